# Optimizing a Trainium2 kernel written in Bass

```python
import jax, jax.numpy as jnp
from jax import lax
import numpy as np

D_MODEL = 1024
BATCH = 4
SEQ = 4096
DEPTH = 1
DEC_BATCH = 32
DEC_SEQ = 1
PAST_LEN = 8192
PAGE_SIZE = 128

CONV_CH = 512
CONV_WIDTH = 31
N_HEADS = 8
HEAD_DIM = 64
ATTN_W = N_HEADS * HEAD_DIM
MOBA_BLOCK = 256
MOBA_TOPK = 3
Q_BLOCK = 32
N_MEM = 256
MEM_HEADS = 4
MEM_HEAD_DIM = 128
MEM_W = MEM_HEADS * MEM_HEAD_DIM
N_BRANCH = 3
D_FF = 2816
FFN_CONV_WIDTH = 3
EPS = 1e-6
N_IN = 2 * CONV_CH + 3 * ATTN_W + MEM_W + N_BRANCH * D_MODEL

kernel_name = 'hybrid_conformer_moba_memxattn_convffn_step'


def rmsnorm(x, g):
    xf = x.astype(jnp.float32)
    y = xf * lax.rsqrt(jnp.mean(xf * xf, axis=-1, keepdims=True) + EPS)
    return (y * g.astype(jnp.float32)).astype(x.dtype)


def layernorm(x, g, b):
    xf = x.astype(jnp.float32)
    mu = jnp.mean(xf, axis=-1, keepdims=True)
    xc = xf - mu
    var = jnp.mean(xc * xc, axis=-1, keepdims=True)
    y = xc * lax.rsqrt(var + EPS) * g.astype(jnp.float32) + b.astype(jnp.float32)
    return y.astype(x.dtype)


def alibi_slopes():
    return jnp.asarray(2.0 ** (-8.0 * np.arange(1, N_HEADS + 1) / N_HEADS), dtype=jnp.float32)


def causal_dwconv(u, prev, w, b):
    full = jnp.concatenate([prev.astype(u.dtype), u], axis=1)
    out = lax.conv_general_dilated(
        full, w[:, None, :].astype(u.dtype), window_strides=(1,), padding='VALID',
        dimension_numbers=('NWC', 'WIO', 'NWC'), feature_group_count=u.shape[-1])
    return out + b.astype(u.dtype), full[:, -(w.shape[0] - 1):]


def pre_mix(x, norm1_g, w_in, q_norm_g, k_norm_g, mq_norm_g):
    n, t = x.shape[:2]
    z = rmsnorm(x, norm1_g) @ w_in
    cuts = [int(c) for c in np.cumsum([2 * CONV_CH, ATTN_W, ATTN_W, ATTN_W, MEM_W])]
    glu, q, k, v, mq, gl = jnp.split(z, cuts, axis=-1)
    a, g = jnp.split(glu, 2, axis=-1)
    u = a * jax.nn.sigmoid(g)
    q = rmsnorm(q.reshape(n, t, N_HEADS, HEAD_DIM), q_norm_g)
    k = rmsnorm(k.reshape(n, t, N_HEADS, HEAD_DIM), k_norm_g)
    v = v.reshape(n, t, N_HEADS, HEAD_DIM)
    mq = rmsnorm(mq.reshape(n, t, MEM_HEADS, MEM_HEAD_DIM), mq_norm_g)
    return u, q, k, v, mq, gl


def conv_branch(u, prev, w_dw_a, b_dw_a, ln_a_g, ln_a_b, w_proj_a):
    c, new_prev = causal_dwconv(u, prev, w_dw_a, b_dw_a)
    c = jax.nn.silu(layernorm(c, ln_a_g, ln_a_b))
    return c @ w_proj_a, new_prev


def mem_kv(mem, mem_norm_g, w_mem_kv, mk_norm_g):
    n, m = mem.shape[:2]
    k, v = jnp.split(rmsnorm(mem, mem_norm_g) @ w_mem_kv, 2, axis=-1)
    k = rmsnorm(k.reshape(n, m, MEM_HEADS, MEM_HEAD_DIM), mk_norm_g)
    return k, v.reshape(n, m, MEM_HEADS, MEM_HEAD_DIM)


def mem_attend(mq, mk, mv, w_proj_c):
    n, t = mq.shape[:2]
    s = jnp.einsum('nthd,nmhd->nhtm', mq, mk.astype(mq.dtype)).astype(jnp.float32) * MEM_HEAD_DIM ** -0.5
    p = jax.nn.softmax(s, axis=-1).astype(mq.dtype)
    o = jnp.einsum('nhtm,nmhd->nthd', p, mv.astype(mq.dtype))
    return o.reshape(n, t, MEM_W) @ w_proj_c


def to_blocks(k):
    n, t, h, d = k.shape
    nb = -(-t // MOBA_BLOCK)
    k = jnp.pad(k, ((0, 0), (0, nb * MOBA_BLOCK - t), (0, 0), (0, 0)))
    return k.reshape(n, nb, MOBA_BLOCK, h, d).transpose(0, 3, 1, 2, 4)


def block_means(kb):
    return jnp.mean(kb.astype(jnp.float32), axis=3).astype(kb.dtype)


def moba_attend(q, t_pos, kb, vb, kmean, slopes):
    n, h, nb, bs, d = kb.shape
    q_len = q.shape[2]
    n_top = min(MOBA_TOPK, nb)
    cur = t_pos // bs
    past = jnp.arange(nb, dtype=jnp.int32)[None, :] < cur[:, None]
    gate = jnp.einsum('nhqd,nhbd->nhqb', q, kmean).astype(jnp.float32)
    gate = jnp.where(past[None, None], gate, -jnp.inf)
    _, top = lax.top_k(gate, n_top)
    own = jnp.broadcast_to(cur[None, None, :, None], (n, h, q_len, 1))
    blocks = jnp.concatenate([top.astype(jnp.int32), own], axis=-1)
    valid = jnp.concatenate([top < cur[None, None, :, None], jnp.ones((n, h, q_len, 1), bool)], axis=-1)
    flat = (jnp.arange(n, dtype=jnp.int32)[:, None, None, None] * h
            + jnp.arange(h, dtype=jnp.int32)[None, :, None, None]) * nb + blocks
    ksel = kb.reshape(n * h * nb, bs, d)[flat]
    vsel = vb.reshape(n * h * nb, bs, d)[flat]
    s_pos = blocks[..., None] * bs + jnp.arange(bs, dtype=jnp.int32)
    dist = (t_pos[None, None, :, None, None] - s_pos).astype(jnp.float32)
    logits = (jnp.einsum('nhqd,nhqsjd->nhqsj', q, ksel).astype(jnp.float32) * HEAD_DIM ** -0.5
              - slopes[None, :, None, None, None] * dist)
    logits = jnp.where(valid[..., None] & (dist >= 0), logits, -jnp.inf)
    p = jax.nn.softmax(logits.reshape(n, h, q_len, -1), axis=-1).reshape(logits.shape)
    return jnp.einsum('nhqsj,nhqsjd->nhqd', p.astype(vb.dtype), vsel)


def post_mix(x, ya, yb, yc, gl, b_gate, w_out, norm2_g, w_up, w_dw_f, b_dw_f, w_down, ffn_prev):
    n, t = x.shape[:2]
    gates = jax.nn.sigmoid((gl + b_gate).reshape(n, t, N_BRANCH, D_MODEL))
    merged = gates[..., 0, :] * ya + gates[..., 1, :] * yb + gates[..., 2, :] * yc
    x1 = x + merged @ w_out
    up, new_prev = causal_dwconv(rmsnorm(x1, norm2_g) @ w_up, ffn_prev, w_dw_f, b_dw_f)
    a, b = jnp.split(up, 2, axis=-1)
    return x1 + (jax.nn.silu(a) * b) @ w_down, new_prev


def setup_inputs(seed: int = 0) -> dict:
    key = jax.random.key(seed)
    ks = iter(jax.random.split(key, 40))

    def nrm(shape, scale=1.0):
        return jax.random.normal(next(ks), shape, jnp.float32) * scale

    def gain(m):
        return 1.0 + 0.05 * nrm((m,))

    n_pages = PAST_LEN // PAGE_SIZE
    n_pool = (DEC_BATCH * n_pages * 5) // 4
    perm = jax.random.permutation(next(ks), n_pool)[: DEC_BATCH * n_pages]
    page_table = perm.reshape(DEC_BATCH, n_pages).astype(jnp.int32)
    return {
        'x_prompt': nrm((BATCH, SEQ, D_MODEL)),
        'x_sample': nrm((DEC_BATCH, DEC_SEQ, D_MODEL)),
        'mem_prompt': nrm((BATCH, N_MEM, D_MODEL)),
        'cache_k': nrm((n_pool, PAGE_SIZE, N_HEADS, HEAD_DIM)),
        'cache_v': nrm((n_pool, PAGE_SIZE, N_HEADS, HEAD_DIM)),
        'page_table': page_table,
        'state_conv': nrm((DEC_BATCH, CONV_WIDTH - 1, CONV_CH), 0.5),
        'state_ffn_conv': nrm((DEC_BATCH, FFN_CONV_WIDTH - 1, 2 * D_FF)),
        'cache_mem_k': nrm((DEC_BATCH, N_MEM, MEM_HEADS, MEM_HEAD_DIM)),
        'cache_mem_v': nrm((DEC_BATCH, N_MEM, MEM_HEADS, MEM_HEAD_DIM)),
        'norm1_g': gain(D_MODEL),
        'w_in': nrm((D_MODEL, N_IN), D_MODEL ** -0.5),
        'b_gate': nrm((N_BRANCH * D_MODEL,), 0.01),
        'w_dw_a': nrm((CONV_WIDTH, CONV_CH), CONV_WIDTH ** -0.5),
        'b_dw_a': nrm((CONV_CH,), 0.01),
        'ln_a_g': gain(CONV_CH),
        'ln_a_b': nrm((CONV_CH,), 0.01),
        'w_proj_a': nrm((CONV_CH, D_MODEL), CONV_CH ** -0.5),
        'q_norm_g': gain(HEAD_DIM),
        'k_norm_g': gain(HEAD_DIM),
        'w_proj_b': nrm((ATTN_W, D_MODEL), ATTN_W ** -0.5),
        'mem_norm_g': gain(D_MODEL),
        'w_mem_kv': nrm((D_MODEL, 2 * MEM_W), D_MODEL ** -0.5),
        'mq_norm_g': gain(MEM_HEAD_DIM),
        'mk_norm_g': gain(MEM_HEAD_DIM),
        'w_proj_c': nrm((MEM_W, D_MODEL), MEM_W ** -0.5),
        'w_out': nrm((D_MODEL, D_MODEL), D_MODEL ** -0.5),
        'norm2_g': gain(D_MODEL),
        'w_up': nrm((D_MODEL, 2 * D_FF), D_MODEL ** -0.5),
        'w_dw_f': nrm((FFN_CONV_WIDTH, 2 * D_FF), FFN_CONV_WIDTH ** -0.5),
        'b_dw_f': nrm((2 * D_FF,), 0.01),
        'w_down': nrm((D_FF, D_MODEL), D_FF ** -0.5),
    }


def reference(x_prompt, x_sample, mem_prompt, cache_k, cache_v, page_table, state_conv,
              state_ffn_conv, cache_mem_k, cache_mem_v, norm1_g, w_in, b_gate, w_dw_a, b_dw_a,
              ln_a_g, ln_a_b, w_proj_a, q_norm_g, k_norm_g, w_proj_b, mem_norm_g, w_mem_kv,
              mq_norm_g, mk_norm_g, w_proj_c, w_out, norm2_g, w_up, w_dw_f, b_dw_f, w_down):
    slopes = alibi_slopes()
    h_p, h_s = x_prompt, x_sample
    for _ in range(DEPTH):
        n, t = h_p.shape[:2]
        u_p, q_p, k_p, v_p, mq_p, gl_p = pre_mix(h_p, norm1_g, w_in, q_norm_g, k_norm_g, mq_norm_g)
        ya_p, conv_p = conv_branch(u_p, jnp.zeros((n, CONV_WIDTH - 1, CONV_CH), u_p.dtype),
                                   w_dw_a, b_dw_a, ln_a_g, ln_a_b, w_proj_a)
        kb_p, vb_p = to_blocks(k_p), to_blocks(v_p)
        km_p = block_means(kb_p)
        n_chunks = t // Q_BLOCK
        qc = q_p.reshape(n, n_chunks, Q_BLOCK, N_HEADS, HEAD_DIM).transpose(1, 0, 3, 2, 4)
        pc = jnp.arange(t, dtype=jnp.int32).reshape(n_chunks, Q_BLOCK)
        oc = lax.map(lambda a: moba_attend(a[0], a[1], kb_p, vb_p, km_p, slopes), (qc, pc))
        yb_p = oc.transpose(1, 0, 3, 2, 4).reshape(n, t, ATTN_W) @ w_proj_b
        mk_p, mv_p = mem_kv(mem_prompt, mem_norm_g, w_mem_kv, mk_norm_g)
        yc_p = mem_attend(mq_p, mk_p, mv_p, w_proj_c)
        y_p, ffn_p = post_mix(h_p, ya_p, yb_p, yc_p, gl_p, b_gate, w_out, norm2_g, w_up, w_dw_f,
                              b_dw_f, w_down,
                              jnp.zeros((n, FFN_CONV_WIDTH - 1, 2 * D_FF), h_p.dtype))
        nd, td = h_s.shape[:2]
        past_len = page_table.shape[1] * PAGE_SIZE
        u_s, q_s, k_s, v_s, mq_s, gl_s = pre_mix(h_s, norm1_g, w_in, q_norm_g, k_norm_g, mq_norm_g)
        ya_s, conv_s = conv_branch(u_s, state_conv, w_dw_a, b_dw_a, ln_a_g, ln_a_b, w_proj_a)
        past_k = cache_k[page_table].reshape(nd, past_len, N_HEADS, HEAD_DIM).astype(k_s.dtype)
        past_v = cache_v[page_table].reshape(nd, past_len, N_HEADS, HEAD_DIM).astype(v_s.dtype)
        kb_s = to_blocks(jnp.concatenate([past_k, k_s], axis=1))
        vb_s = to_blocks(jnp.concatenate([past_v, v_s], axis=1))
        t_s = past_len + jnp.arange(td, dtype=jnp.int32)
        o_s = moba_attend(q_s.transpose(0, 2, 1, 3), t_s, kb_s, vb_s, block_means(kb_s), slopes)
        yb_s = o_s.transpose(0, 2, 1, 3).reshape(nd, td, ATTN_W) @ w_proj_b
        yc_s = mem_attend(mq_s, cache_mem_k, cache_mem_v, w_proj_c)
        y_s, ffn_s = post_mix(h_s, ya_s, yb_s, yc_s, gl_s, b_gate, w_out, norm2_g, w_up, w_dw_f,
                              b_dw_f, w_down, state_ffn_conv)
        h_p, h_s = y_p, y_s
    return (h_p, h_s, k_p, v_p, k_s, v_s, conv_p, conv_s, ffn_p, ffn_s, mk_p, mv_p)
```

```python
import contextlib
import numpy as np
import concourse.bass as bass
import concourse.mybir as mybir
from concourse.bass_utils import run_bass_kernel_spmd

F32 = mybir.dt.float32
BF16 = mybir.dt.bfloat16
I32 = mybir.dt.int32
AF = mybir.ActivationFunctionType
ALU = mybir.AluOpType
AX = mybir.AxisListType

D = 1024
SEQ = 4096
NT = 512
NCHUNK = SEQ // NT
CONV_CH = 512
CW = 31
NH = 8
HD = 64
DFF = 2816
NUP = 2 * DFF
NIN = 6144
EPS = 1e-6
NEG = -30000.0
N_POOL = 2560


class Sched:
    ENGS = ("pe", "act", "dve", "pool", "sp")

    def __init__(self, nc):
        self.nc = nc
        self.q = {e: [] for e in self.ENGS}
        self.last_w = {}
        self.readers = {}
        self.dma_cnt = {}
        self.n_ops = 0
        self.alias = {}

    def op(self, eng, fn, reads=(), writes=(), dma=False, semkey=None):
        rec = {"fn": fn, "eng": eng, "deps": [], "need_inc": False, "dma": dma,
               "semkey": None, "dval": 0}
        self.n_ops += 1
        deps = []
        for k in reads:
            w = self.last_w.get(k)
            if w is not None:
                deps.append(w)
        for k in writes:
            w = self.last_w.get(k)
            if w is not None:
                deps.append(w)
            for r in self.readers.get(k, ()):
                deps.append(r)
            for a in self.alias.get(k, ()):
                w = self.last_w.get(a)
                if w is not None:
                    deps.append(w)
                for r in self.readers.get(a, ()):
                    deps.append(r)
        if dma:
            assert semkey is not None
            rec["semkey"] = semkey
            self.dma_cnt[semkey] = self.dma_cnt.get(semkey, 0) + 16
            rec["dval"] = self.dma_cnt[semkey]
            me = ("d", semkey, rec["dval"])
        else:
            me = ("c", rec)
        seen = set()
        for d in deps:
            if d[0] == "c":
                p = d[1]
                if p is rec:
                    continue
                if p["eng"] == eng and eng == "pe":
                    continue
                if id(p) in seen:
                    continue
                seen.add(id(p))
                p["need_inc"] = True
                rec["deps"].append(d)
            else:
                key = (d[1], d[2])
                if key in seen:
                    continue
                seen.add(key)
                rec["deps"].append(d)
        for k in writes:
            self.last_w[k] = me
            self.readers[k] = []
        for k in reads:
            if k in writes:
                continue
            self.readers.setdefault(k, []).append(me)
        self.q[eng].append(rec)
        return rec

    def emit(self, st):
        nc = self.nc
        esem = {e: st.enter_context(nc.semaphore("e_" + e)) for e in self.ENGS}
        dsem = {}
        for i, k in enumerate(self.dma_cnt):
            dsem[k] = st.enter_context(nc.semaphore("d%d" % i))
        for e in self.ENGS:
            c = 0
            for rec in self.q[e]:
                if rec["need_inc"] and not rec["dma"]:
                    c += 1
                    rec["inc_val"] = c
        block = st.enter_context(nc.Block())
        hw = {"pe": nc.tensor, "act": nc.scalar, "dve": nc.vector, "pool": nc.gpsimd, "sp": nc.sync}

        def run(e):
            eng = hw[e]
            waited = {}
            for rec in self.q[e]:
                for d in rec["deps"]:
                    if d[0] == "c":
                        p = d[1]
                        sem = esem[p["eng"]]
                        val = p["inc_val"]
                        key = ("e", p["eng"])
                    else:
                        sem = dsem[d[1]]
                        val = d[2]
                        key = ("d", d[1])
                    if waited.get(key, 0) >= val:
                        continue
                    waited[key] = val
                    eng.wait_ge(sem, val)
                ins = rec["fn"](eng)
                if rec["dma"]:
                    ins.then_inc(dsem[rec["semkey"]], 16)
                elif rec["need_inc"]:
                    ins.then_inc(esem[e], 1)
            if e == "sp":
                for k, v in self.dma_cnt.items():
                    eng.wait_ge(dsem[k], v)

        block.tensor(lambda _e: run("pe"))
        block.scalar(lambda _e: run("act"))
        block.vector(lambda _e: run("dve"))
        block.gpsimd(lambda _e: run("pool"))
        block.sync(lambda _e: run("sp"))


FM_G1, FM_G2, FM_GM, FM_BG, FM_WDA, FM_BDA, FM_LAG, FM_LAB, FM_WDF, FM_BDF = 0, 8, 16, 24, 48, 172, 176, 180, 184, 316
FM_N = 360
BC_GQ, BC_GK, BC_GMQ, BC_GMK = 0, 512, 1024, 1536
BC_N = 2048


SC_RROW, SC_CS, SC_ST, SC_REP, SC_E8, SC_AL, SC_PI, SC_IND = 0, 512, 643, 898, 1026, 2050, 2562, 2563
SC_N = 2691


def build(nch=NCHUNK, do_sample=True, dbg=False):
    nc = bass.Bass("TRN2", target_bir_lowering=False)

    def din(name, shape, dt=F32):
        return nc.dram_tensor(name, list(shape), dt, kind="ExternalInput").ap()

    def dout(name, shape, dt=F32):
        return nc.dram_tensor(name, list(shape), dt, kind="ExternalOutput").ap()

    xin = din("x", [SEQ, D])
    memin = din("mem", [256, D])
    w_in = din("w_in", [D, NIN])
    w_pa = din("w_proj_a", [512, D])
    w_pb = din("w_proj_b", [512, D])
    w_pc = din("w_proj_c", [512, D])
    w_mkv = din("w_mem_kv", [D, D])
    w_out = din("w_out", [D, D])
    w_up = din("w_up", [D, NUP])
    w_down = din("w_down", [DFF, D])
    fm_in = din("fm", [128, FM_N])
    bc_in = din("bc", [128, BC_N])
    ident_in = din("ident", [128, 128])
    tri_in = din("tri", [128, 128])
    abase_in = din("abase", [128, NH * 32])
    qal_in = din("qal", [128, 4 * NH * 2])

    if do_sample:
        xs_in = din("xs", [128, D])
        stc_in = din("stc", [120, 512])
        w120_in = din("w120", [120, 512])
        stf_in = din("stf", [4, 2 * NUP])
        cmk_in = din("cmk", [4, 256, 512])
        cmv_in = din("cmv", [4, 256, 512])
        pt_in = din("ptrep", [128, 256], I32)
        ck_in = din("cache_k", [N_POOL * 128, 512])
        cv_in = din("cache_v", [N_POOL * 128, 512])
        sc_in = din("sconst", [128, SC_N])
        vrow_in = din("vrows", [9, 5632])
        ys_o = dout("ys", [4, D])
        ks_o = dout("ks", [4, 512])
        vs_o = dout("vs", [4, 512])
        convs_o = dout("convs", [4, 30, 512])
        ffns_o = dout("ffns", [4, 2, NUP])
    y_o = dout("y", [SEQ, D])
    k_o = dout("k", [SEQ, 512])
    v_o = dout("v", [SEQ, 512])
    conv_o = dout("conv", [32, 512])
    ffnc_o = dout("ffnc", [88, 128])
    mk_o = dout("mk", [256, 512])
    mv_o = dout("mv", [256, 512])
    kt_scr = nc.dram_tensor("kt_scr", [NH, 82, SEQ], BF16, kind="Internal").ap()
    v_scr = nc.dram_tensor("v_scr", [NH, SEQ // 128, 128, 128], BF16, kind="Internal").ap()

    st = contextlib.ExitStack()
    with st:
        def sb(name, shape, dt):
            return st.enter_context(nc.sbuf_tensor("s_" + name, list(shape), dt))

        def psum(name, shape, dt):
            return st.enter_context(nc.psum_tensor("p_" + name, list(shape), dt))

        S = Sched(nc)
        op = S.op
        dbg_n = [0]

        def dbgout(src_ap, shape, keys, dt=F32):
            if not dbg:
                return
            name = "dbg%d" % dbg_n[0]
            dbg_n[0] += 1
            t = nc.dram_tensor(name, list(shape), dt, kind="ExternalOutput").ap()
            op("pool", lambda e: e.dma_start(out=t, in_=src_ap), reads=keys, writes=["hbm_" + name], dma=True, semkey="dbg")

        identb = sb("identb", [128, 128], BF16)
        identf = sb("identf", [128, 128], F32)
        trib = sb("trib", [128, 128], BF16)
        onesb = sb("onesb", [128, 128], BF16)
        onesf = sb("onesf", [128, 128], F32)
        fm = sb("fm", [128, FM_N], F32)
        bc = sb("bc", [128, BC_N], F32)
        btab = sb("btab", [128, NH * 32], F32)
        mvals = sb("mvals", [128, 8], F32)
        epsb = sb("epsb", [128, 1], F32)
        X = sb("X", [128, 4, D], F32)
        xn = sb("xn", [128, D], BF16)
        ss = sb("ss", [128, 16], F32)
        hT = sb("hT", [128, 8, NT], BF16)
        uT = sb("uT", [128, 4, 30 + NT], F32)
        cT = sb("cT", [128, 4, NT], F32)
        sT = sb("sT", [128, 4, NT], BF16)
        tmpA = sb("tmpA", [128, NT], F32)
        tmpB = sb("tmpB", [128, NT], F32)
        Qaug = sb("Qaug", [128, 4, NH, 82], BF16)
        Kaug = sb("Kaug", [128, 4, NH, 82], BF16)
        Vloc = sb("Vloc", [128, 4, NH, 128], BF16)
        mqT = sb("mqT", [128, 4, NT], BF16)
        qn_bf = sb("qn_bf", [128, 512], BF16)
        kn_bf = sb("kn_bf", [128, 2, 512], BF16)
        kst = sb("kst", [128, 2, 512], F32)
        vst = sb("vst", [128, 2, 512], F32)
        kmBD = sb("kmBD", [128, 4, 2, 16], BF16)
        gatef = sb("gatef", [128, NH, 16], F32)
        top8 = sb("top8", [128, NH, 8], F32)
        thr = sb("thr", [128, NH], F32)
        nsel = sb("nsel", [128, NH, 16], F32)
        PT = sb("PT", [128, 3, NT], BF16)
        OT = sb("OT", [128, 4, NT], BF16)
        OmT = sb("OmT", [128, 4, NT], BF16)
        rden = sb("rden", [128, NT], F32)
        mkT = sb("mkT", [128, 4, 256], BF16)
        mVa = sb("mVa", [128, 2, 512], BF16)
        merged = sb("merged", [128, 8, NT], BF16)
        scrA = sb("scrA", [128, 22 * NT], BF16)
        sa = scrA[:, :].rearrange("p (j t) -> p j t", j=22)
        KTloc = scrA[:, 0:4096].rearrange("p (j t) -> p j t", j=NH)
        qTa = scrA[:, 4096:8192].rearrange("p (j t) -> p j t", j=NH)
        mqn = scrA[:, 8192:10240].rearrange("p (j t) -> p j t", j=4)
        qTp = scrA[:, 10240:10752].rearrange("p (j t) -> p j t", j=4)
        cbuf = sb("cbuf", [128, 3, NT], F32)
        tmpC = cbuf[:, 0, :]
        tmpD = cbuf[:, 1, :]
        sig = cT
        junk = merged[:, 0:2, :].rearrange("p a t -> p (a t)")
        for _k in ("KTloc", "qTa", "mqn", "qTp"):
            S.alias[_k] = [("sa", j) for j in range(22)]
        for j in range(22):
            S.alias[("sa", j)] = ["KTloc", "qTa", "mqn", "qTp"]
        hist = sb("hist", [128, 44, 2], F32)
        ring = sb("ring", [128, 4, 4096], BF16)
        gring = sb("gring", [128, 4, 1024], BF16)
        kvK = sb("kvK", [128, 4, NT], BF16)
        kvV = sb("kvV", [128, 4, 4, 128], BF16)
        outst = sb("outst", [128, 512], F32)
        idx_t = sb("idx_t", [128, 256], I32)
        ptf = sb("ptf", [128, 256], F32)

        psA = [psum("psA%d" % i, [128, 512], F32) for i in range(6)]
        psT = psum("psT", [128, 1024], BF16)
        psM = psum("psM", [128, 512], F32)
        PA = ["psA%d" % i for i in range(6)]

        def dma(eng, out, in_, reads, writes, semkey):
            return op(eng, lambda e: e.dma_start(out=out, in_=in_), reads=reads, writes=writes, dma=True, semkey=semkey)

        def mm(out, lhsT, rhs, start, stop, reads, writes):
            return op("pe", lambda e: e.matmul(out, lhsT=lhsT, rhs=rhs, start=start, stop=stop, skip_group_check=True),
                      reads=reads, writes=writes)

        def tr(out, in_, ident, reads, writes):
            return op("pe", lambda e: e.transpose(out=out, in_=in_, identity=ident), reads=reads, writes=writes)

        rstate = {"i": 0, "g": 0}

        def load_w(view, nk, ncols):
            i = rstate["i"] % 4
            rstate["i"] += 1
            dst = ring[:, i, 0:nk * ncols].rearrange("p (k n) -> p k n", k=nk)
            dma("pool", dst, view, [], [("ring", i)], "ring%d" % i)
            return dst, ("ring", i)

        def load_g(view):
            i = rstate["g"] % 4
            rstate["g"] += 1
            dst = gring[:, i, :].rearrange("p (k n) -> p k n", k=8)
            dma("pool", dst, view, [], [("gring", i)], "gring%d" % i)
            return dst, ("gring", i)

        win_v = w_in.rearrange("(kc p) n -> p kc n", p=128)
        wup_v = w_up.rearrange("(kc p) n -> p kc n", p=128)
        wout_v = w_out.rearrange("(kc p) n -> p kc n", p=128)
        wdn_v = w_down.rearrange("(kc p) n -> p kc n", p=128)
        wmkv_v = w_mkv.rearrange("(kc p) n -> p kc n", p=128)
        wpa_v = w_pa.rearrange("(kc p) n -> p kc n", p=128)
        wpb_v = w_pb.rearrange("(kc p) n -> p kc n", p=128)
        wpc_v = w_pc.rearrange("(kc p) n -> p kc n", p=128)

        dma("sp", fm[:, :], fm_in[:, :], [], ["fm"], "c1")
        dma("sp", bc[:, :], bc_in[:, :], [], ["bc"], "c2")
        dma("sp", identf[:, :], ident_in[:, :], [], ["identf"], "c3")
        dma("sp", btab[:, :], abase_in[:, :], [], ["btab"], "c4")
        dma("sp", tmpA[:, 0:128], tri_in[:, :], [], ["tmpA"], "c5")
        dma("sp", tmpB[:, 0:64], qal_in[:, :], [], ["tmpB"], "c6")
        op("dve", lambda e: e.tensor_copy(out=identb[:, :], in_=identf[:, :]), ["identf"], ["identb"])
        op("dve", lambda e: e.tensor_copy(out=trib[:, :], in_=tmpA[:, 0:128]), ["tmpA"], ["trib"])
        op("dve", lambda e: e.memset(onesb[:, :], 1.0), [], ["onesb"])
        op("dve", lambda e: e.memset(onesf[:, :], 1.0), [], ["onesf"])
        op("dve", lambda e: e.memset(epsb[:, :], EPS), [], ["epsb"])
        op("dve", lambda e: e.memset(uT[:, :, :], 0.0), [], ["uT"])
        op("dve", lambda e: e.memset(hist[:, :, :], 0.0), [], ["hist"])
        op("dve", lambda e: e.memset(kmBD[:, :, :, :], 0.0), [], ["kmBD"])
        op("dve", lambda e: e.memset(gatef[:, :, :], -1e30), [], ["gatef"])
        op("dve", lambda e: e.memset(Vloc[:, :, :, 0:64], 1.0), [], ["Vloc"])
        op("dve", lambda e: e.memset(Kaug[:, :, :, 80:82], 1.0), [], ["Kaug"])
        op("dve", lambda e: e.tensor_copy(out=Qaug[:, :, :, 80:82],
                                          in_=tmpB[:, 0:64].rearrange("p (s h r) -> p s h r", s=4, h=NH)),
           ["tmpB"], ["Qaug"])
        for j, (off, n) in enumerate(((BC_GQ, 64), (BC_GK, 64), (BC_GMQ, 128), (BC_GMK, 128))):
            op("dve", lambda e, j=j, off=off, n=n: e.tensor_reduce(out=mvals[:, j:j + 1], in_=bc[:, off:off + n], axis=AX.X,
                                                                  op=ALU.max, apply_absolute_value=True),
               ["bc"], ["mvals"])
        op("dve", lambda e: e.tensor_tensor(out=mvals[:, 4:5], in0=mvals[:, 0:1], in1=mvals[:, 1:2], op=ALU.mult), ["mvals"], ["mvals"])
        op("dve", lambda e: e.tensor_scalar(out=mvals[:, 4:5], in0=mvals[:, 4:5], scalar1=8.0, scalar2=None, op0=ALU.mult), ["mvals"], ["mvals"])
        op("dve", lambda e: e.tensor_tensor(out=mvals[:, 5:6], in0=mvals[:, 2:3], in1=mvals[:, 3:4], op=ALU.mult), ["mvals"], ["mvals"])
        op("dve", lambda e: e.tensor_scalar(out=mvals[:, 5:6], in0=mvals[:, 5:6], scalar1=-(128.0 ** 0.5), scalar2=None, op0=ALU.mult), ["mvals"], ["mvals"])
        op("dve", lambda e: e.tensor_scalar(out=btab[:, :], in0=btab[:, :], scalar1=mvals[:, 4:5], scalar2=None, op0=ALU.subtract),
           ["btab", "mvals"], ["btab"])
        op("dve", lambda e: e.tensor_scalar(out=bc[:, BC_GQ:BC_GQ + 512], in0=bc[:, BC_GQ:BC_GQ + 512], scalar1=HD ** -0.5, scalar2=None, op0=ALU.mult), ["bc"], ["bc"])
        op("dve", lambda e: e.tensor_scalar(out=bc[:, BC_GMQ:BC_GMQ + 512], in0=bc[:, BC_GMQ:BC_GMQ + 512], scalar1=128.0 ** -0.5, scalar2=None, op0=ALU.mult), ["bc"], ["bc"])

        def rms_rows(src3, sub, col):
            op("dve", lambda e: e.memset(ss[:, col:col + 1], 0.0), [], [("ss", col)])
            op("act", lambda e: e.activation(out=junk[:, :], in_=src3[:, sub, :], func=AF.Square, accum_out=ss[:, col:col + 1]),
               ["X", ("ss", col)], ["merged", ("ss", col)])
            op("act", lambda e: e.activation(out=ss[:, col:col + 1], in_=ss[:, col:col + 1], func=AF.Sqrt, bias=epsb[:, 0:1], scale=1.0 / D),
               [("ss", col), "epsb"], [("ss", col)])
            op("dve", lambda e: e.reciprocal(out=ss[:, col:col + 1], in_=ss[:, col:col + 1]), [("ss", col)], [("ss", col)])

        def norm_to_T(src3, nsub, goff, dstT, dkey, srckey):
            for sub in range(nsub):
                rms_rows(src3, sub, sub)
                op("dve", lambda e, sub=sub: e.tensor_scalar(out=xn[:, :], in0=src3[:, sub, :], scalar1=ss[:, sub:sub + 1], scalar2=None, op0=ALU.mult),
                   [srckey, ("ss", sub)], ["xn"])
                for kc in range(8):
                    tr(psT[:, kc * 128:(kc + 1) * 128], xn[:, kc * 128:(kc + 1) * 128], identb[:, :], ["xn", "identb"], ["psT"])
                op("dve", lambda e, sub=sub: e.tensor_tensor(
                    out=dstT[:, :, sub * 128:(sub + 1) * 128], in0=psT[:, :].rearrange("p (k t) -> p k t", k=8),
                    in1=fm[:, goff:goff + 8].unsqueeze(2).broadcast_to([128, 8, 128]), op=ALU.mult),
                   ["psT", "fm"], [dkey])

        def headnorm(ps, pkey, nh, hd, goff, out_ap, okey, extra_reads=()):
            op("act", lambda e: e.activation(out=tmpA[:, :], in_=ps[:, :], func=AF.Square), [pkey], ["tmpA"])
            op("dve", lambda e: e.tensor_reduce(out=ss[:, 8:8 + nh], in_=tmpA[:, :].rearrange("p (h d) -> p h d", h=nh), axis=AX.X, op=ALU.add),
               ["tmpA"], ["ss8"])
            op("act", lambda e: e.activation(out=ss[:, 8:8 + nh], in_=ss[:, 8:8 + nh], func=AF.Sqrt, bias=epsb[:, 0:1], scale=1.0 / hd),
               ["ss8", "epsb"], ["ss8"])
            op("dve", lambda e: e.reciprocal(out=ss[:, 8:8 + nh], in_=ss[:, 8:8 + nh]), ["ss8"], ["ss8"])
            op("dve", lambda e: e.tensor_tensor(out=tmpB[:, :].rearrange("p (h d) -> p h d", h=nh), in0=ps[:, :].rearrange("p (h d) -> p h d", h=nh),
                                                in1=ss[:, 8:8 + nh].unsqueeze(2).broadcast_to([128, nh, hd]), op=ALU.mult),
               [pkey, "ss8"], ["tmpB"])
            op("dve", lambda e: e.tensor_tensor(out=out_ap, in0=tmpB[:, :].rearrange("p (h d) -> p h d", h=nh),
                                                in1=bc[:, goff:goff + 512].rearrange("p (h d) -> p h d", h=nh), op=ALU.mult),
               ["tmpB", "bc"] + list(extra_reads), [okey])

        dma("sp", X[:, 0:2, :], memin.rearrange("(s p) d -> p s d", p=128), [], ["X"], "xload")
        norm_to_T(X, 2, FM_GM, hT, "hT", "X")
        for half in range(2):
            W, wk = load_w(wmkv_v[:, :, half * 512:(half + 1) * 512], 8, 512)
            for sub in range(2):
                ps = psA[sub]
                for kc in range(8):
                    mm(ps[:, :], hT[:, kc, sub * 128:(sub + 1) * 128], W[:, kc, :], kc == 0, kc == 7, ["hT", wk], [PA[sub]])
                if half == 0:
                    headnorm(ps, PA[sub], 4, 128, BC_GMK, kst[:, sub, :].rearrange("p (h d) -> p h d", h=4), ("kst", sub))
                    dma("sp", mk_o[sub * 128:(sub + 1) * 128, :], kst[:, sub, :], [("kst", sub)], ["hbm_mk"], "st_k%d" % sub)
                    op("dve", lambda e, sub=sub: e.tensor_copy(out=mqn[:, sub, :], in_=kst[:, sub, :]), [("kst", sub)], ["mqn"])
                    for mh in range(4):
                        tr(psT[:, mh * 128:(mh + 1) * 128], mqn[:, sub, mh * 128:(mh + 1) * 128], identb[:, :], ["mqn", "identb"], ["psT"])
                    op("dve", lambda e, sub=sub: e.tensor_copy(out=mkT[:, :, sub * 128:(sub + 1) * 128],
                                                               in_=psT[:, 0:512].rearrange("p (k t) -> p k t", k=4)), ["psT"], ["mkT"])
                else:
                    op("act", lambda e, sub=sub, ps=ps: e.activation(out=vst[:, sub, :], in_=ps[:, :], func=AF.Copy), [PA[sub]], [("vst", sub)])
                    dma("sp", mv_o[sub * 128:(sub + 1) * 128, :], vst[:, sub, :], [("vst", sub)], ["hbm_mv"], "st_v%d" % sub)
                    op("dve", lambda e, sub=sub: e.tensor_copy(out=mVa[:, sub, :], in_=vst[:, sub, :]), [("vst", sub)], ["mVa"])

        kvstate = {"i": 0}
        for c in range(nch):
            t0 = c * NT
            dma("sp", X[:, :, :], xin[t0:t0 + NT, :].rearrange("(s p) d -> p s d", p=128), [], ["X"], "xload")
            norm_to_T(X, 4, FM_G1, hT, "hT", "X")

            Wa, wka = load_w(win_v[:, :, 0:512], 8, 512)
            Wg, wkg = load_w(win_v[:, :, 512:1024], 8, 512)
            for cc in range(4):
                pa, pg = psA[2 * (cc % 2)], psA[2 * (cc % 2) + 1]
                ka, kg = PA[2 * (cc % 2)], PA[2 * (cc % 2) + 1]
                for kc in range(8):
                    mm(pa[:, :], Wa[:, kc, cc * 128:(cc + 1) * 128], hT[:, kc, :], kc == 0, kc == 7, ["hT", wka], [ka])
                for kc in range(8):
                    mm(pg[:, :], Wg[:, kc, cc * 128:(cc + 1) * 128], hT[:, kc, :], kc == 0, kc == 7, ["hT", wkg], [kg])
                tk = ("cbuf", 0) if cc % 2 == 0 else ("cbuf", 1)
                tt = tmpC if cc % 2 == 0 else tmpD
                op("act", lambda e, pg=pg, tt=tt: e.activation(out=tt[:, :], in_=pg[:, :], func=AF.Sigmoid), [kg], [tk])
                op("dve", lambda e, pa=pa, tt=tt, cc=cc: e.tensor_tensor(out=uT[:, cc, 30:30 + NT], in0=pa[:, :], in1=tt[:, :], op=ALU.mult),
                   [ka, tk], ["uT"])
            for cc in range(4):
                op("dve", lambda e, cc=cc: e.tensor_scalar(out=cT[:, cc, :], in0=uT[:, cc, 0:NT], scalar1=fm[:, FM_WDA + cc * 31:FM_WDA + cc * 31 + 1],
                                                          scalar2=fm[:, FM_BDA + cc:FM_BDA + cc + 1], op0=ALU.mult, op1=ALU.add),
                   ["uT", "fm"], [("cT", cc)])
                for k in range(1, CW):
                    op("dve", lambda e, cc=cc, k=k: e.scalar_tensor_tensor(out=cT[:, cc, :], in0=uT[:, cc, k:k + NT],
                                                                          scalar=fm[:, FM_WDA + cc * 31 + k:FM_WDA + cc * 31 + k + 1],
                                                                          in1=cT[:, cc, :], op0=ALU.mult, op1=ALU.add),
                       ["uT", "fm", ("cT", cc)], [("cT", cc)])
            if c == nch - 1:
                for cc in range(4):
                    op("pe", lambda e, cc=cc: e.transpose(out=psM[0:32, cc * 128:(cc + 1) * 128], in_=uT[:, cc, 30 + NT - 32:30 + NT], identity=identf[:, :]),
                       ["uT", "identf"], ["psM"])
                op("dve", lambda e: e.tensor_copy(out=outst[0:32, :], in_=psM[0:32, :]), ["psM"], ["outst"])
                dma("sp", conv_o[:, :], outst[0:32, :], ["outst"], ["hbm_conv"], "st_misc")
            op("dve", lambda e: e.tensor_copy(out=uT[:, :, 0:30], in_=uT[:, :, NT:NT + 30]), ["uT"], ["uT"])
            for cc in range(4):
                mm(psA[4][:, :], onesf[:, :], cT[:, cc, :], cc == 0, cc == 3, ["onesf", ("cT", cc)], [PA[4]])
            for cc in range(4):
                tt, tk = (tmpC, ("cbuf", 0)) if cc % 2 == 0 else (tmpD, ("cbuf", 1))
                op("act", lambda e, cc=cc, tt=tt: e.activation(out=tt[:, :], in_=cT[:, cc, :], func=AF.Square), [("cT", cc)], [tk])
                mm(psA[5][:, :], onesf[:, :], tt[:, :], cc == 0, cc == 3, ["onesf", tk], [PA[5]])
            op("dve", lambda e: e.tensor_scalar(out=tmpA[:, :], in0=psA[4][:, :], scalar1=1.0 / 512, scalar2=None, op0=ALU.mult), [PA[4]], ["tmpA"])
            op("dve", lambda e: e.tensor_tensor(out=tmpB[:, :], in0=tmpA[:, :], in1=tmpA[:, :], op=ALU.mult), ["tmpA"], ["tmpB"])
            op("dve", lambda e: e.scalar_tensor_tensor(out=tmpB[:, :], in0=psA[5][:, :], scalar=1.0 / 512, in1=tmpB[:, :], op0=ALU.mult, op1=ALU.subtract),
               [PA[5], "tmpB"], ["tmpB"])
            op("act", lambda e: e.activation(out=tmpB[:, :], in_=tmpB[:, :], func=AF.Sqrt, bias=epsb[:, 0:1], scale=1.0), ["tmpB", "epsb"], ["tmpB"])
            op("dve", lambda e: e.reciprocal(out=tmpB[:, :], in_=tmpB[:, :]), ["tmpB"], ["tmpB"])
            for cc in range(4):
                op("dve", lambda e, cc=cc: e.tensor_tensor(out=cT[:, cc, :], in0=cT[:, cc, :], in1=tmpA[:, :], op=ALU.subtract), [("cT", cc), "tmpA"], [("cT", cc)])
                op("dve", lambda e, cc=cc: e.tensor_tensor(out=cT[:, cc, :], in0=cT[:, cc, :], in1=tmpB[:, :], op=ALU.mult), [("cT", cc), "tmpB"], [("cT", cc)])
                op("act", lambda e, cc=cc: e.activation(out=sT[:, cc, :], in_=cT[:, cc, :], func=AF.Silu, scale=fm[:, FM_LAG + cc:FM_LAG + cc + 1],
                                                        bias=fm[:, FM_LAB + cc:FM_LAB + cc + 1]), [("cT", cc), "fm"], ["sT"])

            op("dve", lambda e: e.memset(Kaug[:, :, :, 64:80], 0.0), [], ["Kaug"])
            op("dve", lambda e, c=c: e.memset(Kaug[:, 0:2, :, 64 + 2 * c:65 + 2 * c], 1.0), [], ["Kaug"])
            op("dve", lambda e, c=c: e.memset(Kaug[:, 2:4, :, 65 + 2 * c:66 + 2 * c], 1.0), [], ["Kaug"])
            for name, col0 in (("k", 1536), ("v", 2048), ("q", 1024), ("mq", 2560)):
                W, wk = load_w(win_v[:, :, col0:col0 + 512], 8, 512)
                for sub in range(4):
                    ps, pk = psA[sub % 4], PA[sub % 4]
                    for kc in range(8):
                        mm(ps[:, :], hT[:, kc, sub * 128:(sub + 1) * 128], W[:, kc, :], kc == 0, kc == 7, ["hT", wk], [pk])
                    if name == "k":
                        headnorm(ps, pk, NH, HD, BC_GK, kst[:, sub % 2, :].rearrange("p (h d) -> p h d", h=NH), ("kst", sub % 2))
                        dma("sp", k_o[t0 + sub * 128:t0 + (sub + 1) * 128, :], kst[:, sub % 2, :], [("kst", sub % 2)], ["hbm_k"], "st_k%d" % (sub % 2))
                        op("dve", lambda e, sub=sub: e.tensor_copy(out=Kaug[:, sub, :, 0:64], in_=kst[:, sub % 2, :].rearrange("p (h d) -> p h d", h=NH)),
                           [("kst", sub % 2)], ["Kaug"])
                        op("dve", lambda e, sub=sub: e.tensor_copy(out=kn_bf[:, sub % 2, :], in_=kst[:, sub % 2, :]), [("kst", sub % 2)], ["kn_bf"])
                        for h in range(NH):
                            tr(psT[0:82, h * 128:(h + 1) * 128], Kaug[:, sub, h, :], identb[:, :], ["Kaug", "identb"], ["psT"])
                        op("dve", lambda e, sub=sub: e.tensor_copy(out=KTloc[0:82, :, sub * 128:(sub + 1) * 128],
                                                                   in_=psT[0:82, :].rearrange("p (k t) -> p k t", k=NH)), ["psT"], ["KTloc"])
                        if sub % 2 == 1:
                            blk = 2 * c + sub // 2
                            for pair in range(4):
                                for s2 in range(2):
                                    mm(psM[:, pair:pair + 1], kn_bf[:, s2, pair * 128:(pair + 1) * 128], onesb[:, 0:1], s2 == 0, s2 == 1,
                                       ["kn_bf", "onesb"], ["psM"])
                            op("dve", lambda e, blk=blk: e.tensor_scalar(out=kmBD[0:64, :, 0, blk], in0=psM[0:64, 0:4], scalar1=1.0 / 256, scalar2=None, op0=ALU.mult),
                               ["psM"], ["kmBD"])
                            op("dve", lambda e, blk=blk: e.tensor_scalar(out=kmBD[64:128, :, 1, blk], in0=psM[64:128, 0:4], scalar1=1.0 / 256, scalar2=None, op0=ALU.mult),
                               ["psM"], ["kmBD"])
                    elif name == "v":
                        op("act", lambda e, sub=sub, ps=ps: e.activation(out=vst[:, sub % 2, :], in_=ps[:, :], func=AF.Copy), [pk], [("vst", sub % 2)])
                        dma("sp", v_o[t0 + sub * 128:t0 + (sub + 1) * 128, :], vst[:, sub % 2, :], [("vst", sub % 2)], ["hbm_v"], "st_v%d" % (sub % 2))
                        op("dve", lambda e, sub=sub: e.tensor_copy(out=Vloc[:, sub, :, 64:128], in_=vst[:, sub % 2, :].rearrange("p (h d) -> p h d", h=NH)),
                           [("vst", sub % 2)], ["Vloc"])
                    elif name == "q":
                        cur = 2 * c + sub // 2
                        headnorm(ps, pk, NH, HD, BC_GQ, Qaug[:, sub, :, 0:64], "Qaug")
                        if cur == 0:
                            op("dve", lambda e, sub=sub: e.memset(Qaug[:, sub, :, 64:80], NEG), [], ["Qaug"])
                        else:
                            op("dve", lambda e, sub=sub: e.tensor_copy(out=qn_bf[:, :].rearrange("p (h d) -> p h d", h=NH), in_=Qaug[:, sub, :, 0:64]), ["Qaug"], ["qn_bf"])
                            for pair in range(4):
                                tr(psT[:, pair * 128:(pair + 1) * 128], qn_bf[:, pair * 128:(pair + 1) * 128], identb[:, :], ["qn_bf", "identb"], ["psT"])
                            op("dve", lambda e: e.tensor_copy(out=qTp[:, :, :], in_=psT[:, 0:512].rearrange("p (k t) -> p k t", k=4)), ["psT"], ["qTp"])
                            for pair in range(4):
                                mm(psM[:, 32 * pair:32 * pair + 32], qTp[:, pair, :], kmBD[:, pair, :, :].rearrange("p a b -> p (a b)"), True, True, ["qTp", "kmBD"], ["psM"])
                            op("dve", lambda e, cur=cur: e.tensor_copy(out=gatef[:, :, 0:cur], in_=psM[:, 0:128].rearrange("p (h b) -> p h b", h=NH)[:, :, 0:cur]),
                               ["psM"], ["gatef"])
                            for h in range(NH):
                                op("dve", lambda e, h=h: e.max(out=top8[:, h, :], in_=gatef[:, h, :]), ["gatef"], ["top8"])
                            op("dve", lambda e: e.tensor_scalar(out=thr[:, :], in0=top8[:, :, 2], scalar1=-1e29, scalar2=None, op0=ALU.max), ["top8"], ["thr"])
                            op("dve", lambda e: e.tensor_tensor(out=nsel[:, :, :], in0=gatef[:, :, :], in1=thr[:, :].unsqueeze(2).broadcast_to([128, NH, 16]), op=ALU.is_lt),
                               ["gatef", "thr"], ["nsel"])
                            op("dve", lambda e, sub=sub: e.tensor_scalar(out=Qaug[:, sub, :, 64:80], in0=nsel[:, :, :], scalar1=NEG, scalar2=None, op0=ALU.mult),
                               ["nsel"], ["Qaug"])
                        op("dve", lambda e, sub=sub, cur=cur: e.memset(Qaug[:, sub, :, 64 + cur:65 + cur], 0.0), [], ["Qaug"])
                        for h in range(NH):
                            tr(psT[0:82, h * 128:(h + 1) * 128], Qaug[:, sub, h, :], identb[:, :], ["Qaug", "identb"], ["psT"])
                        op("dve", lambda e, sub=sub: e.tensor_copy(out=qTa[0:82, :, sub * 128:(sub + 1) * 128],
                                                                   in_=psT[0:82, :].rearrange("p (k t) -> p k t", k=NH)), ["psT"], ["qTa"])
                    else:
                        headnorm(ps, pk, 4, 128, BC_GMQ, mqn[:, sub, :].rearrange("p (h d) -> p h d", h=4), "mqn")
                        for mh in range(4):
                            tr(psT[:, mh * 128:(mh + 1) * 128], mqn[:, sub, mh * 128:(mh + 1) * 128], identb[:, :], ["mqn", "identb"], ["psT"])
                        op("dve", lambda e, sub=sub: e.tensor_copy(out=mqT[:, :, sub * 128:(sub + 1) * 128],
                                                                   in_=psT[:, 0:512].rearrange("p (k t) -> p k t", k=4)), ["psT"], ["mqT"])
                if name == "v":
                    dma("sp", kt_scr[:, :, t0:t0 + NT].rearrange("h r k -> r h k"), KTloc[0:82, :, :], ["KTloc"], [("kscr", c)], "st_kscr")
                    for t4 in range(4):
                        dma("sp", v_scr[:, 4 * c + t4, :, :].rearrange("h k c -> k h c"), Vloc[:, t4, :, :], ["Vloc"], [("vscr", c)], "st_vscr")

            nexp = 0
            for h in range(NH):
                acc, ak = psA[h % 4], PA[h % 4]
                first = True
                for g in range(c + 1):
                    sl = kvstate["i"] % 4
                    kvstate["i"] += 1
                    dma("sp", kvK[0:82, sl, :], kt_scr[h, :, g * NT:(g + 1) * NT], [("kscr", g)], [("kvK", sl)], "kvK%d" % sl)
                    dma("sp", kvV[:, sl, :, :], v_scr[h, 4 * g:4 * g + 4, :, :].rearrange("t k c -> k t c"), [("vscr", g)], [("kvV", sl)], "kvV%d" % sl)
                    for tile in range(4):
                        kt = 4 * g + tile
                        diag = (g == c)
                        col0 = 128 * tile if diag else 0
                        last = diag and tile == 3
                        sp_, sk = psA[4 + nexp % 2], PA[4 + nexp % 2]
                        pslot = nexp % 3
                        nexp += 1
                        mm(sp_[:, col0:NT], kvK[0:82, sl, tile * 128:(tile + 1) * 128], qTa[0:82, h, col0:NT], True, True, [("kvK", sl), "qTa"], [sk])
                        if diag:
                            mm(sp_[:, col0:col0 + 128], identb[:, :], trib[:, :], False, True, ["identb", "trib"], [sk])
                        bcol = h * 32 + (kt - 4 * c + 28)
                        op("act", lambda e, sp_=sp_, pslot=pslot, col0=col0, bcol=bcol: e.activation(
                            out=PT[:, pslot, col0:NT], in_=sp_[:, col0:NT], func=AF.Exp, bias=btab[:, bcol:bcol + 1], scale=1.0),
                           [sk, "btab"], [("PT", pslot)])
                        mm(acc[:, col0:NT], kvV[:, sl, tile, :], PT[:, pslot, col0:NT], first, last, [("kvV", sl), ("PT", pslot)], [ak])
                        first = False
                op("dve", lambda e, acc=acc: e.reciprocal(out=rden[0:64, :], in_=acc[0:64, :]), [ak], ["rden"])
                op("dve", lambda e, acc=acc, h=h: e.tensor_tensor(out=OT[64 * (h % 2):64 * (h % 2) + 64, h // 2, :], in0=acc[64:128, :], in1=rden[0:64, :], op=ALU.mult),
                   [ak, "rden"], ["OT"])

            for mh in range(4):
                o_ps, ok = psA[2 * (mh % 2)], PA[2 * (mh % 2)]
                d_ps, dk = psA[2 * (mh % 2) + 1], PA[2 * (mh % 2) + 1]
                for t in range(2):
                    sp_, sk = psA[4 + nexp % 2], PA[4 + nexp % 2]
                    pslot = nexp % 3
                    nexp += 1
                    mm(sp_[:, :], mkT[:, mh, t * 128:(t + 1) * 128], mqT[:, mh, :], True, True, ["mkT", "mqT"], [sk])
                    op("act", lambda e, sp_=sp_, pslot=pslot: e.activation(out=PT[:, pslot, :], in_=sp_[:, :], func=AF.Exp, bias=mvals[:, 5:6], scale=1.0),
                       [sk, "mvals"], [("PT", pslot)])
                    mm(o_ps[:, :], mVa[:, t, mh * 128:(mh + 1) * 128], PT[:, pslot, :], t == 0, t == 1, ["mVa", ("PT", pslot)], [ok])
                    mm(d_ps[:, :], onesb[:, :], PT[:, pslot, :], t == 0, t == 1, ["onesb", ("PT", pslot)], [dk])
                op("dve", lambda e, d_ps=d_ps: e.reciprocal(out=rden[:, :], in_=d_ps[:, :]), [dk], ["rden"])
                op("dve", lambda e, o_ps=o_ps, mh=mh: e.tensor_tensor(out=OmT[:, mh, :], in0=o_ps[:, :], in1=rden[:, :], op=ALU.mult), [ok, "rden"], ["OmT"])

            Wpa, kpa = load_w(wpa_v, 4, 1024)
            Wpb, kpb = load_w(wpb_v, 4, 1024)
            Wpc, kpc = load_w(wpc_v, 4, 1024)
            for dc in range(8):
                srcs = ((Wpa, kpa, sT, "sT"), (Wpb, kpb, OT, "OT"), (Wpc, kpc, OmT, "OmT"))
                for j in range(3):
                    Wp, kp, src, skey = srcs[j]
                    for kc in range(4):
                        mm(psA[j][:, :], Wp[:, kc, dc * 128:(dc + 1) * 128], src[:, kc, :], kc == 0, kc == 3, [kp, skey], [PA[j]])
                for j in range(3):
                    Wgt, kgt = load_g(win_v[:, :, 3072 + j * 1024 + dc * 128:3072 + j * 1024 + (dc + 1) * 128])
                    for kc in range(8):
                        mm(psA[3 + j][:, :], Wgt[:, kc, :], hT[:, kc, :], kc == 0, kc == 7, [kgt, "hT"], [PA[3 + j]])
                    op("act", lambda e, j=j, dc=dc: e.activation(out=sig[:, j, :], in_=psA[3 + j][:, :], func=AF.Sigmoid,
                                                                 bias=fm[:, FM_BG + j * 8 + dc:FM_BG + j * 8 + dc + 1], scale=1.0),
                       [PA[3 + j], "fm"], [("cT", j)])
                op("dve", lambda e: e.tensor_tensor(out=tmpA[:, :], in0=psA[0][:, :], in1=sig[:, 0, :], op=ALU.mult), [PA[0], ("cT", 0)], ["tmpA"])
                op("dve", lambda e: e.tensor_tensor(out=tmpB[:, :], in0=psA[1][:, :], in1=sig[:, 1, :], op=ALU.mult), [PA[1], ("cT", 1)], ["tmpB"])
                op("dve", lambda e: e.tensor_tensor(out=tmpA[:, :], in0=tmpA[:, :], in1=tmpB[:, :], op=ALU.add), ["tmpA", "tmpB"], ["tmpA"])
                op("dve", lambda e: e.tensor_tensor(out=tmpB[:, :], in0=psA[2][:, :], in1=sig[:, 2, :], op=ALU.mult), [PA[2], ("cT", 2)], ["tmpB"])
                op("dve", lambda e, dc=dc: e.tensor_tensor(out=merged[:, dc, :], in0=tmpA[:, :], in1=tmpB[:, :], op=ALU.add), ["tmpA", "tmpB"], ["merged"])
            for half in range(2):
                W, wk = load_w(wout_v[:, :, half * 512:(half + 1) * 512], 8, 512)
                for sub in range(4):
                    for kc in range(8):
                        mm(psA[sub][:, :], merged[:, kc, sub * 128:(sub + 1) * 128], W[:, kc, :], kc == 0, kc == 7, ["merged", wk], [PA[sub]])
                    op("dve", lambda e, sub=sub, half=half: e.tensor_tensor(out=X[:, sub, half * 512:(half + 1) * 512], in0=X[:, sub, half * 512:(half + 1) * 512],
                                                                            in1=psA[sub][:, :], op=ALU.add), ["X", PA[sub]], ["X"])

            norm_to_T(X, 4, FM_G2, hT, "hT", "X")
            for piece in range(11):
                W, wk = load_w(wup_v[:, :, piece * 512:(piece + 1) * 512], 8, 512)
                for jj in range(4):
                    j = piece * 4 + jj
                    ps, pk = psA[j % 4], PA[j % 4]
                    cb, ck = cbuf[:, j % 3, :], ("cbuf", j % 3)
                    for kc in range(8):
                        mm(ps[:, :], W[:, kc, jj * 128:(jj + 1) * 128], hT[:, kc, :], kc == 0, kc == 7, [wk, "hT"], [pk])
                    w0 = fm[:, FM_WDF + 3 * j:FM_WDF + 3 * j + 1]
                    w1 = fm[:, FM_WDF + 3 * j + 1:FM_WDF + 3 * j + 2]
                    w2 = fm[:, FM_WDF + 3 * j + 2:FM_WDF + 3 * j + 3]
                    bb = fm[:, FM_BDF + j:FM_BDF + j + 1]
                    op("act", lambda e, ps=ps, cb=cb, w2=w2, bb=bb: e.activation(out=cb, in_=ps[:, :], func=AF.Identity, scale=w2, bias=bb), [pk, "fm"], [ck])
                    op("dve", lambda e, ps=ps, cb=cb, w1=w1: e.scalar_tensor_tensor(out=cb[:, 1:NT], in0=ps[:, 0:NT - 1], scalar=w1, in1=cb[:, 1:NT], op0=ALU.mult, op1=ALU.add),
                       [pk, ck, "fm"], [ck])
                    op("dve", lambda e, ps=ps, cb=cb, w0=w0: e.scalar_tensor_tensor(out=cb[:, 2:NT], in0=ps[:, 0:NT - 2], scalar=w0, in1=cb[:, 2:NT], op0=ALU.mult, op1=ALU.add),
                       [pk, ck, "fm"], [ck])
                    op("dve", lambda e, cb=cb, w0=w0, j=j: e.scalar_tensor_tensor(out=cb[:, 0:2], in0=hist[:, j, 0:2], scalar=w0, in1=cb[:, 0:2], op0=ALU.mult, op1=ALU.add),
                       [ck, ("hist", j), "fm"], [ck])
                    op("dve", lambda e, cb=cb, w1=w1, j=j: e.scalar_tensor_tensor(out=cb[:, 0:1], in0=hist[:, j, 1:2], scalar=w1, in1=cb[:, 0:1], op0=ALU.mult, op1=ALU.add),
                       [ck, ("hist", j), "fm"], [ck])
                    op("dve", lambda e, ps=ps, j=j: e.tensor_copy(out=hist[:, j, :], in_=ps[:, NT - 2:NT]), [pk], [("hist", j)])
                    if j < 22:
                        op("act", lambda e, cb=cb, j=j: e.activation(out=sa[:, j, :], in_=cb, func=AF.Silu), [ck], [("sa", j)])
                    else:
                        op("dve", lambda e, cb=cb, j=j: e.tensor_tensor(out=sa[:, j - 22, :], in0=sa[:, j - 22, :], in1=cb, op=ALU.mult), [ck, ("sa", j - 22)], [("sa", j - 22)])
            sakeys = [("sa", j) for j in range(22)]
            for half in range(2):
                for kp, (k0, k1) in enumerate(((0, 8), (8, 16), (16, 22))):
                    W, wk = load_w(wdn_v[:, k0:k1, half * 512:(half + 1) * 512], k1 - k0, 512)
                    for sub in range(4):
                        for kc in range(k0, k1):
                            mm(psA[sub][:, :], sa[:, kc, sub * 128:(sub + 1) * 128], W[:, kc - k0, :], kc == 0, kc == 21, sakeys + [wk], [PA[sub]])
                for sub in range(4):
                    op("dve", lambda e, sub=sub, half=half: e.tensor_tensor(out=X[:, sub, half * 512:(half + 1) * 512], in0=X[:, sub, half * 512:(half + 1) * 512],
                                                                            in1=psA[sub][:, :], op=ALU.add), ["X", PA[sub]], ["X"])
            dma("sp", y_o[t0:t0 + NT, :].rearrange("(s p) d -> p s d", p=128), X[:, :, :], ["X"], ["hbm_y"], "st_y")


        if do_sample:
            sc = X[:, 1:4, :].rearrange("p a t -> p (a t)")
            dma("sp", X[:, 0, :], xs_in[:, :], [], ["X"], "xload")
            dma("sp", sc[:, 0:SC_N], sc_in[:, :], [], ["X"], "xload")
            RROW = lambda n: sc[:, SC_RROW + n * 128:SC_RROW + (n + 1) * 128]
            ONEROW = lambda n: sc[:, SC_CS + 3 - n:SC_CS + 131 - n]
            ONECOL = lambda r: sc[:, SC_ST + 127 - r:SC_ST + 255 - r]
            REP = sc[:, SC_REP:SC_REP + 128]
            E8 = lambda h: sc[0:8, SC_E8 + h * 128:SC_E8 + (h + 1) * 128]
            ALT = sc[:, SC_AL:SC_AL + 512]
            PIOTA = sc[:, SC_PI:SC_PI + 1]
            IND120 = sc[0:120, SC_IND:SC_IND + 128]
            dma("sp", idx_t[:, :], pt_in[:, :], [], ["idx_t"], "c7")
            op("dve", lambda e: e.tensor_copy(out=ptf[:, :], in_=idx_t[:, :]), ["idx_t"], ["ptf"])
            op("dve", lambda e: e.tensor_scalar(out=ptf[:, :], in0=ptf[:, :], scalar1=128.0, scalar2=None, op0=ALU.mult), ["ptf"], ["ptf"])
            op("dve", lambda e: e.tensor_scalar(out=ptf[:, :], in0=ptf[:, :], scalar1=PIOTA, scalar2=None, op0=ALU.add), ["ptf", "X"], ["ptf"])
            op("dve", lambda e: e.tensor_copy(out=idx_t[:, :], in_=ptf[:, :]), ["ptf"], ["idx_t"])

            for _k in ("stc", "prd", "qb", "mg"):
                S.alias[_k] = ["uT"]
            S.alias["mg"] = ["uT", "stc", "prd"]
            S.alias["cs"] = [("sa", j) for j in range(22)] + ["KTloc", "qTa", "mqn", "qTp"]
            S.alias["stf"] = ["Vloc"]
            norm_to_T(X, 1, FM_G1, hT, "hT", "X")
            us, qs, mqs, ks = cT[:, 0, :], cT[:, 1, :], cT[:, 2, :], cT[:, 3, :]
            vs, stc, prd, qb = uT[:, 0, 0:512], uT[:, 1, 0:512], uT[:, 2, 0:512], uT[:, 3, 0:512]

            def bload(dst, dkey, src_row):
                dma("sp", dst, src_row.partition_broadcast(128), [], [dkey], "bl_" + str(dkey))

            def zpiece(col0):
                W, wk = load_w(win_v[:, :, col0:col0 + 512], 8, 512)
                for kc in range(8):
                    mm(psA[0][:, :], hT[:, kc, 0:128], W[:, kc, :], kc == 0, kc == 7, ["hT", wk], [PA[0]])
                return psA[0], PA[0]

            ps, pk = zpiece(0)
            op("act", lambda e: e.activation(out=tmpA[:, :], in_=psA[0][:, :], func=AF.Copy), [pk], ["tmpA"])
            op("dve", lambda e: e.tensor_copy(out=rden[:, :], in_=tmpA[:, :]), ["tmpA"], ["rden"])
            ps, pk = zpiece(512)
            op("act", lambda e: e.activation(out=tmpB[:, :], in_=psA[0][:, :], func=AF.Sigmoid), [pk], ["tmpB"])
            op("dve", lambda e: e.tensor_tensor(out=us, in0=rden[:, :], in1=tmpB[:, :], op=ALU.mult), ["rden", "tmpB"], [("cT", 0)])
            ps, pk = zpiece(1024)
            headnorm(ps, pk, NH, HD, BC_GQ, qs.rearrange("p (h d) -> p h d", h=NH), ("cT", 1))
            ps, pk = zpiece(1536)
            headnorm(ps, pk, NH, HD, BC_GK, ks.rearrange("p (h d) -> p h d", h=NH), ("cT", 3))
            dma("sp", ks_o[:, :], cT[0:4, 3, :], [("cT", 3)], ["hbm_ks"], "st_s1")
            ps, pk = zpiece(2048)
            op("act", lambda e: e.activation(out=vs, in_=psA[0][:, :], func=AF.Copy), [pk], ["uT"])
            dma("sp", vs_o[:, :], uT[0:4, 0, 0:512], ["uT"], ["hbm_vs"], "st_s2")
            ps, pk = zpiece(2560)
            headnorm(ps, pk, 4, 128, BC_GMQ, mqs.rearrange("p (h d) -> p h d", h=4), ("cT", 2))

            dma("sp", uT[0:120, 1, 0:512], stc_in[:, :], [], ["stc"], "c8")
            dma("sp", uT[0:120, 2, 0:512], w120_in[:, :], [], ["prd"], "c9")
            for n in range(4):
                dma("sp", convs_o[n, 0:29, :], uT[n * 30 + 1:n * 30 + 30, 1, 0:512], ["stc"], ["hbm_convs"], "st_s3")
            dma("sp", convs_o[:, 29, :], cT[0:4, 0, :], [("cT", 0)], ["hbm_convs"], "st_s3")
            op("dve", lambda e: e.tensor_tensor(out=uT[0:120, 2, 0:512], in0=uT[0:120, 2, 0:512], in1=uT[0:120, 1, 0:512], op=ALU.mult), ["stc", "prd"], ["prd"])
            mm(psA[1][:, :], IND120, uT[0:120, 2, 0:512], True, True, ["prd", "X"], [PA[1]])
            bload(cbuf[:, 0, :], ("cbuf", 0), vrow_in[0, 0:512])
            bload(cbuf[:, 1, :], ("cbuf", 1), vrow_in[1, 0:512])
            op("dve", lambda e: e.tensor_tensor(out=tmpA[:, :], in0=us, in1=cbuf[:, 0, :], op=ALU.mult), [("cT", 0), ("cbuf", 0)], ["tmpA"])
            op("dve", lambda e: e.tensor_tensor(out=tmpA[:, :], in0=tmpA[:, :], in1=cbuf[:, 1, :], op=ALU.add), ["tmpA", ("cbuf", 1)], ["tmpA"])
            op("dve", lambda e: e.tensor_tensor(out=tmpA[:, :], in0=tmpA[:, :], in1=psA[1][:, :], op=ALU.add), ["tmpA", PA[1]], ["tmpA"])
            op("dve", lambda e: e.tensor_reduce(out=ss[:, 0:1], in_=tmpA[:, :], axis=AX.X, op=ALU.add), ["tmpA"], [("ss", 0)])
            op("dve", lambda e: e.tensor_scalar(out=ss[:, 0:1], in0=ss[:, 0:1], scalar1=1.0 / 512, scalar2=None, op0=ALU.mult), [("ss", 0)], [("ss", 0)])
            op("dve", lambda e: e.tensor_scalar(out=tmpA[:, :], in0=tmpA[:, :], scalar1=ss[:, 0:1], scalar2=None, op0=ALU.subtract), ["tmpA", ("ss", 0)], ["tmpA"])
            op("dve", lambda e: e.memset(ss[:, 1:2], 0.0), [], [("ss", 1)])
            op("act", lambda e: e.activation(out=tmpB[:, :], in_=tmpA[:, :], func=AF.Square, accum_out=ss[:, 1:2]), ["tmpA", ("ss", 1)], ["tmpB", ("ss", 1)])
            op("act", lambda e: e.activation(out=ss[:, 1:2], in_=ss[:, 1:2], func=AF.Sqrt, bias=epsb[:, 0:1], scale=1.0 / 512), [("ss", 1), "epsb"], [("ss", 1)])
            op("dve", lambda e: e.reciprocal(out=ss[:, 1:2], in_=ss[:, 1:2]), [("ss", 1)], [("ss", 1)])
            bload(cbuf[:, 0, :], ("cbuf", 0), vrow_in[2, 0:512])
            bload(cbuf[:, 1, :], ("cbuf", 1), vrow_in[3, 0:512])
            op("dve", lambda e: e.tensor_scalar(out=tmpA[:, :], in0=tmpA[:, :], scalar1=ss[:, 1:2], scalar2=None, op0=ALU.mult), ["tmpA", ("ss", 1)], ["tmpA"])
            op("dve", lambda e: e.tensor_tensor(out=tmpA[:, :], in0=tmpA[:, :], in1=cbuf[:, 0, :], op=ALU.mult), ["tmpA", ("cbuf", 0)], ["tmpA"])
            op("dve", lambda e: e.tensor_tensor(out=tmpA[:, :], in0=tmpA[:, :], in1=cbuf[:, 1, :], op=ALU.add), ["tmpA", ("cbuf", 1)], ["tmpA"])
            op("act", lambda e: e.activation(out=xn[:, 0:512], in_=tmpA[:, :], func=AF.Silu), ["tmpA"], ["xn"])
            dbgout(xn[0:4, 0:512], [4, 512], ["xn"])
            for kc in range(4):
                tr(psT[:, kc * 128:(kc + 1) * 128], xn[:, kc * 128:(kc + 1) * 128], identb[:, :], ["xn", "identb"], ["psT"])
            op("dve", lambda e: e.tensor_copy(out=sT[:, :, 0:128], in_=psT[:, 0:512].rearrange("p (k t) -> p k t", k=4)), ["psT"], ["sT"])

            Lt = bc
            pbufs = [(kst[:, 0, :], ("kst", 0)), (kst[:, 1, :], ("kst", 1)), (vst[:, 0, :], ("vst", 0)), (vst[:, 1, :], ("vst", 1))]
            pstate = {"i": 0}

            def get_page(src_kind, n, pg):
                buf, bk = pbufs[pstate["i"] % 4]
                pstate["i"] += 1
                if src_kind in ("k", "v"):
                    src = ck_in if src_kind == "k" else cv_in
                    col = n * 64 + pg
                    op("pool", lambda e: e.indirect_dma_start(out=buf, out_offset=None, in_=src[:, :],
                                                              in_offset=bass.IndirectOffsetOnAxis(ap=idx_t[:, col:col + 1], axis=0)),
                       ["idx_t"], [bk], dma=True, semkey="pg_" + str(bk))
                else:
                    src = cmk_in if src_kind == "mk" else cmv_in
                    dma("sp", buf, src[n, pg * 128:(pg + 1) * 128, :], [], [bk], "pg_" + str(bk))
                return buf, bk

            def bcast_row(n, src, skey):
                mm(psA[2][:, :], RROW(n), src, True, True, ["X", skey], [PA[2]])
                op("act", lambda e: e.activation(out=qb, in_=psA[2][:, :], func=AF.Copy), [PA[2]], ["qb"])

            def dense_attn(kkind, vkind, npg, nh, hd, qsrc, qkey, moba):
                G = npg * nh
                first_sum = [True]
                for n in range(4):
                    bcast_row(n, qsrc, qkey)
                    for pg in range(npg):
                        buf, bk = get_page(kkind, n, pg)
                        op("dve", lambda e, buf=buf: e.tensor_tensor(out=tmpA[:, :], in0=buf, in1=qb, op=ALU.mult), [bk, "qb"], ["tmpA"])
                        o0 = n * G + pg * nh
                        op("dve", lambda e, o0=o0: e.tensor_reduce(out=Lt[:, o0:o0 + nh], in_=tmpA[:, :].rearrange("p (h d) -> p h d", h=nh), axis=AX.X, op=ALU.add),
                           ["tmpA"], ["bc"])
                        if moba:
                            r = n * 32 + pg // 2
                            last = (n == 3 and pg == npg - 1)
                            mm(psA[3][:, :], ONECOL(r), buf, first_sum[0], last, ["X", bk], [PA[3]])
                            first_sum[0] = False
                if moba:
                    op("dve", lambda e: e.tensor_scalar(out=tmpB[:, :], in0=psA[3][:, :], scalar1=1.0 / 256, scalar2=None, op0=ALU.mult), [PA[3]], ["tmpB"])
                    mm(psA[2][:, :], REP, qsrc, True, True, ["X", qkey], [PA[2]])
                    op("dve", lambda e: e.tensor_tensor(out=tmpA[:, :], in0=tmpB[:, :], in1=psA[2][:, :], op=ALU.mult), ["tmpB", PA[2]], ["tmpA"])
                    op("dve", lambda e: e.tensor_reduce(out=gatef[:, 0, 0:8], in_=tmpA[:, :].rearrange("p (h d) -> p h d", h=NH), axis=AX.X, op=ALU.add), ["tmpA"], ["gatef"])
                    op("pe", lambda e: e.transpose(out=psM[0:8, 0:128], in_=gatef[:, 0, 0:8], identity=identf[:, :]), ["gatef", "identf"], ["psM"])
                    op("dve", lambda e: e.tensor_copy(out=rden[0:8, 0:128], in_=psM[0:8, 0:128]), ["psM"], ["rden"])
                    for n in range(4):
                        op("dve", lambda e, n=n: e.max(out=top8[0:8, n, :], in_=rden[0:8, n * 32:(n + 1) * 32]), ["rden"], ["top8"])
                    op("dve", lambda e: e.tensor_tensor(out=rden[0:8, 128:256].rearrange("p (n b) -> p n b", n=4), in0=rden[0:8, 0:128].rearrange("p (n b) -> p n b", n=4),
                                                        in1=top8[0:8, 0:4, 2:3].broadcast_to([8, 4, 32]), op=ALU.is_ge), ["rden", "top8"], ["rden"])
                    op("dve", lambda e: e.tensor_copy(out=tmpB[0:8, 0:256].rearrange("p (g e) -> p g e", e=2), in_=rden[0:8, 128:256].unsqueeze(2).broadcast_to([8, 128, 2])),
                       ["rden"], ["tmpB"])
                for n in range(4):
                    if moba:
                        op("dve", lambda e, n=n: e.tensor_tensor(out=Lt[:, n * G:(n + 1) * G], in0=Lt[:, n * G:(n + 1) * G], in1=ALT, op=ALU.add), ["bc", "X"], ["bc"])
                    op("dve", lambda e, n=n: e.tensor_reduce(out=thr[:, n:n + 1], in_=Lt[:, n * G:(n + 1) * G], axis=AX.X, op=ALU.max), ["bc"], ["thr"])
                op("pe", lambda e: e.transpose(out=psM[0:4, 128:256], in_=thr[:, 0:4], identity=identf[:, :]), ["thr", "identf"], ["psM"])
                op("dve", lambda e: e.tensor_reduce(out=mvals[0:4, 6:7], in_=psM[0:4, 128:256], axis=AX.X, op=ALU.max), ["psM"], ["mvals"])
                if moba:
                    op("dve", lambda e: e.tensor_tensor(out=tmpA[:, :], in0=qs, in1=ks, op=ALU.mult), [("cT", 1), ("cT", 3)], ["tmpA"])
                    op("dve", lambda e: e.tensor_reduce(out=nsel[:, 0, 0:8], in_=tmpA[:, :].rearrange("p (h d) -> p h d", h=NH), axis=AX.X, op=ALU.add), ["tmpA"], ["nsel"])
                    op("dve", lambda e: e.tensor_reduce(out=mvals[0:4, 7:8], in_=nsel[0:4, 0, 0:8], axis=AX.X, op=ALU.max), ["nsel"], ["mvals"])
                    op("dve", lambda e: e.tensor_tensor(out=mvals[0:4, 6:7], in0=mvals[0:4, 6:7], in1=mvals[0:4, 7:8], op=ALU.max), ["mvals"], ["mvals"])
                op("dve", lambda e: e.tensor_scalar(out=mvals[0:4, 6:7], in0=mvals[0:4, 6:7], scalar1=-1.0, scalar2=None, op0=ALU.mult), ["mvals"], ["mvals"])
                op("dve", lambda e: e.tensor_scalar(out=nsel[0:4, 1, 0:4], in0=identf[0:4, 0:4], scalar1=mvals[0:4, 6:7], scalar2=None, op0=ALU.mult), ["mvals", "identf"], ["nsel"])
                mm(psM[:, 256:260], onesf[0:4, :], nsel[0:4, 1, 0:4], True, True, ["onesf", "nsel"], ["psM"])
                op("dve", lambda e: e.tensor_copy(out=thr[:, 4:8], in_=psM[:, 256:260]), ["psM"], ["thr"])
                for n in range(4):
                    op("act", lambda e, n=n: e.activation(out=Lt[:, n * G:(n + 1) * G], in_=Lt[:, n * G:(n + 1) * G], func=AF.Exp, bias=thr[:, 4 + n:5 + n], scale=1.0),
                       ["bc", "thr"], ["bc"])
                if moba:
                    op("act", lambda e: e.activation(out=nsel[0:4, 0, 0:8], in_=nsel[0:4, 0, 0:8], func=AF.Exp, bias=mvals[0:4, 6:7], scale=1.0), ["nsel", "mvals"], ["nsel"])
                    for h in range(NH):
                        mm(psA[h // 2][:, (h % 2) * 256:(h % 2) * 256 + 256], E8(h), tmpB[0:8, 0:256], True, True, ["X", "tmpB"], [PA[h // 2]])
                    for hp in range(4):
                        op("dve", lambda e, hp=hp: e.tensor_tensor(
                            out=Lt[:, :].rearrange("p (g h) -> p g h", h=NH)[:, :, 2 * hp:2 * hp + 2],
                            in0=Lt[:, :].rearrange("p (g h) -> p g h", h=NH)[:, :, 2 * hp:2 * hp + 2],
                            in1=psA[hp][:, :].rearrange("p (h g) -> p g h", h=2), op=ALU.mult), ["bc", PA[hp]], ["bc"])
                for n in range(4):
                    for c0 in range(0, G, 512):
                        w = min(512, G - c0)
                        mm(psA[4][:, 0:w], ONEROW(n), Lt[:, n * G + c0:n * G + c0 + w], n == 0 and c0 == 0, n == 3 and c0 + w >= G, ["X", "bc"], [PA[4]])
                Gc = min(G, 512)
                op("dve", lambda e: e.tensor_reduce(out=ss[:, 8:8 + nh], in_=psA[4][:, 0:Gc].rearrange("p (g h) -> p h g", h=nh), axis=AX.X, op=ALU.add), [PA[4]], ["ss8"])
                firstv = [True]
                for n in range(4):
                    for pg in range(npg):
                        buf, bk = get_page(vkind, n, pg)
                        o0 = n * G + pg * nh
                        op("dve", lambda e, buf=buf, o0=o0: e.tensor_tensor(out=tmpA[:, :].rearrange("p (h d) -> p h d", h=nh), in0=buf.rearrange("p (h d) -> p h d", h=nh),
                                                                          in1=Lt[:, o0:o0 + nh].unsqueeze(2).broadcast_to([128, nh, hd]), op=ALU.mult), [bk, "bc"], ["tmpA"])
                        mm(psA[5][:, :], ONEROW(n), tmpA[:, :], firstv[0], n == 3 and pg == npg - 1, ["X", "tmpA"], [PA[5]])
                        firstv[0] = False

            dense_attn("k", "v", 64, NH, HD, qs, ("cT", 1), True)
            op("dve", lambda e: e.tensor_tensor(out=ss[:, 8:16], in0=ss[:, 8:16], in1=nsel[:, 0, 0:8], op=ALU.add), ["ss8", "nsel"], ["ss8"])
            op("dve", lambda e: e.tensor_scalar(out=ss[:, 8:16], in0=ss[:, 8:16], scalar1=1e-30, scalar2=None, op0=ALU.add), ["ss8"], ["ss8"])
            op("dve", lambda e: e.reciprocal(out=ss[:, 8:16], in_=ss[:, 8:16]), ["ss8"], ["ss8"])
            op("dve", lambda e: e.tensor_tensor(out=tmpB[:, :].rearrange("p (h d) -> p h d", h=NH), in0=vs.rearrange("p (h d) -> p h d", h=NH),
                                                in1=nsel[:, 0, 0:8].unsqueeze(2).broadcast_to([128, NH, HD]), op=ALU.mult), ["uT", "nsel"], ["tmpB"])
            op("dve", lambda e: e.tensor_tensor(out=tmpB[:, :], in0=tmpB[:, :], in1=psA[5][:, :], op=ALU.add), ["tmpB", PA[5]], ["tmpB"])
            op("dve", lambda e: e.tensor_tensor(out=xn[:, 0:512].rearrange("p (h d) -> p h d", h=NH), in0=tmpB[:, :].rearrange("p (h d) -> p h d", h=NH),
                                                in1=ss[:, 8:16].unsqueeze(2).broadcast_to([128, NH, HD]), op=ALU.mult), ["tmpB", "ss8"], ["xn"])
            dbgout(rden[0:8, 0:256], [8, 256], ["rden"])
            dbgout(ss[0:4, 8:16], [4, 8], ["ss8"])
            dbgout(xn[0:4, 0:512], [4, 512], ["xn"])
            dbgout(bc[:, :], [128, 2048], ["bc"])
            dbgout(nsel[0:4, 0, 0:8], [4, 8], ["nsel"])
            dbgout(tmpB[0:4, :], [4, 512], ["tmpB"])
            for kc in range(4):
                tr(psT[:, kc * 128:(kc + 1) * 128], xn[:, kc * 128:(kc + 1) * 128], identb[:, :], ["xn", "identb"], ["psT"])
            op("dve", lambda e: e.tensor_copy(out=OT[:, :, 0:128], in_=psT[:, 0:512].rearrange("p (k t) -> p k t", k=4)), ["psT"], ["OT"])

            dense_attn("mk", "mv", 2, 4, 128, mqs, ("cT", 2), False)
            op("dve", lambda e: e.tensor_scalar(out=ss[:, 8:12], in0=ss[:, 8:12], scalar1=1e-30, scalar2=None, op0=ALU.add), ["ss8"], ["ss8"])
            op("dve", lambda e: e.reciprocal(out=ss[:, 8:12], in_=ss[:, 8:12]), ["ss8"], ["ss8"])
            op("dve", lambda e: e.tensor_tensor(out=xn[:, 0:512].rearrange("p (h d) -> p h d", h=4), in0=psA[5][:, :].rearrange("p (h d) -> p h d", h=4),
                                                in1=ss[:, 8:12].unsqueeze(2).broadcast_to([128, 4, 128]), op=ALU.mult), [PA[5], "ss8"], ["xn"])
            dbgout(xn[0:4, 0:512], [4, 512], ["xn"])
            for kc in range(4):
                tr(psT[:, kc * 128:(kc + 1) * 128], xn[:, kc * 128:(kc + 1) * 128], identb[:, :], ["xn", "identb"], ["psT"])
            op("dve", lambda e: e.tensor_copy(out=OmT[:, :, 0:128], in_=psT[:, 0:512].rearrange("p (k t) -> p k t", k=4)), ["psT"], ["OmT"])

            mg = uT[:, 1:3, :].rearrange("p a t -> p (a t)")[:, 0:1024]
            for half in range(2):
                srcs = ((wpa_v, sT, "sT"), (wpb_v, OT, "OT"), (wpc_v, OmT, "OmT"))
                for j in range(3):
                    wpv, src, skey = srcs[j]
                    Wp, kp = load_w(wpv[:, :, half * 512:(half + 1) * 512], 4, 512)
                    for kc in range(4):
                        mm(psA[j][:, :], src[:, kc, 0:128], Wp[:, kc, :], kc == 0, kc == 3, [kp, skey], [PA[j]])
                    col0 = 3072 + j * 1024 + half * 512
                    W, wk = load_w(win_v[:, :, col0:col0 + 512], 8, 512)
                    for kc in range(8):
                        mm(psA[3][:, :], hT[:, kc, 0:128], W[:, kc, :], kc == 0, kc == 7, ["hT", wk], [PA[3]])
                    bload(cbuf[:, 2, :], ("cbuf", 2), vrow_in[4, j * 1024 + half * 512:j * 1024 + half * 512 + 512])
                    op("dve", lambda e: e.tensor_tensor(out=tmpA[:, :], in0=psA[3][:, :], in1=cbuf[:, 2, :], op=ALU.add), [PA[3], ("cbuf", 2)], ["tmpA"])
                    op("act", lambda e: e.activation(out=tmpA[:, :], in_=tmpA[:, :], func=AF.Sigmoid), ["tmpA"], ["tmpA"])
                    if j == 0:
                        op("dve", lambda e, half=half: e.tensor_tensor(out=mg[:, half * 512:(half + 1) * 512], in0=psA[0][:, :], in1=tmpA[:, :], op=ALU.mult), [PA[0], "tmpA"], ["mg"])
                    else:
                        op("dve", lambda e, j=j: e.tensor_tensor(out=tmpB[:, :], in0=psA[j][:, :], in1=tmpA[:, :], op=ALU.mult), [PA[j], "tmpA"], ["tmpB"])
                        op("dve", lambda e, half=half: e.tensor_tensor(out=mg[:, half * 512:(half + 1) * 512], in0=mg[:, half * 512:(half + 1) * 512], in1=tmpB[:, :], op=ALU.add),
                           ["mg", "tmpB"], ["mg"])
            op("dve", lambda e: e.tensor_copy(out=xn[:, :], in_=mg), ["mg"], ["xn"])
            dbgout(xn[0:4, :], [4, 1024], ["xn"])
            for kc in range(8):
                tr(psT[:, kc * 128:(kc + 1) * 128], xn[:, kc * 128:(kc + 1) * 128], identb[:, :], ["xn", "identb"], ["psT"])
            op("dve", lambda e: e.tensor_copy(out=merged[:, :, 0:128], in_=psT[:, :].rearrange("p (k t) -> p k t", k=8)), ["psT"], ["merged"])
            for half in range(2):
                W, wk = load_w(wout_v[:, :, half * 512:(half + 1) * 512], 8, 512)
                for kc in range(8):
                    mm(psA[0][:, :], merged[:, kc, 0:128], W[:, kc, :], kc == 0, kc == 7, ["merged", wk], [PA[0]])
                op("dve", lambda e, half=half: e.tensor_tensor(out=X[:, 0, half * 512:(half + 1) * 512], in0=X[:, 0, half * 512:(half + 1) * 512], in1=psA[0][:, :], op=ALU.add),
                   ["X", PA[0]], ["X"])

            dbgout(X[0:4, 0, :], [4, 1024], ["X"])
            norm_to_T(X, 1, FM_G2, hT, "hT", "X")
            cs = scrA[:, :].bitcast(F32)
            stf = Vloc[:, :, :, :].rearrange("p a b c -> p (a b c)").bitcast(F32)
            op("dve", lambda e: e.memset(stf[:, 0:1024], 0.0), [], ["stf"])
            for piece in range(11):
                c0 = piece * 512
                W, wk = load_w(wup_v[:, :, c0:c0 + 512], 8, 512)
                for kc in range(8):
                    mm(psA[piece % 2][:, :], hT[:, kc, 0:128], W[:, kc, :], kc == 0, kc == 7, ["hT", wk], [PA[piece % 2]])
                pp, pkk = psA[piece % 2], PA[piece % 2]
                op("act", lambda e, pp=pp: e.activation(out=tmpA[:, :], in_=pp[:, :], func=AF.Copy), [pkk], ["tmpA"])
                dma("sp", ffns_o[:, 1, c0:c0 + 512], tmpA[0:4, :], ["tmpA"], ["hbm_ffns"], "st_s4")
                dma("sp", stf[0:4, 0:512], stf_in[:, c0:c0 + 512], [], ["stf"], "ld_stf")
                dma("sp", stf[0:4, 512:1024], stf_in[:, NUP + c0:NUP + c0 + 512], [], ["stf"], "ld_stf")
                dma("sp", ffns_o[:, 0, c0:c0 + 512], stf[0:4, 512:1024], ["stf"], ["hbm_ffns"], "st_s5")
                bload(cbuf[:, 0, :], ("cbuf", 0), vrow_in[5, c0:c0 + 512])
                op("dve", lambda e, c0=c0: e.tensor_tensor(out=cs[:, c0:c0 + 512], in0=stf[:, 0:512], in1=cbuf[:, 0, :], op=ALU.mult), ["stf", ("cbuf", 0)], ["cs"])
                bload(cbuf[:, 1, :], ("cbuf", 1), vrow_in[6, c0:c0 + 512])
                op("dve", lambda e: e.tensor_tensor(out=tmpB[:, :], in0=stf[:, 512:1024], in1=cbuf[:, 1, :], op=ALU.mult), ["stf", ("cbuf", 1)], ["tmpB"])
                op("dve", lambda e, c0=c0: e.tensor_tensor(out=cs[:, c0:c0 + 512], in0=cs[:, c0:c0 + 512], in1=tmpB[:, :], op=ALU.add), [("sa", 0), "tmpB"], ["cs"])
                bload(cbuf[:, 2, :], ("cbuf", 2), vrow_in[7, c0:c0 + 512])
                op("dve", lambda e: e.tensor_tensor(out=tmpB[:, :], in0=tmpA[:, :], in1=cbuf[:, 2, :], op=ALU.mult), ["tmpA", ("cbuf", 2)], ["tmpB"])
                op("dve", lambda e, c0=c0: e.tensor_tensor(out=cs[:, c0:c0 + 512], in0=cs[:, c0:c0 + 512], in1=tmpB[:, :], op=ALU.add), [("sa", 0), "tmpB"], ["cs"])
                bload(cbuf[:, 0, :], ("cbuf", 0), vrow_in[8, c0:c0 + 512])
                op("dve", lambda e, c0=c0: e.tensor_tensor(out=cs[:, c0:c0 + 512], in0=cs[:, c0:c0 + 512], in1=cbuf[:, 0, :], op=ALU.add), [("sa", 0), ("cbuf", 0)], ["cs"])
            actb = hT[:, 0:6, :].rearrange("p a t -> p (a t)")
            for c0 in range(0, DFF, 512):
                w = min(512, DFF - c0)
                op("act", lambda e, c0=c0, w=w: e.activation(out=tmpA[:, 0:w], in_=cs[:, c0:c0 + w], func=AF.Silu), ["cs"], ["tmpA"])
                op("dve", lambda e, c0=c0, w=w: e.tensor_tensor(out=actb[:, c0:c0 + w], in0=tmpA[:, 0:w], in1=cs[:, DFF + c0:DFF + c0 + w], op=ALU.mult), ["tmpA", ("sa", 0)], ["hT"])
            actT = qTa_s = None
            aT = sT[:, :, :].rearrange("p a t -> p (a t)")
            aT2 = OT[:, :, :].rearrange("p a t -> p (a t)")
            for grp in range(3):
                k0 = grp * 8
                nk = min(8, 22 - k0)
                for kk in range(nk):
                    tr(psT[:, kk * 128:(kk + 1) * 128], actb[:, (k0 + kk) * 128:(k0 + kk + 1) * 128], identb[:, :], ["hT", "identb"], ["psT"])
                if grp < 2:
                    op("dve", lambda e, k0=k0, nk=nk: e.tensor_copy(out=aT[:, k0 * 128:(k0 + nk) * 128], in_=psT[:, 0:nk * 128]), ["psT"], ["sT"])
                else:
                    op("dve", lambda e, nk=nk: e.tensor_copy(out=aT2[:, 0:nk * 128], in_=psT[:, 0:nk * 128]), ["psT"], ["OT"])

            def aTk(kc):
                return aT[:, kc * 128:(kc + 1) * 128] if kc < 16 else aT2[:, (kc - 16) * 128:(kc - 15) * 128]

            for half in range(2):
                for kp, (k0, k1) in enumerate(((0, 8), (8, 16), (16, 22))):
                    W, wk = load_w(wdn_v[:, k0:k1, half * 512:(half + 1) * 512], k1 - k0, 512)
                    for kc in range(k0, k1):
                        mm(psA[2][:, :], aTk(kc), W[:, kc - k0, :], kc == 0, kc == 21, ["sT", "OT", wk], [PA[2]])
                op("dve", lambda e, half=half: e.tensor_tensor(out=X[:, 0, half * 512:(half + 1) * 512], in0=X[:, 0, half * 512:(half + 1) * 512], in1=psA[2][:, :], op=ALU.add),
                   ["X", PA[2]], ["X"])
            dma("sp", ys_o[:, :], X[0:4, 0, :], ["X"], ["hbm_ys"], "st_y")

        allhist = [("hist", j) for j in range(44)]
        op("pe", lambda e: e.transpose(out=psM[0:88, 0:128], in_=hist[:, :, :].rearrange("p j r -> p (j r)"), identity=identf[:, :]), allhist + ["identf"], ["psM"])
        op("dve", lambda e: e.tensor_copy(out=outst[0:88, 0:128], in_=psM[0:88, 0:128]), ["psM"], ["outst"])
        dma("sp", ffnc_o[:, :], outst[0:88, 0:128], ["outst"], ["hbm_ffnc"], "st_misc")

        S.emit(st)
    return nc


def host_tables(inp):
    f = np.float32
    fm = np.zeros((128, FM_N), f)

    def fmaj(v):
        return np.ascontiguousarray(np.asarray(v, f).reshape(-1, 128).T)

    fm[:, FM_G1:FM_G1 + 8] = fmaj(inp["norm1_g"])
    fm[:, FM_G2:FM_G2 + 8] = fmaj(inp["norm2_g"])
    fm[:, FM_GM:FM_GM + 8] = fmaj(inp["mem_norm_g"])
    fm[:, FM_BG:FM_BG + 24] = fmaj(inp["b_gate"])
    wda = np.asarray(inp["w_dw_a"], f)
    fm[:, FM_WDA:FM_WDA + 124] = wda.T.reshape(4, 128, 31).transpose(1, 0, 2).reshape(128, 124)
    fm[:, FM_BDA:FM_BDA + 4] = fmaj(inp["b_dw_a"])
    fm[:, FM_LAG:FM_LAG + 4] = fmaj(inp["ln_a_g"])
    fm[:, FM_LAB:FM_LAB + 4] = fmaj(inp["ln_a_b"])
    wdf = np.asarray(inp["w_dw_f"], f)
    fm[:, FM_WDF:FM_WDF + 132] = wdf.T.reshape(44, 128, 3).transpose(1, 0, 2).reshape(128, 132)
    fm[:, FM_BDF:FM_BDF + 44] = fmaj(inp["b_dw_f"])
    bc = np.zeros((128, BC_N), f)
    bc[:, BC_GQ:BC_GQ + 512] = np.tile(np.asarray(inp["q_norm_g"], f), 8)[None, :]
    bc[:, BC_GK:BC_GK + 512] = np.tile(np.asarray(inp["k_norm_g"], f), 8)[None, :]
    bc[:, BC_GMQ:BC_GMQ + 512] = np.tile(np.asarray(inp["mq_norm_g"], f), 4)[None, :]
    bc[:, BC_GMK:BC_GMK + 512] = np.tile(np.asarray(inp["mk_norm_g"], f), 4)[None, :]
    ident = np.eye(128, dtype=f)
    p = np.arange(128)
    tri = np.where(p[:, None] > p[None, :], f(NEG), f(0)).astype(f)
    slopes = (2.0 ** (-np.arange(1, NH + 1, dtype=np.float64))).astype(f)
    j = np.arange(32)
    abase = (slopes[None, :, None] * (p[:, None, None] + 128.0 * (j[None, None, :] - 28))).astype(f).reshape(128, NH * 32)
    dt = (np.arange(4)[None, :] * 128 + p[:, None]).astype(np.int64)
    hi = (16 * (dt // 16)).astype(f)
    lo = (dt % 16).astype(f)
    qal = np.stack([-slopes[None, None, :] * hi[:, :, None], -slopes[None, None, :] * lo[:, :, None]], axis=-1).astype(f).reshape(128, 64)
    return {"fm": fm, "bc": bc, "ident": ident, "tri": tri, "abase": abase, "qal": qal}


_NC_CACHE = {}


def run_prompt(inp, nch=NCHUNK):
    key = ("p", nch)
    if key not in _NC_CACHE:
        _NC_CACHE[key] = build(nch=nch, do_sample=False)
    nc = _NC_CACHE[key]
    tabs = host_tables(inp)
    f = np.float32
    wnames = ["w_in", "w_proj_a", "w_proj_b", "w_proj_c", "w_mem_kv", "w_out", "w_up", "w_down"]
    shared = {n: np.ascontiguousarray(np.asarray(inp[n], f)) for n in wnames}
    shared.update(tabs)
    in_maps = []
    for c in range(8):
        s = c % 4
        m = dict(shared)
        m["x"] = np.ascontiguousarray(np.asarray(inp["x_prompt"][s], f))
        m["mem"] = np.ascontiguousarray(np.asarray(inp["mem_prompt"][s], f))
        in_maps.append(m)
    res = run_bass_kernel_spmd(nc, in_maps, core_ids=list(range(8)))
    return res.results


def sample_tables(inp):
    f = np.float32
    sc = np.zeros((128, SC_N), f)
    for n in range(4):
        sc[n, SC_RROW + n * 128:SC_RROW + (n + 1) * 128] = 1.0
    sc[:, SC_CS + 3] = 1.0
    sc[:, SC_ST + 127] = 1.0
    for k in range(4):
        sc[k, SC_REP + k * 32:SC_REP + (k + 1) * 32] = 1.0
    for h in range(8):
        sc[h, SC_E8 + h * 128:SC_E8 + (h + 1) * 128] = 1.0
    slopes = (2.0 ** (-np.arange(1, NH + 1, dtype=np.float64)))
    p = np.arange(128)[:, None, None]
    pg = np.arange(64)[None, :, None]
    sc[:, SC_AL:SC_AL + 512] = (-slopes[None, None, :] * (8192.0 - 128.0 * pg - p)).astype(f).reshape(128, 512)
    sc[:, SC_PI] = np.arange(128, dtype=f)
    for n in range(4):
        sc[n * 30:(n + 1) * 30, SC_IND + n] = 1.0
    vr = np.zeros((9, 5632), f)
    vr[0, :512] = np.asarray(inp["w_dw_a"], f)[30]
    vr[1, :512] = np.asarray(inp["b_dw_a"], f)
    vr[2, :512] = np.asarray(inp["ln_a_g"], f)
    vr[3, :512] = np.asarray(inp["ln_a_b"], f)
    vr[4, :3072] = np.asarray(inp["b_gate"], f)
    vr[5:8, :] = np.asarray(inp["w_dw_f"], f)
    vr[8, :] = np.asarray(inp["b_dw_f"], f)
    w120 = np.ascontiguousarray(np.tile(np.asarray(inp["w_dw_a"], f)[:30], (4, 1)))
    return sc, vr, w120


def run_all(inp):
    key = ("all",)
    if key not in _NC_CACHE:
        _NC_CACHE[key] = build(nch=NCHUNK, do_sample=True)
    nc = _NC_CACHE[key]
    tabs = host_tables(inp)
    f = np.float32
    wnames = ["w_in", "w_proj_a", "w_proj_b", "w_proj_c", "w_mem_kv", "w_out", "w_up", "w_down"]
    shared = {n: np.ascontiguousarray(np.asarray(inp[n], f)) for n in wnames}
    shared.update(tabs)
    sc, vr, w120 = sample_tables(inp)
    shared["sconst"] = sc
    shared["vrows"] = vr
    shared["w120"] = w120
    shared["cache_k"] = np.ascontiguousarray(np.asarray(inp["cache_k"], f)).reshape(N_POOL * 128, 512)
    shared["cache_v"] = np.ascontiguousarray(np.asarray(inp["cache_v"], f)).reshape(N_POOL * 128, 512)
    pt = np.asarray(inp["page_table"]).astype(np.int32)
    in_maps = []
    for c in range(8):
        s = c % 4
        m = dict(shared)
        m["x"] = np.ascontiguousarray(np.asarray(inp["x_prompt"][s], f))
        m["mem"] = np.ascontiguousarray(np.asarray(inp["mem_prompt"][s], f))
        sl = slice(4 * c, 4 * c + 4)
        xs = np.zeros((128, D), f)
        xs[0:4] = np.asarray(inp["x_sample"], f)[sl, 0, :]
        m["xs"] = xs
        m["stc"] = np.ascontiguousarray(np.asarray(inp["state_conv"], f)[sl].reshape(120, 512))
        m["stf"] = np.ascontiguousarray(np.asarray(inp["state_ffn_conv"], f)[sl].reshape(4, 2 * NUP))
        m["cmk"] = np.ascontiguousarray(np.asarray(inp["cache_mem_k"], f)[sl].reshape(4, 256, 512))
        m["cmv"] = np.ascontiguousarray(np.asarray(inp["cache_mem_v"], f)[sl].reshape(4, 256, 512))
        m["ptrep"] = np.ascontiguousarray(np.broadcast_to(pt[sl].reshape(1, 256), (128, 256))).astype(np.int32)
        in_maps.append(m)
    res = run_bass_kernel_spmd(nc, in_maps, core_ids=list(range(8)))
    return res.results


def kernel(**inp):
    r = run_all(inp)
    f = np.float32
    y_p = np.stack([r[s]["y"] for s in range(4)]).astype(f)
    k_p = np.stack([r[s]["k"] for s in range(4)]).reshape(4, SEQ, NH, HD).astype(f)
    v_p = np.stack([r[s]["v"] for s in range(4)]).reshape(4, SEQ, NH, HD).astype(f)
    conv_p = np.stack([r[s]["conv"][2:32] for s in range(4)]).astype(f)
    ffn_p = np.stack([r[s]["ffnc"].reshape(44, 2, 128).transpose(1, 0, 2).reshape(2, NUP) for s in range(4)]).astype(f)
    mk_p = np.stack([r[s]["mk"] for s in range(4)]).reshape(4, 256, 4, 128).astype(f)
    mv_p = np.stack([r[s]["mv"] for s in range(4)]).reshape(4, 256, 4, 128).astype(f)
    y_s = np.concatenate([r[c]["ys"] for c in range(8)]).reshape(32, 1, D).astype(f)
    k_s = np.concatenate([r[c]["ks"] for c in range(8)]).reshape(32, 1, NH, HD).astype(f)
    v_s = np.concatenate([r[c]["vs"] for c in range(8)]).reshape(32, 1, NH, HD).astype(f)
    conv_s = np.concatenate([r[c]["convs"] for c in range(8)]).reshape(32, 30, 512).astype(f)
    ffn_s = np.concatenate([r[c]["ffns"] for c in range(8)]).reshape(32, 2, NUP).astype(f)
    return (y_p, y_s, k_p, v_p, k_s, v_s, conv_p, conv_s, ffn_p, ffn_s, mk_p, mv_p)
```

```python
import contextlib
import numpy as np
import concourse.bass as bass
import concourse.mybir as mybir
from concourse.bass_utils import run_bass_kernel_spmd

F32 = mybir.dt.float32
BF16 = mybir.dt.bfloat16
I32 = mybir.dt.int32
AF = mybir.ActivationFunctionType
ALU = mybir.AluOpType
AX = mybir.AxisListType

D = 1024
SEQ = 4096
NT = 512
NCHUNK = SEQ // NT
CONV_CH = 512
CW = 31
NH = 8
HD = 64
DFF = 2816
NUP = 2 * DFF
NIN = 6144
EPS = 1e-6
NEG = -30000.0
FFN_MUL_ENG = "dve"
N_POOL = 2560


class Sched:
    ENGS = ("pe", "act", "dve", "pool", "sp")

    def __init__(self, nc):
        self.nc = nc
        self.q = {e: [] for e in self.ENGS}
        self.last_w = {}
        self.readers = {}
        self.dma_cnt = {}
        self.n_ops = 0
        self.alias = {}

    def op(self, eng, fn, reads=(), writes=(), dma=False, semkey=None):
        rec = {"fn": fn, "eng": eng, "deps": [], "need_inc": False, "dma": dma,
               "semkey": None, "dval": 0}
        self.n_ops += 1
        deps = []
        for k in reads:
            w = self.last_w.get(k)
            if w is not None:
                deps.append(w)
        for k in writes:
            w = self.last_w.get(k)
            if w is not None:
                deps.append(w)
            for r in self.readers.get(k, ()):
                deps.append(r)
            for a in self.alias.get(k, ()):
                w = self.last_w.get(a)
                if w is not None:
                    deps.append(w)
                for r in self.readers.get(a, ()):
                    deps.append(r)
        if dma:
            assert semkey is not None
            rec["semkey"] = semkey
            self.dma_cnt[semkey] = self.dma_cnt.get(semkey, 0) + 16
            rec["dval"] = self.dma_cnt[semkey]
            me = ("d", semkey, rec["dval"])
        else:
            me = ("c", rec)
        seen = set()
        for d in deps:
            if d[0] == "c":
                p = d[1]
                if p is rec:
                    continue
                if p["eng"] == eng and eng == "pe":
                    continue
                if id(p) in seen:
                    continue
                seen.add(id(p))
                p["need_inc"] = True
                rec["deps"].append(d)
            else:
                key = (d[1], d[2])
                if key in seen:
                    continue
                seen.add(key)
                rec["deps"].append(d)
        for k in writes:
            self.last_w[k] = me
            self.readers[k] = []
        for k in reads:
            if k in writes:
                continue
            self.readers.setdefault(k, []).append(me)
        self.q[eng].append(rec)
        return rec

    def emit(self, st):
        nc = self.nc
        esem = {e: st.enter_context(nc.semaphore("e_" + e)) for e in self.ENGS}
        dsem = {}
        for i, k in enumerate(self.dma_cnt):
            dsem[k] = st.enter_context(nc.semaphore("d%d" % i))
        for e in self.ENGS:
            c = 0
            for rec in self.q[e]:
                if rec["need_inc"] and not rec["dma"]:
                    c += 1
                    rec["inc_val"] = c
        block = st.enter_context(nc.Block())
        hw = {"pe": nc.tensor, "act": nc.scalar, "dve": nc.vector, "pool": nc.gpsimd, "sp": nc.sync}

        def run(e):
            eng = hw[e]
            waited = {}
            for rec in self.q[e]:
                for d in rec["deps"]:
                    if d[0] == "c":
                        p = d[1]
                        sem = esem[p["eng"]]
                        val = p["inc_val"]
                        key = ("e", p["eng"])
                    else:
                        sem = dsem[d[1]]
                        val = d[2]
                        key = ("d", d[1])
                    if waited.get(key, 0) >= val:
                        continue
                    waited[key] = val
                    eng.wait_ge(sem, val)
                ins = rec["fn"](eng)
                if rec["dma"]:
                    ins.then_inc(dsem[rec["semkey"]], 16)
                elif rec["need_inc"]:
                    ins.then_inc(esem[e], 1)
            if e == "sp":
                for k, v in self.dma_cnt.items():
                    eng.wait_ge(dsem[k], v)

        block.tensor(lambda _e: run("pe"))
        block.scalar(lambda _e: run("act"))
        block.vector(lambda _e: run("dve"))
        block.gpsimd(lambda _e: run("pool"))
        block.sync(lambda _e: run("sp"))


FM_G1, FM_G2, FM_GM, FM_BG, FM_WDA, FM_BDA, FM_LAG, FM_LAB, FM_WDF, FM_BDF = 0, 8, 16, 24, 48, 172, 176, 180, 184, 316
FM_N = 360
BC_GQ, BC_GK, BC_GMQ, BC_GMK = 0, 512, 1024, 1536
BC_N = 2048


SC_RROW, SC_CS, SC_ST, SC_REP, SC_E8, SC_AL, SC_PI, SC_IND = 0, 512, 643, 898, 1026, 2050, 2562, 2563
SC_N = 2691


def build(nch=NCHUNK, do_sample=True, dbg=False):
    nc = bass.Bass("TRN2", target_bir_lowering=False)

    def din(name, shape, dt=F32):
        return nc.dram_tensor(name, list(shape), dt, kind="ExternalInput").ap()

    def dout(name, shape, dt=F32):
        return nc.dram_tensor(name, list(shape), dt, kind="ExternalOutput").ap()

    xin = din("x", [SEQ, D])
    memin = din("mem", [256, D])
    w_in = din("w_in", [D, NIN])
    w_pa = din("w_proj_a", [512, D])
    w_pb = din("w_proj_b", [512, D])
    w_pc = din("w_proj_c", [512, D])
    w_mkv = din("w_mem_kv", [D, D])
    w_out = din("w_out", [D, D])
    w_up = din("w_up", [D, NUP])
    w_down = din("w_down", [DFF, D])
    fm_in = din("fm", [128, FM_N])
    bc_in = din("bc", [128, BC_N])
    ident_in = din("ident", [128, 128])
    tri_in = din("tri", [128, 128])
    abase_in = din("abase", [128, NH * 32])
    qal_in = din("qal", [128, 4 * NH * 2])

    if do_sample:
        xs_in = din("xs", [128, D])
        stc_in = din("stc", [120, 512])
        w120_in = din("w120", [120, 512])
        stf_in = din("stf", [4, 2 * NUP])
        cmk_in = din("cmk", [4, 256, 512])
        cmv_in = din("cmv", [4, 256, 512])
        pt_in = din("ptrep", [128, 256], I32)
        ck_in = din("cache_k", [N_POOL * 128, 512])
        cv_in = din("cache_v", [N_POOL * 128, 512])
        sc_in = din("sconst", [128, SC_N])
        vrow_in = din("vrows", [9, 5632])
        ys_o = dout("ys", [4, D])
        ks_o = dout("ks", [4, 512])
        vs_o = dout("vs", [4, 512])
        convs_o = dout("convs", [4, 30, 512])
        ffns_o = dout("ffns", [4, 2, NUP])
    y_o = dout("y", [SEQ, D])
    k_o = dout("k", [SEQ, 512])
    v_o = dout("v", [SEQ, 512])
    conv_o = dout("conv", [32, 512])
    ffnc_o = dout("ffnc", [88, 128])
    mk_o = dout("mk", [256, 512])
    mv_o = dout("mv", [256, 512])
    kt_scr = nc.dram_tensor("kt_scr", [NH, 82, SEQ], BF16, kind="Internal").ap()
    v_scr = nc.dram_tensor("v_scr", [NH, SEQ // 128, 128, 128], BF16, kind="Internal").ap()

    st = contextlib.ExitStack()
    with st:
        def sb(name, shape, dt):
            return st.enter_context(nc.sbuf_tensor("s_" + name, list(shape), dt))

        def psum(name, shape, dt):
            return st.enter_context(nc.psum_tensor("p_" + name, list(shape), dt))

        S = Sched(nc)
        op = S.op
        dbg_n = [0]

        def dbgout(src_ap, shape, keys, dt=F32):
            if not dbg:
                return
            name = "dbg%d" % dbg_n[0]
            dbg_n[0] += 1
            t = nc.dram_tensor(name, list(shape), dt, kind="ExternalOutput").ap()
            op("pool", lambda e: e.dma_start(out=t, in_=src_ap), reads=keys, writes=["hbm_" + name], dma=True, semkey="dbg")

        identb = sb("identb", [128, 128], BF16)
        identf = sb("identf", [128, 128], F32)
        trib = sb("trib", [128, 128], BF16)
        onesb = sb("onesb", [128, 128], BF16)
        onesf = sb("onesf", [128, 128], F32)
        fm = sb("fm", [128, FM_N], F32)
        bc = sb("bc", [128, BC_N], F32)
        btab = sb("btab", [128, NH * 32], F32)
        mvals = sb("mvals", [128, 8], F32)
        epsb = sb("epsb", [128, 1], F32)
        X = sb("X", [128, 4, D], F32)
        xn = sb("xn", [128, D], BF16)
        ss = sb("ss", [128, 16], F32)
        hT = sb("hT", [128, 8, NT], BF16)
        uT = sb("uT", [128, 4, 30 + NT], F32)
        cT = sb("cT", [128, 4, NT], F32)
        sT = sb("sT", [128, 4, NT], BF16)
        tmpA = sb("tmpA", [128, NT], F32)
        tmpB = sb("tmpB", [128, NT], F32)
        Qaug = sb("Qaug", [128, 4, NH, 82], BF16)
        Kaug = sb("Kaug", [128, 4, NH, 82], BF16)
        Vloc = sb("Vloc", [128, 4, NH, 128], BF16)
        mqT = sb("mqT", [128, 4, NT], BF16)
        qn_bf = sb("qn_bf", [128, 512], BF16)
        kn_bf = sb("kn_bf", [128, 2, 512], BF16)
        kst = sb("kst", [128, 2, 512], F32)
        vst = sb("vst", [128, 2, 512], F32)
        kmBD = sb("kmBD", [128, 4, 2, 16], BF16)
        gatef = sb("gatef", [128, NH, 16], F32)
        top8 = sb("top8", [128, NH, 8], F32)
        thr = sb("thr", [128, NH], F32)
        nsel = sb("nsel", [128, NH, 16], F32)
        PT = sb("PT", [128, 3, NT], BF16)
        OT = sb("OT", [128, 4, NT], BF16)
        OmT = sb("OmT", [128, 4, NT], BF16)
        rden = sb("rden", [128, NT], F32)
        mkT = sb("mkT", [128, 4, 256], BF16)
        mVa = sb("mVa", [128, 2, 512], BF16)
        merged = sb("merged", [128, 8, NT], BF16)
        scrA = sb("scrA", [128, 22 * NT], BF16)
        sa = scrA[:, :].rearrange("p (j t) -> p j t", j=22)
        KTloc = scrA[:, 0:4096].rearrange("p (j t) -> p j t", j=NH)
        qTa = scrA[:, 4096:8192].rearrange("p (j t) -> p j t", j=NH)
        mqn = scrA[:, 8192:10240].rearrange("p (j t) -> p j t", j=4)
        qTp = scrA[:, 10240:10752].rearrange("p (j t) -> p j t", j=4)
        cbuf = sb("cbuf", [128, 3, NT], F32)
        tmpC = cbuf[:, 0, :]
        tmpD = cbuf[:, 1, :]
        sig = cT
        junk = merged[:, 0:2, :].rearrange("p a t -> p (a t)")
        for _k in ("KTloc", "qTa", "mqn", "qTp"):
            S.alias[_k] = [("sa", j) for j in range(22)]
        for j in range(22):
            S.alias[("sa", j)] = ["KTloc", "qTa", "mqn", "qTp"]
        hist = sb("hist", [128, 44, 2], F32)
        corr = sb("corr", [128, 44, 2], F32)
        corrt = sb("corrt", [128, 44], F32)
        ring = sb("ring", [128, 4, 4096], BF16)
        gring = sb("gring", [128, 4, 1024], BF16)
        kvK = sb("kvK", [128, 4, NT], BF16)
        kvV = sb("kvV", [128, 4, 4, 128], BF16)
        outst = sb("outst", [128, 512], F32)
        idx_t = sb("idx_t", [128, 256], I32)
        orowb = sb("orowb", [128, 132], BF16)
        ptf = sb("ptf", [128, 256], F32)

        psA = [psum("psA%d" % i, [128, 512], F32) for i in range(6)]
        psT = psum("psT", [128, 1024], BF16)
        psM = psum("psM", [128, 512], F32)
        PA = ["psA%d" % i for i in range(6)]

        def dma(eng, out, in_, reads, writes, semkey):
            return op(eng, lambda e: e.dma_start(out=out, in_=in_), reads=reads, writes=writes, dma=True, semkey=semkey)

        def mm(out, lhsT, rhs, start, stop, reads, writes):
            return op("pe", lambda e: e.matmul(out, lhsT=lhsT, rhs=rhs, start=start, stop=stop, skip_group_check=True),
                      reads=reads, writes=writes)

        def tr(out, in_, ident, reads, writes):
            return op("pe", lambda e: e.transpose(out=out, in_=in_, identity=ident), reads=reads, writes=writes)

        rstate = {"i": 0, "g": 0}

        def load_w(view, nk, ncols):
            i = rstate["i"] % 4
            rstate["i"] += 1
            dst = ring[:, i, 0:nk * ncols].rearrange("p (k n) -> p k n", k=nk)
            dma("pool", dst, view, [], [("ring", i)], "ring%d" % i)
            return dst, ("ring", i)

        def load_g(view):
            i = rstate["g"] % 4
            rstate["g"] += 1
            dst = gring[:, i, :].rearrange("p (k n) -> p k n", k=8)
            dma("pool", dst, view, [], [("gring", i)], "gring%d" % i)
            return dst, ("gring", i)

        win_v = w_in.rearrange("(kc p) n -> p kc n", p=128)
        wup_v = w_up.rearrange("(kc p) n -> p kc n", p=128)
        wout_v = w_out.rearrange("(kc p) n -> p kc n", p=128)
        wdn_v = w_down.rearrange("(kc p) n -> p kc n", p=128)
        wmkv_v = w_mkv.rearrange("(kc p) n -> p kc n", p=128)
        wpa_v = w_pa.rearrange("(kc p) n -> p kc n", p=128)
        wpb_v = w_pb.rearrange("(kc p) n -> p kc n", p=128)
        wpc_v = w_pc.rearrange("(kc p) n -> p kc n", p=128)

        dma("sp", fm[:, :], fm_in[:, :], [], ["fm"], "c1")
        dma("sp", bc[:, :], bc_in[:, :], [], ["bc"], "c2")
        dma("sp", identf[:, :], ident_in[:, :], [], ["identf"], "c3")
        dma("sp", btab[:, :], abase_in[:, :], [], ["btab"], "c4")
        dma("sp", tmpA[:, 0:128], tri_in[:, :], [], ["tmpA"], "c5")
        dma("sp", tmpB[:, 0:64], qal_in[:, :], [], ["tmpB"], "c6")
        op("dve", lambda e: e.tensor_copy(out=identb[:, :], in_=identf[:, :]), ["identf"], ["identb"])
        op("dve", lambda e: e.tensor_copy(out=trib[:, :], in_=tmpA[:, 0:128]), ["tmpA"], ["trib"])
        op("dve", lambda e: e.memset(onesb[:, :], 1.0), [], ["onesb"])
        op("dve", lambda e: e.memset(onesf[:, :], 1.0), [], ["onesf"])
        op("dve", lambda e: e.memset(epsb[:, :], EPS), [], ["epsb"])
        op("dve", lambda e: e.memset(uT[:, :, :], 0.0), [], ["uT"])
        op("dve", lambda e: e.memset(hist[:, :, :], 0.0), [], ["hist"])
        op("dve", lambda e: e.memset(kmBD[:, :, :, :], 0.0), [], ["kmBD"])
        op("dve", lambda e: e.memset(gatef[:, :, :], -1e30), [], ["gatef"])
        op("dve", lambda e: e.memset(Vloc[:, :, :, 0:64], 1.0), [], ["Vloc"])
        op("dve", lambda e: e.memset(Kaug[:, :, :, 80:82], 1.0), [], ["Kaug"])
        op("dve", lambda e: e.tensor_copy(out=Qaug[:, :, :, 80:82],
                                          in_=tmpB[:, 0:64].rearrange("p (s h r) -> p s h r", s=4, h=NH)),
           ["tmpB"], ["Qaug"])
        for j, (off, n) in enumerate(((BC_GQ, 64), (BC_GK, 64), (BC_GMQ, 128), (BC_GMK, 128))):
            op("dve", lambda e, j=j, off=off, n=n: e.tensor_reduce(out=mvals[:, j:j + 1], in_=bc[:, off:off + n], axis=AX.X,
                                                                  op=ALU.max, apply_absolute_value=True),
               ["bc"], ["mvals"])
        op("dve", lambda e: e.tensor_tensor(out=mvals[:, 4:5], in0=mvals[:, 0:1], in1=mvals[:, 1:2], op=ALU.mult), ["mvals"], ["mvals"])
        op("dve", lambda e: e.tensor_scalar(out=mvals[:, 4:5], in0=mvals[:, 4:5], scalar1=8.0, scalar2=None, op0=ALU.mult), ["mvals"], ["mvals"])
        op("dve", lambda e: e.tensor_tensor(out=mvals[:, 5:6], in0=mvals[:, 2:3], in1=mvals[:, 3:4], op=ALU.mult), ["mvals"], ["mvals"])
        op("dve", lambda e: e.tensor_scalar(out=mvals[:, 5:6], in0=mvals[:, 5:6], scalar1=-(128.0 ** 0.5), scalar2=None, op0=ALU.mult), ["mvals"], ["mvals"])
        op("dve", lambda e: e.tensor_scalar(out=btab[:, :], in0=btab[:, :], scalar1=mvals[:, 4:5], scalar2=None, op0=ALU.subtract),
           ["btab", "mvals"], ["btab"])
        op("dve", lambda e: e.tensor_scalar(out=bc[:, BC_GQ:BC_GQ + 512], in0=bc[:, BC_GQ:BC_GQ + 512], scalar1=HD ** -0.5, scalar2=None, op0=ALU.mult), ["bc"], ["bc"])
        op("dve", lambda e: e.tensor_scalar(out=bc[:, BC_GMQ:BC_GMQ + 512], in0=bc[:, BC_GMQ:BC_GMQ + 512], scalar1=128.0 ** -0.5, scalar2=None, op0=ALU.mult), ["bc"], ["bc"])

        def rms_rows(src3, sub, col):
            op("dve", lambda e: e.memset(ss[:, col:col + 1], 0.0), [], [("ss", col)])
            op("act", lambda e: e.activation(out=junk[:, :], in_=src3[:, sub, :], func=AF.Square, accum_out=ss[:, col:col + 1]),
               ["X", ("ss", col)], ["merged", ("ss", col)])
            op("act", lambda e: e.activation(out=ss[:, col:col + 1], in_=ss[:, col:col + 1], func=AF.Sqrt, bias=epsb[:, 0:1], scale=1.0 / D),
               [("ss", col), "epsb"], [("ss", col)])
            op("dve", lambda e: e.reciprocal(out=ss[:, col:col + 1], in_=ss[:, col:col + 1]), [("ss", col)], [("ss", col)])

        def norm_to_T(src3, nsub, goff, dstT, dkey, srckey):
            for sub in range(nsub):
                rms_rows(src3, sub, sub)
            for sub in range(nsub):
                op("dve", lambda e, sub=sub: e.tensor_scalar(out=xn[:, :], in0=src3[:, sub, :], scalar1=ss[:, sub:sub + 1], scalar2=None, op0=ALU.mult),
                   [srckey, ("ss", sub)], ["xn"])
                for kc in range(8):
                    tr(psT[:, kc * 128:(kc + 1) * 128], xn[:, kc * 128:(kc + 1) * 128], identb[:, :], ["xn", "identb"], ["psT"])
                op("dve", lambda e, sub=sub: e.tensor_tensor(
                    out=dstT[:, :, sub * 128:(sub + 1) * 128], in0=psT[:, :].rearrange("p (k t) -> p k t", k=8),
                    in1=fm[:, goff:goff + 8].unsqueeze(2).broadcast_to([128, 8, 128]), op=ALU.mult),
                   ["psT", "fm"], [dkey])

        def headnorm(ps, pkey, nh, hd, goff, out_ap, okey, extra_reads=()):
            op("act", lambda e: e.activation(out=tmpA[:, :], in_=ps[:, :], func=AF.Square), [pkey], ["tmpA"])
            op("dve", lambda e: e.tensor_reduce(out=ss[:, 8:8 + nh], in_=tmpA[:, :].rearrange("p (h d) -> p h d", h=nh), axis=AX.X, op=ALU.add),
               ["tmpA"], ["ss8"])
            op("act", lambda e: e.activation(out=ss[:, 8:8 + nh], in_=ss[:, 8:8 + nh], func=AF.Sqrt, bias=epsb[:, 0:1], scale=1.0 / hd),
               ["ss8", "epsb"], ["ss8"])
            op("dve", lambda e: e.reciprocal(out=ss[:, 8:8 + nh], in_=ss[:, 8:8 + nh]), ["ss8"], ["ss8"])
            op("dve", lambda e: e.tensor_tensor(out=tmpB[:, :].rearrange("p (h d) -> p h d", h=nh), in0=ps[:, :].rearrange("p (h d) -> p h d", h=nh),
                                                in1=ss[:, 8:8 + nh].unsqueeze(2).broadcast_to([128, nh, hd]), op=ALU.mult),
               [pkey, "ss8"], ["tmpB"])
            op("dve", lambda e: e.tensor_tensor(out=out_ap, in0=tmpB[:, :].rearrange("p (h d) -> p h d", h=nh),
                                                in1=bc[:, goff:goff + 512].rearrange("p (h d) -> p h d", h=nh), op=ALU.mult),
               ["tmpB", "bc"] + list(extra_reads), [okey])

        dma("sp", X[:, 0:2, :], memin.rearrange("(s p) d -> p s d", p=128), [], ["X"], "xload")
        norm_to_T(X, 2, FM_GM, hT, "hT", "X")
        for half in range(2):
            W, wk = load_w(wmkv_v[:, :, half * 512:(half + 1) * 512], 8, 512)
            for sub in range(2):
                ps = psA[sub]
                for kc in range(8):
                    mm(ps[:, :], hT[:, kc, sub * 128:(sub + 1) * 128], W[:, kc, :], kc == 0, kc == 7, ["hT", wk], [PA[sub]])
                if half == 0:
                    headnorm(ps, PA[sub], 4, 128, BC_GMK, kst[:, sub, :].rearrange("p (h d) -> p h d", h=4), ("kst", sub))
                    dma("sp", mk_o[sub * 128:(sub + 1) * 128, :], kst[:, sub, :], [("kst", sub)], ["hbm_mk"], "st_k%d" % sub)
                    op("dve", lambda e, sub=sub: e.tensor_copy(out=mqn[:, sub, :], in_=kst[:, sub, :]), [("kst", sub)], ["mqn"])
                    for mh in range(4):
                        tr(psT[:, mh * 128:(mh + 1) * 128], mqn[:, sub, mh * 128:(mh + 1) * 128], identb[:, :], ["mqn", "identb"], ["psT"])
                    op("dve", lambda e, sub=sub: e.tensor_copy(out=mkT[:, :, sub * 128:(sub + 1) * 128],
                                                               in_=psT[:, 0:512].rearrange("p (k t) -> p k t", k=4)), ["psT"], ["mkT"])
                else:
                    op("act", lambda e, sub=sub, ps=ps: e.activation(out=vst[:, sub, :], in_=ps[:, :], func=AF.Copy), [PA[sub]], [("vst", sub)])
                    dma("sp", mv_o[sub * 128:(sub + 1) * 128, :], vst[:, sub, :], [("vst", sub)], ["hbm_mv"], "st_v%d" % sub)
                    op("dve", lambda e, sub=sub: e.tensor_copy(out=mVa[:, sub, :], in_=vst[:, sub, :]), [("vst", sub)], ["mVa"])

        kvstate = {"i": 0}
        for c in range(nch):
            t0 = c * NT
            dma("sp", X[:, :, :], xin[t0:t0 + NT, :].rearrange("(s p) d -> p s d", p=128), [], ["X"], "xload")
            norm_to_T(X, 4, FM_G1, hT, "hT", "X")

            Wa, wka = load_w(win_v[:, :, 0:512], 8, 512)
            Wg, wkg = load_w(win_v[:, :, 512:1024], 8, 512)
            for cc in range(4):
                pa, pg = psA[2 * (cc % 2)], psA[2 * (cc % 2) + 1]
                ka, kg = PA[2 * (cc % 2)], PA[2 * (cc % 2) + 1]
                for kc in range(8):
                    mm(pa[:, :], Wa[:, kc, cc * 128:(cc + 1) * 128], hT[:, kc, :], kc == 0, kc == 7, ["hT", wka], [ka])
                for kc in range(8):
                    mm(pg[:, :], Wg[:, kc, cc * 128:(cc + 1) * 128], hT[:, kc, :], kc == 0, kc == 7, ["hT", wkg], [kg])
                tk = ("cbuf", 0) if cc % 2 == 0 else ("cbuf", 1)
                tt = tmpC if cc % 2 == 0 else tmpD
                op("act", lambda e, pg=pg, tt=tt: e.activation(out=tt[:, :], in_=pg[:, :], func=AF.Sigmoid), [kg], [tk])
                op("dve", lambda e, pa=pa, tt=tt, cc=cc: e.tensor_tensor(out=uT[:, cc, 30:30 + NT], in0=pa[:, :], in1=tt[:, :], op=ALU.mult),
                   [ka, tk], ["uT"])
            conv_steps = []
            for cc in range(4):
                conv_steps.append(lambda cc=cc: op("dve", lambda e: e.tensor_scalar(
                    out=cT[:, cc, :], in0=uT[:, cc, 0:NT], scalar1=fm[:, FM_WDA + cc * 31:FM_WDA + cc * 31 + 1],
                    scalar2=fm[:, FM_BDA + cc:FM_BDA + cc + 1], op0=ALU.mult, op1=ALU.add), ["uT", "fm"], [("cT", cc)]))
                for k in range(1, CW):
                    conv_steps.append(lambda cc=cc, k=k: op("dve", lambda e: e.scalar_tensor_tensor(
                        out=cT[:, cc, :], in0=uT[:, cc, k:k + NT], scalar=fm[:, FM_WDA + cc * 31 + k:FM_WDA + cc * 31 + k + 1],
                        in1=cT[:, cc, :], op0=ALU.mult, op1=ALU.add), ["uT", "fm", ("cT", cc)], [("cT", cc)]))

            def conv_finish(c=c):
                if c == nch - 1:
                    for cc in range(4):
                        op("pe", lambda e, cc=cc: e.transpose(out=psM[0:32, cc * 128:(cc + 1) * 128], in_=uT[:, cc, 30 + NT - 32:30 + NT], identity=identf[:, :]),
                           ["uT", "identf"], ["psM"])
                    op("dve", lambda e: e.tensor_copy(out=outst[0:32, :], in_=psM[0:32, :]), ["psM"], ["outst"])
                    dma("sp", conv_o[:, :], outst[0:32, :], ["outst"], ["hbm_conv"], "st_misc")
                op("dve", lambda e: e.tensor_copy(out=uT[:, :, 0:30], in_=uT[:, :, NT:NT + 30]), ["uT"], ["uT"])
                for cc in range(4):
                    mm(psA[4][:, :], onesf[:, :], cT[:, cc, :], cc == 0, cc == 3, ["onesf", ("cT", cc)], [PA[4]])
                for cc in range(4):
                    tt, tk = (tmpC, ("cbuf", 0)) if cc % 2 == 0 else (tmpD, ("cbuf", 1))
                    op("act", lambda e, cc=cc, tt=tt: e.activation(out=tt[:, :], in_=cT[:, cc, :], func=AF.Square), [("cT", cc)], [tk])
                    mm(psA[5][:, :], onesf[:, :], tt[:, :], cc == 0, cc == 3, ["onesf", tk], [PA[5]])
                op("dve", lambda e: e.tensor_scalar(out=tmpA[:, :], in0=psA[4][:, :], scalar1=1.0 / 512, scalar2=None, op0=ALU.mult), [PA[4]], ["tmpA"])
                op("dve", lambda e: e.tensor_tensor(out=tmpB[:, :], in0=tmpA[:, :], in1=tmpA[:, :], op=ALU.mult), ["tmpA"], ["tmpB"])
                op("dve", lambda e: e.scalar_tensor_tensor(out=tmpB[:, :], in0=psA[5][:, :], scalar=1.0 / 512, in1=tmpB[:, :], op0=ALU.mult, op1=ALU.subtract),
                   [PA[5], "tmpB"], ["tmpB"])
                op("act", lambda e: e.activation(out=tmpB[:, :], in_=tmpB[:, :], func=AF.Sqrt, bias=epsb[:, 0:1], scale=1.0), ["tmpB", "epsb"], ["tmpB"])
                op("dve", lambda e: e.reciprocal(out=tmpB[:, :], in_=tmpB[:, :]), ["tmpB"], ["tmpB"])
                for cc in range(4):
                    op("dve", lambda e, cc=cc: e.tensor_tensor(out=cT[:, cc, :], in0=cT[:, cc, :], in1=tmpA[:, :], op=ALU.subtract), [("cT", cc), "tmpA"], [("cT", cc)])
                    op("dve", lambda e, cc=cc: e.tensor_tensor(out=cT[:, cc, :], in0=cT[:, cc, :], in1=tmpB[:, :], op=ALU.mult), [("cT", cc), "tmpB"], [("cT", cc)])
                    op("act", lambda e, cc=cc: e.activation(out=sT[:, cc, :], in_=cT[:, cc, :], func=AF.Silu, scale=fm[:, FM_LAG + cc:FM_LAG + cc + 1],
                                                            bias=fm[:, FM_LAB + cc:FM_LAB + cc + 1]), [("cT", cc), "fm"], ["sT"])

            op("dve", lambda e: e.memset(Kaug[:, :, :, 64:80], 0.0), [], ["Kaug"])
            op("dve", lambda e, c=c: e.memset(Kaug[:, 0:2, :, 64 + 2 * c:65 + 2 * c], 1.0), [], ["Kaug"])
            op("dve", lambda e, c=c: e.memset(Kaug[:, 2:4, :, 65 + 2 * c:66 + 2 * c], 1.0), [], ["Kaug"])
            kvqm = (("k", 1536), ("v", 2048), ("q", 1024), ("mq", 2560))
            wcur = {}

            def emit_mm(i):
                name, col0 = kvqm[i // 4]
                sub = i % 4
                if sub == 0:
                    wcur[name] = load_w(win_v[:, :, col0:col0 + 512], 8, 512)
                W, wk = wcur[name]
                ps, pk = psA[sub], PA[sub]
                for kc in range(8):
                    mm(ps[:, :], hT[:, kc, sub * 128:(sub + 1) * 128], W[:, kc, :], kc == 0, kc == 7, ["hT", wk], [pk])

            def post(i, c=c, t0=t0):
                name = kvqm[i // 4][0]
                sub = i % 4
                ps, pk = psA[sub], PA[sub]
                if name == "k":
                    headnorm(ps, pk, NH, HD, BC_GK, kst[:, sub % 2, :].rearrange("p (h d) -> p h d", h=NH), ("kst", sub % 2))
                    dma("sp", k_o[t0 + sub * 128:t0 + (sub + 1) * 128, :], kst[:, sub % 2, :], [("kst", sub % 2)], ["hbm_k"], "st_k%d" % (sub % 2))
                    op("dve", lambda e, sub=sub: e.tensor_copy(out=Kaug[:, sub, :, 0:64], in_=kst[:, sub % 2, :].rearrange("p (h d) -> p h d", h=NH)),
                       [("kst", sub % 2)], ["Kaug"])
                    op("dve", lambda e, sub=sub: e.tensor_copy(out=kn_bf[:, sub % 2, :], in_=kst[:, sub % 2, :]), [("kst", sub % 2)], ["kn_bf"])
                    for h in range(NH):
                        tr(psT[0:82, h * 128:(h + 1) * 128], Kaug[:, sub, h, :], identb[:, :], ["Kaug", "identb"], ["psT"])
                    op("dve", lambda e, sub=sub: e.tensor_copy(out=KTloc[0:82, :, sub * 128:(sub + 1) * 128],
                                                               in_=psT[0:82, :].rearrange("p (k t) -> p k t", k=NH)), ["psT"], ["KTloc"])
                    if sub % 2 == 1:
                        blk = 2 * c + sub // 2
                        for pair in range(4):
                            for s2 in range(2):
                                mm(psM[:, pair:pair + 1], kn_bf[:, s2, pair * 128:(pair + 1) * 128], onesb[:, 0:1], s2 == 0, s2 == 1,
                                   ["kn_bf", "onesb"], ["psM"])
                        op("dve", lambda e, blk=blk: e.tensor_scalar(out=kmBD[0:64, :, 0, blk], in0=psM[0:64, 0:4], scalar1=1.0 / 256, scalar2=None, op0=ALU.mult),
                           ["psM"], ["kmBD"])
                        op("dve", lambda e, blk=blk: e.tensor_scalar(out=kmBD[64:128, :, 1, blk], in0=psM[64:128, 0:4], scalar1=1.0 / 256, scalar2=None, op0=ALU.mult),
                           ["psM"], ["kmBD"])
                elif name == "v":
                    op("act", lambda e, sub=sub, ps=ps: e.activation(out=vst[:, sub % 2, :], in_=ps[:, :], func=AF.Copy), [pk], [("vst", sub % 2)])
                    dma("sp", v_o[t0 + sub * 128:t0 + (sub + 1) * 128, :], vst[:, sub % 2, :], [("vst", sub % 2)], ["hbm_v"], "st_v%d" % (sub % 2))
                    op("dve", lambda e, sub=sub: e.tensor_copy(out=Vloc[:, sub, :, 64:128], in_=vst[:, sub % 2, :].rearrange("p (h d) -> p h d", h=NH)),
                       [("vst", sub % 2)], ["Vloc"])
                elif name == "q":
                    cur = 2 * c + sub // 2
                    headnorm(ps, pk, NH, HD, BC_GQ, Qaug[:, sub, :, 0:64], "Qaug")
                    if cur == 0:
                        op("dve", lambda e, sub=sub: e.memset(Qaug[:, sub, :, 64:80], NEG), [], ["Qaug"])
                    else:
                        op("dve", lambda e, sub=sub: e.tensor_copy(out=qn_bf[:, :].rearrange("p (h d) -> p h d", h=NH), in_=Qaug[:, sub, :, 0:64]), ["Qaug"], ["qn_bf"])
                        for pair in range(4):
                            tr(psT[:, pair * 128:(pair + 1) * 128], qn_bf[:, pair * 128:(pair + 1) * 128], identb[:, :], ["qn_bf", "identb"], ["psT"])
                        op("dve", lambda e: e.tensor_copy(out=qTp[:, :, :], in_=psT[:, 0:512].rearrange("p (k t) -> p k t", k=4)), ["psT"], ["qTp"])
                        for pair in range(4):
                            mm(psM[:, 32 * pair:32 * pair + 32], qTp[:, pair, :], kmBD[:, pair, :, :].rearrange("p a b -> p (a b)"), True, True, ["qTp", "kmBD"], ["psM"])
                        op("dve", lambda e, cur=cur: e.tensor_copy(out=gatef[:, :, 0:cur], in_=psM[:, 0:128].rearrange("p (h b) -> p h b", h=NH)[:, :, 0:cur]),
                           ["psM"], ["gatef"])
                        for h in range(NH):
                            op("dve", lambda e, h=h: e.max(out=top8[:, h, :], in_=gatef[:, h, :]), ["gatef"], ["top8"])
                        op("dve", lambda e: e.tensor_scalar(out=thr[:, :], in0=top8[:, :, 2], scalar1=-1e29, scalar2=None, op0=ALU.max), ["top8"], ["thr"])
                        op("dve", lambda e: e.tensor_tensor(out=nsel[:, :, :], in0=gatef[:, :, :], in1=thr[:, :].unsqueeze(2).broadcast_to([128, NH, 16]), op=ALU.is_lt),
                           ["gatef", "thr"], ["nsel"])
                        op("dve", lambda e, sub=sub: e.tensor_scalar(out=Qaug[:, sub, :, 64:80], in0=nsel[:, :, :], scalar1=NEG, scalar2=None, op0=ALU.mult),
                           ["nsel"], ["Qaug"])
                    op("dve", lambda e, sub=sub, cur=cur: e.memset(Qaug[:, sub, :, 64 + cur:65 + cur], 0.0), [], ["Qaug"])
                    for h in range(NH):
                        tr(psT[0:82, h * 128:(h + 1) * 128], Qaug[:, sub, h, :], identb[:, :], ["Qaug", "identb"], ["psT"])
                    op("dve", lambda e, sub=sub: e.tensor_copy(out=qTa[0:82, :, sub * 128:(sub + 1) * 128],
                                                               in_=psT[0:82, :].rearrange("p (k t) -> p k t", k=NH)), ["psT"], ["qTa"])
                else:
                    headnorm(ps, pk, 4, 128, BC_GMQ, mqn[:, sub, :].rearrange("p (h d) -> p h d", h=4), "mqn")
                    for mh in range(4):
                        tr(psT[:, mh * 128:(mh + 1) * 128], mqn[:, sub, mh * 128:(mh + 1) * 128], identb[:, :], ["mqn", "identb"], ["psT"])
                    op("dve", lambda e, sub=sub: e.tensor_copy(out=mqT[:, :, sub * 128:(sub + 1) * 128],
                                                               in_=psT[:, 0:512].rearrange("p (k t) -> p k t", k=4)), ["psT"], ["mqT"])
                if name == "v" and sub == 3:
                    dma("sp", kt_scr[:, :, t0:t0 + NT].rearrange("h r k -> r h k"), KTloc[0:82, :, :], ["KTloc"], [("kscr", c)], "st_kscr")
                    for t4 in range(4):
                        dma("sp", v_scr[:, 4 * c + t4, :, :].rearrange("h k c -> k h c"), Vloc[:, t4, :, :], ["Vloc"], [("vscr", c)], "st_vscr")


            emit_mm(0)
            emit_mm(1)
            for i in range(16):
                if i + 2 < 16:
                    emit_mm(i + 2)
                post(i)

            nexp = 0
            for h in range(NH):
                acc, ak = psA[h % 4], PA[h % 4]
                first = True
                for g in range(c + 1):
                    sl = kvstate["i"] % 4
                    kvstate["i"] += 1
                    dma("sp", kvK[0:82, sl, :], kt_scr[h, :, g * NT:(g + 1) * NT], [("kscr", g)], [("kvK", sl)], "kvK%d" % sl)
                    dma("sp", kvV[:, sl, :, :], v_scr[h, 4 * g:4 * g + 4, :, :].rearrange("t k c -> k t c"), [("vscr", g)], [("kvV", sl)], "kvV%d" % sl)
                    for tile in range(4):
                        kt = 4 * g + tile
                        diag = (g == c)
                        col0 = 128 * tile if diag else 0
                        last = diag and tile == 3
                        sp_, sk = psA[4 + nexp % 2], PA[4 + nexp % 2]
                        pslot = nexp % 3
                        nexp += 1
                        mm(sp_[:, col0:NT], kvK[0:82, sl, tile * 128:(tile + 1) * 128], qTa[0:82, h, col0:NT], True, True, [("kvK", sl), "qTa"], [sk])
                        if diag:
                            mm(sp_[:, col0:col0 + 128], identb[:, :], trib[:, :], False, True, ["identb", "trib"], [sk])
                        bcol = h * 32 + (kt - 4 * c + 28)
                        op("act", lambda e, sp_=sp_, pslot=pslot, col0=col0, bcol=bcol: e.activation(
                            out=PT[:, pslot, col0:NT], in_=sp_[:, col0:NT], func=AF.Exp, bias=btab[:, bcol:bcol + 1], scale=1.0),
                           [sk, "btab"], [("PT", pslot)])
                        mm(acc[:, col0:NT], kvV[:, sl, tile, :], PT[:, pslot, col0:NT], first, last, [("kvV", sl), ("PT", pslot)], [ak])
                        first = False
                op("dve", lambda e, acc=acc: e.reciprocal(out=rden[0:64, :], in_=acc[0:64, :]), [ak], ["rden"])
                op("dve", lambda e, acc=acc, h=h: e.tensor_tensor(out=OT[64 * (h % 2):64 * (h % 2) + 64, h // 2, :], in0=acc[64:128, :], in1=rden[0:64, :], op=ALU.mult),
                   [ak, "rden"], ["OT"])
                for stp in conv_steps[16 * h:16 * (h + 1)]:
                    stp()
            conv_finish()

            for mh in range(4):
                o_ps, ok = psA[2 * (mh % 2)], PA[2 * (mh % 2)]
                d_ps, dk = psA[2 * (mh % 2) + 1], PA[2 * (mh % 2) + 1]
                for t in range(2):
                    sp_, sk = psA[4 + nexp % 2], PA[4 + nexp % 2]
                    pslot = nexp % 3
                    nexp += 1
                    mm(sp_[:, :], mkT[:, mh, t * 128:(t + 1) * 128], mqT[:, mh, :], True, True, ["mkT", "mqT"], [sk])
                    op("act", lambda e, sp_=sp_, pslot=pslot: e.activation(out=PT[:, pslot, :], in_=sp_[:, :], func=AF.Exp, bias=mvals[:, 5:6], scale=1.0),
                       [sk, "mvals"], [("PT", pslot)])
                    mm(o_ps[:, :], mVa[:, t, mh * 128:(mh + 1) * 128], PT[:, pslot, :], t == 0, t == 1, ["mVa", ("PT", pslot)], [ok])
                    mm(d_ps[:, :], onesb[:, :], PT[:, pslot, :], t == 0, t == 1, ["onesb", ("PT", pslot)], [dk])
                op("dve", lambda e, d_ps=d_ps: e.reciprocal(out=rden[:, :], in_=d_ps[:, :]), [dk], ["rden"])
                op("dve", lambda e, o_ps=o_ps, mh=mh: e.tensor_tensor(out=OmT[:, mh, :], in0=o_ps[:, :], in1=rden[:, :], op=ALU.mult), [ok, "rden"], ["OmT"])

            Wpa, kpa = load_w(wpa_v, 4, 1024)
            Wpb, kpb = load_w(wpb_v, 4, 1024)
            Wpc, kpc = load_w(wpc_v, 4, 1024)
            for dc in range(8):
                srcs = ((Wpa, kpa, sT, "sT"), (Wpb, kpb, OT, "OT"), (Wpc, kpc, OmT, "OmT"))
                for j in range(3):
                    Wp, kp, src, skey = srcs[j]
                    for kc in range(4):
                        mm(psA[j][:, :], Wp[:, kc, dc * 128:(dc + 1) * 128], src[:, kc, :], kc == 0, kc == 3, [kp, skey], [PA[j]])
                for j in range(3):
                    Wgt, kgt = load_g(win_v[:, :, 3072 + j * 1024 + dc * 128:3072 + j * 1024 + (dc + 1) * 128])
                    for kc in range(8):
                        mm(psA[3 + j][:, :], Wgt[:, kc, :], hT[:, kc, :], kc == 0, kc == 7, [kgt, "hT"], [PA[3 + j]])
                    op("act", lambda e, j=j, dc=dc: e.activation(out=sig[:, j, :], in_=psA[3 + j][:, :], func=AF.Sigmoid,
                                                                 bias=fm[:, FM_BG + j * 8 + dc:FM_BG + j * 8 + dc + 1], scale=1.0),
                       [PA[3 + j], "fm"], [("cT", j)])
                op("dve", lambda e: e.tensor_tensor(out=tmpA[:, :], in0=psA[0][:, :], in1=sig[:, 0, :], op=ALU.mult), [PA[0], ("cT", 0)], ["tmpA"])
                op("dve", lambda e: e.tensor_tensor(out=tmpB[:, :], in0=psA[1][:, :], in1=sig[:, 1, :], op=ALU.mult), [PA[1], ("cT", 1)], ["tmpB"])
                op("dve", lambda e: e.tensor_tensor(out=tmpA[:, :], in0=tmpA[:, :], in1=tmpB[:, :], op=ALU.add), ["tmpA", "tmpB"], ["tmpA"])
                op("dve", lambda e: e.tensor_tensor(out=tmpB[:, :], in0=psA[2][:, :], in1=sig[:, 2, :], op=ALU.mult), [PA[2], ("cT", 2)], ["tmpB"])
                op("dve", lambda e, dc=dc: e.tensor_tensor(out=merged[:, dc, :], in0=tmpA[:, :], in1=tmpB[:, :], op=ALU.add), ["tmpA", "tmpB"], ["merged"])
            for half in range(2):
                W, wk = load_w(wout_v[:, :, half * 512:(half + 1) * 512], 8, 512)
                for sub in range(4):
                    for kc in range(8):
                        mm(psA[sub][:, :], merged[:, kc, sub * 128:(sub + 1) * 128], W[:, kc, :], kc == 0, kc == 7, ["merged", wk], [PA[sub]])
                    op("dve", lambda e, sub=sub, half=half: e.tensor_tensor(out=X[:, sub, half * 512:(half + 1) * 512], in0=X[:, sub, half * 512:(half + 1) * 512],
                                                                            in1=psA[sub][:, :], op=ALU.add), ["X", PA[sub]], ["X"])

            norm_to_T(X, 4, FM_G2, hT, "hT", "X")
            wdf3 = fm[:, FM_WDF:FM_WDF + 132].rearrange("p (j k) -> p j k", k=3)
            allhist_k = [("hist", j) for j in range(44)]
            op("dve", lambda e: e.tensor_tensor(out=corr[:, :, 0], in0=hist[:, :, 0], in1=wdf3[:, :, 0], op=ALU.mult), allhist_k + ["fm"], ["corr"])
            op("dve", lambda e: e.tensor_tensor(out=corrt[:, :], in0=hist[:, :, 1], in1=wdf3[:, :, 1], op=ALU.mult), allhist_k + ["fm"], ["corrt"])
            op("dve", lambda e: e.tensor_tensor(out=corr[:, :, 0], in0=corr[:, :, 0], in1=corrt[:, :], op=ALU.add), ["corr", "corrt"], ["corr"])
            op("dve", lambda e: e.tensor_tensor(out=corr[:, :, 1], in0=hist[:, :, 1], in1=wdf3[:, :, 0], op=ALU.mult), allhist_k + ["fm"], ["corr"])
            upw = {}

            def emit_up(j):
                piece, jj = j // 4, j % 4
                if jj == 0:
                    upw[piece] = load_w(wup_v[:, :, piece * 512:(piece + 1) * 512], 8, 512)
                W, wk = upw[piece]
                ps, pk = psA[j % 4], PA[j % 4]
                cb, ck = cbuf[:, j % 3, :], ("cbuf", j % 3)
                for kc in range(8):
                    mm(ps[:, :], W[:, kc, jj * 128:(jj + 1) * 128], hT[:, kc, :], kc == 0, kc == 7, [wk, "hT"], [pk])
                w2 = fm[:, FM_WDF + 3 * j + 2:FM_WDF + 3 * j + 3]
                bb = fm[:, FM_BDF + j:FM_BDF + j + 1]
                op("act", lambda e: e.activation(out=cb, in_=ps[:, :], func=AF.Identity, scale=w2, bias=bb), [pk, "fm"], [ck])
                op("act", lambda e: e.activation(out=hist[:, j, :], in_=ps[:, NT - 2:NT], func=AF.Copy), [pk], [("hist", j)])

            def chain_up(j):
                ps, pk = psA[j % 4], PA[j % 4]
                cb, ck = cbuf[:, j % 3, :], ("cbuf", j % 3)
                w0 = fm[:, FM_WDF + 3 * j:FM_WDF + 3 * j + 1]
                w1 = fm[:, FM_WDF + 3 * j + 1:FM_WDF + 3 * j + 2]
                op("dve", lambda e: e.scalar_tensor_tensor(out=cb[:, 1:NT], in0=ps[:, 0:NT - 1], scalar=w1, in1=cb[:, 1:NT], op0=ALU.mult, op1=ALU.add),
                   [pk, ck, "fm"], [ck])
                op("dve", lambda e: e.scalar_tensor_tensor(out=cb[:, 2:NT], in0=ps[:, 0:NT - 2], scalar=w0, in1=cb[:, 2:NT], op0=ALU.mult, op1=ALU.add),
                   [pk, ck, "fm"], [ck])
                op("dve", lambda e: e.tensor_tensor(out=cb[:, 0:2], in0=cb[:, 0:2], in1=corr[:, j, :], op=ALU.add), [ck, "corr"], [ck])
                if j < 22:
                    op("act", lambda e: e.activation(out=sa[:, j, :], in_=cb, func=AF.Silu), [ck], [("sa", j)])
                else:
                    op(FFN_MUL_ENG, lambda e: e.tensor_tensor(out=sa[:, j - 22, :], in0=sa[:, j - 22, :], in1=cb, op=ALU.mult), [ck, ("sa", j - 22)], [("sa", j - 22)])

            emit_up(0)
            for j in range(44):
                if j + 1 < 44:
                    emit_up(j + 1)
                chain_up(j)
            sakeys = [("sa", j) for j in range(22)]
            for half in range(2):
                for kp, (k0, k1) in enumerate(((0, 8), (8, 16), (16, 22))):
                    W, wk = load_w(wdn_v[:, k0:k1, half * 512:(half + 1) * 512], k1 - k0, 512)
                    for sub in range(4):
                        for kc in range(k0, k1):
                            mm(psA[sub][:, :], sa[:, kc, sub * 128:(sub + 1) * 128], W[:, kc - k0, :], kc == 0, kc == 21, sakeys + [wk], [PA[sub]])
                for sub in range(4):
                    op("dve", lambda e, sub=sub, half=half: e.tensor_tensor(out=X[:, sub, half * 512:(half + 1) * 512], in0=X[:, sub, half * 512:(half + 1) * 512],
                                                                            in1=psA[sub][:, :], op=ALU.add), ["X", PA[sub]], ["X"])
            dma("sp", y_o[t0:t0 + NT, :].rearrange("(s p) d -> p s d", p=128), X[:, :, :], ["X"], ["hbm_y"], "st_y")


        if do_sample:
            sc = X[:, 1:4, :].rearrange("p a t -> p (a t)")
            dma("sp", X[:, 0, :], xs_in[:, :], [], ["X"], "xload")
            dma("sp", sc[:, 0:SC_N], sc_in[:, :], [], ["X"], "xload")
            RROW = lambda n: sc[:, SC_RROW + n * 128:SC_RROW + (n + 1) * 128]
            ONEROW = lambda n: sc[:, SC_CS + 3 - n:SC_CS + 131 - n]
            ONECOL = lambda r: sc[:, SC_ST + 127 - r:SC_ST + 255 - r]
            REP = sc[:, SC_REP:SC_REP + 128]
            E8 = lambda h: sc[0:8, SC_E8 + h * 128:SC_E8 + (h + 1) * 128]
            ALT = sc[:, SC_AL:SC_AL + 512]
            PIOTA = sc[:, SC_PI:SC_PI + 1]
            IND120 = sc[0:120, SC_IND:SC_IND + 128]
            op("dve", lambda e: e.tensor_copy(out=orowb[:, 0:131], in_=sc[:, SC_CS:SC_CS + 131]), ["X"], ["orowb"])
            ONEROWB = lambda n: orowb[:, 3 - n:131 - n]
            HCOL = lambda h: sc[:, SC_ST + 127 - h:SC_ST + 135 - h]
            dma("sp", idx_t[:, :], pt_in[:, :], [], ["idx_t"], "c7")
            op("dve", lambda e: e.tensor_copy(out=ptf[:, :], in_=idx_t[:, :]), ["idx_t"], ["ptf"])
            op("dve", lambda e: e.tensor_scalar(out=ptf[:, :], in0=ptf[:, :], scalar1=128.0, scalar2=None, op0=ALU.mult), ["ptf"], ["ptf"])
            op("dve", lambda e: e.tensor_scalar(out=ptf[:, :], in0=ptf[:, :], scalar1=PIOTA, scalar2=None, op0=ALU.add), ["ptf", "X"], ["ptf"])
            op("dve", lambda e: e.tensor_copy(out=idx_t[:, :], in_=ptf[:, :]), ["ptf"], ["idx_t"])

            for _k in ("stc", "prd", "qb", "mg"):
                S.alias[_k] = ["uT"]
            S.alias["mg"] = ["uT", "stc", "prd"]
            S.alias["cs"] = [("sa", j) for j in range(22)] + ["KTloc", "qTa", "mqn", "qTp"]
            S.alias["stf"] = ["Vloc"]
            norm_to_T(X, 1, FM_G1, hT, "hT", "X")
            us, qs, mqs, ks = cT[:, 0, :], cT[:, 1, :], cT[:, 2, :], cT[:, 3, :]
            vs, stc, prd, qb = uT[:, 0, 0:512], uT[:, 1, 0:512], uT[:, 2, 0:512], uT[:, 3, 0:512]

            def bload(dst, dkey, src_row):
                dma("sp", dst, src_row.partition_broadcast(128), [], [dkey], "bl_" + str(dkey))

            def zpiece(col0):
                W, wk = load_w(win_v[:, :, col0:col0 + 512], 8, 512)
                for kc in range(8):
                    mm(psA[0][:, :], hT[:, kc, 0:128], W[:, kc, :], kc == 0, kc == 7, ["hT", wk], [PA[0]])
                return psA[0], PA[0]

            ps, pk = zpiece(0)
            op("act", lambda e: e.activation(out=tmpA[:, :], in_=psA[0][:, :], func=AF.Copy), [pk], ["tmpA"])
            op("dve", lambda e: e.tensor_copy(out=rden[:, :], in_=tmpA[:, :]), ["tmpA"], ["rden"])
            ps, pk = zpiece(512)
            op("act", lambda e: e.activation(out=tmpB[:, :], in_=psA[0][:, :], func=AF.Sigmoid), [pk], ["tmpB"])
            op("dve", lambda e: e.tensor_tensor(out=us, in0=rden[:, :], in1=tmpB[:, :], op=ALU.mult), ["rden", "tmpB"], [("cT", 0)])
            ps, pk = zpiece(1024)
            headnorm(ps, pk, NH, HD, BC_GQ, qs.rearrange("p (h d) -> p h d", h=NH), ("cT", 1))
            ps, pk = zpiece(1536)
            headnorm(ps, pk, NH, HD, BC_GK, ks.rearrange("p (h d) -> p h d", h=NH), ("cT", 3))
            dma("sp", ks_o[:, :], cT[0:4, 3, :], [("cT", 3)], ["hbm_ks"], "st_s1")
            ps, pk = zpiece(2048)
            op("act", lambda e: e.activation(out=vs, in_=psA[0][:, :], func=AF.Copy), [pk], ["uT"])
            dma("sp", vs_o[:, :], uT[0:4, 0, 0:512], ["uT"], ["hbm_vs"], "st_s2")
            ps, pk = zpiece(2560)
            headnorm(ps, pk, 4, 128, BC_GMQ, mqs.rearrange("p (h d) -> p h d", h=4), ("cT", 2))

            dma("sp", uT[0:120, 1, 0:512], stc_in[:, :], [], ["stc"], "c8")
            dma("sp", uT[0:120, 2, 0:512], w120_in[:, :], [], ["prd"], "c9")
            for n in range(4):
                dma("sp", convs_o[n, 0:29, :], uT[n * 30 + 1:n * 30 + 30, 1, 0:512], ["stc"], ["hbm_convs"], "st_s3")
            dma("sp", convs_o[:, 29, :], cT[0:4, 0, :], [("cT", 0)], ["hbm_convs"], "st_s3")
            op("dve", lambda e: e.tensor_tensor(out=uT[0:120, 2, 0:512], in0=uT[0:120, 2, 0:512], in1=uT[0:120, 1, 0:512], op=ALU.mult), ["stc", "prd"], ["prd"])
            mm(psA[1][:, :], IND120, uT[0:120, 2, 0:512], True, True, ["prd", "X"], [PA[1]])
            bload(cbuf[:, 0, :], ("cbuf", 0), vrow_in[0, 0:512])
            bload(cbuf[:, 1, :], ("cbuf", 1), vrow_in[1, 0:512])
            op("dve", lambda e: e.tensor_tensor(out=tmpA[:, :], in0=us, in1=cbuf[:, 0, :], op=ALU.mult), [("cT", 0), ("cbuf", 0)], ["tmpA"])
            op("dve", lambda e: e.tensor_tensor(out=tmpA[:, :], in0=tmpA[:, :], in1=cbuf[:, 1, :], op=ALU.add), ["tmpA", ("cbuf", 1)], ["tmpA"])
            op("dve", lambda e: e.tensor_tensor(out=tmpA[:, :], in0=tmpA[:, :], in1=psA[1][:, :], op=ALU.add), ["tmpA", PA[1]], ["tmpA"])
            op("dve", lambda e: e.tensor_reduce(out=ss[:, 0:1], in_=tmpA[:, :], axis=AX.X, op=ALU.add), ["tmpA"], [("ss", 0)])
            op("dve", lambda e: e.tensor_scalar(out=ss[:, 0:1], in0=ss[:, 0:1], scalar1=1.0 / 512, scalar2=None, op0=ALU.mult), [("ss", 0)], [("ss", 0)])
            op("dve", lambda e: e.tensor_scalar(out=tmpA[:, :], in0=tmpA[:, :], scalar1=ss[:, 0:1], scalar2=None, op0=ALU.subtract), ["tmpA", ("ss", 0)], ["tmpA"])
            op("dve", lambda e: e.memset(ss[:, 1:2], 0.0), [], [("ss", 1)])
            op("act", lambda e: e.activation(out=tmpB[:, :], in_=tmpA[:, :], func=AF.Square, accum_out=ss[:, 1:2]), ["tmpA", ("ss", 1)], ["tmpB", ("ss", 1)])
            op("act", lambda e: e.activation(out=ss[:, 1:2], in_=ss[:, 1:2], func=AF.Sqrt, bias=epsb[:, 0:1], scale=1.0 / 512), [("ss", 1), "epsb"], [("ss", 1)])
            op("dve", lambda e: e.reciprocal(out=ss[:, 1:2], in_=ss[:, 1:2]), [("ss", 1)], [("ss", 1)])
            bload(cbuf[:, 0, :], ("cbuf", 0), vrow_in[2, 0:512])
            bload(cbuf[:, 1, :], ("cbuf", 1), vrow_in[3, 0:512])
            op("dve", lambda e: e.tensor_scalar(out=tmpA[:, :], in0=tmpA[:, :], scalar1=ss[:, 1:2], scalar2=None, op0=ALU.mult), ["tmpA", ("ss", 1)], ["tmpA"])
            op("dve", lambda e: e.tensor_tensor(out=tmpA[:, :], in0=tmpA[:, :], in1=cbuf[:, 0, :], op=ALU.mult), ["tmpA", ("cbuf", 0)], ["tmpA"])
            op("dve", lambda e: e.tensor_tensor(out=tmpA[:, :], in0=tmpA[:, :], in1=cbuf[:, 1, :], op=ALU.add), ["tmpA", ("cbuf", 1)], ["tmpA"])
            op("act", lambda e: e.activation(out=xn[:, 0:512], in_=tmpA[:, :], func=AF.Silu), ["tmpA"], ["xn"])
            dbgout(xn[0:4, 0:512], [4, 512], ["xn"])
            for kc in range(4):
                tr(psT[:, kc * 128:(kc + 1) * 128], xn[:, kc * 128:(kc + 1) * 128], identb[:, :], ["xn", "identb"], ["psT"])
            op("dve", lambda e: e.tensor_copy(out=sT[:, :, 0:128], in_=psT[:, 0:512].rearrange("p (k t) -> p k t", k=4)), ["psT"], ["sT"])

            Lt = bc
            pbufs = [(kst[:, 0, :], ("kst", 0)), (kst[:, 1, :], ("kst", 1)), (vst[:, 0, :], ("vst", 0)), (vst[:, 1, :], ("vst", 1))]
            pstate = {"i": 0}
            pvstate = {"i": 0}

            def get_page(src_kind, n, pg):
                buf, bk = pbufs[pstate["i"] % 4]
                pstate["i"] += 1
                if src_kind in ("k", "v"):
                    src = ck_in if src_kind == "k" else cv_in
                    col = n * 64 + pg
                    op("pool", lambda e: e.indirect_dma_start(out=buf, out_offset=None, in_=src[:, :],
                                                              in_offset=bass.IndirectOffsetOnAxis(ap=idx_t[:, col:col + 1], axis=0)),
                       ["idx_t"], [bk], dma=True, semkey="pg_" + str(bk))
                else:
                    src = cmk_in if src_kind == "mk" else cmv_in
                    dma("sp", buf, src[n, pg * 128:(pg + 1) * 128, :], [], [bk], "pg_" + str(bk))
                return buf, bk

            def bcast_row(n, src, skey):
                mm(psA[2][:, :], RROW(n), src, True, True, ["X", skey], [PA[2]])
                op("act", lambda e: e.activation(out=qb, in_=psA[2][:, :], func=AF.Copy), [PA[2]], ["qb"])

            def dense_attn(kkind, vkind, npg, nh, hd, qsrc, qkey, moba):
                G = npg * nh
                first_sum = [True]
                for n in range(4):
                    bcast_row(n, qsrc, qkey)
                    for pg in range(npg):
                        buf, bk = get_page(kkind, n, pg)
                        op("dve", lambda e, buf=buf: e.tensor_tensor(out=tmpA[:, :], in0=buf, in1=qb, op=ALU.mult), [bk, "qb"], ["tmpA"])
                        o0 = n * G + pg * nh
                        op("dve", lambda e, o0=o0: e.tensor_reduce(out=Lt[:, o0:o0 + nh], in_=tmpA[:, :].rearrange("p (h d) -> p h d", h=nh), axis=AX.X, op=ALU.add),
                           ["tmpA"], ["bc"])
                if moba:
                    Lv = Lt[:, :].rearrange("p (g h) -> p g h", h=NH)
                    for h in range(NH):
                        mm(psA[3][0:8, 0:256], HCOL(h), Lv[:, :, h], h == 0, h == NH - 1, ["X", "bc"], [PA[3]])
                    op("dve", lambda e: e.tensor_reduce(out=rden[0:8, 0:128], in_=psA[3][0:8, 0:256].rearrange("p (g e) -> p g e", e=2), axis=AX.X, op=ALU.add),
                       [PA[3]], ["rden"])
                    for n in range(4):
                        op("dve", lambda e, n=n: e.max(out=top8[0:8, n, :], in_=rden[0:8, n * 32:(n + 1) * 32]), ["rden"], ["top8"])
                    op("dve", lambda e: e.tensor_tensor(out=rden[0:8, 128:256].rearrange("p (n b) -> p n b", n=4), in0=rden[0:8, 0:128].rearrange("p (n b) -> p n b", n=4),
                                                        in1=top8[0:8, 0:4, 2:3].broadcast_to([8, 4, 32]), op=ALU.is_ge), ["rden", "top8"], ["rden"])
                    op("dve", lambda e: e.tensor_copy(out=tmpB[0:8, 0:256].rearrange("p (g e) -> p g e", e=2), in_=rden[0:8, 128:256].unsqueeze(2).broadcast_to([8, 128, 2])),
                       ["rden"], ["tmpB"])
                for n in range(4):
                    if moba:
                        op("dve", lambda e, n=n: e.tensor_tensor(out=Lt[:, n * G:(n + 1) * G], in0=Lt[:, n * G:(n + 1) * G], in1=ALT, op=ALU.add), ["bc", "X"], ["bc"])
                    op("dve", lambda e, n=n: e.tensor_reduce(out=thr[:, n:n + 1], in_=Lt[:, n * G:(n + 1) * G], axis=AX.X, op=ALU.max), ["bc"], ["thr"])
                op("pe", lambda e: e.transpose(out=psM[0:4, 128:256], in_=thr[:, 0:4], identity=identf[:, :]), ["thr", "identf"], ["psM"])
                op("dve", lambda e: e.tensor_reduce(out=mvals[0:4, 6:7], in_=psM[0:4, 128:256], axis=AX.X, op=ALU.max), ["psM"], ["mvals"])
                if moba:
                    op("dve", lambda e: e.tensor_tensor(out=tmpA[:, :], in0=qs, in1=ks, op=ALU.mult), [("cT", 1), ("cT", 3)], ["tmpA"])
                    op("dve", lambda e: e.tensor_reduce(out=nsel[:, 0, 0:8], in_=tmpA[:, :].rearrange("p (h d) -> p h d", h=NH), axis=AX.X, op=ALU.add), ["tmpA"], ["nsel"])
                    op("dve", lambda e: e.tensor_reduce(out=mvals[0:4, 7:8], in_=nsel[0:4, 0, 0:8], axis=AX.X, op=ALU.max), ["nsel"], ["mvals"])
                    op("dve", lambda e: e.tensor_tensor(out=mvals[0:4, 6:7], in0=mvals[0:4, 6:7], in1=mvals[0:4, 7:8], op=ALU.max), ["mvals"], ["mvals"])
                op("dve", lambda e: e.tensor_scalar(out=mvals[0:4, 6:7], in0=mvals[0:4, 6:7], scalar1=-1.0, scalar2=None, op0=ALU.mult), ["mvals"], ["mvals"])
                op("dve", lambda e: e.tensor_scalar(out=nsel[0:4, 1, 0:4], in0=identf[0:4, 0:4], scalar1=mvals[0:4, 6:7], scalar2=None, op0=ALU.mult), ["mvals", "identf"], ["nsel"])
                mm(psM[:, 256:260], onesf[0:4, :], nsel[0:4, 1, 0:4], True, True, ["onesf", "nsel"], ["psM"])
                op("dve", lambda e: e.tensor_copy(out=thr[:, 4:8], in_=psM[:, 256:260]), ["psM"], ["thr"])
                for n in range(4):
                    op("act", lambda e, n=n: e.activation(out=Lt[:, n * G:(n + 1) * G], in_=Lt[:, n * G:(n + 1) * G], func=AF.Exp, bias=thr[:, 4 + n:5 + n], scale=1.0),
                       ["bc", "thr"], ["bc"])
                if moba:
                    op("act", lambda e: e.activation(out=nsel[0:4, 0, 0:8], in_=nsel[0:4, 0, 0:8], func=AF.Exp, bias=mvals[0:4, 6:7], scale=1.0), ["nsel", "mvals"], ["nsel"])
                    for h in range(NH):
                        mm(psA[h // 2][:, (h % 2) * 256:(h % 2) * 256 + 256], E8(h), tmpB[0:8, 0:256], True, True, ["X", "tmpB"], [PA[h // 2]])
                    for hp in range(4):
                        op("dve", lambda e, hp=hp: e.tensor_tensor(
                            out=Lt[:, :].rearrange("p (g h) -> p g h", h=NH)[:, :, 2 * hp:2 * hp + 2],
                            in0=Lt[:, :].rearrange("p (g h) -> p g h", h=NH)[:, :, 2 * hp:2 * hp + 2],
                            in1=psA[hp][:, :].rearrange("p (h g) -> p g h", h=2), op=ALU.mult), ["bc", PA[hp]], ["bc"])
                for n in range(4):
                    for c0 in range(0, G, 512):
                        w = min(512, G - c0)
                        mm(psA[4][:, 0:w], ONEROW(n), Lt[:, n * G + c0:n * G + c0 + w], n == 0 and c0 == 0, n == 3 and c0 + w >= G, ["X", "bc"], [PA[4]])
                Gc = min(G, 512)
                op("dve", lambda e: e.tensor_reduce(out=ss[:, 8:8 + nh], in_=psA[4][:, 0:Gc].rearrange("p (g h) -> p h g", h=nh), axis=AX.X, op=ALU.add), [PA[4]], ["ss8"])
                firstv = [True]
                for n in range(4):
                    for pg in range(npg):
                        buf, bk = get_page(vkind, n, pg)
                        o0 = n * G + pg * nh
                        slot = pvstate["i"] % 3
                        pvstate["i"] += 1
                        op("dve", lambda e, buf=buf, o0=o0, slot=slot: e.tensor_tensor(out=PT[:, slot, :].rearrange("p (h d) -> p h d", h=nh), in0=buf.rearrange("p (h d) -> p h d", h=nh),
                                                                                     in1=Lt[:, o0:o0 + nh].unsqueeze(2).broadcast_to([128, nh, hd]), op=ALU.mult), [bk, "bc"], [("PT", slot)])
                        mm(psA[5][:, :], ONEROWB(n), PT[:, slot, :], firstv[0], n == 3 and pg == npg - 1, ["orowb", ("PT", slot)], [PA[5]])
                        firstv[0] = False

            dense_attn("k", "v", 64, NH, HD, qs, ("cT", 1), True)
            op("dve", lambda e: e.tensor_tensor(out=ss[:, 8:16], in0=ss[:, 8:16], in1=nsel[:, 0, 0:8], op=ALU.add), ["ss8", "nsel"], ["ss8"])
            op("dve", lambda e: e.tensor_scalar(out=ss[:, 8:16], in0=ss[:, 8:16], scalar1=1e-30, scalar2=None, op0=ALU.add), ["ss8"], ["ss8"])
            op("dve", lambda e: e.reciprocal(out=ss[:, 8:16], in_=ss[:, 8:16]), ["ss8"], ["ss8"])
            op("dve", lambda e: e.tensor_tensor(out=tmpB[:, :].rearrange("p (h d) -> p h d", h=NH), in0=vs.rearrange("p (h d) -> p h d", h=NH),
                                                in1=nsel[:, 0, 0:8].unsqueeze(2).broadcast_to([128, NH, HD]), op=ALU.mult), ["uT", "nsel"], ["tmpB"])
            op("dve", lambda e: e.tensor_tensor(out=tmpB[:, :], in0=tmpB[:, :], in1=psA[5][:, :], op=ALU.add), ["tmpB", PA[5]], ["tmpB"])
            op("dve", lambda e: e.tensor_tensor(out=xn[:, 0:512].rearrange("p (h d) -> p h d", h=NH), in0=tmpB[:, :].rearrange("p (h d) -> p h d", h=NH),
                                                in1=ss[:, 8:16].unsqueeze(2).broadcast_to([128, NH, HD]), op=ALU.mult), ["tmpB", "ss8"], ["xn"])
            dbgout(rden[0:8, 0:256], [8, 256], ["rden"])
            dbgout(ss[0:4, 8:16], [4, 8], ["ss8"])
            dbgout(xn[0:4, 0:512], [4, 512], ["xn"])
            dbgout(bc[:, :], [128, 2048], ["bc"])
            dbgout(nsel[0:4, 0, 0:8], [4, 8], ["nsel"])
            dbgout(tmpB[0:4, :], [4, 512], ["tmpB"])
            for kc in range(4):
                tr(psT[:, kc * 128:(kc + 1) * 128], xn[:, kc * 128:(kc + 1) * 128], identb[:, :], ["xn", "identb"], ["psT"])
            op("dve", lambda e: e.tensor_copy(out=OT[:, :, 0:128], in_=psT[:, 0:512].rearrange("p (k t) -> p k t", k=4)), ["psT"], ["OT"])

            dense_attn("mk", "mv", 2, 4, 128, mqs, ("cT", 2), False)
            op("dve", lambda e: e.tensor_scalar(out=ss[:, 8:12], in0=ss[:, 8:12], scalar1=1e-30, scalar2=None, op0=ALU.add), ["ss8"], ["ss8"])
            op("dve", lambda e: e.reciprocal(out=ss[:, 8:12], in_=ss[:, 8:12]), ["ss8"], ["ss8"])
            op("dve", lambda e: e.tensor_tensor(out=xn[:, 0:512].rearrange("p (h d) -> p h d", h=4), in0=psA[5][:, :].rearrange("p (h d) -> p h d", h=4),
                                                in1=ss[:, 8:12].unsqueeze(2).broadcast_to([128, 4, 128]), op=ALU.mult), [PA[5], "ss8"], ["xn"])
            dbgout(xn[0:4, 0:512], [4, 512], ["xn"])
            for kc in range(4):
                tr(psT[:, kc * 128:(kc + 1) * 128], xn[:, kc * 128:(kc + 1) * 128], identb[:, :], ["xn", "identb"], ["psT"])
            op("dve", lambda e: e.tensor_copy(out=OmT[:, :, 0:128], in_=psT[:, 0:512].rearrange("p (k t) -> p k t", k=4)), ["psT"], ["OmT"])

            mg = uT[:, 1:3, :].rearrange("p a t -> p (a t)")[:, 0:1024]
            for half in range(2):
                srcs = ((wpa_v, sT, "sT"), (wpb_v, OT, "OT"), (wpc_v, OmT, "OmT"))
                for j in range(3):
                    wpv, src, skey = srcs[j]
                    Wp, kp = load_w(wpv[:, :, half * 512:(half + 1) * 512], 4, 512)
                    for kc in range(4):
                        mm(psA[j][:, :], src[:, kc, 0:128], Wp[:, kc, :], kc == 0, kc == 3, [kp, skey], [PA[j]])
                    col0 = 3072 + j * 1024 + half * 512
                    W, wk = load_w(win_v[:, :, col0:col0 + 512], 8, 512)
                    for kc in range(8):
                        mm(psA[3][:, :], hT[:, kc, 0:128], W[:, kc, :], kc == 0, kc == 7, ["hT", wk], [PA[3]])
                    bload(cbuf[:, 2, :], ("cbuf", 2), vrow_in[4, j * 1024 + half * 512:j * 1024 + half * 512 + 512])
                    op("dve", lambda e: e.tensor_tensor(out=tmpA[:, :], in0=psA[3][:, :], in1=cbuf[:, 2, :], op=ALU.add), [PA[3], ("cbuf", 2)], ["tmpA"])
                    op("act", lambda e: e.activation(out=tmpA[:, :], in_=tmpA[:, :], func=AF.Sigmoid), ["tmpA"], ["tmpA"])
                    if j == 0:
                        op("dve", lambda e, half=half: e.tensor_tensor(out=mg[:, half * 512:(half + 1) * 512], in0=psA[0][:, :], in1=tmpA[:, :], op=ALU.mult), [PA[0], "tmpA"], ["mg"])
                    else:
                        op("dve", lambda e, j=j: e.tensor_tensor(out=tmpB[:, :], in0=psA[j][:, :], in1=tmpA[:, :], op=ALU.mult), [PA[j], "tmpA"], ["tmpB"])
                        op("dve", lambda e, half=half: e.tensor_tensor(out=mg[:, half * 512:(half + 1) * 512], in0=mg[:, half * 512:(half + 1) * 512], in1=tmpB[:, :], op=ALU.add),
                           ["mg", "tmpB"], ["mg"])
            op("dve", lambda e: e.tensor_copy(out=xn[:, :], in_=mg), ["mg"], ["xn"])
            dbgout(xn[0:4, :], [4, 1024], ["xn"])
            for kc in range(8):
                tr(psT[:, kc * 128:(kc + 1) * 128], xn[:, kc * 128:(kc + 1) * 128], identb[:, :], ["xn", "identb"], ["psT"])
            op("dve", lambda e: e.tensor_copy(out=merged[:, :, 0:128], in_=psT[:, :].rearrange("p (k t) -> p k t", k=8)), ["psT"], ["merged"])
            for half in range(2):
                W, wk = load_w(wout_v[:, :, half * 512:(half + 1) * 512], 8, 512)
                for kc in range(8):
                    mm(psA[0][:, :], merged[:, kc, 0:128], W[:, kc, :], kc == 0, kc == 7, ["merged", wk], [PA[0]])
                op("dve", lambda e, half=half: e.tensor_tensor(out=X[:, 0, half * 512:(half + 1) * 512], in0=X[:, 0, half * 512:(half + 1) * 512], in1=psA[0][:, :], op=ALU.add),
                   ["X", PA[0]], ["X"])

            dbgout(X[0:4, 0, :], [4, 1024], ["X"])
            norm_to_T(X, 1, FM_G2, hT, "hT", "X")
            cs = scrA[:, :].bitcast(F32)
            stf = Vloc[:, :, :, :].rearrange("p a b c -> p (a b c)").bitcast(F32)
            op("dve", lambda e: e.memset(stf[:, 0:1024], 0.0), [], ["stf"])
            for piece in range(11):
                c0 = piece * 512
                W, wk = load_w(wup_v[:, :, c0:c0 + 512], 8, 512)
                for kc in range(8):
                    mm(psA[piece % 2][:, :], hT[:, kc, 0:128], W[:, kc, :], kc == 0, kc == 7, ["hT", wk], [PA[piece % 2]])
                pp, pkk = psA[piece % 2], PA[piece % 2]
                op("act", lambda e, pp=pp: e.activation(out=tmpA[:, :], in_=pp[:, :], func=AF.Copy), [pkk], ["tmpA"])
                dma("sp", ffns_o[:, 1, c0:c0 + 512], tmpA[0:4, :], ["tmpA"], ["hbm_ffns"], "st_s4")
                dma("sp", stf[0:4, 0:512], stf_in[:, c0:c0 + 512], [], ["stf"], "ld_stf")
                dma("sp", stf[0:4, 512:1024], stf_in[:, NUP + c0:NUP + c0 + 512], [], ["stf"], "ld_stf")
                dma("sp", ffns_o[:, 0, c0:c0 + 512], stf[0:4, 512:1024], ["stf"], ["hbm_ffns"], "st_s5")
                bload(cbuf[:, 0, :], ("cbuf", 0), vrow_in[5, c0:c0 + 512])
                op("dve", lambda e, c0=c0: e.tensor_tensor(out=cs[:, c0:c0 + 512], in0=stf[:, 0:512], in1=cbuf[:, 0, :], op=ALU.mult), ["stf", ("cbuf", 0)], ["cs"])
                bload(cbuf[:, 1, :], ("cbuf", 1), vrow_in[6, c0:c0 + 512])
                op("dve", lambda e: e.tensor_tensor(out=tmpB[:, :], in0=stf[:, 512:1024], in1=cbuf[:, 1, :], op=ALU.mult), ["stf", ("cbuf", 1)], ["tmpB"])
                op("dve", lambda e, c0=c0: e.tensor_tensor(out=cs[:, c0:c0 + 512], in0=cs[:, c0:c0 + 512], in1=tmpB[:, :], op=ALU.add), [("sa", 0), "tmpB"], ["cs"])
                bload(cbuf[:, 2, :], ("cbuf", 2), vrow_in[7, c0:c0 + 512])
                op("dve", lambda e: e.tensor_tensor(out=tmpB[:, :], in0=tmpA[:, :], in1=cbuf[:, 2, :], op=ALU.mult), ["tmpA", ("cbuf", 2)], ["tmpB"])
                op("dve", lambda e, c0=c0: e.tensor_tensor(out=cs[:, c0:c0 + 512], in0=cs[:, c0:c0 + 512], in1=tmpB[:, :], op=ALU.add), [("sa", 0), "tmpB"], ["cs"])
                bload(cbuf[:, 0, :], ("cbuf", 0), vrow_in[8, c0:c0 + 512])
                op("dve", lambda e, c0=c0: e.tensor_tensor(out=cs[:, c0:c0 + 512], in0=cs[:, c0:c0 + 512], in1=cbuf[:, 0, :], op=ALU.add), [("sa", 0), ("cbuf", 0)], ["cs"])
            actb = hT[:, 0:6, :].rearrange("p a t -> p (a t)")
            for c0 in range(0, DFF, 512):
                w = min(512, DFF - c0)
                op("act", lambda e, c0=c0, w=w: e.activation(out=tmpA[:, 0:w], in_=cs[:, c0:c0 + w], func=AF.Silu), ["cs"], ["tmpA"])
                op("dve", lambda e, c0=c0, w=w: e.tensor_tensor(out=actb[:, c0:c0 + w], in0=tmpA[:, 0:w], in1=cs[:, DFF + c0:DFF + c0 + w], op=ALU.mult), ["tmpA", ("sa", 0)], ["hT"])
            actT = qTa_s = None
            aT = sT[:, :, :].rearrange("p a t -> p (a t)")
            aT2 = OT[:, :, :].rearrange("p a t -> p (a t)")
            for grp in range(3):
                k0 = grp * 8
                nk = min(8, 22 - k0)
                for kk in range(nk):
                    tr(psT[:, kk * 128:(kk + 1) * 128], actb[:, (k0 + kk) * 128:(k0 + kk + 1) * 128], identb[:, :], ["hT", "identb"], ["psT"])
                if grp < 2:
                    op("dve", lambda e, k0=k0, nk=nk: e.tensor_copy(out=aT[:, k0 * 128:(k0 + nk) * 128], in_=psT[:, 0:nk * 128]), ["psT"], ["sT"])
                else:
                    op("dve", lambda e, nk=nk: e.tensor_copy(out=aT2[:, 0:nk * 128], in_=psT[:, 0:nk * 128]), ["psT"], ["OT"])

            def aTk(kc):
                return aT[:, kc * 128:(kc + 1) * 128] if kc < 16 else aT2[:, (kc - 16) * 128:(kc - 15) * 128]

            for half in range(2):
                for kp, (k0, k1) in enumerate(((0, 8), (8, 16), (16, 22))):
                    W, wk = load_w(wdn_v[:, k0:k1, half * 512:(half + 1) * 512], k1 - k0, 512)
                    for kc in range(k0, k1):
                        mm(psA[2][:, :], aTk(kc), W[:, kc - k0, :], kc == 0, kc == 21, ["sT", "OT", wk], [PA[2]])
                op("dve", lambda e, half=half: e.tensor_tensor(out=X[:, 0, half * 512:(half + 1) * 512], in0=X[:, 0, half * 512:(half + 1) * 512], in1=psA[2][:, :], op=ALU.add),
                   ["X", PA[2]], ["X"])
            dma("sp", ys_o[:, :], X[0:4, 0, :], ["X"], ["hbm_ys"], "st_y")

        allhist = [("hist", j) for j in range(44)]
        op("pe", lambda e: e.transpose(out=psM[0:88, 0:128], in_=hist[:, :, :].rearrange("p j r -> p (j r)"), identity=identf[:, :]), allhist + ["identf"], ["psM"])
        op("dve", lambda e: e.tensor_copy(out=outst[0:88, 0:128], in_=psM[0:88, 0:128]), ["psM"], ["outst"])
        dma("sp", ffnc_o[:, :], outst[0:88, 0:128], ["outst"], ["hbm_ffnc"], "st_misc")

        S.emit(st)
    return nc


def host_tables(inp):
    f = np.float32
    fm = np.zeros((128, FM_N), f)

    def fmaj(v):
        return np.ascontiguousarray(np.asarray(v, f).reshape(-1, 128).T)

    fm[:, FM_G1:FM_G1 + 8] = fmaj(inp["norm1_g"])
    fm[:, FM_G2:FM_G2 + 8] = fmaj(inp["norm2_g"])
    fm[:, FM_GM:FM_GM + 8] = fmaj(inp["mem_norm_g"])
    fm[:, FM_BG:FM_BG + 24] = fmaj(inp["b_gate"])
    wda = np.asarray(inp["w_dw_a"], f)
    fm[:, FM_WDA:FM_WDA + 124] = wda.T.reshape(4, 128, 31).transpose(1, 0, 2).reshape(128, 124)
    fm[:, FM_BDA:FM_BDA + 4] = fmaj(inp["b_dw_a"])
    fm[:, FM_LAG:FM_LAG + 4] = fmaj(inp["ln_a_g"])
    fm[:, FM_LAB:FM_LAB + 4] = fmaj(inp["ln_a_b"])
    wdf = np.asarray(inp["w_dw_f"], f)
    fm[:, FM_WDF:FM_WDF + 132] = wdf.T.reshape(44, 128, 3).transpose(1, 0, 2).reshape(128, 132)
    fm[:, FM_BDF:FM_BDF + 44] = fmaj(inp["b_dw_f"])
    bc = np.zeros((128, BC_N), f)
    bc[:, BC_GQ:BC_GQ + 512] = np.tile(np.asarray(inp["q_norm_g"], f), 8)[None, :]
    bc[:, BC_GK:BC_GK + 512] = np.tile(np.asarray(inp["k_norm_g"], f), 8)[None, :]
    bc[:, BC_GMQ:BC_GMQ + 512] = np.tile(np.asarray(inp["mq_norm_g"], f), 4)[None, :]
    bc[:, BC_GMK:BC_GMK + 512] = np.tile(np.asarray(inp["mk_norm_g"], f), 4)[None, :]
    ident = np.eye(128, dtype=f)
    p = np.arange(128)
    tri = np.where(p[:, None] > p[None, :], f(NEG), f(0)).astype(f)
    slopes = (2.0 ** (-np.arange(1, NH + 1, dtype=np.float64))).astype(f)
    j = np.arange(32)
    abase = (slopes[None, :, None] * (p[:, None, None] + 128.0 * (j[None, None, :] - 28))).astype(f).reshape(128, NH * 32)
    dt = (np.arange(4)[None, :] * 128 + p[:, None]).astype(np.int64)
    hi = (16 * (dt // 16)).astype(f)
    lo = (dt % 16).astype(f)
    qal = np.stack([-slopes[None, None, :] * hi[:, :, None], -slopes[None, None, :] * lo[:, :, None]], axis=-1).astype(f).reshape(128, 64)
    return {"fm": fm, "bc": bc, "ident": ident, "tri": tri, "abase": abase, "qal": qal}


_NC_CACHE = {}


def run_prompt(inp, nch=NCHUNK):
    key = ("p", nch)
    if key not in _NC_CACHE:
        _NC_CACHE[key] = build(nch=nch, do_sample=False)
    nc = _NC_CACHE[key]
    tabs = host_tables(inp)
    f = np.float32
    wnames = ["w_in", "w_proj_a", "w_proj_b", "w_proj_c", "w_mem_kv", "w_out", "w_up", "w_down"]
    shared = {n: np.ascontiguousarray(np.asarray(inp[n], f)) for n in wnames}
    shared.update(tabs)
    in_maps = []
    for c in range(8):
        s = c % 4
        m = dict(shared)
        m["x"] = np.ascontiguousarray(np.asarray(inp["x_prompt"][s], f))
        m["mem"] = np.ascontiguousarray(np.asarray(inp["mem_prompt"][s], f))
        in_maps.append(m)
    res = run_bass_kernel_spmd(nc, in_maps, core_ids=list(range(8)))
    return res.results


def sample_tables(inp):
    f = np.float32
    sc = np.zeros((128, SC_N), f)
    for n in range(4):
        sc[n, SC_RROW + n * 128:SC_RROW + (n + 1) * 128] = 1.0
    sc[:, SC_CS + 3] = 1.0
    sc[:, SC_ST + 127] = 1.0
    for k in range(4):
        sc[k, SC_REP + k * 32:SC_REP + (k + 1) * 32] = 1.0
    for h in range(8):
        sc[h, SC_E8 + h * 128:SC_E8 + (h + 1) * 128] = 1.0
    slopes = (2.0 ** (-np.arange(1, NH + 1, dtype=np.float64)))
    p = np.arange(128)[:, None, None]
    pg = np.arange(64)[None, :, None]
    sc[:, SC_AL:SC_AL + 512] = (-slopes[None, None, :] * (8192.0 - 128.0 * pg - p)).astype(f).reshape(128, 512)
    sc[:, SC_PI] = np.arange(128, dtype=f)
    for n in range(4):
        sc[n * 30:(n + 1) * 30, SC_IND + n] = 1.0
    vr = np.zeros((9, 5632), f)
    vr[0, :512] = np.asarray(inp["w_dw_a"], f)[30]
    vr[1, :512] = np.asarray(inp["b_dw_a"], f)
    vr[2, :512] = np.asarray(inp["ln_a_g"], f)
    vr[3, :512] = np.asarray(inp["ln_a_b"], f)
    vr[4, :3072] = np.asarray(inp["b_gate"], f)
    vr[5:8, :] = np.asarray(inp["w_dw_f"], f)
    vr[8, :] = np.asarray(inp["b_dw_f"], f)
    w120 = np.ascontiguousarray(np.tile(np.asarray(inp["w_dw_a"], f)[:30], (4, 1)))
    return sc, vr, w120


def run_all(inp):
    key = ("all",)
    if key not in _NC_CACHE:
        _NC_CACHE[key] = build(nch=NCHUNK, do_sample=True)
    nc = _NC_CACHE[key]
    tabs = host_tables(inp)
    f = np.float32
    wnames = ["w_in", "w_proj_a", "w_proj_b", "w_proj_c", "w_mem_kv", "w_out", "w_up", "w_down"]
    shared = {n: np.ascontiguousarray(np.asarray(inp[n], f)) for n in wnames}
    shared.update(tabs)
    sc, vr, w120 = sample_tables(inp)
    shared["sconst"] = sc
    shared["vrows"] = vr
    shared["w120"] = w120
    shared["cache_k"] = np.ascontiguousarray(np.asarray(inp["cache_k"], f)).reshape(N_POOL * 128, 512)
    shared["cache_v"] = np.ascontiguousarray(np.asarray(inp["cache_v"], f)).reshape(N_POOL * 128, 512)
    pt = np.asarray(inp["page_table"]).astype(np.int32)
    in_maps = []
    for c in range(8):
        s = c % 4
        m = dict(shared)
        m["x"] = np.ascontiguousarray(np.asarray(inp["x_prompt"][s], f))
        m["mem"] = np.ascontiguousarray(np.asarray(inp["mem_prompt"][s], f))
        sl = slice(4 * c, 4 * c + 4)
        xs = np.zeros((128, D), f)
        xs[0:4] = np.asarray(inp["x_sample"], f)[sl, 0, :]
        m["xs"] = xs
        m["stc"] = np.ascontiguousarray(np.asarray(inp["state_conv"], f)[sl].reshape(120, 512))
        m["stf"] = np.ascontiguousarray(np.asarray(inp["state_ffn_conv"], f)[sl].reshape(4, 2 * NUP))
        m["cmk"] = np.ascontiguousarray(np.asarray(inp["cache_mem_k"], f)[sl].reshape(4, 256, 512))
        m["cmv"] = np.ascontiguousarray(np.asarray(inp["cache_mem_v"], f)[sl].reshape(4, 256, 512))
        m["ptrep"] = np.ascontiguousarray(np.broadcast_to(pt[sl].reshape(1, 256), (128, 256))).astype(np.int32)
        in_maps.append(m)
    res = run_bass_kernel_spmd(nc, in_maps, core_ids=list(range(8)))
    return res.results


def kernel(**inp):
    r = run_all(inp)
    f = np.float32
    y_p = np.stack([r[s]["y"] for s in range(4)]).astype(f)
    k_p = np.stack([r[s]["k"] for s in range(4)]).reshape(4, SEQ, NH, HD).astype(f)
    v_p = np.stack([r[s]["v"] for s in range(4)]).reshape(4, SEQ, NH, HD).astype(f)
    conv_p = np.stack([r[s]["conv"][2:32] for s in range(4)]).astype(f)
    ffn_p = np.stack([r[s]["ffnc"].reshape(44, 2, 128).transpose(1, 0, 2).reshape(2, NUP) for s in range(4)]).astype(f)
    mk_p = np.stack([r[s]["mk"] for s in range(4)]).reshape(4, 256, 4, 128).astype(f)
    mv_p = np.stack([r[s]["mv"] for s in range(4)]).reshape(4, 256, 4, 128).astype(f)
    y_s = np.concatenate([r[c]["ys"] for c in range(8)]).reshape(32, 1, D).astype(f)
    k_s = np.concatenate([r[c]["ks"] for c in range(8)]).reshape(32, 1, NH, HD).astype(f)
    v_s = np.concatenate([r[c]["vs"] for c in range(8)]).reshape(32, 1, NH, HD).astype(f)
    conv_s = np.concatenate([r[c]["convs"] for c in range(8)]).reshape(32, 30, 512).astype(f)
    ffn_s = np.concatenate([r[c]["ffns"] for c in range(8)]).reshape(32, 2, NUP).astype(f)
    return (y_p, y_s, k_p, v_p, k_s, v_s, conv_p, conv_s, ffn_p, ffn_s, mk_p, mv_p)
```

```python
import contextlib
import numpy as np
import concourse.bass as bass
import concourse.mybir as mybir
from concourse.bass_utils import run_bass_kernel_spmd

F32 = mybir.dt.float32
BF16 = mybir.dt.bfloat16
I32 = mybir.dt.int32
AF = mybir.ActivationFunctionType
ALU = mybir.AluOpType
AX = mybir.AxisListType

D = 1024
SEQ = 4096
NT = 512
NCHUNK = SEQ // NT
CONV_CH = 512
CW = 31
NH = 8
HD = 64
DFF = 2816
NUP = 2 * DFF
NIN = 6144
EPS = 1e-6
NEG = -30000.0
FFN_MUL_ENG = "dve"
N_POOL = 2560


class Sched:
    ENGS = ("pe", "act", "dve", "pool", "sp")

    def __init__(self, nc):
        self.nc = nc
        self.q = {e: [] for e in self.ENGS}
        self.last_w = {}
        self.readers = {}
        self.dma_cnt = {}
        self.n_ops = 0
        self.alias = {}

    def op(self, eng, fn, reads=(), writes=(), dma=False, semkey=None):
        rec = {"fn": fn, "eng": eng, "deps": [], "need_inc": False, "dma": dma,
               "semkey": None, "dval": 0}
        self.n_ops += 1
        deps = []
        for k in reads:
            w = self.last_w.get(k)
            if w is not None:
                deps.append(w)
        for k in writes:
            w = self.last_w.get(k)
            if w is not None:
                deps.append(w)
            for r in self.readers.get(k, ()):
                deps.append(r)
            for a in self.alias.get(k, ()):
                w = self.last_w.get(a)
                if w is not None:
                    deps.append(w)
                for r in self.readers.get(a, ()):
                    deps.append(r)
        if dma:
            assert semkey is not None
            rec["semkey"] = semkey
            self.dma_cnt[semkey] = self.dma_cnt.get(semkey, 0) + 16
            rec["dval"] = self.dma_cnt[semkey]
            me = ("d", semkey, rec["dval"])
        else:
            me = ("c", rec)
        seen = set()
        for d in deps:
            if d[0] == "c":
                p = d[1]
                if p is rec:
                    continue
                if p["eng"] == eng and eng == "pe":
                    continue
                if id(p) in seen:
                    continue
                seen.add(id(p))
                p["need_inc"] = True
                rec["deps"].append(d)
            else:
                key = (d[1], d[2])
                if key in seen:
                    continue
                seen.add(key)
                rec["deps"].append(d)
        for k in writes:
            self.last_w[k] = me
            self.readers[k] = []
        for k in reads:
            if k in writes:
                continue
            self.readers.setdefault(k, []).append(me)
        self.q[eng].append(rec)
        return rec

    def emit(self, st):
        nc = self.nc
        esem = {e: st.enter_context(nc.semaphore("e_" + e)) for e in self.ENGS}
        dsem = {}
        for i, k in enumerate(self.dma_cnt):
            dsem[k] = st.enter_context(nc.semaphore("d%d" % i))
        for e in self.ENGS:
            c = 0
            for rec in self.q[e]:
                if rec["need_inc"] and not rec["dma"]:
                    c += 1
                    rec["inc_val"] = c
        block = st.enter_context(nc.Block())
        hw = {"pe": nc.tensor, "act": nc.scalar, "dve": nc.vector, "pool": nc.gpsimd, "sp": nc.sync}

        def run(e):
            eng = hw[e]
            waited = {}
            for rec in self.q[e]:
                for d in rec["deps"]:
                    if d[0] == "c":
                        p = d[1]
                        sem = esem[p["eng"]]
                        val = p["inc_val"]
                        key = ("e", p["eng"])
                    else:
                        sem = dsem[d[1]]
                        val = d[2]
                        key = ("d", d[1])
                    if waited.get(key, 0) >= val:
                        continue
                    waited[key] = val
                    eng.wait_ge(sem, val)
                ins = rec["fn"](eng)
                if rec["dma"]:
                    ins.then_inc(dsem[rec["semkey"]], 16)
                elif rec["need_inc"]:
                    ins.then_inc(esem[e], 1)
            if e == "sp":
                for k, v in self.dma_cnt.items():
                    eng.wait_ge(dsem[k], v)

        block.tensor(lambda _e: run("pe"))
        block.scalar(lambda _e: run("act"))
        block.vector(lambda _e: run("dve"))
        block.gpsimd(lambda _e: run("pool"))
        block.sync(lambda _e: run("sp"))


FM_G1, FM_G2, FM_GM, FM_BG, FM_WDA, FM_BDA, FM_LAG, FM_LAB, FM_WDF, FM_BDF = 0, 8, 16, 24, 48, 172, 176, 180, 184, 316
FM_N = 360
BC_GQ, BC_GK, BC_GMQ, BC_GMK = 0, 512, 1024, 1536
BC_N = 2048


SC_RROW, SC_CS, SC_ST, SC_REP, SC_E8, SC_AL, SC_PI, SC_IND = 0, 512, 643, 898, 1026, 2050, 2562, 2563
SC_N = 2691


def build(nch=NCHUNK, do_sample=True, dbg=False):
    nc = bass.Bass("TRN2", target_bir_lowering=False)

    def din(name, shape, dt=F32):
        return nc.dram_tensor(name, list(shape), dt, kind="ExternalInput").ap()

    def dout(name, shape, dt=F32):
        return nc.dram_tensor(name, list(shape), dt, kind="ExternalOutput").ap()

    xin = din("x", [SEQ, D])
    memin = din("mem", [256, D])
    w_in = din("w_in", [D, NIN])
    w_pa = din("w_proj_a", [512, D])
    w_pb = din("w_proj_b", [512, D])
    w_pc = din("w_proj_c", [512, D])
    w_mkv = din("w_mem_kv", [D, D])
    w_out = din("w_out", [D, D])
    w_up = din("w_up", [D, NUP])
    w_down = din("w_down", [DFF, D])
    fm_in = din("fm", [128, FM_N])
    bc_in = din("bc", [128, BC_N])
    ident_in = din("ident", [128, 128])
    tri_in = din("tri", [128, 128])
    abase_in = din("abase", [128, NH * 32])
    qal_in = din("qal", [128, 4 * NH * 2])

    if do_sample:
        xs_in = din("xs", [128, D])
        stc_in = din("stc", [120, 512])
        w120_in = din("w120", [120, 512])
        stf_in = din("stf", [4, 2 * NUP])
        cmk_in = din("cmk", [4, 256, 512])
        cmv_in = din("cmv", [4, 256, 512])
        pt_in = din("ptrep", [128, 256], I32)
        ck_in = din("cache_k", [N_POOL * 128, 512])
        cv_in = din("cache_v", [N_POOL * 128, 512])
        sc_in = din("sconst", [128, SC_N])
        vrow_in = din("vrows", [9, 5632])
        ys_o = dout("ys", [4, D])
        ks_o = dout("ks", [4, 512])
        vs_o = dout("vs", [4, 512])
        convs_o = dout("convs", [4, 30, 512])
        ffns_o = dout("ffns", [4, 2, NUP])
    y_o = dout("y", [SEQ, D])
    k_o = dout("k", [SEQ, 512])
    v_o = dout("v", [SEQ, 512])
    conv_o = dout("conv", [32, 512])
    ffnc_o = dout("ffnc", [88, 128])
    mk_o = dout("mk", [256, 512])
    mv_o = dout("mv", [256, 512])
    kt_scr = nc.dram_tensor("kt_scr", [NH, 82, SEQ], BF16, kind="Internal").ap()
    v_scr = nc.dram_tensor("v_scr", [NH, SEQ // 128, 128, 128], BF16, kind="Internal").ap()

    st = contextlib.ExitStack()
    with st:
        def sb(name, shape, dt):
            return st.enter_context(nc.sbuf_tensor("s_" + name, list(shape), dt))

        def psum(name, shape, dt):
            return st.enter_context(nc.psum_tensor("p_" + name, list(shape), dt))

        S = Sched(nc)
        op = S.op
        dbg_n = [0]

        def dbgout(src_ap, shape, keys, dt=F32):
            if not dbg:
                return
            name = "dbg%d" % dbg_n[0]
            dbg_n[0] += 1
            t = nc.dram_tensor(name, list(shape), dt, kind="ExternalOutput").ap()
            op("pool", lambda e: e.dma_start(out=t, in_=src_ap), reads=keys, writes=["hbm_" + name], dma=True, semkey="dbg")

        identb = sb("identb", [128, 128], BF16)
        identf = sb("identf", [128, 128], F32)
        trib = sb("trib", [128, 128], BF16)
        onesb = sb("onesb", [128, 128], BF16)
        onesf = sb("onesf", [128, 128], F32)
        fm = sb("fm", [128, FM_N], F32)
        bc = sb("bc", [128, BC_N], F32)
        btab = sb("btab", [128, NH * 32], F32)
        mvals = sb("mvals", [128, 8], F32)
        epsb = sb("epsb", [128, 1], F32)
        X = sb("X", [128, 4, D], F32)
        xn = sb("xn", [128, D], BF16)
        ss = sb("ss", [128, 16], F32)
        hT = sb("hT", [128, 8, NT], BF16)
        uT = sb("uT", [128, 4, 30 + NT], F32)
        cT = sb("cT", [128, 4, NT], F32)
        sT = sb("sT", [128, 4, NT], BF16)
        tmpA = sb("tmpA", [128, NT], F32)
        tmpB = sb("tmpB", [128, NT], F32)
        Qaug = sb("Qaug", [128, 4, NH, 82], BF16)
        Kaug = sb("Kaug", [128, 4, NH, 82], BF16)
        Vloc = sb("Vloc", [128, 4, NH, 128], BF16)
        mqT = sb("mqT", [128, 4, NT], BF16)
        qn_bf = sb("qn_bf", [128, 512], BF16)
        kn_bf = sb("kn_bf", [128, 2, 512], BF16)
        kst = sb("kst", [128, 2, 512], F32)
        vst = sb("vst", [128, 2, 512], F32)
        kmBD = sb("kmBD", [128, 4, 2, 16], BF16)
        gatef = sb("gatef", [128, NH, 16], F32)
        top8 = sb("top8", [128, NH, 8], F32)
        thr = sb("thr", [128, NH], F32)
        nsel = sb("nsel", [128, NH, 16], F32)
        PT = sb("PT", [128, 3, NT], BF16)
        OT = sb("OT", [128, 4, NT], BF16)
        OmT = sb("OmT", [128, 4, NT], BF16)
        rden = sb("rden", [128, NT], F32)
        mkT = sb("mkT", [128, 4, 256], BF16)
        mVa = sb("mVa", [128, 2, 512], BF16)
        merged = sb("merged", [128, 8, NT], BF16)
        scrA = sb("scrA", [128, 22 * NT], BF16)
        sa = scrA[:, :].rearrange("p (j t) -> p j t", j=22)
        KTloc = scrA[:, 0:4096].rearrange("p (j t) -> p j t", j=NH)
        qTa = scrA[:, 4096:8192].rearrange("p (j t) -> p j t", j=NH)
        mqn = scrA[:, 8192:10240].rearrange("p (j t) -> p j t", j=4)
        qTp = scrA[:, 10240:10752].rearrange("p (j t) -> p j t", j=4)
        cbuf = sb("cbuf", [128, 3, NT], F32)
        tmpC = cbuf[:, 0, :]
        tmpD = cbuf[:, 1, :]
        sig = cT
        junk = merged[:, 0:2, :].rearrange("p a t -> p (a t)")
        for _k in ("KTloc", "qTa", "mqn", "qTp"):
            S.alias[_k] = [("sa", j) for j in range(22)]
        for j in range(22):
            S.alias[("sa", j)] = ["KTloc", "qTa", "mqn", "qTp"]
        hist = sb("hist", [128, 44, 2], F32)
        corr = sb("corr", [128, 44, 2], F32)
        corrt = sb("corrt", [128, 44], F32)
        ring = sb("ring", [128, 4, 4096], BF16)
        gring = sb("gring", [128, 4, 1024], BF16)
        kvK = sb("kvK", [128, 4, NT], BF16)
        kvV = sb("kvV", [128, 4, 4, 128], BF16)
        outst = sb("outst", [128, 512], F32)
        idx_t = sb("idx_t", [128, 256], I32)
        orowb = sb("orowb", [128, 132], BF16)
        ptf = sb("ptf", [128, 256], F32)

        psA = [psum("psA%d" % i, [128, 512], F32) for i in range(6)]
        psT = psum("psT", [128, 1024], BF16)
        psM = psum("psM", [128, 512], F32)
        PA = ["psA%d" % i for i in range(6)]

        def dma(eng, out, in_, reads, writes, semkey):
            return op(eng, lambda e: e.dma_start(out=out, in_=in_), reads=reads, writes=writes, dma=True, semkey=semkey)

        def mm(out, lhsT, rhs, start, stop, reads, writes):
            return op("pe", lambda e: e.matmul(out, lhsT=lhsT, rhs=rhs, start=start, stop=stop, skip_group_check=True),
                      reads=reads, writes=writes)

        def tr(out, in_, ident, reads, writes):
            return op("pe", lambda e: e.transpose(out=out, in_=in_, identity=ident), reads=reads, writes=writes)

        rstate = {"i": 0, "g": 0}

        def load_w(view, nk, ncols):
            i = rstate["i"] % 4
            rstate["i"] += 1
            dst = ring[:, i, 0:nk * ncols].rearrange("p (k n) -> p k n", k=nk)
            dma("pool", dst, view, [], [("ring", i)], "ring%d" % i)
            return dst, ("ring", i)

        def load_g(view):
            i = rstate["g"] % 4
            rstate["g"] += 1
            dst = gring[:, i, :].rearrange("p (k n) -> p k n", k=8)
            dma("pool", dst, view, [], [("gring", i)], "gring%d" % i)
            return dst, ("gring", i)

        win_v = w_in.rearrange("(kc p) n -> p kc n", p=128)
        wup_v = w_up.rearrange("(kc p) n -> p kc n", p=128)
        wout_v = w_out.rearrange("(kc p) n -> p kc n", p=128)
        wdn_v = w_down.rearrange("(kc p) n -> p kc n", p=128)
        wmkv_v = w_mkv.rearrange("(kc p) n -> p kc n", p=128)
        wpa_v = w_pa.rearrange("(kc p) n -> p kc n", p=128)
        wpb_v = w_pb.rearrange("(kc p) n -> p kc n", p=128)
        wpc_v = w_pc.rearrange("(kc p) n -> p kc n", p=128)

        dma("sp", fm[:, :], fm_in[:, :], [], ["fm"], "c1")
        dma("sp", bc[:, :], bc_in[:, :], [], ["bc"], "c2")
        dma("sp", identf[:, :], ident_in[:, :], [], ["identf"], "c3")
        dma("sp", btab[:, :], abase_in[:, :], [], ["btab"], "c4")
        dma("sp", tmpA[:, 0:128], tri_in[:, :], [], ["tmpA"], "c5")
        dma("sp", tmpB[:, 0:64], qal_in[:, :], [], ["tmpB"], "c6")
        op("dve", lambda e: e.tensor_copy(out=identb[:, :], in_=identf[:, :]), ["identf"], ["identb"])
        op("dve", lambda e: e.tensor_copy(out=trib[:, :], in_=tmpA[:, 0:128]), ["tmpA"], ["trib"])
        op("dve", lambda e: e.memset(onesb[:, :], 1.0), [], ["onesb"])
        op("dve", lambda e: e.memset(onesf[:, :], 1.0), [], ["onesf"])
        op("dve", lambda e: e.memset(epsb[:, :], EPS), [], ["epsb"])
        op("dve", lambda e: e.memset(uT[:, :, :], 0.0), [], ["uT"])
        op("dve", lambda e: e.memset(hist[:, :, :], 0.0), [], ["hist"])
        op("dve", lambda e: e.memset(kmBD[:, :, :, :], 0.0), [], ["kmBD"])
        op("dve", lambda e: e.memset(gatef[:, :, :], -1e30), [], ["gatef"])
        op("dve", lambda e: e.memset(Vloc[:, :, :, 0:64], 1.0), [], ["Vloc"])
        op("dve", lambda e: e.memset(Kaug[:, :, :, 80:82], 1.0), [], ["Kaug"])
        op("dve", lambda e: e.tensor_copy(out=Qaug[:, :, :, 80:82],
                                          in_=tmpB[:, 0:64].rearrange("p (s h r) -> p s h r", s=4, h=NH)),
           ["tmpB"], ["Qaug"])
        for j, (off, n) in enumerate(((BC_GQ, 64), (BC_GK, 64), (BC_GMQ, 128), (BC_GMK, 128))):
            op("dve", lambda e, j=j, off=off, n=n: e.tensor_reduce(out=mvals[:, j:j + 1], in_=bc[:, off:off + n], axis=AX.X,
                                                                  op=ALU.max, apply_absolute_value=True),
               ["bc"], ["mvals"])
        op("dve", lambda e: e.tensor_tensor(out=mvals[:, 4:5], in0=mvals[:, 0:1], in1=mvals[:, 1:2], op=ALU.mult), ["mvals"], ["mvals"])
        op("dve", lambda e: e.tensor_scalar(out=mvals[:, 4:5], in0=mvals[:, 4:5], scalar1=8.0, scalar2=None, op0=ALU.mult), ["mvals"], ["mvals"])
        op("dve", lambda e: e.tensor_tensor(out=mvals[:, 5:6], in0=mvals[:, 2:3], in1=mvals[:, 3:4], op=ALU.mult), ["mvals"], ["mvals"])
        op("dve", lambda e: e.tensor_scalar(out=mvals[:, 5:6], in0=mvals[:, 5:6], scalar1=-(128.0 ** 0.5), scalar2=None, op0=ALU.mult), ["mvals"], ["mvals"])
        op("dve", lambda e: e.tensor_scalar(out=btab[:, :], in0=btab[:, :], scalar1=mvals[:, 4:5], scalar2=None, op0=ALU.subtract),
           ["btab", "mvals"], ["btab"])
        op("dve", lambda e: e.tensor_scalar(out=bc[:, BC_GQ:BC_GQ + 512], in0=bc[:, BC_GQ:BC_GQ + 512], scalar1=HD ** -0.5, scalar2=None, op0=ALU.mult), ["bc"], ["bc"])
        op("dve", lambda e: e.tensor_scalar(out=bc[:, BC_GMQ:BC_GMQ + 512], in0=bc[:, BC_GMQ:BC_GMQ + 512], scalar1=128.0 ** -0.5, scalar2=None, op0=ALU.mult), ["bc"], ["bc"])

        def rms_rows(src3, sub, col):
            op("dve", lambda e: e.memset(ss[:, col:col + 1], 0.0), [], [("ss", col)])
            op("act", lambda e: e.activation(out=junk[:, :], in_=src3[:, sub, :], func=AF.Square, accum_out=ss[:, col:col + 1]),
               ["X", ("ss", col)], ["merged", ("ss", col)])
            op("act", lambda e: e.activation(out=ss[:, col:col + 1], in_=ss[:, col:col + 1], func=AF.Sqrt, bias=epsb[:, 0:1], scale=1.0 / D),
               [("ss", col), "epsb"], [("ss", col)])
            op("dve", lambda e: e.reciprocal(out=ss[:, col:col + 1], in_=ss[:, col:col + 1]), [("ss", col)], [("ss", col)])

        def norm_to_T(src3, nsub, goff, dstT, dkey, srckey):
            for sub in range(nsub):
                rms_rows(src3, sub, sub)
            for sub in range(nsub):
                op("dve", lambda e, sub=sub: e.tensor_scalar(out=xn[:, :], in0=src3[:, sub, :], scalar1=ss[:, sub:sub + 1], scalar2=None, op0=ALU.mult),
                   [srckey, ("ss", sub)], ["xn"])
                for kc in range(8):
                    tr(psT[:, kc * 128:(kc + 1) * 128], xn[:, kc * 128:(kc + 1) * 128], identb[:, :], ["xn", "identb"], ["psT"])
                op("dve", lambda e, sub=sub: e.tensor_tensor(
                    out=dstT[:, :, sub * 128:(sub + 1) * 128], in0=psT[:, :].rearrange("p (k t) -> p k t", k=8),
                    in1=fm[:, goff:goff + 8].unsqueeze(2).broadcast_to([128, 8, 128]), op=ALU.mult),
                   ["psT", "fm"], [dkey])

        def headnorm(ps, pkey, nh, hd, goff, out_ap, okey, extra_reads=()):
            op("act", lambda e: e.activation(out=tmpA[:, :], in_=ps[:, :], func=AF.Square), [pkey], ["tmpA"])
            op("dve", lambda e: e.tensor_reduce(out=ss[:, 8:8 + nh], in_=tmpA[:, :].rearrange("p (h d) -> p h d", h=nh), axis=AX.X, op=ALU.add),
               ["tmpA"], ["ss8"])
            op("act", lambda e: e.activation(out=ss[:, 8:8 + nh], in_=ss[:, 8:8 + nh], func=AF.Sqrt, bias=epsb[:, 0:1], scale=1.0 / hd),
               ["ss8", "epsb"], ["ss8"])
            op("dve", lambda e: e.reciprocal(out=ss[:, 8:8 + nh], in_=ss[:, 8:8 + nh]), ["ss8"], ["ss8"])
            op("dve", lambda e: e.tensor_tensor(out=tmpB[:, :].rearrange("p (h d) -> p h d", h=nh), in0=ps[:, :].rearrange("p (h d) -> p h d", h=nh),
                                                in1=ss[:, 8:8 + nh].unsqueeze(2).broadcast_to([128, nh, hd]), op=ALU.mult),
               [pkey, "ss8"], ["tmpB"])
            op("dve", lambda e: e.tensor_tensor(out=out_ap, in0=tmpB[:, :].rearrange("p (h d) -> p h d", h=nh),
                                                in1=bc[:, goff:goff + 512].rearrange("p (h d) -> p h d", h=nh), op=ALU.mult),
               ["tmpB", "bc"] + list(extra_reads), [okey])

        dma("sp", X[:, 0:2, :], memin.rearrange("(s p) d -> p s d", p=128), [], ["X"], "xload")
        norm_to_T(X, 2, FM_GM, hT, "hT", "X")
        for half in range(2):
            W, wk = load_w(wmkv_v[:, :, half * 512:(half + 1) * 512], 8, 512)
            for sub in range(2):
                ps = psA[sub]
                for kc in range(8):
                    mm(ps[:, :], hT[:, kc, sub * 128:(sub + 1) * 128], W[:, kc, :], kc == 0, kc == 7, ["hT", wk], [PA[sub]])
                if half == 0:
                    headnorm(ps, PA[sub], 4, 128, BC_GMK, kst[:, sub, :].rearrange("p (h d) -> p h d", h=4), ("kst", sub))
                    dma("sp", mk_o[sub * 128:(sub + 1) * 128, :], kst[:, sub, :], [("kst", sub)], ["hbm_mk"], "st_k%d" % sub)
                    op("dve", lambda e, sub=sub: e.tensor_copy(out=mqn[:, sub, :], in_=kst[:, sub, :]), [("kst", sub)], ["mqn"])
                    for mh in range(4):
                        tr(psT[:, mh * 128:(mh + 1) * 128], mqn[:, sub, mh * 128:(mh + 1) * 128], identb[:, :], ["mqn", "identb"], ["psT"])
                    op("dve", lambda e, sub=sub: e.tensor_copy(out=mkT[:, :, sub * 128:(sub + 1) * 128],
                                                               in_=psT[:, 0:512].rearrange("p (k t) -> p k t", k=4)), ["psT"], ["mkT"])
                else:
                    op("act", lambda e, sub=sub, ps=ps: e.activation(out=vst[:, sub, :], in_=ps[:, :], func=AF.Copy), [PA[sub]], [("vst", sub)])
                    dma("sp", mv_o[sub * 128:(sub + 1) * 128, :], vst[:, sub, :], [("vst", sub)], ["hbm_mv"], "st_v%d" % sub)
                    op("dve", lambda e, sub=sub: e.tensor_copy(out=mVa[:, sub, :], in_=vst[:, sub, :]), [("vst", sub)], ["mVa"])

        kvstate = {"i": 0}
        for c in range(nch):
            t0 = c * NT
            dma("sp", X[:, :, :], xin[t0:t0 + NT, :].rearrange("(s p) d -> p s d", p=128), [], ["X"], "xload")
            norm_to_T(X, 4, FM_G1, hT, "hT", "X")

            Wa, wka = load_w(win_v[:, :, 0:512], 8, 512)
            Wg, wkg = load_w(win_v[:, :, 512:1024], 8, 512)
            for cc in range(4):
                pa, pg = psA[2 * (cc % 2)], psA[2 * (cc % 2) + 1]
                ka, kg = PA[2 * (cc % 2)], PA[2 * (cc % 2) + 1]
                for kc in range(8):
                    mm(pa[:, :], Wa[:, kc, cc * 128:(cc + 1) * 128], hT[:, kc, :], kc == 0, kc == 7, ["hT", wka], [ka])
                for kc in range(8):
                    mm(pg[:, :], Wg[:, kc, cc * 128:(cc + 1) * 128], hT[:, kc, :], kc == 0, kc == 7, ["hT", wkg], [kg])
                tk = ("cbuf", 0) if cc % 2 == 0 else ("cbuf", 1)
                tt = tmpC if cc % 2 == 0 else tmpD
                op("act", lambda e, pg=pg, tt=tt: e.activation(out=tt[:, :], in_=pg[:, :], func=AF.Sigmoid), [kg], [tk])
                op("dve", lambda e, pa=pa, tt=tt, cc=cc: e.tensor_tensor(out=uT[:, cc, 30:30 + NT], in0=pa[:, :], in1=tt[:, :], op=ALU.mult),
                   [ka, tk], ["uT"])
            conv_steps = []
            for cc in range(4):
                conv_steps.append(lambda cc=cc: op("dve", lambda e: e.tensor_scalar(
                    out=cT[:, cc, :], in0=uT[:, cc, 0:NT], scalar1=fm[:, FM_WDA + cc * 31:FM_WDA + cc * 31 + 1],
                    scalar2=fm[:, FM_BDA + cc:FM_BDA + cc + 1], op0=ALU.mult, op1=ALU.add), ["uT", "fm"], [("cT", cc)]))
                for k in range(1, CW):
                    conv_steps.append(lambda cc=cc, k=k: op("dve", lambda e: e.scalar_tensor_tensor(
                        out=cT[:, cc, :], in0=uT[:, cc, k:k + NT], scalar=fm[:, FM_WDA + cc * 31 + k:FM_WDA + cc * 31 + k + 1],
                        in1=cT[:, cc, :], op0=ALU.mult, op1=ALU.add), ["uT", "fm", ("cT", cc)], [("cT", cc)]))

            def conv_finish(c=c):
                if c == nch - 1:
                    for cc in range(4):
                        op("pe", lambda e, cc=cc: e.transpose(out=psM[0:32, cc * 128:(cc + 1) * 128], in_=uT[:, cc, 30 + NT - 32:30 + NT], identity=identf[:, :]),
                           ["uT", "identf"], ["psM"])
                    op("dve", lambda e: e.tensor_copy(out=outst[0:32, :], in_=psM[0:32, :]), ["psM"], ["outst"])
                    dma("sp", conv_o[:, :], outst[0:32, :], ["outst"], ["hbm_conv"], "st_misc")
                op("dve", lambda e: e.tensor_copy(out=uT[:, :, 0:30], in_=uT[:, :, NT:NT + 30]), ["uT"], ["uT"])
                for cc in range(4):
                    mm(psA[4][:, :], onesf[:, :], cT[:, cc, :], cc == 0, cc == 3, ["onesf", ("cT", cc)], [PA[4]])
                for cc in range(4):
                    tt, tk = (tmpC, ("cbuf", 0)) if cc % 2 == 0 else (tmpD, ("cbuf", 1))
                    op("act", lambda e, cc=cc, tt=tt: e.activation(out=tt[:, :], in_=cT[:, cc, :], func=AF.Square), [("cT", cc)], [tk])
                    mm(psA[5][:, :], onesf[:, :], tt[:, :], cc == 0, cc == 3, ["onesf", tk], [PA[5]])
                op("dve", lambda e: e.tensor_scalar(out=tmpA[:, :], in0=psA[4][:, :], scalar1=1.0 / 512, scalar2=None, op0=ALU.mult), [PA[4]], ["tmpA"])
                op("dve", lambda e: e.tensor_tensor(out=tmpB[:, :], in0=tmpA[:, :], in1=tmpA[:, :], op=ALU.mult), ["tmpA"], ["tmpB"])
                op("dve", lambda e: e.scalar_tensor_tensor(out=tmpB[:, :], in0=psA[5][:, :], scalar=1.0 / 512, in1=tmpB[:, :], op0=ALU.mult, op1=ALU.subtract),
                   [PA[5], "tmpB"], ["tmpB"])
                op("act", lambda e: e.activation(out=tmpB[:, :], in_=tmpB[:, :], func=AF.Sqrt, bias=epsb[:, 0:1], scale=1.0), ["tmpB", "epsb"], ["tmpB"])
                op("dve", lambda e: e.reciprocal(out=tmpB[:, :], in_=tmpB[:, :]), ["tmpB"], ["tmpB"])
                for cc in range(4):
                    op("dve", lambda e, cc=cc: e.tensor_tensor(out=cT[:, cc, :], in0=cT[:, cc, :], in1=tmpA[:, :], op=ALU.subtract), [("cT", cc), "tmpA"], [("cT", cc)])
                    op("dve", lambda e, cc=cc: e.tensor_tensor(out=cT[:, cc, :], in0=cT[:, cc, :], in1=tmpB[:, :], op=ALU.mult), [("cT", cc), "tmpB"], [("cT", cc)])
                    op("act", lambda e, cc=cc: e.activation(out=sT[:, cc, :], in_=cT[:, cc, :], func=AF.Silu, scale=fm[:, FM_LAG + cc:FM_LAG + cc + 1],
                                                            bias=fm[:, FM_LAB + cc:FM_LAB + cc + 1]), [("cT", cc), "fm"], ["sT"])

            op("dve", lambda e: e.memset(Kaug[:, :, :, 64:80], 0.0), [], ["Kaug"])
            op("dve", lambda e, c=c: e.memset(Kaug[:, 0:2, :, 64 + 2 * c:65 + 2 * c], 1.0), [], ["Kaug"])
            op("dve", lambda e, c=c: e.memset(Kaug[:, 2:4, :, 65 + 2 * c:66 + 2 * c], 1.0), [], ["Kaug"])
            kvqm = (("k", 1536), ("v", 2048), ("q", 1024), ("mq", 2560))
            wcur = {}

            def emit_mm(i):
                name, col0 = kvqm[i // 4]
                sub = i % 4
                if sub == 0:
                    wcur[name] = load_w(win_v[:, :, col0:col0 + 512], 8, 512)
                W, wk = wcur[name]
                ps, pk = psA[sub], PA[sub]
                for kc in range(8):
                    mm(ps[:, :], hT[:, kc, sub * 128:(sub + 1) * 128], W[:, kc, :], kc == 0, kc == 7, ["hT", wk], [pk])

            def post(i, c=c, t0=t0):
                name = kvqm[i // 4][0]
                sub = i % 4
                ps, pk = psA[sub], PA[sub]
                if name == "k":
                    headnorm(ps, pk, NH, HD, BC_GK, kst[:, sub % 2, :].rearrange("p (h d) -> p h d", h=NH), ("kst", sub % 2))
                    dma("sp", k_o[t0 + sub * 128:t0 + (sub + 1) * 128, :], kst[:, sub % 2, :], [("kst", sub % 2)], ["hbm_k"], "st_k%d" % (sub % 2))
                    op("dve", lambda e, sub=sub: e.tensor_copy(out=Kaug[:, sub, :, 0:64], in_=kst[:, sub % 2, :].rearrange("p (h d) -> p h d", h=NH)),
                       [("kst", sub % 2)], ["Kaug"])
                    op("dve", lambda e, sub=sub: e.tensor_copy(out=kn_bf[:, sub % 2, :], in_=kst[:, sub % 2, :]), [("kst", sub % 2)], ["kn_bf"])
                    for h in range(NH):
                        tr(psT[0:82, h * 128:(h + 1) * 128], Kaug[:, sub, h, :], identb[:, :], ["Kaug", "identb"], ["psT"])
                    op("dve", lambda e, sub=sub: e.tensor_copy(out=KTloc[0:82, :, sub * 128:(sub + 1) * 128],
                                                               in_=psT[0:82, :].rearrange("p (k t) -> p k t", k=NH)), ["psT"], ["KTloc"])
                    if sub % 2 == 1:
                        blk = 2 * c + sub // 2
                        for pair in range(4):
                            for s2 in range(2):
                                mm(psM[:, pair:pair + 1], kn_bf[:, s2, pair * 128:(pair + 1) * 128], onesb[:, 0:1], s2 == 0, s2 == 1,
                                   ["kn_bf", "onesb"], ["psM"])
                        op("dve", lambda e, blk=blk: e.tensor_scalar(out=kmBD[0:64, :, 0, blk], in0=psM[0:64, 0:4], scalar1=1.0 / 256, scalar2=None, op0=ALU.mult),
                           ["psM"], ["kmBD"])
                        op("dve", lambda e, blk=blk: e.tensor_scalar(out=kmBD[64:128, :, 1, blk], in0=psM[64:128, 0:4], scalar1=1.0 / 256, scalar2=None, op0=ALU.mult),
                           ["psM"], ["kmBD"])
                elif name == "v":
                    op("act", lambda e, sub=sub, ps=ps: e.activation(out=vst[:, sub % 2, :], in_=ps[:, :], func=AF.Copy), [pk], [("vst", sub % 2)])
                    dma("sp", v_o[t0 + sub * 128:t0 + (sub + 1) * 128, :], vst[:, sub % 2, :], [("vst", sub % 2)], ["hbm_v"], "st_v%d" % (sub % 2))
                    op("dve", lambda e, sub=sub: e.tensor_copy(out=Vloc[:, sub, :, 64:128], in_=vst[:, sub % 2, :].rearrange("p (h d) -> p h d", h=NH)),
                       [("vst", sub % 2)], ["Vloc"])
                elif name == "q":
                    cur = 2 * c + sub // 2
                    headnorm(ps, pk, NH, HD, BC_GQ, Qaug[:, sub, :, 0:64], "Qaug")
                    if cur == 0:
                        op("dve", lambda e, sub=sub: e.memset(Qaug[:, sub, :, 64:80], NEG), [], ["Qaug"])
                    else:
                        op("dve", lambda e, sub=sub: e.tensor_copy(out=qn_bf[:, :].rearrange("p (h d) -> p h d", h=NH), in_=Qaug[:, sub, :, 0:64]), ["Qaug"], ["qn_bf"])
                        for pair in range(4):
                            tr(psT[:, pair * 128:(pair + 1) * 128], qn_bf[:, pair * 128:(pair + 1) * 128], identb[:, :], ["qn_bf", "identb"], ["psT"])
                        op("dve", lambda e: e.tensor_copy(out=qTp[:, :, :], in_=psT[:, 0:512].rearrange("p (k t) -> p k t", k=4)), ["psT"], ["qTp"])
                        for pair in range(4):
                            mm(psM[:, 32 * pair:32 * pair + 32], qTp[:, pair, :], kmBD[:, pair, :, :].rearrange("p a b -> p (a b)"), True, True, ["qTp", "kmBD"], ["psM"])
                        op("dve", lambda e, cur=cur: e.tensor_copy(out=gatef[:, :, 0:cur], in_=psM[:, 0:128].rearrange("p (h b) -> p h b", h=NH)[:, :, 0:cur]),
                           ["psM"], ["gatef"])
                        for h in range(NH):
                            op("dve", lambda e, h=h: e.max(out=top8[:, h, :], in_=gatef[:, h, :]), ["gatef"], ["top8"])
                        op("dve", lambda e: e.tensor_scalar(out=thr[:, :], in0=top8[:, :, 2], scalar1=-1e29, scalar2=None, op0=ALU.max), ["top8"], ["thr"])
                        op("dve", lambda e: e.tensor_tensor(out=nsel[:, :, :], in0=gatef[:, :, :], in1=thr[:, :].unsqueeze(2).broadcast_to([128, NH, 16]), op=ALU.is_lt),
                           ["gatef", "thr"], ["nsel"])
                        op("dve", lambda e, sub=sub: e.tensor_scalar(out=Qaug[:, sub, :, 64:80], in0=nsel[:, :, :], scalar1=NEG, scalar2=None, op0=ALU.mult),
                           ["nsel"], ["Qaug"])
                    op("dve", lambda e, sub=sub, cur=cur: e.memset(Qaug[:, sub, :, 64 + cur:65 + cur], 0.0), [], ["Qaug"])
                    for h in range(NH):
                        tr(psT[0:82, h * 128:(h + 1) * 128], Qaug[:, sub, h, :], identb[:, :], ["Qaug", "identb"], ["psT"])
                    op("dve", lambda e, sub=sub: e.tensor_copy(out=qTa[0:82, :, sub * 128:(sub + 1) * 128],
                                                               in_=psT[0:82, :].rearrange("p (k t) -> p k t", k=NH)), ["psT"], ["qTa"])
                else:
                    headnorm(ps, pk, 4, 128, BC_GMQ, mqn[:, sub, :].rearrange("p (h d) -> p h d", h=4), "mqn")
                    for mh in range(4):
                        tr(psT[:, mh * 128:(mh + 1) * 128], mqn[:, sub, mh * 128:(mh + 1) * 128], identb[:, :], ["mqn", "identb"], ["psT"])
                    op("dve", lambda e, sub=sub: e.tensor_copy(out=mqT[:, :, sub * 128:(sub + 1) * 128],
                                                               in_=psT[:, 0:512].rearrange("p (k t) -> p k t", k=4)), ["psT"], ["mqT"])
                if name == "v" and sub == 3:
                    dma("sp", kt_scr[:, :, t0:t0 + NT].rearrange("h r k -> r h k"), KTloc[0:82, :, :], ["KTloc"], [("kscr", c)], "st_kscr")
                    for t4 in range(4):
                        dma("sp", v_scr[:, 4 * c + t4, :, :].rearrange("h k c -> k h c"), Vloc[:, t4, :, :], ["Vloc"], [("vscr", c)], "st_vscr")


            emit_mm(0)
            emit_mm(1)
            for i in range(16):
                if i + 2 < 16:
                    emit_mm(i + 2)
                post(i)

            nexp = 0
            steps = []
            for h in range(NH):
                for g in range(c + 1):
                    for tile in range(4):
                        steps.append((h, g, tile))
            LA = 2
            slots = {}

            def qk(i):
                h, g, tile = steps[i]
                if tile == 0:
                    sl = kvstate["i"] % 4
                    kvstate["i"] += 1
                    slots[(h, g)] = sl
                    dma("sp", kvK[0:82, sl, :], kt_scr[h, :, g * NT:(g + 1) * NT], [("kscr", g)], [("kvK", sl)], "kvK%d" % sl)
                    dma("sp", kvV[:, sl, :, :], v_scr[h, 4 * g:4 * g + 4, :, :].rearrange("t k c -> k t c"), [("vscr", g)], [("kvV", sl)], "kvV%d" % sl)
                sl = slots[(h, g)]
                diag = (g == c)
                col0 = 128 * tile if diag else 0
                sp_, sk = psA[2 + i % 4], PA[2 + i % 4]
                mm(sp_[:, col0:NT], kvK[0:82, sl, tile * 128:(tile + 1) * 128], qTa[0:82, h, col0:NT], True, True, [("kvK", sl), "qTa"], [sk])
                if diag:
                    mm(sp_[:, col0:col0 + 128], identb[:, :], trib[:, :], False, True, ["identb", "trib"], [sk])

            def expv(i):
                h, g, tile = steps[i]
                sl = slots[(h, g)]
                acc, ak = psA[h % 2], PA[h % 2]
                kt = 4 * g + tile
                diag = (g == c)
                col0 = 128 * tile if diag else 0
                sp_, sk = psA[2 + i % 4], PA[2 + i % 4]
                pslot = i % 3
                bcol = h * 32 + (kt - 4 * c + 28)
                op("act", lambda e: e.activation(out=PT[:, pslot, col0:NT], in_=sp_[:, col0:NT], func=AF.Exp, bias=btab[:, bcol:bcol + 1], scale=1.0),
                   [sk, "btab"], [("PT", pslot)])
                mm(acc[:, col0:NT], kvV[:, sl, tile, :], PT[:, pslot, col0:NT], g == 0 and tile == 0, diag and tile == 3, [("kvV", sl), ("PT", pslot)], [ak])
                if diag and tile == 3:
                    op("dve", lambda e: e.reciprocal(out=rden[0:64, :], in_=acc[0:64, :]), [ak], ["rden"])
                    op("dve", lambda e: e.tensor_tensor(out=OT[64 * (h % 2):64 * (h % 2) + 64, h // 2, :], in0=acc[64:128, :], in1=rden[0:64, :], op=ALU.mult),
                       [ak, "rden"], ["OT"])
                    for stp in conv_steps[16 * h:16 * (h + 1)]:
                        stp()

            for i in range(min(LA, len(steps))):
                qk(i)
            for i in range(len(steps)):
                if i + LA < len(steps):
                    qk(i + LA)
                expv(i)
            nexp = len(steps)
            conv_finish()

            for mh in range(4):
                o_ps, ok = psA[2 * (mh % 2)], PA[2 * (mh % 2)]
                d_ps, dk = psA[2 * (mh % 2) + 1], PA[2 * (mh % 2) + 1]
                for t in range(2):
                    sp_, sk = psA[4 + nexp % 2], PA[4 + nexp % 2]
                    pslot = nexp % 3
                    nexp += 1
                    mm(sp_[:, :], mkT[:, mh, t * 128:(t + 1) * 128], mqT[:, mh, :], True, True, ["mkT", "mqT"], [sk])
                    op("act", lambda e, sp_=sp_, pslot=pslot: e.activation(out=PT[:, pslot, :], in_=sp_[:, :], func=AF.Exp, bias=mvals[:, 5:6], scale=1.0),
                       [sk, "mvals"], [("PT", pslot)])
                    mm(o_ps[:, :], mVa[:, t, mh * 128:(mh + 1) * 128], PT[:, pslot, :], t == 0, t == 1, ["mVa", ("PT", pslot)], [ok])
                    mm(d_ps[:, :], onesb[:, :], PT[:, pslot, :], t == 0, t == 1, ["onesb", ("PT", pslot)], [dk])
                op("dve", lambda e, d_ps=d_ps: e.reciprocal(out=rden[:, :], in_=d_ps[:, :]), [dk], ["rden"])
                op("dve", lambda e, o_ps=o_ps, mh=mh: e.tensor_tensor(out=OmT[:, mh, :], in0=o_ps[:, :], in1=rden[:, :], op=ALU.mult), [ok, "rden"], ["OmT"])

            Wpa, kpa = load_w(wpa_v, 4, 1024)
            Wpb, kpb = load_w(wpb_v, 4, 1024)
            Wpc, kpc = load_w(wpc_v, 4, 1024)
            for dc in range(8):
                srcs = ((Wpa, kpa, sT, "sT"), (Wpb, kpb, OT, "OT"), (Wpc, kpc, OmT, "OmT"))
                for j in range(3):
                    Wp, kp, src, skey = srcs[j]
                    for kc in range(4):
                        mm(psA[j][:, :], Wp[:, kc, dc * 128:(dc + 1) * 128], src[:, kc, :], kc == 0, kc == 3, [kp, skey], [PA[j]])
                for j in range(3):
                    Wgt, kgt = load_g(win_v[:, :, 3072 + j * 1024 + dc * 128:3072 + j * 1024 + (dc + 1) * 128])
                    for kc in range(8):
                        mm(psA[3 + j][:, :], Wgt[:, kc, :], hT[:, kc, :], kc == 0, kc == 7, [kgt, "hT"], [PA[3 + j]])
                    op("act", lambda e, j=j, dc=dc: e.activation(out=sig[:, j, :], in_=psA[3 + j][:, :], func=AF.Sigmoid,
                                                                 bias=fm[:, FM_BG + j * 8 + dc:FM_BG + j * 8 + dc + 1], scale=1.0),
                       [PA[3 + j], "fm"], [("cT", j)])
                op("dve", lambda e: e.tensor_tensor(out=tmpA[:, :], in0=psA[0][:, :], in1=sig[:, 0, :], op=ALU.mult), [PA[0], ("cT", 0)], ["tmpA"])
                op("dve", lambda e: e.tensor_tensor(out=tmpB[:, :], in0=psA[1][:, :], in1=sig[:, 1, :], op=ALU.mult), [PA[1], ("cT", 1)], ["tmpB"])
                op("dve", lambda e: e.tensor_tensor(out=tmpA[:, :], in0=tmpA[:, :], in1=tmpB[:, :], op=ALU.add), ["tmpA", "tmpB"], ["tmpA"])
                op("dve", lambda e: e.tensor_tensor(out=tmpB[:, :], in0=psA[2][:, :], in1=sig[:, 2, :], op=ALU.mult), [PA[2], ("cT", 2)], ["tmpB"])
                op("dve", lambda e, dc=dc: e.tensor_tensor(out=merged[:, dc, :], in0=tmpA[:, :], in1=tmpB[:, :], op=ALU.add), ["tmpA", "tmpB"], ["merged"])
            for half in range(2):
                W, wk = load_w(wout_v[:, :, half * 512:(half + 1) * 512], 8, 512)
                for sub in range(4):
                    for kc in range(8):
                        mm(psA[sub][:, :], merged[:, kc, sub * 128:(sub + 1) * 128], W[:, kc, :], kc == 0, kc == 7, ["merged", wk], [PA[sub]])
                    op("dve", lambda e, sub=sub, half=half: e.tensor_tensor(out=X[:, sub, half * 512:(half + 1) * 512], in0=X[:, sub, half * 512:(half + 1) * 512],
                                                                            in1=psA[sub][:, :], op=ALU.add), ["X", PA[sub]], ["X"])

            norm_to_T(X, 4, FM_G2, hT, "hT", "X")
            wdf3 = fm[:, FM_WDF:FM_WDF + 132].rearrange("p (j k) -> p j k", k=3)
            allhist_k = [("hist", j) for j in range(44)]
            op("dve", lambda e: e.tensor_tensor(out=corr[:, :, 0], in0=hist[:, :, 0], in1=wdf3[:, :, 0], op=ALU.mult), allhist_k + ["fm"], ["corr"])
            op("dve", lambda e: e.tensor_tensor(out=corrt[:, :], in0=hist[:, :, 1], in1=wdf3[:, :, 1], op=ALU.mult), allhist_k + ["fm"], ["corrt"])
            op("dve", lambda e: e.tensor_tensor(out=corr[:, :, 0], in0=corr[:, :, 0], in1=corrt[:, :], op=ALU.add), ["corr", "corrt"], ["corr"])
            op("dve", lambda e: e.tensor_tensor(out=corr[:, :, 1], in0=hist[:, :, 1], in1=wdf3[:, :, 0], op=ALU.mult), allhist_k + ["fm"], ["corr"])
            upw = {}

            def emit_up(j):
                piece, jj = j // 4, j % 4
                if jj == 0:
                    upw[piece] = load_w(wup_v[:, :, piece * 512:(piece + 1) * 512], 8, 512)
                W, wk = upw[piece]
                ps, pk = psA[j % 4], PA[j % 4]
                cb, ck = cbuf[:, j % 3, :], ("cbuf", j % 3)
                for kc in range(8):
                    mm(ps[:, :], W[:, kc, jj * 128:(jj + 1) * 128], hT[:, kc, :], kc == 0, kc == 7, [wk, "hT"], [pk])
                w2 = fm[:, FM_WDF + 3 * j + 2:FM_WDF + 3 * j + 3]
                bb = fm[:, FM_BDF + j:FM_BDF + j + 1]
                op("act", lambda e: e.activation(out=cb, in_=ps[:, :], func=AF.Identity, scale=w2, bias=bb), [pk, "fm"], [ck])
                op("act", lambda e: e.activation(out=hist[:, j, :], in_=ps[:, NT - 2:NT], func=AF.Copy), [pk], [("hist", j)])

            def chain_up(j):
                ps, pk = psA[j % 4], PA[j % 4]
                cb, ck = cbuf[:, j % 3, :], ("cbuf", j % 3)
                w0 = fm[:, FM_WDF + 3 * j:FM_WDF + 3 * j + 1]
                w1 = fm[:, FM_WDF + 3 * j + 1:FM_WDF + 3 * j + 2]
                op("dve", lambda e: e.scalar_tensor_tensor(out=cb[:, 1:NT], in0=ps[:, 0:NT - 1], scalar=w1, in1=cb[:, 1:NT], op0=ALU.mult, op1=ALU.add),
                   [pk, ck, "fm"], [ck])
                op("dve", lambda e: e.scalar_tensor_tensor(out=cb[:, 2:NT], in0=ps[:, 0:NT - 2], scalar=w0, in1=cb[:, 2:NT], op0=ALU.mult, op1=ALU.add),
                   [pk, ck, "fm"], [ck])
                op("dve", lambda e: e.tensor_tensor(out=cb[:, 0:2], in0=cb[:, 0:2], in1=corr[:, j, :], op=ALU.add), [ck, "corr"], [ck])
                if j < 22:
                    op("act", lambda e: e.activation(out=sa[:, j, :], in_=cb, func=AF.Silu), [ck], [("sa", j)])
                else:
                    op(FFN_MUL_ENG, lambda e: e.tensor_tensor(out=sa[:, j - 22, :], in0=sa[:, j - 22, :], in1=cb, op=ALU.mult), [ck, ("sa", j - 22)], [("sa", j - 22)])

            emit_up(0)
            for j in range(44):
                if j + 1 < 44:
                    emit_up(j + 1)
                chain_up(j)
            sakeys = [("sa", j) for j in range(22)]
            for half in range(2):
                for kp, (k0, k1) in enumerate(((0, 8), (8, 16), (16, 22))):
                    W, wk = load_w(wdn_v[:, k0:k1, half * 512:(half + 1) * 512], k1 - k0, 512)
                    for sub in range(4):
                        for kc in range(k0, k1):
                            mm(psA[sub][:, :], sa[:, kc, sub * 128:(sub + 1) * 128], W[:, kc - k0, :], kc == 0, kc == 21, sakeys + [wk], [PA[sub]])
                for sub in range(4):
                    op("dve", lambda e, sub=sub, half=half: e.tensor_tensor(out=X[:, sub, half * 512:(half + 1) * 512], in0=X[:, sub, half * 512:(half + 1) * 512],
                                                                            in1=psA[sub][:, :], op=ALU.add), ["X", PA[sub]], ["X"])
            dma("sp", y_o[t0:t0 + NT, :].rearrange("(s p) d -> p s d", p=128), X[:, :, :], ["X"], ["hbm_y"], "st_y")


        if do_sample:
            sc = X[:, 1:4, :].rearrange("p a t -> p (a t)")
            dma("sp", X[:, 0, :], xs_in[:, :], [], ["X"], "xload")
            dma("sp", sc[:, 0:SC_N], sc_in[:, :], [], ["X"], "xload")
            RROW = lambda n: sc[:, SC_RROW + n * 128:SC_RROW + (n + 1) * 128]
            ONEROW = lambda n: sc[:, SC_CS + 3 - n:SC_CS + 131 - n]
            ONECOL = lambda r: sc[:, SC_ST + 127 - r:SC_ST + 255 - r]
            REP = sc[:, SC_REP:SC_REP + 128]
            E8 = lambda h: sc[0:8, SC_E8 + h * 128:SC_E8 + (h + 1) * 128]
            ALT = sc[:, SC_AL:SC_AL + 512]
            PIOTA = sc[:, SC_PI:SC_PI + 1]
            IND120 = sc[0:120, SC_IND:SC_IND + 128]
            op("dve", lambda e: e.tensor_copy(out=orowb[:, 0:131], in_=sc[:, SC_CS:SC_CS + 131]), ["X"], ["orowb"])
            ONEROWB = lambda n: orowb[:, 3 - n:131 - n]
            HCOL = lambda h: sc[:, SC_ST + 127 - h:SC_ST + 135 - h]
            dma("sp", idx_t[:, :], pt_in[:, :], [], ["idx_t"], "c7")
            op("dve", lambda e: e.tensor_copy(out=ptf[:, :], in_=idx_t[:, :]), ["idx_t"], ["ptf"])
            op("dve", lambda e: e.tensor_scalar(out=ptf[:, :], in0=ptf[:, :], scalar1=128.0, scalar2=None, op0=ALU.mult), ["ptf"], ["ptf"])
            op("dve", lambda e: e.tensor_scalar(out=ptf[:, :], in0=ptf[:, :], scalar1=PIOTA, scalar2=None, op0=ALU.add), ["ptf", "X"], ["ptf"])
            op("dve", lambda e: e.tensor_copy(out=idx_t[:, :], in_=ptf[:, :]), ["ptf"], ["idx_t"])

            for _k in ("stc", "prd", "qb", "mg"):
                S.alias[_k] = ["uT"]
            S.alias["mg"] = ["uT", "stc", "prd"]
            S.alias["cs"] = [("sa", j) for j in range(22)] + ["KTloc", "qTa", "mqn", "qTp"]
            S.alias["stf"] = ["Vloc"]
            norm_to_T(X, 1, FM_G1, hT, "hT", "X")
            us, qs, mqs, ks = cT[:, 0, :], cT[:, 1, :], cT[:, 2, :], cT[:, 3, :]
            vs, stc, prd, qb = uT[:, 0, 0:512], uT[:, 1, 0:512], uT[:, 2, 0:512], uT[:, 3, 0:512]

            def bload(dst, dkey, src_row):
                dma("sp", dst, src_row.partition_broadcast(128), [], [dkey], "bl_" + str(dkey))

            def zpiece(col0):
                W, wk = load_w(win_v[:, :, col0:col0 + 512], 8, 512)
                for kc in range(8):
                    mm(psA[0][:, :], hT[:, kc, 0:128], W[:, kc, :], kc == 0, kc == 7, ["hT", wk], [PA[0]])
                return psA[0], PA[0]

            ps, pk = zpiece(0)
            op("act", lambda e: e.activation(out=tmpA[:, :], in_=psA[0][:, :], func=AF.Copy), [pk], ["tmpA"])
            op("dve", lambda e: e.tensor_copy(out=rden[:, :], in_=tmpA[:, :]), ["tmpA"], ["rden"])
            ps, pk = zpiece(512)
            op("act", lambda e: e.activation(out=tmpB[:, :], in_=psA[0][:, :], func=AF.Sigmoid), [pk], ["tmpB"])
            op("dve", lambda e: e.tensor_tensor(out=us, in0=rden[:, :], in1=tmpB[:, :], op=ALU.mult), ["rden", "tmpB"], [("cT", 0)])
            ps, pk = zpiece(1024)
            headnorm(ps, pk, NH, HD, BC_GQ, qs.rearrange("p (h d) -> p h d", h=NH), ("cT", 1))
            ps, pk = zpiece(1536)
            headnorm(ps, pk, NH, HD, BC_GK, ks.rearrange("p (h d) -> p h d", h=NH), ("cT", 3))
            dma("sp", ks_o[:, :], cT[0:4, 3, :], [("cT", 3)], ["hbm_ks"], "st_s1")
            ps, pk = zpiece(2048)
            op("act", lambda e: e.activation(out=vs, in_=psA[0][:, :], func=AF.Copy), [pk], ["uT"])
            dma("sp", vs_o[:, :], uT[0:4, 0, 0:512], ["uT"], ["hbm_vs"], "st_s2")
            ps, pk = zpiece(2560)
            headnorm(ps, pk, 4, 128, BC_GMQ, mqs.rearrange("p (h d) -> p h d", h=4), ("cT", 2))

            dma("sp", uT[0:120, 1, 0:512], stc_in[:, :], [], ["stc"], "c8")
            dma("sp", uT[0:120, 2, 0:512], w120_in[:, :], [], ["prd"], "c9")
            for n in range(4):
                dma("sp", convs_o[n, 0:29, :], uT[n * 30 + 1:n * 30 + 30, 1, 0:512], ["stc"], ["hbm_convs"], "st_s3")
            dma("sp", convs_o[:, 29, :], cT[0:4, 0, :], [("cT", 0)], ["hbm_convs"], "st_s3")
            op("dve", lambda e: e.tensor_tensor(out=uT[0:120, 2, 0:512], in0=uT[0:120, 2, 0:512], in1=uT[0:120, 1, 0:512], op=ALU.mult), ["stc", "prd"], ["prd"])
            mm(psA[1][:, :], IND120, uT[0:120, 2, 0:512], True, True, ["prd", "X"], [PA[1]])
            bload(cbuf[:, 0, :], ("cbuf", 0), vrow_in[0, 0:512])
            bload(cbuf[:, 1, :], ("cbuf", 1), vrow_in[1, 0:512])
            op("dve", lambda e: e.tensor_tensor(out=tmpA[:, :], in0=us, in1=cbuf[:, 0, :], op=ALU.mult), [("cT", 0), ("cbuf", 0)], ["tmpA"])
            op("dve", lambda e: e.tensor_tensor(out=tmpA[:, :], in0=tmpA[:, :], in1=cbuf[:, 1, :], op=ALU.add), ["tmpA", ("cbuf", 1)], ["tmpA"])
            op("dve", lambda e: e.tensor_tensor(out=tmpA[:, :], in0=tmpA[:, :], in1=psA[1][:, :], op=ALU.add), ["tmpA", PA[1]], ["tmpA"])
            op("dve", lambda e: e.tensor_reduce(out=ss[:, 0:1], in_=tmpA[:, :], axis=AX.X, op=ALU.add), ["tmpA"], [("ss", 0)])
            op("dve", lambda e: e.tensor_scalar(out=ss[:, 0:1], in0=ss[:, 0:1], scalar1=1.0 / 512, scalar2=None, op0=ALU.mult), [("ss", 0)], [("ss", 0)])
            op("dve", lambda e: e.tensor_scalar(out=tmpA[:, :], in0=tmpA[:, :], scalar1=ss[:, 0:1], scalar2=None, op0=ALU.subtract), ["tmpA", ("ss", 0)], ["tmpA"])
            op("dve", lambda e: e.memset(ss[:, 1:2], 0.0), [], [("ss", 1)])
            op("act", lambda e: e.activation(out=tmpB[:, :], in_=tmpA[:, :], func=AF.Square, accum_out=ss[:, 1:2]), ["tmpA", ("ss", 1)], ["tmpB", ("ss", 1)])
            op("act", lambda e: e.activation(out=ss[:, 1:2], in_=ss[:, 1:2], func=AF.Sqrt, bias=epsb[:, 0:1], scale=1.0 / 512), [("ss", 1), "epsb"], [("ss", 1)])
            op("dve", lambda e: e.reciprocal(out=ss[:, 1:2], in_=ss[:, 1:2]), [("ss", 1)], [("ss", 1)])
            bload(cbuf[:, 0, :], ("cbuf", 0), vrow_in[2, 0:512])
            bload(cbuf[:, 1, :], ("cbuf", 1), vrow_in[3, 0:512])
            op("dve", lambda e: e.tensor_scalar(out=tmpA[:, :], in0=tmpA[:, :], scalar1=ss[:, 1:2], scalar2=None, op0=ALU.mult), ["tmpA", ("ss", 1)], ["tmpA"])
            op("dve", lambda e: e.tensor_tensor(out=tmpA[:, :], in0=tmpA[:, :], in1=cbuf[:, 0, :], op=ALU.mult), ["tmpA", ("cbuf", 0)], ["tmpA"])
            op("dve", lambda e: e.tensor_tensor(out=tmpA[:, :], in0=tmpA[:, :], in1=cbuf[:, 1, :], op=ALU.add), ["tmpA", ("cbuf", 1)], ["tmpA"])
            op("act", lambda e: e.activation(out=xn[:, 0:512], in_=tmpA[:, :], func=AF.Silu), ["tmpA"], ["xn"])
            dbgout(xn[0:4, 0:512], [4, 512], ["xn"])
            for kc in range(4):
                tr(psT[:, kc * 128:(kc + 1) * 128], xn[:, kc * 128:(kc + 1) * 128], identb[:, :], ["xn", "identb"], ["psT"])
            op("dve", lambda e: e.tensor_copy(out=sT[:, :, 0:128], in_=psT[:, 0:512].rearrange("p (k t) -> p k t", k=4)), ["psT"], ["sT"])

            Lt = bc
            pbufs = [(kst[:, 0, :], ("kst", 0)), (kst[:, 1, :], ("kst", 1)), (vst[:, 0, :], ("vst", 0)), (vst[:, 1, :], ("vst", 1))]
            pstate = {"i": 0}
            pvstate = {"i": 0}

            def get_page(src_kind, n, pg):
                buf, bk = pbufs[pstate["i"] % 4]
                pstate["i"] += 1
                if src_kind in ("k", "v"):
                    src = ck_in if src_kind == "k" else cv_in
                    col = n * 64 + pg
                    op("pool", lambda e: e.indirect_dma_start(out=buf, out_offset=None, in_=src[:, :],
                                                              in_offset=bass.IndirectOffsetOnAxis(ap=idx_t[:, col:col + 1], axis=0)),
                       ["idx_t"], [bk], dma=True, semkey="pg_" + str(bk))
                else:
                    src = cmk_in if src_kind == "mk" else cmv_in
                    dma("sp", buf, src[n, pg * 128:(pg + 1) * 128, :], [], [bk], "pg_" + str(bk))
                return buf, bk

            def bcast_row(n, src, skey):
                mm(psA[2][:, :], RROW(n), src, True, True, ["X", skey], [PA[2]])
                op("act", lambda e: e.activation(out=qb, in_=psA[2][:, :], func=AF.Copy), [PA[2]], ["qb"])

            def dense_attn(kkind, vkind, npg, nh, hd, qsrc, qkey, moba):
                G = npg * nh
                first_sum = [True]
                for n in range(4):
                    bcast_row(n, qsrc, qkey)
                    for pg in range(npg):
                        buf, bk = get_page(kkind, n, pg)
                        op("dve", lambda e, buf=buf: e.tensor_tensor(out=tmpA[:, :], in0=buf, in1=qb, op=ALU.mult), [bk, "qb"], ["tmpA"])
                        o0 = n * G + pg * nh
                        op("dve", lambda e, o0=o0: e.tensor_reduce(out=Lt[:, o0:o0 + nh], in_=tmpA[:, :].rearrange("p (h d) -> p h d", h=nh), axis=AX.X, op=ALU.add),
                           ["tmpA"], ["bc"])
                if moba:
                    Lv = Lt[:, :].rearrange("p (g h) -> p g h", h=NH)
                    for h in range(NH):
                        mm(psA[3][0:8, 0:256], HCOL(h), Lv[:, :, h], h == 0, h == NH - 1, ["X", "bc"], [PA[3]])
                    op("dve", lambda e: e.tensor_reduce(out=rden[0:8, 0:128], in_=psA[3][0:8, 0:256].rearrange("p (g e) -> p g e", e=2), axis=AX.X, op=ALU.add),
                       [PA[3]], ["rden"])
                    for n in range(4):
                        op("dve", lambda e, n=n: e.max(out=top8[0:8, n, :], in_=rden[0:8, n * 32:(n + 1) * 32]), ["rden"], ["top8"])
                    op("dve", lambda e: e.tensor_tensor(out=rden[0:8, 128:256].rearrange("p (n b) -> p n b", n=4), in0=rden[0:8, 0:128].rearrange("p (n b) -> p n b", n=4),
                                                        in1=top8[0:8, 0:4, 2:3].broadcast_to([8, 4, 32]), op=ALU.is_ge), ["rden", "top8"], ["rden"])
                    op("dve", lambda e: e.tensor_copy(out=tmpB[0:8, 0:256].rearrange("p (g e) -> p g e", e=2), in_=rden[0:8, 128:256].unsqueeze(2).broadcast_to([8, 128, 2])),
                       ["rden"], ["tmpB"])
                for n in range(4):
                    if moba:
                        op("dve", lambda e, n=n: e.tensor_tensor(out=Lt[:, n * G:(n + 1) * G], in0=Lt[:, n * G:(n + 1) * G], in1=ALT, op=ALU.add), ["bc", "X"], ["bc"])
                    op("dve", lambda e, n=n: e.tensor_reduce(out=thr[:, n:n + 1], in_=Lt[:, n * G:(n + 1) * G], axis=AX.X, op=ALU.max), ["bc"], ["thr"])
                op("pe", lambda e: e.transpose(out=psM[0:4, 128:256], in_=thr[:, 0:4], identity=identf[:, :]), ["thr", "identf"], ["psM"])
                op("dve", lambda e: e.tensor_reduce(out=mvals[0:4, 6:7], in_=psM[0:4, 128:256], axis=AX.X, op=ALU.max), ["psM"], ["mvals"])
                if moba:
                    op("dve", lambda e: e.tensor_tensor(out=tmpA[:, :], in0=qs, in1=ks, op=ALU.mult), [("cT", 1), ("cT", 3)], ["tmpA"])
                    op("dve", lambda e: e.tensor_reduce(out=nsel[:, 0, 0:8], in_=tmpA[:, :].rearrange("p (h d) -> p h d", h=NH), axis=AX.X, op=ALU.add), ["tmpA"], ["nsel"])
                    op("dve", lambda e: e.tensor_reduce(out=mvals[0:4, 7:8], in_=nsel[0:4, 0, 0:8], axis=AX.X, op=ALU.max), ["nsel"], ["mvals"])
                    op("dve", lambda e: e.tensor_tensor(out=mvals[0:4, 6:7], in0=mvals[0:4, 6:7], in1=mvals[0:4, 7:8], op=ALU.max), ["mvals"], ["mvals"])
                op("dve", lambda e: e.tensor_scalar(out=mvals[0:4, 6:7], in0=mvals[0:4, 6:7], scalar1=-1.0, scalar2=None, op0=ALU.mult), ["mvals"], ["mvals"])
                op("dve", lambda e: e.tensor_scalar(out=nsel[0:4, 1, 0:4], in0=identf[0:4, 0:4], scalar1=mvals[0:4, 6:7], scalar2=None, op0=ALU.mult), ["mvals", "identf"], ["nsel"])
                mm(psM[:, 256:260], onesf[0:4, :], nsel[0:4, 1, 0:4], True, True, ["onesf", "nsel"], ["psM"])
                op("dve", lambda e: e.tensor_copy(out=thr[:, 4:8], in_=psM[:, 256:260]), ["psM"], ["thr"])
                for n in range(4):
                    op("act", lambda e, n=n: e.activation(out=Lt[:, n * G:(n + 1) * G], in_=Lt[:, n * G:(n + 1) * G], func=AF.Exp, bias=thr[:, 4 + n:5 + n], scale=1.0),
                       ["bc", "thr"], ["bc"])
                if moba:
                    op("act", lambda e: e.activation(out=nsel[0:4, 0, 0:8], in_=nsel[0:4, 0, 0:8], func=AF.Exp, bias=mvals[0:4, 6:7], scale=1.0), ["nsel", "mvals"], ["nsel"])
                    for h in range(NH):
                        mm(psA[h // 2][:, (h % 2) * 256:(h % 2) * 256 + 256], E8(h), tmpB[0:8, 0:256], True, True, ["X", "tmpB"], [PA[h // 2]])
                    for hp in range(4):
                        op("dve", lambda e, hp=hp: e.tensor_tensor(
                            out=Lt[:, :].rearrange("p (g h) -> p g h", h=NH)[:, :, 2 * hp:2 * hp + 2],
                            in0=Lt[:, :].rearrange("p (g h) -> p g h", h=NH)[:, :, 2 * hp:2 * hp + 2],
                            in1=psA[hp][:, :].rearrange("p (h g) -> p g h", h=2), op=ALU.mult), ["bc", PA[hp]], ["bc"])
                for n in range(4):
                    for c0 in range(0, G, 512):
                        w = min(512, G - c0)
                        mm(psA[4][:, 0:w], ONEROW(n), Lt[:, n * G + c0:n * G + c0 + w], n == 0 and c0 == 0, n == 3 and c0 + w >= G, ["X", "bc"], [PA[4]])
                Gc = min(G, 512)
                op("dve", lambda e: e.tensor_reduce(out=ss[:, 8:8 + nh], in_=psA[4][:, 0:Gc].rearrange("p (g h) -> p h g", h=nh), axis=AX.X, op=ALU.add), [PA[4]], ["ss8"])
                firstv = [True]
                for n in range(4):
                    for pg in range(npg):
                        buf, bk = get_page(vkind, n, pg)
                        o0 = n * G + pg * nh
                        slot = pvstate["i"] % 3
                        pvstate["i"] += 1
                        op("dve", lambda e, buf=buf, o0=o0, slot=slot: e.tensor_tensor(out=PT[:, slot, :].rearrange("p (h d) -> p h d", h=nh), in0=buf.rearrange("p (h d) -> p h d", h=nh),
                                                                                     in1=Lt[:, o0:o0 + nh].unsqueeze(2).broadcast_to([128, nh, hd]), op=ALU.mult), [bk, "bc"], [("PT", slot)])
                        mm(psA[5][:, :], ONEROWB(n), PT[:, slot, :], firstv[0], n == 3 and pg == npg - 1, ["orowb", ("PT", slot)], [PA[5]])
                        firstv[0] = False

            dense_attn("k", "v", 64, NH, HD, qs, ("cT", 1), True)
            op("dve", lambda e: e.tensor_tensor(out=ss[:, 8:16], in0=ss[:, 8:16], in1=nsel[:, 0, 0:8], op=ALU.add), ["ss8", "nsel"], ["ss8"])
            op("dve", lambda e: e.tensor_scalar(out=ss[:, 8:16], in0=ss[:, 8:16], scalar1=1e-30, scalar2=None, op0=ALU.add), ["ss8"], ["ss8"])
            op("dve", lambda e: e.reciprocal(out=ss[:, 8:16], in_=ss[:, 8:16]), ["ss8"], ["ss8"])
            op("dve", lambda e: e.tensor_tensor(out=tmpB[:, :].rearrange("p (h d) -> p h d", h=NH), in0=vs.rearrange("p (h d) -> p h d", h=NH),
                                                in1=nsel[:, 0, 0:8].unsqueeze(2).broadcast_to([128, NH, HD]), op=ALU.mult), ["uT", "nsel"], ["tmpB"])
            op("dve", lambda e: e.tensor_tensor(out=tmpB[:, :], in0=tmpB[:, :], in1=psA[5][:, :], op=ALU.add), ["tmpB", PA[5]], ["tmpB"])
            op("dve", lambda e: e.tensor_tensor(out=xn[:, 0:512].rearrange("p (h d) -> p h d", h=NH), in0=tmpB[:, :].rearrange("p (h d) -> p h d", h=NH),
                                                in1=ss[:, 8:16].unsqueeze(2).broadcast_to([128, NH, HD]), op=ALU.mult), ["tmpB", "ss8"], ["xn"])
            dbgout(rden[0:8, 0:256], [8, 256], ["rden"])
            dbgout(ss[0:4, 8:16], [4, 8], ["ss8"])
            dbgout(xn[0:4, 0:512], [4, 512], ["xn"])
            dbgout(bc[:, :], [128, 2048], ["bc"])
            dbgout(nsel[0:4, 0, 0:8], [4, 8], ["nsel"])
            dbgout(tmpB[0:4, :], [4, 512], ["tmpB"])
            for kc in range(4):
                tr(psT[:, kc * 128:(kc + 1) * 128], xn[:, kc * 128:(kc + 1) * 128], identb[:, :], ["xn", "identb"], ["psT"])
            op("dve", lambda e: e.tensor_copy(out=OT[:, :, 0:128], in_=psT[:, 0:512].rearrange("p (k t) -> p k t", k=4)), ["psT"], ["OT"])

            dense_attn("mk", "mv", 2, 4, 128, mqs, ("cT", 2), False)
            op("dve", lambda e: e.tensor_scalar(out=ss[:, 8:12], in0=ss[:, 8:12], scalar1=1e-30, scalar2=None, op0=ALU.add), ["ss8"], ["ss8"])
            op("dve", lambda e: e.reciprocal(out=ss[:, 8:12], in_=ss[:, 8:12]), ["ss8"], ["ss8"])
            op("dve", lambda e: e.tensor_tensor(out=xn[:, 0:512].rearrange("p (h d) -> p h d", h=4), in0=psA[5][:, :].rearrange("p (h d) -> p h d", h=4),
                                                in1=ss[:, 8:12].unsqueeze(2).broadcast_to([128, 4, 128]), op=ALU.mult), [PA[5], "ss8"], ["xn"])
            dbgout(xn[0:4, 0:512], [4, 512], ["xn"])
            for kc in range(4):
                tr(psT[:, kc * 128:(kc + 1) * 128], xn[:, kc * 128:(kc + 1) * 128], identb[:, :], ["xn", "identb"], ["psT"])
            op("dve", lambda e: e.tensor_copy(out=OmT[:, :, 0:128], in_=psT[:, 0:512].rearrange("p (k t) -> p k t", k=4)), ["psT"], ["OmT"])

            mg = uT[:, 1:3, :].rearrange("p a t -> p (a t)")[:, 0:1024]
            for half in range(2):
                srcs = ((wpa_v, sT, "sT"), (wpb_v, OT, "OT"), (wpc_v, OmT, "OmT"))
                for j in range(3):
                    wpv, src, skey = srcs[j]
                    Wp, kp = load_w(wpv[:, :, half * 512:(half + 1) * 512], 4, 512)
                    for kc in range(4):
                        mm(psA[j][:, :], src[:, kc, 0:128], Wp[:, kc, :], kc == 0, kc == 3, [kp, skey], [PA[j]])
                    col0 = 3072 + j * 1024 + half * 512
                    W, wk = load_w(win_v[:, :, col0:col0 + 512], 8, 512)
                    for kc in range(8):
                        mm(psA[3][:, :], hT[:, kc, 0:128], W[:, kc, :], kc == 0, kc == 7, ["hT", wk], [PA[3]])
                    bload(cbuf[:, 2, :], ("cbuf", 2), vrow_in[4, j * 1024 + half * 512:j * 1024 + half * 512 + 512])
                    op("dve", lambda e: e.tensor_tensor(out=tmpA[:, :], in0=psA[3][:, :], in1=cbuf[:, 2, :], op=ALU.add), [PA[3], ("cbuf", 2)], ["tmpA"])
                    op("act", lambda e: e.activation(out=tmpA[:, :], in_=tmpA[:, :], func=AF.Sigmoid), ["tmpA"], ["tmpA"])
                    if j == 0:
                        op("dve", lambda e, half=half: e.tensor_tensor(out=mg[:, half * 512:(half + 1) * 512], in0=psA[0][:, :], in1=tmpA[:, :], op=ALU.mult), [PA[0], "tmpA"], ["mg"])
                    else:
                        op("dve", lambda e, j=j: e.tensor_tensor(out=tmpB[:, :], in0=psA[j][:, :], in1=tmpA[:, :], op=ALU.mult), [PA[j], "tmpA"], ["tmpB"])
                        op("dve", lambda e, half=half: e.tensor_tensor(out=mg[:, half * 512:(half + 1) * 512], in0=mg[:, half * 512:(half + 1) * 512], in1=tmpB[:, :], op=ALU.add),
                           ["mg", "tmpB"], ["mg"])
            op("dve", lambda e: e.tensor_copy(out=xn[:, :], in_=mg), ["mg"], ["xn"])
            dbgout(xn[0:4, :], [4, 1024], ["xn"])
            for kc in range(8):
                tr(psT[:, kc * 128:(kc + 1) * 128], xn[:, kc * 128:(kc + 1) * 128], identb[:, :], ["xn", "identb"], ["psT"])
            op("dve", lambda e: e.tensor_copy(out=merged[:, :, 0:128], in_=psT[:, :].rearrange("p (k t) -> p k t", k=8)), ["psT"], ["merged"])
            for half in range(2):
                W, wk = load_w(wout_v[:, :, half * 512:(half + 1) * 512], 8, 512)
                for kc in range(8):
                    mm(psA[0][:, :], merged[:, kc, 0:128], W[:, kc, :], kc == 0, kc == 7, ["merged", wk], [PA[0]])
                op("dve", lambda e, half=half: e.tensor_tensor(out=X[:, 0, half * 512:(half + 1) * 512], in0=X[:, 0, half * 512:(half + 1) * 512], in1=psA[0][:, :], op=ALU.add),
                   ["X", PA[0]], ["X"])

            dbgout(X[0:4, 0, :], [4, 1024], ["X"])
            norm_to_T(X, 1, FM_G2, hT, "hT", "X")
            cs = scrA[:, :].bitcast(F32)
            stf = Vloc[:, :, :, :].rearrange("p a b c -> p (a b c)").bitcast(F32)
            op("dve", lambda e: e.memset(stf[:, 0:1024], 0.0), [], ["stf"])
            for piece in range(11):
                c0 = piece * 512
                W, wk = load_w(wup_v[:, :, c0:c0 + 512], 8, 512)
                for kc in range(8):
                    mm(psA[piece % 2][:, :], hT[:, kc, 0:128], W[:, kc, :], kc == 0, kc == 7, ["hT", wk], [PA[piece % 2]])
                pp, pkk = psA[piece % 2], PA[piece % 2]
                op("act", lambda e, pp=pp: e.activation(out=tmpA[:, :], in_=pp[:, :], func=AF.Copy), [pkk], ["tmpA"])
                dma("sp", ffns_o[:, 1, c0:c0 + 512], tmpA[0:4, :], ["tmpA"], ["hbm_ffns"], "st_s4")
                dma("sp", stf[0:4, 0:512], stf_in[:, c0:c0 + 512], [], ["stf"], "ld_stf")
                dma("sp", stf[0:4, 512:1024], stf_in[:, NUP + c0:NUP + c0 + 512], [], ["stf"], "ld_stf")
                dma("sp", ffns_o[:, 0, c0:c0 + 512], stf[0:4, 512:1024], ["stf"], ["hbm_ffns"], "st_s5")
                bload(cbuf[:, 0, :], ("cbuf", 0), vrow_in[5, c0:c0 + 512])
                op("dve", lambda e, c0=c0: e.tensor_tensor(out=cs[:, c0:c0 + 512], in0=stf[:, 0:512], in1=cbuf[:, 0, :], op=ALU.mult), ["stf", ("cbuf", 0)], ["cs"])
                bload(cbuf[:, 1, :], ("cbuf", 1), vrow_in[6, c0:c0 + 512])
                op("dve", lambda e: e.tensor_tensor(out=tmpB[:, :], in0=stf[:, 512:1024], in1=cbuf[:, 1, :], op=ALU.mult), ["stf", ("cbuf", 1)], ["tmpB"])
                op("dve", lambda e, c0=c0: e.tensor_tensor(out=cs[:, c0:c0 + 512], in0=cs[:, c0:c0 + 512], in1=tmpB[:, :], op=ALU.add), [("sa", 0), "tmpB"], ["cs"])
                bload(cbuf[:, 2, :], ("cbuf", 2), vrow_in[7, c0:c0 + 512])
                op("dve", lambda e: e.tensor_tensor(out=tmpB[:, :], in0=tmpA[:, :], in1=cbuf[:, 2, :], op=ALU.mult), ["tmpA", ("cbuf", 2)], ["tmpB"])
                op("dve", lambda e, c0=c0: e.tensor_tensor(out=cs[:, c0:c0 + 512], in0=cs[:, c0:c0 + 512], in1=tmpB[:, :], op=ALU.add), [("sa", 0), "tmpB"], ["cs"])
                bload(cbuf[:, 0, :], ("cbuf", 0), vrow_in[8, c0:c0 + 512])
                op("dve", lambda e, c0=c0: e.tensor_tensor(out=cs[:, c0:c0 + 512], in0=cs[:, c0:c0 + 512], in1=cbuf[:, 0, :], op=ALU.add), [("sa", 0), ("cbuf", 0)], ["cs"])
            actb = hT[:, 0:6, :].rearrange("p a t -> p (a t)")
            for c0 in range(0, DFF, 512):
                w = min(512, DFF - c0)
                op("act", lambda e, c0=c0, w=w: e.activation(out=tmpA[:, 0:w], in_=cs[:, c0:c0 + w], func=AF.Silu), ["cs"], ["tmpA"])
                op("dve", lambda e, c0=c0, w=w: e.tensor_tensor(out=actb[:, c0:c0 + w], in0=tmpA[:, 0:w], in1=cs[:, DFF + c0:DFF + c0 + w], op=ALU.mult), ["tmpA", ("sa", 0)], ["hT"])
            actT = qTa_s = None
            aT = sT[:, :, :].rearrange("p a t -> p (a t)")
            aT2 = OT[:, :, :].rearrange("p a t -> p (a t)")
            for grp in range(3):
                k0 = grp * 8
                nk = min(8, 22 - k0)
                for kk in range(nk):
                    tr(psT[:, kk * 128:(kk + 1) * 128], actb[:, (k0 + kk) * 128:(k0 + kk + 1) * 128], identb[:, :], ["hT", "identb"], ["psT"])
                if grp < 2:
                    op("dve", lambda e, k0=k0, nk=nk: e.tensor_copy(out=aT[:, k0 * 128:(k0 + nk) * 128], in_=psT[:, 0:nk * 128]), ["psT"], ["sT"])
                else:
                    op("dve", lambda e, nk=nk: e.tensor_copy(out=aT2[:, 0:nk * 128], in_=psT[:, 0:nk * 128]), ["psT"], ["OT"])

            def aTk(kc):
                return aT[:, kc * 128:(kc + 1) * 128] if kc < 16 else aT2[:, (kc - 16) * 128:(kc - 15) * 128]

            for half in range(2):
                for kp, (k0, k1) in enumerate(((0, 8), (8, 16), (16, 22))):
                    W, wk = load_w(wdn_v[:, k0:k1, half * 512:(half + 1) * 512], k1 - k0, 512)
                    for kc in range(k0, k1):
                        mm(psA[2][:, :], aTk(kc), W[:, kc - k0, :], kc == 0, kc == 21, ["sT", "OT", wk], [PA[2]])
                op("dve", lambda e, half=half: e.tensor_tensor(out=X[:, 0, half * 512:(half + 1) * 512], in0=X[:, 0, half * 512:(half + 1) * 512], in1=psA[2][:, :], op=ALU.add),
                   ["X", PA[2]], ["X"])
            dma("sp", ys_o[:, :], X[0:4, 0, :], ["X"], ["hbm_ys"], "st_y")

        allhist = [("hist", j) for j in range(44)]
        op("pe", lambda e: e.transpose(out=psM[0:88, 0:128], in_=hist[:, :, :].rearrange("p j r -> p (j r)"), identity=identf[:, :]), allhist + ["identf"], ["psM"])
        op("dve", lambda e: e.tensor_copy(out=outst[0:88, 0:128], in_=psM[0:88, 0:128]), ["psM"], ["outst"])
        dma("sp", ffnc_o[:, :], outst[0:88, 0:128], ["outst"], ["hbm_ffnc"], "st_misc")

        S.emit(st)
    return nc


def host_tables(inp):
    f = np.float32
    fm = np.zeros((128, FM_N), f)

    def fmaj(v):
        return np.ascontiguousarray(np.asarray(v, f).reshape(-1, 128).T)

    fm[:, FM_G1:FM_G1 + 8] = fmaj(inp["norm1_g"])
    fm[:, FM_G2:FM_G2 + 8] = fmaj(inp["norm2_g"])
    fm[:, FM_GM:FM_GM + 8] = fmaj(inp["mem_norm_g"])
    fm[:, FM_BG:FM_BG + 24] = fmaj(inp["b_gate"])
    wda = np.asarray(inp["w_dw_a"], f)
    fm[:, FM_WDA:FM_WDA + 124] = wda.T.reshape(4, 128, 31).transpose(1, 0, 2).reshape(128, 124)
    fm[:, FM_BDA:FM_BDA + 4] = fmaj(inp["b_dw_a"])
    fm[:, FM_LAG:FM_LAG + 4] = fmaj(inp["ln_a_g"])
    fm[:, FM_LAB:FM_LAB + 4] = fmaj(inp["ln_a_b"])
    wdf = np.asarray(inp["w_dw_f"], f)
    fm[:, FM_WDF:FM_WDF + 132] = wdf.T.reshape(44, 128, 3).transpose(1, 0, 2).reshape(128, 132)
    fm[:, FM_BDF:FM_BDF + 44] = fmaj(inp["b_dw_f"])
    bc = np.zeros((128, BC_N), f)
    bc[:, BC_GQ:BC_GQ + 512] = np.tile(np.asarray(inp["q_norm_g"], f), 8)[None, :]
    bc[:, BC_GK:BC_GK + 512] = np.tile(np.asarray(inp["k_norm_g"], f), 8)[None, :]
    bc[:, BC_GMQ:BC_GMQ + 512] = np.tile(np.asarray(inp["mq_norm_g"], f), 4)[None, :]
    bc[:, BC_GMK:BC_GMK + 512] = np.tile(np.asarray(inp["mk_norm_g"], f), 4)[None, :]
    ident = np.eye(128, dtype=f)
    p = np.arange(128)
    tri = np.where(p[:, None] > p[None, :], f(NEG), f(0)).astype(f)
    slopes = (2.0 ** (-np.arange(1, NH + 1, dtype=np.float64))).astype(f)
    j = np.arange(32)
    abase = (slopes[None, :, None] * (p[:, None, None] + 128.0 * (j[None, None, :] - 28))).astype(f).reshape(128, NH * 32)
    dt = (np.arange(4)[None, :] * 128 + p[:, None]).astype(np.int64)
    hi = (16 * (dt // 16)).astype(f)
    lo = (dt % 16).astype(f)
    qal = np.stack([-slopes[None, None, :] * hi[:, :, None], -slopes[None, None, :] * lo[:, :, None]], axis=-1).astype(f).reshape(128, 64)
    return {"fm": fm, "bc": bc, "ident": ident, "tri": tri, "abase": abase, "qal": qal}


_NC_CACHE = {}


def run_prompt(inp, nch=NCHUNK):
    key = ("p", nch)
    if key not in _NC_CACHE:
        _NC_CACHE[key] = build(nch=nch, do_sample=False)
    nc = _NC_CACHE[key]
    tabs = host_tables(inp)
    f = np.float32
    wnames = ["w_in", "w_proj_a", "w_proj_b", "w_proj_c", "w_mem_kv", "w_out", "w_up", "w_down"]
    shared = {n: np.ascontiguousarray(np.asarray(inp[n], f)) for n in wnames}
    shared.update(tabs)
    in_maps = []
    for c in range(8):
        s = c % 4
        m = dict(shared)
        m["x"] = np.ascontiguousarray(np.asarray(inp["x_prompt"][s], f))
        m["mem"] = np.ascontiguousarray(np.asarray(inp["mem_prompt"][s], f))
        in_maps.append(m)
    res = run_bass_kernel_spmd(nc, in_maps, core_ids=list(range(8)))
    return res.results


def sample_tables(inp):
    f = np.float32
    sc = np.zeros((128, SC_N), f)
    for n in range(4):
        sc[n, SC_RROW + n * 128:SC_RROW + (n + 1) * 128] = 1.0
    sc[:, SC_CS + 3] = 1.0
    sc[:, SC_ST + 127] = 1.0
    for k in range(4):
        sc[k, SC_REP + k * 32:SC_REP + (k + 1) * 32] = 1.0
    for h in range(8):
        sc[h, SC_E8 + h * 128:SC_E8 + (h + 1) * 128] = 1.0
    slopes = (2.0 ** (-np.arange(1, NH + 1, dtype=np.float64)))
    p = np.arange(128)[:, None, None]
    pg = np.arange(64)[None, :, None]
    sc[:, SC_AL:SC_AL + 512] = (-slopes[None, None, :] * (8192.0 - 128.0 * pg - p)).astype(f).reshape(128, 512)
    sc[:, SC_PI] = np.arange(128, dtype=f)
    for n in range(4):
        sc[n * 30:(n + 1) * 30, SC_IND + n] = 1.0
    vr = np.zeros((9, 5632), f)
    vr[0, :512] = np.asarray(inp["w_dw_a"], f)[30]
    vr[1, :512] = np.asarray(inp["b_dw_a"], f)
    vr[2, :512] = np.asarray(inp["ln_a_g"], f)
    vr[3, :512] = np.asarray(inp["ln_a_b"], f)
    vr[4, :3072] = np.asarray(inp["b_gate"], f)
    vr[5:8, :] = np.asarray(inp["w_dw_f"], f)
    vr[8, :] = np.asarray(inp["b_dw_f"], f)
    w120 = np.ascontiguousarray(np.tile(np.asarray(inp["w_dw_a"], f)[:30], (4, 1)))
    return sc, vr, w120


def run_all(inp):
    key = ("all",)
    if key not in _NC_CACHE:
        _NC_CACHE[key] = build(nch=NCHUNK, do_sample=True)
    nc = _NC_CACHE[key]
    tabs = host_tables(inp)
    f = np.float32
    wnames = ["w_in", "w_proj_a", "w_proj_b", "w_proj_c", "w_mem_kv", "w_out", "w_up", "w_down"]
    shared = {n: np.ascontiguousarray(np.asarray(inp[n], f)) for n in wnames}
    shared.update(tabs)
    sc, vr, w120 = sample_tables(inp)
    shared["sconst"] = sc
    shared["vrows"] = vr
    shared["w120"] = w120
    shared["cache_k"] = np.ascontiguousarray(np.asarray(inp["cache_k"], f)).reshape(N_POOL * 128, 512)
    shared["cache_v"] = np.ascontiguousarray(np.asarray(inp["cache_v"], f)).reshape(N_POOL * 128, 512)
    pt = np.asarray(inp["page_table"]).astype(np.int32)
    in_maps = []
    for c in range(8):
        s = c % 4
        m = dict(shared)
        m["x"] = np.ascontiguousarray(np.asarray(inp["x_prompt"][s], f))
        m["mem"] = np.ascontiguousarray(np.asarray(inp["mem_prompt"][s], f))
        sl = slice(4 * c, 4 * c + 4)
        xs = np.zeros((128, D), f)
        xs[0:4] = np.asarray(inp["x_sample"], f)[sl, 0, :]
        m["xs"] = xs
        m["stc"] = np.ascontiguousarray(np.asarray(inp["state_conv"], f)[sl].reshape(120, 512))
        m["stf"] = np.ascontiguousarray(np.asarray(inp["state_ffn_conv"], f)[sl].reshape(4, 2 * NUP))
        m["cmk"] = np.ascontiguousarray(np.asarray(inp["cache_mem_k"], f)[sl].reshape(4, 256, 512))
        m["cmv"] = np.ascontiguousarray(np.asarray(inp["cache_mem_v"], f)[sl].reshape(4, 256, 512))
        m["ptrep"] = np.ascontiguousarray(np.broadcast_to(pt[sl].reshape(1, 256), (128, 256))).astype(np.int32)
        in_maps.append(m)
    res = run_bass_kernel_spmd(nc, in_maps, core_ids=list(range(8)))
    return res.results


def kernel(**inp):
    r = run_all(inp)
    f = np.float32
    y_p = np.stack([r[s]["y"] for s in range(4)]).astype(f)
    k_p = np.stack([r[s]["k"] for s in range(4)]).reshape(4, SEQ, NH, HD).astype(f)
    v_p = np.stack([r[s]["v"] for s in range(4)]).reshape(4, SEQ, NH, HD).astype(f)
    conv_p = np.stack([r[s]["conv"][2:32] for s in range(4)]).astype(f)
    ffn_p = np.stack([r[s]["ffnc"].reshape(44, 2, 128).transpose(1, 0, 2).reshape(2, NUP) for s in range(4)]).astype(f)
    mk_p = np.stack([r[s]["mk"] for s in range(4)]).reshape(4, 256, 4, 128).astype(f)
    mv_p = np.stack([r[s]["mv"] for s in range(4)]).reshape(4, 256, 4, 128).astype(f)
    y_s = np.concatenate([r[c]["ys"] for c in range(8)]).reshape(32, 1, D).astype(f)
    k_s = np.concatenate([r[c]["ks"] for c in range(8)]).reshape(32, 1, NH, HD).astype(f)
    v_s = np.concatenate([r[c]["vs"] for c in range(8)]).reshape(32, 1, NH, HD).astype(f)
    conv_s = np.concatenate([r[c]["convs"] for c in range(8)]).reshape(32, 30, 512).astype(f)
    ffn_s = np.concatenate([r[c]["ffns"] for c in range(8)]).reshape(32, 2, NUP).astype(f)
    return (y_p, y_s, k_p, v_p, k_s, v_s, conv_p, conv_s, ffn_p, ffn_s, mk_p, mv_p)
```

```python
import contextlib
import numpy as np
import concourse.bass as bass
import concourse.mybir as mybir
from concourse.bass_utils import run_bass_kernel_spmd

F32 = mybir.dt.float32
BF16 = mybir.dt.bfloat16
I32 = mybir.dt.int32
AF = mybir.ActivationFunctionType
ALU = mybir.AluOpType
AX = mybir.AxisListType

D = 1024
SEQ = 4096
NT = 512
NCHUNK = SEQ // NT
CONV_CH = 512
CW = 31
NH = 8
HD = 64
DFF = 2816
NUP = 2 * DFF
NIN = 6144
EPS = 1e-6
NEG = -30000.0
FFN_MUL_ENG = "dve"
CONV_POOL_CC = 0
CONV_PER_HEAD = (31 * (4 - CONV_POOL_CC) + 7) // 8
N_POOL = 2560


class Sched:
    ENGS = ("pe", "act", "dve", "pool", "sp")

    def __init__(self, nc):
        self.nc = nc
        self.q = {e: [] for e in self.ENGS}
        self.last_w = {}
        self.readers = {}
        self.dma_cnt = {}
        self.n_ops = 0
        self.alias = {}

    def op(self, eng, fn, reads=(), writes=(), dma=False, semkey=None):
        rec = {"fn": fn, "eng": eng, "deps": [], "need_inc": False, "dma": dma,
               "semkey": None, "dval": 0}
        self.n_ops += 1
        deps = []
        for k in reads:
            w = self.last_w.get(k)
            if w is not None:
                deps.append(w)
        for k in writes:
            w = self.last_w.get(k)
            if w is not None:
                deps.append(w)
            for r in self.readers.get(k, ()):
                deps.append(r)
            for a in self.alias.get(k, ()):
                w = self.last_w.get(a)
                if w is not None:
                    deps.append(w)
                for r in self.readers.get(a, ()):
                    deps.append(r)
        if dma:
            assert semkey is not None
            rec["semkey"] = semkey
            self.dma_cnt[semkey] = self.dma_cnt.get(semkey, 0) + 16
            rec["dval"] = self.dma_cnt[semkey]
            me = ("d", semkey, rec["dval"])
        else:
            me = ("c", rec)
        seen = set()
        for d in deps:
            if d[0] == "c":
                p = d[1]
                if p is rec:
                    continue
                if p["eng"] == eng and eng == "pe":
                    continue
                if id(p) in seen:
                    continue
                seen.add(id(p))
                p["need_inc"] = True
                rec["deps"].append(d)
            else:
                key = (d[1], d[2])
                if key in seen:
                    continue
                seen.add(key)
                rec["deps"].append(d)
        for k in writes:
            self.last_w[k] = me
            self.readers[k] = []
        for k in reads:
            if k in writes:
                continue
            self.readers.setdefault(k, []).append(me)
        self.q[eng].append(rec)
        return rec

    def emit(self, st):
        nc = self.nc
        esem = {e: st.enter_context(nc.semaphore("e_" + e)) for e in self.ENGS}
        dsem = {}
        for i, k in enumerate(self.dma_cnt):
            dsem[k] = st.enter_context(nc.semaphore("d%d" % i))
        for e in self.ENGS:
            c = 0
            for rec in self.q[e]:
                if rec["need_inc"] and not rec["dma"]:
                    c += 1
                    rec["inc_val"] = c
        block = st.enter_context(nc.Block())
        hw = {"pe": nc.tensor, "act": nc.scalar, "dve": nc.vector, "pool": nc.gpsimd, "sp": nc.sync}

        def run(e):
            eng = hw[e]
            waited = {}
            for rec in self.q[e]:
                for d in rec["deps"]:
                    if d[0] == "c":
                        p = d[1]
                        sem = esem[p["eng"]]
                        val = p["inc_val"]
                        key = ("e", p["eng"])
                    else:
                        sem = dsem[d[1]]
                        val = d[2]
                        key = ("d", d[1])
                    if waited.get(key, 0) >= val:
                        continue
                    waited[key] = val
                    eng.wait_ge(sem, val)
                ins = rec["fn"](eng)
                if rec["dma"]:
                    ins.then_inc(dsem[rec["semkey"]], 16)
                elif rec["need_inc"]:
                    ins.then_inc(esem[e], 1)
            if e == "sp":
                for k, v in self.dma_cnt.items():
                    eng.wait_ge(dsem[k], v)

        block.tensor(lambda _e: run("pe"))
        block.scalar(lambda _e: run("act"))
        block.vector(lambda _e: run("dve"))
        block.gpsimd(lambda _e: run("pool"))
        block.sync(lambda _e: run("sp"))


FM_G1, FM_G2, FM_GM, FM_BG, FM_WDA, FM_BDA, FM_LAG, FM_LAB, FM_WDF, FM_BDF = 0, 8, 16, 24, 48, 172, 176, 180, 184, 316
FM_N = 360
BC_GQ, BC_GK, BC_GMQ, BC_GMK = 0, 512, 1024, 1536
BC_N = 2048


SC_RROW, SC_CS, SC_ST, SC_REP, SC_E8, SC_AL, SC_PI, SC_IND = 0, 512, 643, 898, 1026, 2050, 2562, 2563
SC_N = 2691


def build(nch=NCHUNK, do_sample=True, dbg=False):
    nc = bass.Bass("TRN2", target_bir_lowering=False)

    def din(name, shape, dt=F32):
        return nc.dram_tensor(name, list(shape), dt, kind="ExternalInput").ap()

    def dout(name, shape, dt=F32):
        return nc.dram_tensor(name, list(shape), dt, kind="ExternalOutput").ap()

    xin = din("x", [SEQ, D])
    memin = din("mem", [256, D])
    w_in = din("w_in", [D, NIN])
    w_pa = din("w_proj_a", [512, D])
    w_pb = din("w_proj_b", [512, D])
    w_pc = din("w_proj_c", [512, D])
    w_mkv = din("w_mem_kv", [D, D])
    w_out = din("w_out", [D, D])
    w_up = din("w_up", [D, NUP])
    w_down = din("w_down", [DFF, D])
    fm_in = din("fm", [128, FM_N])
    bc_in = din("bc", [128, BC_N])
    ident_in = din("ident", [128, 128])
    tri_in = din("tri", [128, 128])
    abase_in = din("abase", [128, NH * 32])
    qal_in = din("qal", [128, 4 * NH * 2])

    if do_sample:
        xs_in = din("xs", [128, D])
        stc_in = din("stc", [120, 512])
        w120_in = din("w120", [120, 512])
        stf_in = din("stf", [4, 2 * NUP])
        cmk_in = din("cmk", [4, 256, 512])
        cmv_in = din("cmv", [4, 256, 512])
        pt_in = din("ptrep", [128, 256], I32)
        ck_in = din("cache_k", [N_POOL * 128, 512])
        cv_in = din("cache_v", [N_POOL * 128, 512])
        sc_in = din("sconst", [128, SC_N])
        vrow_in = din("vrows", [9, 5632])
        ys_o = dout("ys", [4, D])
        ks_o = dout("ks", [4, 512])
        vs_o = dout("vs", [4, 512])
        convs_o = dout("convs", [4, 30, 512])
        ffns_o = dout("ffns", [4, 2, NUP])
    y_o = dout("y", [SEQ, D])
    k_o = dout("k", [SEQ, 512])
    v_o = dout("v", [SEQ, 512])
    conv_o = dout("conv", [32, 512])
    ffnc_o = dout("ffnc", [88, 128])
    mk_o = dout("mk", [256, 512])
    mv_o = dout("mv", [256, 512])
    kt_scr = nc.dram_tensor("kt_scr", [NH, 82, SEQ], BF16, kind="Internal").ap()
    v_scr = nc.dram_tensor("v_scr", [NH, SEQ // 128, 128, 128], BF16, kind="Internal").ap()

    st = contextlib.ExitStack()
    with st:
        def sb(name, shape, dt):
            return st.enter_context(nc.sbuf_tensor("s_" + name, list(shape), dt))

        def psum(name, shape, dt):
            return st.enter_context(nc.psum_tensor("p_" + name, list(shape), dt))

        S = Sched(nc)
        op = S.op
        dbg_n = [0]

        def dbgout(src_ap, shape, keys, dt=F32):
            if not dbg:
                return
            name = "dbg%d" % dbg_n[0]
            dbg_n[0] += 1
            t = nc.dram_tensor(name, list(shape), dt, kind="ExternalOutput").ap()
            op("pool", lambda e: e.dma_start(out=t, in_=src_ap), reads=keys, writes=["hbm_" + name], dma=True, semkey="dbg")

        identb = sb("identb", [128, 128], BF16)
        identf = sb("identf", [128, 128], F32)
        trib = sb("trib", [128, 128], BF16)
        onesb = sb("onesb", [128, 128], BF16)
        onesf = sb("onesf", [128, 128], F32)
        fm = sb("fm", [128, FM_N], F32)
        bc = sb("bc", [128, BC_N], F32)
        btab = sb("btab", [128, NH * 32], F32)
        mvals = sb("mvals", [128, 8], F32)
        epsb = sb("epsb", [128, 1], F32)
        X0 = sb("X0", [128, 4, D], F32)
        Xb = [X0, X0]
        X = Xb[0]
        xn = sb("xn", [128, D], BF16)
        ss = sb("ss", [128, 16], F32)
        hT = sb("hT", [128, 8, NT], BF16)
        uT = sb("uT", [128, 4, 30 + NT], F32)
        cT = sb("cT", [128, 4, NT], F32)
        sT = sb("sT", [128, 4, NT], BF16)
        tmpA = sb("tmpA", [128, NT], F32)
        tmpB = sb("tmpB", [128, NT], F32)
        Qaug = sb("Qaug", [128, 4, NH, 82], BF16)
        Kaug = sb("Kaug", [128, 4, NH, 82], BF16)
        Vloc = sb("Vloc", [128, 4, NH, 128], BF16)
        mqT = sb("mqT", [128, 4, NT], BF16)
        qn_bf = sb("qn_bf", [128, 512], BF16)
        kn_bf = sb("kn_bf", [128, 2, 512], BF16)
        kst = sb("kst", [128, 2, 512], F32)
        vst = sb("vst", [128, 2, 512], F32)
        kmBD = sb("kmBD", [128, 4, 2, 16], BF16)
        gatef = sb("gatef", [128, NH, 16], F32)
        top8 = sb("top8", [128, NH, 8], F32)
        thr = sb("thr", [128, NH], F32)
        nsel = sb("nsel", [128, NH, 16], F32)
        PT = sb("PT", [128, 3, NT], BF16)
        OT = sb("OT", [128, 4, NT], BF16)
        OmT = sb("OmT", [128, 4, NT], BF16)
        rden = sb("rden", [128, NT], F32)
        mkT = sb("mkT", [128, 4, 256], BF16)
        mVa = sb("mVa", [128, 2, 512], BF16)
        merged = sb("merged", [128, 8, NT], BF16)
        scrA = sb("scrA", [128, 22 * NT], BF16)
        sa = scrA[:, :].rearrange("p (j t) -> p j t", j=22)
        KTloc = scrA[:, 0:4096].rearrange("p (j t) -> p j t", j=NH)
        qTa = scrA[:, 4096:8192].rearrange("p (j t) -> p j t", j=NH)
        mqn = scrA[:, 8192:10240].rearrange("p (j t) -> p j t", j=4)
        qTp = scrA[:, 10240:10752].rearrange("p (j t) -> p j t", j=4)
        cbuf = sb("cbuf", [128, 3, NT], F32)
        tmpC = cbuf[:, 0, :]
        tmpD = cbuf[:, 1, :]
        sig = cT
        junk = kn_bf[:, :, :].rearrange("p a t -> p (a t)")
        for _k in ("KTloc", "qTa", "mqn", "qTp"):
            S.alias[_k] = [("sa", j) for j in range(22)]
        for j in range(22):
            S.alias[("sa", j)] = ["KTloc", "qTa", "mqn", "qTp"]
        hist = sb("hist", [128, 44, 2], F32)
        corr = sb("corr", [128, 44, 2], F32)
        corrt = sb("corrt", [128, 44], F32)
        ring = sb("ring", [128, 4, 4096], BF16)
        gring = sb("gring", [128, 4, 1024], BF16)
        kvK = sb("kvK", [128, 4, NT], BF16)
        kvV = sb("kvV", [128, 4, 4, 128], BF16)
        outst = sb("outst", [128, 512], F32)
        idx_t = sb("idx_t", [128, 256], I32)
        orowb = sb("orowb", [128, 132], BF16)
        ptf = sb("ptf", [128, 256], F32)

        psA = [psum("psA%d" % i, [128, 512], F32) for i in range(6)]
        psT = psum("psT", [128, 1024], BF16)
        psM = psum("psM", [128, 512], F32)
        PA = ["psA%d" % i for i in range(6)]

        def dma(eng, out, in_, reads, writes, semkey):
            return op(eng, lambda e: e.dma_start(out=out, in_=in_), reads=reads, writes=writes, dma=True, semkey=semkey)

        def mm(out, lhsT, rhs, start, stop, reads, writes):
            return op("pe", lambda e: e.matmul(out, lhsT=lhsT, rhs=rhs, start=start, stop=stop, skip_group_check=True),
                      reads=reads, writes=writes)

        def tr(out, in_, ident, reads, writes):
            return op("pe", lambda e: e.transpose(out=out, in_=in_, identity=ident), reads=reads, writes=writes)

        rstate = {"i": 0, "g": 0}

        def load_w(view, nk, ncols):
            i = rstate["i"] % 4
            rstate["i"] += 1
            dst = ring[:, i, 0:nk * ncols].rearrange("p (k n) -> p k n", k=nk)
            dma("pool", dst, view, [], [("ring", i)], "ring%d" % i)
            return dst, ("ring", i)

        def load_g(view):
            i = rstate["g"] % 4
            rstate["g"] += 1
            dst = gring[:, i, :].rearrange("p (k n) -> p k n", k=8)
            dma("pool", dst, view, [], [("gring", i)], "gring%d" % i)
            return dst, ("gring", i)

        win_v = w_in.rearrange("(kc p) n -> p kc n", p=128)
        wup_v = w_up.rearrange("(kc p) n -> p kc n", p=128)
        wout_v = w_out.rearrange("(kc p) n -> p kc n", p=128)
        wdn_v = w_down.rearrange("(kc p) n -> p kc n", p=128)
        wmkv_v = w_mkv.rearrange("(kc p) n -> p kc n", p=128)
        wpa_v = w_pa.rearrange("(kc p) n -> p kc n", p=128)
        wpb_v = w_pb.rearrange("(kc p) n -> p kc n", p=128)
        wpc_v = w_pc.rearrange("(kc p) n -> p kc n", p=128)

        dma("sp", fm[:, :], fm_in[:, :], [], ["fm"], "c1")
        dma("sp", bc[:, :], bc_in[:, :], [], ["bc"], "c2")
        dma("sp", identf[:, :], ident_in[:, :], [], ["identf"], "c3")
        dma("sp", btab[:, :], abase_in[:, :], [], ["btab"], "c4")
        dma("sp", tmpA[:, 0:128], tri_in[:, :], [], ["tmpA"], "c5")
        dma("sp", tmpB[:, 0:64], qal_in[:, :], [], ["tmpB"], "c6")
        op("dve", lambda e: e.tensor_copy(out=identb[:, :], in_=identf[:, :]), ["identf"], ["identb"])
        op("dve", lambda e: e.tensor_copy(out=trib[:, :], in_=tmpA[:, 0:128]), ["tmpA"], ["trib"])
        op("dve", lambda e: e.memset(onesb[:, :], 1.0), [], ["onesb"])
        op("dve", lambda e: e.memset(onesf[:, :], 1.0), [], ["onesf"])
        op("dve", lambda e: e.memset(epsb[:, :], EPS), [], ["epsb"])
        op("dve", lambda e: e.memset(uT[:, :, :], 0.0), [], ["uT"])
        op("dve", lambda e: e.memset(hist[:, :, :], 0.0), [], ["hist"])
        op("dve", lambda e: e.memset(kmBD[:, :, :, :], 0.0), [], ["kmBD"])
        op("dve", lambda e: e.memset(gatef[:, :, :], -1e30), [], ["gatef"])
        op("dve", lambda e: e.memset(Vloc[:, :, :, 0:64], 1.0), [], ["Vloc"])
        op("dve", lambda e: e.memset(Kaug[:, :, :, 80:82], 1.0), [], ["Kaug"])
        op("dve", lambda e: e.tensor_copy(out=Qaug[:, :, :, 80:82],
                                          in_=tmpB[:, 0:64].rearrange("p (s h r) -> p s h r", s=4, h=NH)),
           ["tmpB"], ["Qaug"])
        for j, (off, n) in enumerate(((BC_GQ, 64), (BC_GK, 64), (BC_GMQ, 128), (BC_GMK, 128))):
            op("dve", lambda e, j=j, off=off, n=n: e.tensor_reduce(out=mvals[:, j:j + 1], in_=bc[:, off:off + n], axis=AX.X,
                                                                  op=ALU.max, apply_absolute_value=True),
               ["bc"], ["mvals"])
        op("dve", lambda e: e.tensor_tensor(out=mvals[:, 4:5], in0=mvals[:, 0:1], in1=mvals[:, 1:2], op=ALU.mult), ["mvals"], ["mvals"])
        op("dve", lambda e: e.tensor_scalar(out=mvals[:, 4:5], in0=mvals[:, 4:5], scalar1=8.0, scalar2=None, op0=ALU.mult), ["mvals"], ["mvals"])
        op("dve", lambda e: e.tensor_tensor(out=mvals[:, 5:6], in0=mvals[:, 2:3], in1=mvals[:, 3:4], op=ALU.mult), ["mvals"], ["mvals"])
        op("dve", lambda e: e.tensor_scalar(out=mvals[:, 5:6], in0=mvals[:, 5:6], scalar1=-(128.0 ** 0.5), scalar2=None, op0=ALU.mult), ["mvals"], ["mvals"])
        op("dve", lambda e: e.tensor_scalar(out=btab[:, :], in0=btab[:, :], scalar1=mvals[:, 4:5], scalar2=None, op0=ALU.subtract),
           ["btab", "mvals"], ["btab"])
        op("dve", lambda e: e.tensor_scalar(out=bc[:, BC_GQ:BC_GQ + 512], in0=bc[:, BC_GQ:BC_GQ + 512], scalar1=HD ** -0.5, scalar2=None, op0=ALU.mult), ["bc"], ["bc"])
        op("dve", lambda e: e.tensor_scalar(out=bc[:, BC_GMQ:BC_GMQ + 512], in0=bc[:, BC_GMQ:BC_GMQ + 512], scalar1=128.0 ** -0.5, scalar2=None, op0=ALU.mult), ["bc"], ["bc"])

        def XK(b):
            return [("X", 0, sub) for sub in range(4)]

        def rms_rows(src3, sub, col, srckey="X"):
            op("dve", lambda e: e.memset(ss[:, col:col + 1], 0.0), [], [("ss", col)])
            op("act", lambda e: e.activation(out=junk[:, :], in_=src3[:, sub, :], func=AF.Square, accum_out=ss[:, col:col + 1]),
               [srckey, ("ss", col)], ["kn_bf", ("ss", col)])
            op("act", lambda e: e.activation(out=ss[:, col:col + 1], in_=ss[:, col:col + 1], func=AF.Sqrt, bias=epsb[:, 0:1], scale=1.0 / D),
               [("ss", col), "epsb"], [("ss", col)])
            op("dve", lambda e: e.reciprocal(out=ss[:, col:col + 1], in_=ss[:, col:col + 1]), [("ss", col)], [("ss", col)])

        def norm_A(src3, sub, srckey):
            op("dve", lambda e: e.tensor_scalar(out=xn[:, :], in0=src3[:, sub, :], scalar1=ss[:, sub:sub + 1], scalar2=None, op0=ALU.mult),
               [srckey, ("ss", sub)], ["xn"])

        def norm_B(sub, goff, dstT, dkey):
            for kc in range(8):
                tr(psT[:, kc * 128:(kc + 1) * 128], xn[:, kc * 128:(kc + 1) * 128], identb[:, :], ["xn", "identb"], ["psT"])
            op("dve", lambda e: e.tensor_tensor(
                out=dstT[:, :, sub * 128:(sub + 1) * 128], in0=psT[:, :].rearrange("p (k t) -> p k t", k=8),
                in1=fm[:, goff:goff + 8].unsqueeze(2).broadcast_to([128, 8, 128]), op=ALU.mult),
               ["psT", "fm"], [dkey])

        def norm_to_T(src3, nsub, goff, dstT, dkey, srckey):
            keys = srckey if isinstance(srckey, list) else [srckey] * nsub
            for sub in range(nsub):
                rms_rows(src3, sub, sub, keys[sub])
            for sub in range(nsub):
                norm_A(src3, sub, keys[sub])
                norm_B(sub, goff, dstT, dkey)

        def headnorm(ps, pkey, nh, hd, goff, out_ap, okey, extra_reads=()):
            op("act", lambda e: e.activation(out=tmpA[:, :], in_=ps[:, :], func=AF.Square), [pkey], ["tmpA"])
            op("dve", lambda e: e.tensor_reduce(out=ss[:, 8:8 + nh], in_=tmpA[:, :].rearrange("p (h d) -> p h d", h=nh), axis=AX.X, op=ALU.add),
               ["tmpA"], ["ss8"])
            op("act", lambda e: e.activation(out=ss[:, 8:8 + nh], in_=ss[:, 8:8 + nh], func=AF.Sqrt, bias=epsb[:, 0:1], scale=1.0 / hd),
               ["ss8", "epsb"], ["ss8"])
            op("dve", lambda e: e.reciprocal(out=ss[:, 8:8 + nh], in_=ss[:, 8:8 + nh]), ["ss8"], ["ss8"])
            op("dve", lambda e: e.tensor_tensor(out=tmpB[:, :].rearrange("p (h d) -> p h d", h=nh), in0=ps[:, :].rearrange("p (h d) -> p h d", h=nh),
                                                in1=ss[:, 8:8 + nh].unsqueeze(2).broadcast_to([128, nh, hd]), op=ALU.mult),
               [pkey, "ss8"], ["tmpB"])
            op("dve", lambda e: e.tensor_tensor(out=out_ap, in0=tmpB[:, :].rearrange("p (h d) -> p h d", h=nh),
                                                in1=bc[:, goff:goff + 512].rearrange("p (h d) -> p h d", h=nh), op=ALU.mult),
               ["tmpB", "bc"] + list(extra_reads), [okey])

        dma("sp", Xb[1][:, 0:2, :], memin.rearrange("(s p) d -> p s d", p=128), [], XK(1)[0:2], "xload2")
        norm_to_T(Xb[1], 2, FM_GM, hT, "hT", XK(1)[0:2])
        for half in range(2):
            W, wk = load_w(wmkv_v[:, :, half * 512:(half + 1) * 512], 8, 512)
            for sub in range(2):
                ps = psA[sub]
                for kc in range(8):
                    mm(ps[:, :], hT[:, kc, sub * 128:(sub + 1) * 128], W[:, kc, :], kc == 0, kc == 7, ["hT", wk], [PA[sub]])
                if half == 0:
                    headnorm(ps, PA[sub], 4, 128, BC_GMK, kst[:, sub, :].rearrange("p (h d) -> p h d", h=4), ("kst", sub))
                    dma("sp", mk_o[sub * 128:(sub + 1) * 128, :], kst[:, sub, :], [("kst", sub)], ["hbm_mk"], "st_k%d" % sub)
                    op("dve", lambda e, sub=sub: e.tensor_copy(out=mqn[:, sub, :], in_=kst[:, sub, :]), [("kst", sub)], ["mqn"])
                    for mh in range(4):
                        tr(psT[:, mh * 128:(mh + 1) * 128], mqn[:, sub, mh * 128:(mh + 1) * 128], identb[:, :], ["mqn", "identb"], ["psT"])
                    op("dve", lambda e, sub=sub: e.tensor_copy(out=mkT[:, :, sub * 128:(sub + 1) * 128],
                                                               in_=psT[:, 0:512].rearrange("p (k t) -> p k t", k=4)), ["psT"], ["mkT"])
                else:
                    op("act", lambda e, sub=sub, ps=ps: e.activation(out=vst[:, sub, :], in_=ps[:, :], func=AF.Copy), [PA[sub]], [("vst", sub)])
                    dma("sp", mv_o[sub * 128:(sub + 1) * 128, :], vst[:, sub, :], [("vst", sub)], ["hbm_mv"], "st_v%d" % sub)
                    op("dve", lambda e, sub=sub: e.tensor_copy(out=mVa[:, sub, :], in_=vst[:, sub, :]), [("vst", sub)], ["mVa"])

        kvstate = {"i": 0}
        for c in range(nch):
            t0 = c * NT
            xb = c % 2
            X = Xb[xb]
            xks = XK(xb)
            dma("sp", X[:, :, :], xin[t0:t0 + NT, :].rearrange("(s p) d -> p s d", p=128), [], xks, "xload")
            norm_to_T(X, 4, FM_G1, hT, "hT", xks)

            Wa, wka = load_w(win_v[:, :, 0:512], 8, 512)
            Wg, wkg = load_w(win_v[:, :, 512:1024], 8, 512)
            for cc in range(4):
                pa, pg = psA[2 * (cc % 2)], psA[2 * (cc % 2) + 1]
                ka, kg = PA[2 * (cc % 2)], PA[2 * (cc % 2) + 1]
                for kc in range(8):
                    mm(pa[:, :], Wa[:, kc, cc * 128:(cc + 1) * 128], hT[:, kc, :], kc == 0, kc == 7, ["hT", wka], [ka])
                for kc in range(8):
                    mm(pg[:, :], Wg[:, kc, cc * 128:(cc + 1) * 128], hT[:, kc, :], kc == 0, kc == 7, ["hT", wkg], [kg])
                tk = ("cbuf", 0) if cc % 2 == 0 else ("cbuf", 1)
                tt = tmpC if cc % 2 == 0 else tmpD
                op("act", lambda e, pg=pg, tt=tt: e.activation(out=tt[:, :], in_=pg[:, :], func=AF.Sigmoid), [kg], [tk])
                op("dve", lambda e, pa=pa, tt=tt, cc=cc: e.tensor_tensor(out=uT[:, cc, 30:30 + NT], in0=pa[:, :], in1=tt[:, :], op=ALU.mult),
                   [ka, tk], ["uT"])
            conv_steps = []
            pool_steps = []
            for cc in range(4):
                ceng = "pool" if cc >= 4 - CONV_POOL_CC else "dve"
                clist = pool_steps if ceng == "pool" else conv_steps
                clist.append(lambda cc=cc, ceng=ceng: op(ceng, lambda e: e.tensor_scalar(
                    out=cT[:, cc, :], in0=uT[:, cc, 0:NT], scalar1=fm[:, FM_WDA + cc * 31:FM_WDA + cc * 31 + 1],
                    scalar2=fm[:, FM_BDA + cc:FM_BDA + cc + 1], op0=ALU.mult, op1=ALU.add), ["uT", "fm"], [("cT", cc)]))
                for k in range(1, CW):
                    clist.append(lambda cc=cc, k=k, ceng=ceng: op(ceng, lambda e: e.scalar_tensor_tensor(
                        out=cT[:, cc, :], in0=uT[:, cc, k:k + NT], scalar=fm[:, FM_WDA + cc * 31 + k:FM_WDA + cc * 31 + k + 1],
                        in1=cT[:, cc, :], op0=ALU.mult, op1=ALU.add), ["uT", "fm", ("cT", cc)], [("cT", cc)]))

            def conv_finish(c=c):
                if c == nch - 1:
                    for cc in range(4):
                        op("pe", lambda e, cc=cc: e.transpose(out=psM[0:32, cc * 128:(cc + 1) * 128], in_=uT[:, cc, 30 + NT - 32:30 + NT], identity=identf[:, :]),
                           ["uT", "identf"], ["psM"])
                    op("dve", lambda e: e.tensor_copy(out=outst[0:32, :], in_=psM[0:32, :]), ["psM"], ["outst"])
                    dma("sp", conv_o[:, :], outst[0:32, :], ["outst"], ["hbm_conv"], "st_misc")
                op("dve", lambda e: e.tensor_copy(out=uT[:, :, 0:30], in_=uT[:, :, NT:NT + 30]), ["uT"], ["uT"])
                for cc in range(4):
                    mm(psA[4][:, :], onesf[:, :], cT[:, cc, :], cc == 0, cc == 3, ["onesf", ("cT", cc)], [PA[4]])
                for cc in range(4):
                    tt, tk = (tmpC, ("cbuf", 0)) if cc % 2 == 0 else (tmpD, ("cbuf", 1))
                    op("act", lambda e, cc=cc, tt=tt: e.activation(out=tt[:, :], in_=cT[:, cc, :], func=AF.Square), [("cT", cc)], [tk])
                    mm(psA[5][:, :], onesf[:, :], tt[:, :], cc == 0, cc == 3, ["onesf", tk], [PA[5]])
                op("dve", lambda e: e.tensor_scalar(out=tmpA[:, :], in0=psA[4][:, :], scalar1=1.0 / 512, scalar2=None, op0=ALU.mult), [PA[4]], ["tmpA"])
                op("dve", lambda e: e.tensor_tensor(out=tmpB[:, :], in0=tmpA[:, :], in1=tmpA[:, :], op=ALU.mult), ["tmpA"], ["tmpB"])
                op("dve", lambda e: e.scalar_tensor_tensor(out=tmpB[:, :], in0=psA[5][:, :], scalar=1.0 / 512, in1=tmpB[:, :], op0=ALU.mult, op1=ALU.subtract),
                   [PA[5], "tmpB"], ["tmpB"])
                op("act", lambda e: e.activation(out=tmpB[:, :], in_=tmpB[:, :], func=AF.Sqrt, bias=epsb[:, 0:1], scale=1.0), ["tmpB", "epsb"], ["tmpB"])
                op("dve", lambda e: e.reciprocal(out=tmpB[:, :], in_=tmpB[:, :]), ["tmpB"], ["tmpB"])
                for cc in range(4):
                    op("dve", lambda e, cc=cc: e.tensor_tensor(out=cT[:, cc, :], in0=cT[:, cc, :], in1=tmpA[:, :], op=ALU.subtract), [("cT", cc), "tmpA"], [("cT", cc)])
                    op("dve", lambda e, cc=cc: e.tensor_tensor(out=cT[:, cc, :], in0=cT[:, cc, :], in1=tmpB[:, :], op=ALU.mult), [("cT", cc), "tmpB"], [("cT", cc)])
                    op("act", lambda e, cc=cc: e.activation(out=sT[:, cc, :], in_=cT[:, cc, :], func=AF.Silu, scale=fm[:, FM_LAG + cc:FM_LAG + cc + 1],
                                                            bias=fm[:, FM_LAB + cc:FM_LAB + cc + 1]), [("cT", cc), "fm"], ["sT"])

            op("dve", lambda e: e.memset(Kaug[:, :, :, 64:80], 0.0), [], ["Kaug"])
            op("dve", lambda e, c=c: e.memset(Kaug[:, 0:2, :, 64 + 2 * c:65 + 2 * c], 1.0), [], ["Kaug"])
            op("dve", lambda e, c=c: e.memset(Kaug[:, 2:4, :, 65 + 2 * c:66 + 2 * c], 1.0), [], ["Kaug"])
            kvqm = (("k", 1536), ("v", 2048), ("q", 1024), ("mq", 2560))
            wcur = {}

            def emit_mm(i):
                name, col0 = kvqm[i // 4]
                sub = i % 4
                if sub == 0:
                    wcur[name] = load_w(win_v[:, :, col0:col0 + 512], 8, 512)
                W, wk = wcur[name]
                ps, pk = psA[sub], PA[sub]
                for kc in range(8):
                    mm(ps[:, :], hT[:, kc, sub * 128:(sub + 1) * 128], W[:, kc, :], kc == 0, kc == 7, ["hT", wk], [pk])

            def post(i, c=c, t0=t0):
                name = kvqm[i // 4][0]
                sub = i % 4
                ps, pk = psA[sub], PA[sub]
                if name == "k":
                    headnorm(ps, pk, NH, HD, BC_GK, kst[:, sub % 2, :].rearrange("p (h d) -> p h d", h=NH), ("kst", sub % 2))
                    dma("sp", k_o[t0 + sub * 128:t0 + (sub + 1) * 128, :], kst[:, sub % 2, :], [("kst", sub % 2)], ["hbm_k"], "st_k%d" % (sub % 2))
                    op("dve", lambda e, sub=sub: e.tensor_copy(out=Kaug[:, sub, :, 0:64], in_=kst[:, sub % 2, :].rearrange("p (h d) -> p h d", h=NH)),
                       [("kst", sub % 2)], ["Kaug"])
                    op("dve", lambda e, sub=sub: e.tensor_copy(out=kn_bf[:, sub % 2, :], in_=kst[:, sub % 2, :]), [("kst", sub % 2)], ["kn_bf"])
                    for h in range(NH):
                        tr(psT[0:82, h * 128:(h + 1) * 128], Kaug[:, sub, h, :], identb[:, :], ["Kaug", "identb"], ["psT"])
                    op("dve", lambda e, sub=sub: e.tensor_copy(out=KTloc[0:82, :, sub * 128:(sub + 1) * 128],
                                                               in_=psT[0:82, :].rearrange("p (k t) -> p k t", k=NH)), ["psT"], ["KTloc"])
                    if sub % 2 == 1:
                        blk = 2 * c + sub // 2
                        for pair in range(4):
                            for s2 in range(2):
                                mm(psM[:, pair:pair + 1], kn_bf[:, s2, pair * 128:(pair + 1) * 128], onesb[:, 0:1], s2 == 0, s2 == 1,
                                   ["kn_bf", "onesb"], ["psM"])
                        op("dve", lambda e, blk=blk: e.tensor_scalar(out=kmBD[0:64, :, 0, blk], in0=psM[0:64, 0:4], scalar1=1.0 / 256, scalar2=None, op0=ALU.mult),
                           ["psM"], ["kmBD"])
                        op("dve", lambda e, blk=blk: e.tensor_scalar(out=kmBD[64:128, :, 1, blk], in0=psM[64:128, 0:4], scalar1=1.0 / 256, scalar2=None, op0=ALU.mult),
                           ["psM"], ["kmBD"])
                elif name == "v":
                    op("act", lambda e, sub=sub, ps=ps: e.activation(out=vst[:, sub % 2, :], in_=ps[:, :], func=AF.Copy), [pk], [("vst", sub % 2)])
                    dma("sp", v_o[t0 + sub * 128:t0 + (sub + 1) * 128, :], vst[:, sub % 2, :], [("vst", sub % 2)], ["hbm_v"], "st_v%d" % (sub % 2))
                    op("dve", lambda e, sub=sub: e.tensor_copy(out=Vloc[:, sub, :, 64:128], in_=vst[:, sub % 2, :].rearrange("p (h d) -> p h d", h=NH)),
                       [("vst", sub % 2)], ["Vloc"])
                elif name == "q":
                    cur = 2 * c + sub // 2
                    headnorm(ps, pk, NH, HD, BC_GQ, Qaug[:, sub, :, 0:64], "Qaug")
                    if cur == 0:
                        op("dve", lambda e, sub=sub: e.memset(Qaug[:, sub, :, 64:80], NEG), [], ["Qaug"])
                    else:
                        op("dve", lambda e, sub=sub: e.tensor_copy(out=qn_bf[:, :].rearrange("p (h d) -> p h d", h=NH), in_=Qaug[:, sub, :, 0:64]), ["Qaug"], ["qn_bf"])
                        for pair in range(4):
                            tr(psT[:, pair * 128:(pair + 1) * 128], qn_bf[:, pair * 128:(pair + 1) * 128], identb[:, :], ["qn_bf", "identb"], ["psT"])
                        op("dve", lambda e: e.tensor_copy(out=qTp[:, :, :], in_=psT[:, 0:512].rearrange("p (k t) -> p k t", k=4)), ["psT"], ["qTp"])
                        for pair in range(4):
                            mm(psM[:, 32 * pair:32 * pair + 32], qTp[:, pair, :], kmBD[:, pair, :, :].rearrange("p a b -> p (a b)"), True, True, ["qTp", "kmBD"], ["psM"])
                        op("dve", lambda e, cur=cur: e.tensor_copy(out=gatef[:, :, 0:cur], in_=psM[:, 0:128].rearrange("p (h b) -> p h b", h=NH)[:, :, 0:cur]),
                           ["psM"], ["gatef"])
                        for h in range(NH):
                            op("dve", lambda e, h=h: e.max(out=top8[:, h, :], in_=gatef[:, h, :]), ["gatef"], ["top8"])
                        op("dve", lambda e: e.tensor_scalar(out=thr[:, :], in0=top8[:, :, 2], scalar1=-1e29, scalar2=None, op0=ALU.max), ["top8"], ["thr"])
                        op("dve", lambda e: e.tensor_tensor(out=nsel[:, :, :], in0=gatef[:, :, :], in1=thr[:, :].unsqueeze(2).broadcast_to([128, NH, 16]), op=ALU.is_lt),
                           ["gatef", "thr"], ["nsel"])
                        op("dve", lambda e, sub=sub: e.tensor_scalar(out=Qaug[:, sub, :, 64:80], in0=nsel[:, :, :], scalar1=NEG, scalar2=None, op0=ALU.mult),
                           ["nsel"], ["Qaug"])
                    op("dve", lambda e, sub=sub, cur=cur: e.memset(Qaug[:, sub, :, 64 + cur:65 + cur], 0.0), [], ["Qaug"])
                    for h in range(NH):
                        tr(psT[0:82, h * 128:(h + 1) * 128], Qaug[:, sub, h, :], identb[:, :], ["Qaug", "identb"], ["psT"])
                    op("dve", lambda e, sub=sub: e.tensor_copy(out=qTa[0:82, :, sub * 128:(sub + 1) * 128],
                                                               in_=psT[0:82, :].rearrange("p (k t) -> p k t", k=NH)), ["psT"], ["qTa"])
                else:
                    headnorm(ps, pk, 4, 128, BC_GMQ, mqn[:, sub, :].rearrange("p (h d) -> p h d", h=4), "mqn")
                    for mh in range(4):
                        tr(psT[:, mh * 128:(mh + 1) * 128], mqn[:, sub, mh * 128:(mh + 1) * 128], identb[:, :], ["mqn", "identb"], ["psT"])
                    op("dve", lambda e, sub=sub: e.tensor_copy(out=mqT[:, :, sub * 128:(sub + 1) * 128],
                                                               in_=psT[:, 0:512].rearrange("p (k t) -> p k t", k=4)), ["psT"], ["mqT"])
                if name == "v" and sub == 3:
                    dma("sp", kt_scr[:, :, t0:t0 + NT].rearrange("h r k -> r h k"), KTloc[0:82, :, :], ["KTloc"], [("kscr", c)], "st_kscr")
                    for t4 in range(4):
                        dma("sp", v_scr[:, 4 * c + t4, :, :].rearrange("h k c -> k h c"), Vloc[:, t4, :, :], ["Vloc"], [("vscr", c)], "st_vscr")


            emit_mm(0)
            emit_mm(1)
            for i in range(16):
                if i + 2 < 16:
                    emit_mm(i + 2)
                post(i)

            nexp = 0
            steps = []
            for h in range(NH):
                for g in range(c + 1):
                    for tile in range(4):
                        steps.append((h, g, tile))
            LA = 2
            slots = {}

            def qk(i):
                h, g, tile = steps[i]
                if tile == 0:
                    sl = kvstate["i"] % 4
                    kvstate["i"] += 1
                    slots[(h, g)] = sl
                    dma("sp", kvK[0:82, sl, :], kt_scr[h, :, g * NT:(g + 1) * NT], [("kscr", g)], [("kvK", sl)], "kvK%d" % sl)
                    dma("sp", kvV[:, sl, :, :], v_scr[h, 4 * g:4 * g + 4, :, :].rearrange("t k c -> k t c"), [("vscr", g)], [("kvV", sl)], "kvV%d" % sl)
                sl = slots[(h, g)]
                diag = (g == c)
                col0 = 128 * tile if diag else 0
                sp_, sk = psA[2 + i % 4], PA[2 + i % 4]
                mm(sp_[:, col0:NT], kvK[0:82, sl, tile * 128:(tile + 1) * 128], qTa[0:82, h, col0:NT], True, True, [("kvK", sl), "qTa"], [sk])
                if diag:
                    mm(sp_[:, col0:col0 + 128], identb[:, :], trib[:, :], False, True, ["identb", "trib"], [sk])

            def expv(i):
                h, g, tile = steps[i]
                sl = slots[(h, g)]
                acc, ak = psA[h % 2], PA[h % 2]
                kt = 4 * g + tile
                diag = (g == c)
                col0 = 128 * tile if diag else 0
                sp_, sk = psA[2 + i % 4], PA[2 + i % 4]
                pslot = i % 3
                bcol = h * 32 + (kt - 4 * c + 28)
                op("act", lambda e: e.activation(out=PT[:, pslot, col0:NT], in_=sp_[:, col0:NT], func=AF.Exp, bias=btab[:, bcol:bcol + 1], scale=1.0),
                   [sk, "btab"], [("PT", pslot)])
                mm(acc[:, col0:NT], kvV[:, sl, tile, :], PT[:, pslot, col0:NT], g == 0 and tile == 0, diag and tile == 3, [("kvV", sl), ("PT", pslot)], [ak])
                if diag and tile == 3:
                    op("dve", lambda e: e.reciprocal(out=rden[0:64, :], in_=acc[0:64, :]), [ak], ["rden"])
                    op("dve", lambda e: e.tensor_tensor(out=OT[64 * (h % 2):64 * (h % 2) + 64, h // 2, :], in0=acc[64:128, :], in1=rden[0:64, :], op=ALU.mult),
                       [ak, "rden"], ["OT"])
                    for stp in conv_steps[CONV_PER_HEAD * h:CONV_PER_HEAD * (h + 1)]:
                        stp()

            for stp in pool_steps:
                stp()
            for i in range(min(LA, len(steps))):
                qk(i)
            for i in range(len(steps)):
                if i + LA < len(steps):
                    qk(i + LA)
                expv(i)
            nexp = len(steps)
            conv_finish()

            msteps = [(mh, t) for mh in range(4) for t in range(2)]

            def mqk(i):
                mh, t = msteps[i]
                sp_, sk = psA[4 + i % 2], PA[4 + i % 2]
                mm(sp_[:, :], mkT[:, mh, t * 128:(t + 1) * 128], mqT[:, mh, :], True, True, ["mkT", "mqT"], [sk])

            def mpv(i):
                mh, t = msteps[i]
                sp_, sk = psA[4 + i % 2], PA[4 + i % 2]
                pslot = i % 3
                o_ps, ok = psA[2 * (mh % 2)], PA[2 * (mh % 2)]
                d_ps, dk = psA[2 * (mh % 2) + 1], PA[2 * (mh % 2) + 1]
                op("act", lambda e: e.activation(out=PT[:, pslot, :], in_=sp_[:, :], func=AF.Exp, bias=mvals[:, 5:6], scale=1.0),
                   [sk, "mvals"], [("PT", pslot)])
                mm(o_ps[:, :], mVa[:, t, mh * 128:(mh + 1) * 128], PT[:, pslot, :], t == 0, t == 1, ["mVa", ("PT", pslot)], [ok])
                mm(d_ps[:, :], onesb[:, :], PT[:, pslot, :], t == 0, t == 1, ["onesb", ("PT", pslot)], [dk])
                if t == 1:
                    op("dve", lambda e: e.reciprocal(out=rden[:, :], in_=d_ps[:, :]), [dk], ["rden"])
                    op("dve", lambda e: e.tensor_tensor(out=OmT[:, mh, :], in0=o_ps[:, :], in1=rden[:, :], op=ALU.mult), [ok, "rden"], ["OmT"])

            mqk(0)
            for i in range(8):
                if i + 1 < 8:
                    mqk(i + 1)
                mpv(i)

            Wpa, kpa = load_w(wpa_v, 4, 1024)
            Wpb, kpb = load_w(wpb_v, 4, 1024)
            Wpc, kpc = load_w(wpc_v, 4, 1024)
            for dc in range(8):
                srcs = ((Wpa, kpa, sT, "sT"), (Wpb, kpb, OT, "OT"), (Wpc, kpc, OmT, "OmT"))
                for j in range(3):
                    Wp, kp, src, skey = srcs[j]
                    for kc in range(4):
                        mm(psA[j][:, :], Wp[:, kc, dc * 128:(dc + 1) * 128], src[:, kc, :], kc == 0, kc == 3, [kp, skey], [PA[j]])
                for j in range(3):
                    Wgt, kgt = load_g(win_v[:, :, 3072 + j * 1024 + dc * 128:3072 + j * 1024 + (dc + 1) * 128])
                    for kc in range(8):
                        mm(psA[3 + j][:, :], Wgt[:, kc, :], hT[:, kc, :], kc == 0, kc == 7, [kgt, "hT"], [PA[3 + j]])
                    op("act", lambda e, j=j, dc=dc: e.activation(out=sig[:, j, :], in_=psA[3 + j][:, :], func=AF.Sigmoid,
                                                                 bias=fm[:, FM_BG + j * 8 + dc:FM_BG + j * 8 + dc + 1], scale=1.0),
                       [PA[3 + j], "fm"], [("cT", j)])
                op("dve", lambda e: e.tensor_tensor(out=tmpA[:, :], in0=psA[0][:, :], in1=sig[:, 0, :], op=ALU.mult), [PA[0], ("cT", 0)], ["tmpA"])
                op("dve", lambda e: e.tensor_tensor(out=tmpB[:, :], in0=psA[1][:, :], in1=sig[:, 1, :], op=ALU.mult), [PA[1], ("cT", 1)], ["tmpB"])
                op("dve", lambda e: e.tensor_tensor(out=tmpA[:, :], in0=tmpA[:, :], in1=tmpB[:, :], op=ALU.add), ["tmpA", "tmpB"], ["tmpA"])
                op("dve", lambda e: e.tensor_tensor(out=tmpB[:, :], in0=psA[2][:, :], in1=sig[:, 2, :], op=ALU.mult), [PA[2], ("cT", 2)], ["tmpB"])
                op("dve", lambda e, dc=dc: e.tensor_tensor(out=merged[:, dc, :], in0=tmpA[:, :], in1=tmpB[:, :], op=ALU.add), ["tmpA", "tmpB"], ["merged"])
            Wo = [load_w(wout_v[:, :, half * 512:(half + 1) * 512], 8, 512) for half in range(2)]
            for sub in range(4):
                for half in range(2):
                    W, wk = Wo[half]
                    pb, pbk = psA[(2 * sub + half) % 4], PA[(2 * sub + half) % 4]
                    for kc in range(8):
                        mm(pb[:, :], merged[:, kc, sub * 128:(sub + 1) * 128], W[:, kc, :], kc == 0, kc == 7, ["merged", wk], [pbk])
                    op("dve", lambda e, sub=sub, half=half, pb=pb: e.tensor_tensor(out=X[:, sub, half * 512:(half + 1) * 512], in0=X[:, sub, half * 512:(half + 1) * 512],
                                                                                   in1=pb[:, :], op=ALU.add), [xks[sub], pbk], [xks[sub]])
                rms_rows(X, sub, sub, xks[sub])
                if sub >= 1:
                    norm_A(X, sub - 1, xks[sub - 1])
                    norm_B(sub - 1, FM_G2, hT, "hT")
            norm_A(X, 3, xks[3])
            norm_B(3, FM_G2, hT, "hT")

            wdf3 = fm[:, FM_WDF:FM_WDF + 132].rearrange("p (j k) -> p j k", k=3)
            allhist_k = [("hist", j) for j in range(44)]
            op("dve", lambda e: e.tensor_tensor(out=corr[:, :, 0], in0=hist[:, :, 0], in1=wdf3[:, :, 0], op=ALU.mult), allhist_k + ["fm"], ["corr"])
            op("dve", lambda e: e.tensor_tensor(out=corrt[:, :], in0=hist[:, :, 1], in1=wdf3[:, :, 1], op=ALU.mult), allhist_k + ["fm"], ["corrt"])
            op("dve", lambda e: e.tensor_tensor(out=corr[:, :, 0], in0=corr[:, :, 0], in1=corrt[:, :], op=ALU.add), ["corr", "corrt"], ["corr"])
            op("dve", lambda e: e.tensor_tensor(out=corr[:, :, 1], in0=hist[:, :, 1], in1=wdf3[:, :, 0], op=ALU.mult), allhist_k + ["fm"], ["corr"])
            upw = {}

            def emit_up(j):
                piece, jj = j // 4, j % 4
                if jj == 0:
                    upw[piece] = load_w(wup_v[:, :, piece * 512:(piece + 1) * 512], 8, 512)
                W, wk = upw[piece]
                ps, pk = psA[j % 4], PA[j % 4]
                cb, ck = cbuf[:, j % 3, :], ("cbuf", j % 3)
                for kc in range(8):
                    mm(ps[:, :], W[:, kc, jj * 128:(jj + 1) * 128], hT[:, kc, :], kc == 0, kc == 7, [wk, "hT"], [pk])
                w2 = fm[:, FM_WDF + 3 * j + 2:FM_WDF + 3 * j + 3]
                bb = fm[:, FM_BDF + j:FM_BDF + j + 1]
                op("act", lambda e: e.activation(out=cb, in_=ps[:, :], func=AF.Identity, scale=w2, bias=bb), [pk, "fm"], [ck])
                op("act", lambda e: e.activation(out=hist[:, j, :], in_=ps[:, NT - 2:NT], func=AF.Copy), [pk], [("hist", j)])

            def chain_up(j):
                ps, pk = psA[j % 4], PA[j % 4]
                cb, ck = cbuf[:, j % 3, :], ("cbuf", j % 3)
                w0 = fm[:, FM_WDF + 3 * j:FM_WDF + 3 * j + 1]
                w1 = fm[:, FM_WDF + 3 * j + 1:FM_WDF + 3 * j + 2]
                op("dve", lambda e: e.scalar_tensor_tensor(out=cb[:, 1:NT], in0=ps[:, 0:NT - 1], scalar=w1, in1=cb[:, 1:NT], op0=ALU.mult, op1=ALU.add),
                   [pk, ck, "fm"], [ck])
                op("dve", lambda e: e.scalar_tensor_tensor(out=cb[:, 2:NT], in0=ps[:, 0:NT - 2], scalar=w0, in1=cb[:, 2:NT], op0=ALU.mult, op1=ALU.add),
                   [pk, ck, "fm"], [ck])
                op("dve", lambda e: e.tensor_tensor(out=cb[:, 0:2], in0=cb[:, 0:2], in1=corr[:, j, :], op=ALU.add), [ck, "corr"], [ck])
                if j < 22:
                    op("act", lambda e: e.activation(out=sa[:, j, :], in_=cb, func=AF.Silu), [ck], [("sa", j)])
                else:
                    op(FFN_MUL_ENG, lambda e: e.tensor_tensor(out=sa[:, j - 22, :], in0=sa[:, j - 22, :], in1=cb, op=ALU.mult), [ck, ("sa", j - 22)], [("sa", j - 22)])

            emit_up(0)
            for j in range(44):
                if j + 1 < 44:
                    emit_up(j + 1)
                chain_up(j)
            sakeys = [("sa", j) for j in range(22)]
            Xn, xnk = Xb[1 - xb], XK(1 - xb)
            if False:
                for sub in range(4):
                    rms_rows(Xn, sub, sub, xnk[sub])
                norm_A(Xn, 0, xnk[0])
            grp = 0
            for half in range(2):
                for kp, (k0, k1) in enumerate(((0, 8), (8, 16), (16, 22))):
                    W, wk = load_w(wdn_v[:, k0:k1, half * 512:(half + 1) * 512], k1 - k0, 512)
                    for sub in range(4):
                        for kc in range(k0, k1):
                            mm(psA[sub][:, :], sa[:, kc, sub * 128:(sub + 1) * 128], W[:, kc - k0, :], kc == 0, kc == 21, sakeys + [wk], [PA[sub]])
                    if False and grp < 4:
                        norm_B(grp, FM_G1, hT, "hT")
                        if grp + 1 < 4:
                            norm_A(Xn, grp + 1, xnk[grp + 1])
                    grp += 1
                for sub in range(4):
                    op("dve", lambda e, sub=sub, half=half: e.tensor_tensor(out=X[:, sub, half * 512:(half + 1) * 512], in0=X[:, sub, half * 512:(half + 1) * 512],
                                                                            in1=psA[sub][:, :], op=ALU.add), [xks[sub], PA[sub]], [xks[sub]])
            dma("sp", y_o[t0:t0 + NT, :].rearrange("(s p) d -> p s d", p=128), X[:, :, :], xks, ["hbm_y"], "st_y")


        if do_sample:
            X = Xb[0]
            S.alias["X"] = XK(0) + XK(1)
            sc = X[:, 1:4, :].rearrange("p a t -> p (a t)")
            dma("sp", X[:, 0, :], xs_in[:, :], [], ["X"], "xload")
            dma("sp", sc[:, 0:SC_N], sc_in[:, :], [], ["X"], "xload")
            RROW = lambda n: sc[:, SC_RROW + n * 128:SC_RROW + (n + 1) * 128]
            ONEROW = lambda n: sc[:, SC_CS + 3 - n:SC_CS + 131 - n]
            ONECOL = lambda r: sc[:, SC_ST + 127 - r:SC_ST + 255 - r]
            REP = sc[:, SC_REP:SC_REP + 128]
            E8 = lambda h: sc[0:8, SC_E8 + h * 128:SC_E8 + (h + 1) * 128]
            ALT = sc[:, SC_AL:SC_AL + 512]
            PIOTA = sc[:, SC_PI:SC_PI + 1]
            IND120 = sc[0:120, SC_IND:SC_IND + 128]
            op("dve", lambda e: e.tensor_copy(out=orowb[:, 0:131], in_=sc[:, SC_CS:SC_CS + 131]), ["X"], ["orowb"])
            ONEROWB = lambda n: orowb[:, 3 - n:131 - n]
            HCOL = lambda h: sc[:, SC_ST + 127 - h:SC_ST + 135 - h]
            dma("sp", idx_t[:, :], pt_in[:, :], [], ["idx_t"], "c7")
            op("dve", lambda e: e.tensor_copy(out=ptf[:, :], in_=idx_t[:, :]), ["idx_t"], ["ptf"])
            op("dve", lambda e: e.tensor_scalar(out=ptf[:, :], in0=ptf[:, :], scalar1=128.0, scalar2=None, op0=ALU.mult), ["ptf"], ["ptf"])
            op("dve", lambda e: e.tensor_scalar(out=ptf[:, :], in0=ptf[:, :], scalar1=PIOTA, scalar2=None, op0=ALU.add), ["ptf", "X"], ["ptf"])
            op("dve", lambda e: e.tensor_copy(out=idx_t[:, :], in_=ptf[:, :]), ["ptf"], ["idx_t"])

            for _k in ("stc", "prd", "qb", "mg"):
                S.alias[_k] = ["uT"]
            S.alias["mg"] = ["uT", "stc", "prd"]
            S.alias["cs"] = [("sa", j) for j in range(22)] + ["KTloc", "qTa", "mqn", "qTp"]
            S.alias["stf"] = ["Vloc"]
            norm_to_T(X, 1, FM_G1, hT, "hT", "X")
            us, qs, mqs, ks = cT[:, 0, :], cT[:, 1, :], cT[:, 2, :], cT[:, 3, :]
            vs, stc, prd, qb = uT[:, 0, 0:512], uT[:, 1, 0:512], uT[:, 2, 0:512], uT[:, 3, 0:512]

            def bload(dst, dkey, src_row):
                dma("sp", dst, src_row.partition_broadcast(128), [], [dkey], "bl_" + str(dkey))

            def zpiece(col0):
                W, wk = load_w(win_v[:, :, col0:col0 + 512], 8, 512)
                for kc in range(8):
                    mm(psA[0][:, :], hT[:, kc, 0:128], W[:, kc, :], kc == 0, kc == 7, ["hT", wk], [PA[0]])
                return psA[0], PA[0]

            ps, pk = zpiece(0)
            op("act", lambda e: e.activation(out=tmpA[:, :], in_=psA[0][:, :], func=AF.Copy), [pk], ["tmpA"])
            op("dve", lambda e: e.tensor_copy(out=rden[:, :], in_=tmpA[:, :]), ["tmpA"], ["rden"])
            ps, pk = zpiece(512)
            op("act", lambda e: e.activation(out=tmpB[:, :], in_=psA[0][:, :], func=AF.Sigmoid), [pk], ["tmpB"])
            op("dve", lambda e: e.tensor_tensor(out=us, in0=rden[:, :], in1=tmpB[:, :], op=ALU.mult), ["rden", "tmpB"], [("cT", 0)])
            ps, pk = zpiece(1024)
            headnorm(ps, pk, NH, HD, BC_GQ, qs.rearrange("p (h d) -> p h d", h=NH), ("cT", 1))
            ps, pk = zpiece(1536)
            headnorm(ps, pk, NH, HD, BC_GK, ks.rearrange("p (h d) -> p h d", h=NH), ("cT", 3))
            dma("sp", ks_o[:, :], cT[0:4, 3, :], [("cT", 3)], ["hbm_ks"], "st_s1")
            ps, pk = zpiece(2048)
            op("act", lambda e: e.activation(out=vs, in_=psA[0][:, :], func=AF.Copy), [pk], ["uT"])
            dma("sp", vs_o[:, :], uT[0:4, 0, 0:512], ["uT"], ["hbm_vs"], "st_s2")
            ps, pk = zpiece(2560)
            headnorm(ps, pk, 4, 128, BC_GMQ, mqs.rearrange("p (h d) -> p h d", h=4), ("cT", 2))

            dma("sp", uT[0:120, 1, 0:512], stc_in[:, :], [], ["stc"], "c8")
            dma("sp", uT[0:120, 2, 0:512], w120_in[:, :], [], ["prd"], "c9")
            for n in range(4):
                dma("sp", convs_o[n, 0:29, :], uT[n * 30 + 1:n * 30 + 30, 1, 0:512], ["stc"], ["hbm_convs"], "st_s3")
            dma("sp", convs_o[:, 29, :], cT[0:4, 0, :], [("cT", 0)], ["hbm_convs"], "st_s3")
            op("dve", lambda e: e.tensor_tensor(out=uT[0:120, 2, 0:512], in0=uT[0:120, 2, 0:512], in1=uT[0:120, 1, 0:512], op=ALU.mult), ["stc", "prd"], ["prd"])
            mm(psA[1][:, :], IND120, uT[0:120, 2, 0:512], True, True, ["prd", "X"], [PA[1]])
            bload(cbuf[:, 0, :], ("cbuf", 0), vrow_in[0, 0:512])
            bload(cbuf[:, 1, :], ("cbuf", 1), vrow_in[1, 0:512])
            op("dve", lambda e: e.tensor_tensor(out=tmpA[:, :], in0=us, in1=cbuf[:, 0, :], op=ALU.mult), [("cT", 0), ("cbuf", 0)], ["tmpA"])
            op("dve", lambda e: e.tensor_tensor(out=tmpA[:, :], in0=tmpA[:, :], in1=cbuf[:, 1, :], op=ALU.add), ["tmpA", ("cbuf", 1)], ["tmpA"])
            op("dve", lambda e: e.tensor_tensor(out=tmpA[:, :], in0=tmpA[:, :], in1=psA[1][:, :], op=ALU.add), ["tmpA", PA[1]], ["tmpA"])
            op("dve", lambda e: e.tensor_reduce(out=ss[:, 0:1], in_=tmpA[:, :], axis=AX.X, op=ALU.add), ["tmpA"], [("ss", 0)])
            op("dve", lambda e: e.tensor_scalar(out=ss[:, 0:1], in0=ss[:, 0:1], scalar1=1.0 / 512, scalar2=None, op0=ALU.mult), [("ss", 0)], [("ss", 0)])
            op("dve", lambda e: e.tensor_scalar(out=tmpA[:, :], in0=tmpA[:, :], scalar1=ss[:, 0:1], scalar2=None, op0=ALU.subtract), ["tmpA", ("ss", 0)], ["tmpA"])
            op("dve", lambda e: e.memset(ss[:, 1:2], 0.0), [], [("ss", 1)])
            op("act", lambda e: e.activation(out=tmpB[:, :], in_=tmpA[:, :], func=AF.Square, accum_out=ss[:, 1:2]), ["tmpA", ("ss", 1)], ["tmpB", ("ss", 1)])
            op("act", lambda e: e.activation(out=ss[:, 1:2], in_=ss[:, 1:2], func=AF.Sqrt, bias=epsb[:, 0:1], scale=1.0 / 512), [("ss", 1), "epsb"], [("ss", 1)])
            op("dve", lambda e: e.reciprocal(out=ss[:, 1:2], in_=ss[:, 1:2]), [("ss", 1)], [("ss", 1)])
            bload(cbuf[:, 0, :], ("cbuf", 0), vrow_in[2, 0:512])
            bload(cbuf[:, 1, :], ("cbuf", 1), vrow_in[3, 0:512])
            op("dve", lambda e: e.tensor_scalar(out=tmpA[:, :], in0=tmpA[:, :], scalar1=ss[:, 1:2], scalar2=None, op0=ALU.mult), ["tmpA", ("ss", 1)], ["tmpA"])
            op("dve", lambda e: e.tensor_tensor(out=tmpA[:, :], in0=tmpA[:, :], in1=cbuf[:, 0, :], op=ALU.mult), ["tmpA", ("cbuf", 0)], ["tmpA"])
            op("dve", lambda e: e.tensor_tensor(out=tmpA[:, :], in0=tmpA[:, :], in1=cbuf[:, 1, :], op=ALU.add), ["tmpA", ("cbuf", 1)], ["tmpA"])
            op("act", lambda e: e.activation(out=xn[:, 0:512], in_=tmpA[:, :], func=AF.Silu), ["tmpA"], ["xn"])
            dbgout(xn[0:4, 0:512], [4, 512], ["xn"])
            for kc in range(4):
                tr(psT[:, kc * 128:(kc + 1) * 128], xn[:, kc * 128:(kc + 1) * 128], identb[:, :], ["xn", "identb"], ["psT"])
            op("dve", lambda e: e.tensor_copy(out=sT[:, :, 0:128], in_=psT[:, 0:512].rearrange("p (k t) -> p k t", k=4)), ["psT"], ["sT"])

            Lt = bc
            pbufs = [(kst[:, 0, :], ("kst", 0)), (kst[:, 1, :], ("kst", 1)), (vst[:, 0, :], ("vst", 0)), (vst[:, 1, :], ("vst", 1))]
            pstate = {"i": 0}
            pvstate = {"i": 0}

            def get_page(src_kind, n, pg):
                buf, bk = pbufs[pstate["i"] % 4]
                pstate["i"] += 1
                if src_kind in ("k", "v"):
                    src = ck_in if src_kind == "k" else cv_in
                    col = n * 64 + pg
                    op("pool", lambda e: e.indirect_dma_start(out=buf, out_offset=None, in_=src[:, :],
                                                              in_offset=bass.IndirectOffsetOnAxis(ap=idx_t[:, col:col + 1], axis=0)),
                       ["idx_t"], [bk], dma=True, semkey="pg_" + str(bk))
                else:
                    src = cmk_in if src_kind == "mk" else cmv_in
                    dma("sp", buf, src[n, pg * 128:(pg + 1) * 128, :], [], [bk], "pg_" + str(bk))
                return buf, bk

            def bcast_row(n, src, skey):
                mm(psA[2][:, :], RROW(n), src, True, True, ["X", skey], [PA[2]])
                op("act", lambda e: e.activation(out=qb, in_=psA[2][:, :], func=AF.Copy), [PA[2]], ["qb"])

            def dense_attn(kkind, vkind, npg, nh, hd, qsrc, qkey, moba):
                G = npg * nh
                first_sum = [True]
                for n in range(4):
                    bcast_row(n, qsrc, qkey)
                    for pg in range(npg):
                        buf, bk = get_page(kkind, n, pg)
                        op("dve", lambda e, buf=buf: e.tensor_tensor(out=tmpA[:, :], in0=buf, in1=qb, op=ALU.mult), [bk, "qb"], ["tmpA"])
                        o0 = n * G + pg * nh
                        op("dve", lambda e, o0=o0: e.tensor_reduce(out=Lt[:, o0:o0 + nh], in_=tmpA[:, :].rearrange("p (h d) -> p h d", h=nh), axis=AX.X, op=ALU.add),
                           ["tmpA"], ["bc"])
                if moba:
                    Lv = Lt[:, :].rearrange("p (g h) -> p g h", h=NH)
                    for h in range(NH):
                        mm(psA[3][0:8, 0:256], HCOL(h), Lv[:, :, h], h == 0, h == NH - 1, ["X", "bc"], [PA[3]])
                    op("dve", lambda e: e.tensor_reduce(out=rden[0:8, 0:128], in_=psA[3][0:8, 0:256].rearrange("p (g e) -> p g e", e=2), axis=AX.X, op=ALU.add),
                       [PA[3]], ["rden"])
                    for n in range(4):
                        op("dve", lambda e, n=n: e.max(out=top8[0:8, n, :], in_=rden[0:8, n * 32:(n + 1) * 32]), ["rden"], ["top8"])
                    op("dve", lambda e: e.tensor_tensor(out=rden[0:8, 128:256].rearrange("p (n b) -> p n b", n=4), in0=rden[0:8, 0:128].rearrange("p (n b) -> p n b", n=4),
                                                        in1=top8[0:8, 0:4, 2:3].broadcast_to([8, 4, 32]), op=ALU.is_ge), ["rden", "top8"], ["rden"])
                    op("dve", lambda e: e.tensor_copy(out=tmpB[0:8, 0:256].rearrange("p (g e) -> p g e", e=2), in_=rden[0:8, 128:256].unsqueeze(2).broadcast_to([8, 128, 2])),
                       ["rden"], ["tmpB"])
                for n in range(4):
                    if moba:
                        op("dve", lambda e, n=n: e.tensor_tensor(out=Lt[:, n * G:(n + 1) * G], in0=Lt[:, n * G:(n + 1) * G], in1=ALT, op=ALU.add), ["bc", "X"], ["bc"])
                    op("dve", lambda e, n=n: e.tensor_reduce(out=thr[:, n:n + 1], in_=Lt[:, n * G:(n + 1) * G], axis=AX.X, op=ALU.max), ["bc"], ["thr"])
                op("pe", lambda e: e.transpose(out=psM[0:4, 128:256], in_=thr[:, 0:4], identity=identf[:, :]), ["thr", "identf"], ["psM"])
                op("dve", lambda e: e.tensor_reduce(out=mvals[0:4, 6:7], in_=psM[0:4, 128:256], axis=AX.X, op=ALU.max), ["psM"], ["mvals"])
                if moba:
                    op("dve", lambda e: e.tensor_tensor(out=tmpA[:, :], in0=qs, in1=ks, op=ALU.mult), [("cT", 1), ("cT", 3)], ["tmpA"])
                    op("dve", lambda e: e.tensor_reduce(out=nsel[:, 0, 0:8], in_=tmpA[:, :].rearrange("p (h d) -> p h d", h=NH), axis=AX.X, op=ALU.add), ["tmpA"], ["nsel"])
                    op("dve", lambda e: e.tensor_reduce(out=mvals[0:4, 7:8], in_=nsel[0:4, 0, 0:8], axis=AX.X, op=ALU.max), ["nsel"], ["mvals"])
                    op("dve", lambda e: e.tensor_tensor(out=mvals[0:4, 6:7], in0=mvals[0:4, 6:7], in1=mvals[0:4, 7:8], op=ALU.max), ["mvals"], ["mvals"])
                op("dve", lambda e: e.tensor_scalar(out=mvals[0:4, 6:7], in0=mvals[0:4, 6:7], scalar1=-1.0, scalar2=None, op0=ALU.mult), ["mvals"], ["mvals"])
                op("dve", lambda e: e.tensor_scalar(out=nsel[0:4, 1, 0:4], in0=identf[0:4, 0:4], scalar1=mvals[0:4, 6:7], scalar2=None, op0=ALU.mult), ["mvals", "identf"], ["nsel"])
                mm(psM[:, 256:260], onesf[0:4, :], nsel[0:4, 1, 0:4], True, True, ["onesf", "nsel"], ["psM"])
                op("dve", lambda e: e.tensor_copy(out=thr[:, 4:8], in_=psM[:, 256:260]), ["psM"], ["thr"])
                for n in range(4):
                    op("act", lambda e, n=n: e.activation(out=Lt[:, n * G:(n + 1) * G], in_=Lt[:, n * G:(n + 1) * G], func=AF.Exp, bias=thr[:, 4 + n:5 + n], scale=1.0),
                       ["bc", "thr"], ["bc"])
                if moba:
                    op("act", lambda e: e.activation(out=nsel[0:4, 0, 0:8], in_=nsel[0:4, 0, 0:8], func=AF.Exp, bias=mvals[0:4, 6:7], scale=1.0), ["nsel", "mvals"], ["nsel"])
                    for h in range(NH):
                        mm(psA[h // 2][:, (h % 2) * 256:(h % 2) * 256 + 256], E8(h), tmpB[0:8, 0:256], True, True, ["X", "tmpB"], [PA[h // 2]])
                    for hp in range(4):
                        op("dve", lambda e, hp=hp: e.tensor_tensor(
                            out=Lt[:, :].rearrange("p (g h) -> p g h", h=NH)[:, :, 2 * hp:2 * hp + 2],
                            in0=Lt[:, :].rearrange("p (g h) -> p g h", h=NH)[:, :, 2 * hp:2 * hp + 2],
                            in1=psA[hp][:, :].rearrange("p (h g) -> p g h", h=2), op=ALU.mult), ["bc", PA[hp]], ["bc"])
                for n in range(4):
                    for c0 in range(0, G, 512):
                        w = min(512, G - c0)
                        mm(psA[4][:, 0:w], ONEROW(n), Lt[:, n * G + c0:n * G + c0 + w], n == 0 and c0 == 0, n == 3 and c0 + w >= G, ["X", "bc"], [PA[4]])
                Gc = min(G, 512)
                op("dve", lambda e: e.tensor_reduce(out=ss[:, 8:8 + nh], in_=psA[4][:, 0:Gc].rearrange("p (g h) -> p h g", h=nh), axis=AX.X, op=ALU.add), [PA[4]], ["ss8"])
                firstv = [True]
                for n in range(4):
                    for pg in range(npg):
                        buf, bk = get_page(vkind, n, pg)
                        o0 = n * G + pg * nh
                        slot = pvstate["i"] % 3
                        pvstate["i"] += 1
                        op("dve", lambda e, buf=buf, o0=o0, slot=slot: e.tensor_tensor(out=PT[:, slot, :].rearrange("p (h d) -> p h d", h=nh), in0=buf.rearrange("p (h d) -> p h d", h=nh),
                                                                                     in1=Lt[:, o0:o0 + nh].unsqueeze(2).broadcast_to([128, nh, hd]), op=ALU.mult), [bk, "bc"], [("PT", slot)])
                        mm(psA[5][:, :], ONEROWB(n), PT[:, slot, :], firstv[0], n == 3 and pg == npg - 1, ["orowb", ("PT", slot)], [PA[5]])
                        firstv[0] = False

            dense_attn("k", "v", 64, NH, HD, qs, ("cT", 1), True)
            op("dve", lambda e: e.tensor_tensor(out=ss[:, 8:16], in0=ss[:, 8:16], in1=nsel[:, 0, 0:8], op=ALU.add), ["ss8", "nsel"], ["ss8"])
            op("dve", lambda e: e.tensor_scalar(out=ss[:, 8:16], in0=ss[:, 8:16], scalar1=1e-30, scalar2=None, op0=ALU.add), ["ss8"], ["ss8"])
            op("dve", lambda e: e.reciprocal(out=ss[:, 8:16], in_=ss[:, 8:16]), ["ss8"], ["ss8"])
            op("dve", lambda e: e.tensor_tensor(out=tmpB[:, :].rearrange("p (h d) -> p h d", h=NH), in0=vs.rearrange("p (h d) -> p h d", h=NH),
                                                in1=nsel[:, 0, 0:8].unsqueeze(2).broadcast_to([128, NH, HD]), op=ALU.mult), ["uT", "nsel"], ["tmpB"])
            op("dve", lambda e: e.tensor_tensor(out=tmpB[:, :], in0=tmpB[:, :], in1=psA[5][:, :], op=ALU.add), ["tmpB", PA[5]], ["tmpB"])
            op("dve", lambda e: e.tensor_tensor(out=xn[:, 0:512].rearrange("p (h d) -> p h d", h=NH), in0=tmpB[:, :].rearrange("p (h d) -> p h d", h=NH),
                                                in1=ss[:, 8:16].unsqueeze(2).broadcast_to([128, NH, HD]), op=ALU.mult), ["tmpB", "ss8"], ["xn"])
            dbgout(rden[0:8, 0:256], [8, 256], ["rden"])
            dbgout(ss[0:4, 8:16], [4, 8], ["ss8"])
            dbgout(xn[0:4, 0:512], [4, 512], ["xn"])
            dbgout(bc[:, :], [128, 2048], ["bc"])
            dbgout(nsel[0:4, 0, 0:8], [4, 8], ["nsel"])
            dbgout(tmpB[0:4, :], [4, 512], ["tmpB"])
            for kc in range(4):
                tr(psT[:, kc * 128:(kc + 1) * 128], xn[:, kc * 128:(kc + 1) * 128], identb[:, :], ["xn", "identb"], ["psT"])
            op("dve", lambda e: e.tensor_copy(out=OT[:, :, 0:128], in_=psT[:, 0:512].rearrange("p (k t) -> p k t", k=4)), ["psT"], ["OT"])

            dense_attn("mk", "mv", 2, 4, 128, mqs, ("cT", 2), False)
            op("dve", lambda e: e.tensor_scalar(out=ss[:, 8:12], in0=ss[:, 8:12], scalar1=1e-30, scalar2=None, op0=ALU.add), ["ss8"], ["ss8"])
            op("dve", lambda e: e.reciprocal(out=ss[:, 8:12], in_=ss[:, 8:12]), ["ss8"], ["ss8"])
            op("dve", lambda e: e.tensor_tensor(out=xn[:, 0:512].rearrange("p (h d) -> p h d", h=4), in0=psA[5][:, :].rearrange("p (h d) -> p h d", h=4),
                                                in1=ss[:, 8:12].unsqueeze(2).broadcast_to([128, 4, 128]), op=ALU.mult), [PA[5], "ss8"], ["xn"])
            dbgout(xn[0:4, 0:512], [4, 512], ["xn"])
            for kc in range(4):
                tr(psT[:, kc * 128:(kc + 1) * 128], xn[:, kc * 128:(kc + 1) * 128], identb[:, :], ["xn", "identb"], ["psT"])
            op("dve", lambda e: e.tensor_copy(out=OmT[:, :, 0:128], in_=psT[:, 0:512].rearrange("p (k t) -> p k t", k=4)), ["psT"], ["OmT"])

            mg = uT[:, 1:3, :].rearrange("p a t -> p (a t)")[:, 0:1024]
            for half in range(2):
                srcs = ((wpa_v, sT, "sT"), (wpb_v, OT, "OT"), (wpc_v, OmT, "OmT"))
                for j in range(3):
                    wpv, src, skey = srcs[j]
                    Wp, kp = load_w(wpv[:, :, half * 512:(half + 1) * 512], 4, 512)
                    for kc in range(4):
                        mm(psA[j][:, :], src[:, kc, 0:128], Wp[:, kc, :], kc == 0, kc == 3, [kp, skey], [PA[j]])
                    col0 = 3072 + j * 1024 + half * 512
                    W, wk = load_w(win_v[:, :, col0:col0 + 512], 8, 512)
                    for kc in range(8):
                        mm(psA[3][:, :], hT[:, kc, 0:128], W[:, kc, :], kc == 0, kc == 7, ["hT", wk], [PA[3]])
                    bload(cbuf[:, 2, :], ("cbuf", 2), vrow_in[4, j * 1024 + half * 512:j * 1024 + half * 512 + 512])
                    op("dve", lambda e: e.tensor_tensor(out=tmpA[:, :], in0=psA[3][:, :], in1=cbuf[:, 2, :], op=ALU.add), [PA[3], ("cbuf", 2)], ["tmpA"])
                    op("act", lambda e: e.activation(out=tmpA[:, :], in_=tmpA[:, :], func=AF.Sigmoid), ["tmpA"], ["tmpA"])
                    if j == 0:
                        op("dve", lambda e, half=half: e.tensor_tensor(out=mg[:, half * 512:(half + 1) * 512], in0=psA[0][:, :], in1=tmpA[:, :], op=ALU.mult), [PA[0], "tmpA"], ["mg"])
                    else:
                        op("dve", lambda e, j=j: e.tensor_tensor(out=tmpB[:, :], in0=psA[j][:, :], in1=tmpA[:, :], op=ALU.mult), [PA[j], "tmpA"], ["tmpB"])
                        op("dve", lambda e, half=half: e.tensor_tensor(out=mg[:, half * 512:(half + 1) * 512], in0=mg[:, half * 512:(half + 1) * 512], in1=tmpB[:, :], op=ALU.add),
                           ["mg", "tmpB"], ["mg"])
            op("dve", lambda e: e.tensor_copy(out=xn[:, :], in_=mg), ["mg"], ["xn"])
            dbgout(xn[0:4, :], [4, 1024], ["xn"])
            for kc in range(8):
                tr(psT[:, kc * 128:(kc + 1) * 128], xn[:, kc * 128:(kc + 1) * 128], identb[:, :], ["xn", "identb"], ["psT"])
            op("dve", lambda e: e.tensor_copy(out=merged[:, :, 0:128], in_=psT[:, :].rearrange("p (k t) -> p k t", k=8)), ["psT"], ["merged"])
            for half in range(2):
                W, wk = load_w(wout_v[:, :, half * 512:(half + 1) * 512], 8, 512)
                for kc in range(8):
                    mm(psA[0][:, :], merged[:, kc, 0:128], W[:, kc, :], kc == 0, kc == 7, ["merged", wk], [PA[0]])
                op("dve", lambda e, half=half: e.tensor_tensor(out=X[:, 0, half * 512:(half + 1) * 512], in0=X[:, 0, half * 512:(half + 1) * 512], in1=psA[0][:, :], op=ALU.add),
                   ["X", PA[0]], ["X"])

            dbgout(X[0:4, 0, :], [4, 1024], ["X"])
            norm_to_T(X, 1, FM_G2, hT, "hT", "X")
            cs = scrA[:, :].bitcast(F32)
            stf = Vloc[:, :, :, :].rearrange("p a b c -> p (a b c)").bitcast(F32)
            op("dve", lambda e: e.memset(stf[:, 0:1024], 0.0), [], ["stf"])
            for piece in range(11):
                c0 = piece * 512
                W, wk = load_w(wup_v[:, :, c0:c0 + 512], 8, 512)
                for kc in range(8):
                    mm(psA[piece % 2][:, :], hT[:, kc, 0:128], W[:, kc, :], kc == 0, kc == 7, ["hT", wk], [PA[piece % 2]])
                pp, pkk = psA[piece % 2], PA[piece % 2]
                op("act", lambda e, pp=pp: e.activation(out=tmpA[:, :], in_=pp[:, :], func=AF.Copy), [pkk], ["tmpA"])
                dma("sp", ffns_o[:, 1, c0:c0 + 512], tmpA[0:4, :], ["tmpA"], ["hbm_ffns"], "st_s4")
                dma("sp", stf[0:4, 0:512], stf_in[:, c0:c0 + 512], [], ["stf"], "ld_stf")
                dma("sp", stf[0:4, 512:1024], stf_in[:, NUP + c0:NUP + c0 + 512], [], ["stf"], "ld_stf")
                dma("sp", ffns_o[:, 0, c0:c0 + 512], stf[0:4, 512:1024], ["stf"], ["hbm_ffns"], "st_s5")
                bload(cbuf[:, 0, :], ("cbuf", 0), vrow_in[5, c0:c0 + 512])
                op("dve", lambda e, c0=c0: e.tensor_tensor(out=cs[:, c0:c0 + 512], in0=stf[:, 0:512], in1=cbuf[:, 0, :], op=ALU.mult), ["stf", ("cbuf", 0)], ["cs"])
                bload(cbuf[:, 1, :], ("cbuf", 1), vrow_in[6, c0:c0 + 512])
                op("dve", lambda e: e.tensor_tensor(out=tmpB[:, :], in0=stf[:, 512:1024], in1=cbuf[:, 1, :], op=ALU.mult), ["stf", ("cbuf", 1)], ["tmpB"])
                op("dve", lambda e, c0=c0: e.tensor_tensor(out=cs[:, c0:c0 + 512], in0=cs[:, c0:c0 + 512], in1=tmpB[:, :], op=ALU.add), [("sa", 0), "tmpB"], ["cs"])
                bload(cbuf[:, 2, :], ("cbuf", 2), vrow_in[7, c0:c0 + 512])
                op("dve", lambda e: e.tensor_tensor(out=tmpB[:, :], in0=tmpA[:, :], in1=cbuf[:, 2, :], op=ALU.mult), ["tmpA", ("cbuf", 2)], ["tmpB"])
                op("dve", lambda e, c0=c0: e.tensor_tensor(out=cs[:, c0:c0 + 512], in0=cs[:, c0:c0 + 512], in1=tmpB[:, :], op=ALU.add), [("sa", 0), "tmpB"], ["cs"])
                bload(cbuf[:, 0, :], ("cbuf", 0), vrow_in[8, c0:c0 + 512])
                op("dve", lambda e, c0=c0: e.tensor_tensor(out=cs[:, c0:c0 + 512], in0=cs[:, c0:c0 + 512], in1=cbuf[:, 0, :], op=ALU.add), [("sa", 0), ("cbuf", 0)], ["cs"])
            actb = hT[:, 0:6, :].rearrange("p a t -> p (a t)")
            for c0 in range(0, DFF, 512):
                w = min(512, DFF - c0)
                op("act", lambda e, c0=c0, w=w: e.activation(out=tmpA[:, 0:w], in_=cs[:, c0:c0 + w], func=AF.Silu), ["cs"], ["tmpA"])
                op("dve", lambda e, c0=c0, w=w: e.tensor_tensor(out=actb[:, c0:c0 + w], in0=tmpA[:, 0:w], in1=cs[:, DFF + c0:DFF + c0 + w], op=ALU.mult), ["tmpA", ("sa", 0)], ["hT"])
            actT = qTa_s = None
            aT = sT[:, :, :].rearrange("p a t -> p (a t)")
            aT2 = OT[:, :, :].rearrange("p a t -> p (a t)")
            for grp in range(3):
                k0 = grp * 8
                nk = min(8, 22 - k0)
                for kk in range(nk):
                    tr(psT[:, kk * 128:(kk + 1) * 128], actb[:, (k0 + kk) * 128:(k0 + kk + 1) * 128], identb[:, :], ["hT", "identb"], ["psT"])
                if grp < 2:
                    op("dve", lambda e, k0=k0, nk=nk: e.tensor_copy(out=aT[:, k0 * 128:(k0 + nk) * 128], in_=psT[:, 0:nk * 128]), ["psT"], ["sT"])
                else:
                    op("dve", lambda e, nk=nk: e.tensor_copy(out=aT2[:, 0:nk * 128], in_=psT[:, 0:nk * 128]), ["psT"], ["OT"])

            def aTk(kc):
                return aT[:, kc * 128:(kc + 1) * 128] if kc < 16 else aT2[:, (kc - 16) * 128:(kc - 15) * 128]

            for half in range(2):
                for kp, (k0, k1) in enumerate(((0, 8), (8, 16), (16, 22))):
                    W, wk = load_w(wdn_v[:, k0:k1, half * 512:(half + 1) * 512], k1 - k0, 512)
                    for kc in range(k0, k1):
                        mm(psA[2][:, :], aTk(kc), W[:, kc - k0, :], kc == 0, kc == 21, ["sT", "OT", wk], [PA[2]])
                op("dve", lambda e, half=half: e.tensor_tensor(out=X[:, 0, half * 512:(half + 1) * 512], in0=X[:, 0, half * 512:(half + 1) * 512], in1=psA[2][:, :], op=ALU.add),
                   ["X", PA[2]], ["X"])
            dma("sp", ys_o[:, :], X[0:4, 0, :], ["X"], ["hbm_ys"], "st_y")

        allhist = [("hist", j) for j in range(44)]
        op("pe", lambda e: e.transpose(out=psM[0:88, 0:128], in_=hist[:, :, :].rearrange("p j r -> p (j r)"), identity=identf[:, :]), allhist + ["identf"], ["psM"])
        op("dve", lambda e: e.tensor_copy(out=outst[0:88, 0:128], in_=psM[0:88, 0:128]), ["psM"], ["outst"])
        dma("sp", ffnc_o[:, :], outst[0:88, 0:128], ["outst"], ["hbm_ffnc"], "st_misc")

        S.emit(st)
    return nc


def host_tables(inp):
    f = np.float32
    fm = np.zeros((128, FM_N), f)

    def fmaj(v):
        return np.ascontiguousarray(np.asarray(v, f).reshape(-1, 128).T)

    fm[:, FM_G1:FM_G1 + 8] = fmaj(inp["norm1_g"])
    fm[:, FM_G2:FM_G2 + 8] = fmaj(inp["norm2_g"])
    fm[:, FM_GM:FM_GM + 8] = fmaj(inp["mem_norm_g"])
    fm[:, FM_BG:FM_BG + 24] = fmaj(inp["b_gate"])
    wda = np.asarray(inp["w_dw_a"], f)
    fm[:, FM_WDA:FM_WDA + 124] = wda.T.reshape(4, 128, 31).transpose(1, 0, 2).reshape(128, 124)
    fm[:, FM_BDA:FM_BDA + 4] = fmaj(inp["b_dw_a"])
    fm[:, FM_LAG:FM_LAG + 4] = fmaj(inp["ln_a_g"])
    fm[:, FM_LAB:FM_LAB + 4] = fmaj(inp["ln_a_b"])
    wdf = np.asarray(inp["w_dw_f"], f)
    fm[:, FM_WDF:FM_WDF + 132] = wdf.T.reshape(44, 128, 3).transpose(1, 0, 2).reshape(128, 132)
    fm[:, FM_BDF:FM_BDF + 44] = fmaj(inp["b_dw_f"])
    bc = np.zeros((128, BC_N), f)
    bc[:, BC_GQ:BC_GQ + 512] = np.tile(np.asarray(inp["q_norm_g"], f), 8)[None, :]
    bc[:, BC_GK:BC_GK + 512] = np.tile(np.asarray(inp["k_norm_g"], f), 8)[None, :]
    bc[:, BC_GMQ:BC_GMQ + 512] = np.tile(np.asarray(inp["mq_norm_g"], f), 4)[None, :]
    bc[:, BC_GMK:BC_GMK + 512] = np.tile(np.asarray(inp["mk_norm_g"], f), 4)[None, :]
    ident = np.eye(128, dtype=f)
    p = np.arange(128)
    tri = np.where(p[:, None] > p[None, :], f(NEG), f(0)).astype(f)
    slopes = (2.0 ** (-np.arange(1, NH + 1, dtype=np.float64))).astype(f)
    j = np.arange(32)
    abase = (slopes[None, :, None] * (p[:, None, None] + 128.0 * (j[None, None, :] - 28))).astype(f).reshape(128, NH * 32)
    dt = (np.arange(4)[None, :] * 128 + p[:, None]).astype(np.int64)
    hi = (16 * (dt // 16)).astype(f)
    lo = (dt % 16).astype(f)
    qal = np.stack([-slopes[None, None, :] * hi[:, :, None], -slopes[None, None, :] * lo[:, :, None]], axis=-1).astype(f).reshape(128, 64)
    return {"fm": fm, "bc": bc, "ident": ident, "tri": tri, "abase": abase, "qal": qal}


_NC_CACHE = {}


def run_prompt(inp, nch=NCHUNK):
    key = ("p", nch)
    if key not in _NC_CACHE:
        _NC_CACHE[key] = build(nch=nch, do_sample=False)
    nc = _NC_CACHE[key]
    tabs = host_tables(inp)
    f = np.float32
    wnames = ["w_in", "w_proj_a", "w_proj_b", "w_proj_c", "w_mem_kv", "w_out", "w_up", "w_down"]
    shared = {n: np.ascontiguousarray(np.asarray(inp[n], f)) for n in wnames}
    shared.update(tabs)
    in_maps = []
    for c in range(8):
        s = c % 4
        m = dict(shared)
        m["x"] = np.ascontiguousarray(np.asarray(inp["x_prompt"][s], f))
        m["mem"] = np.ascontiguousarray(np.asarray(inp["mem_prompt"][s], f))
        in_maps.append(m)
    res = run_bass_kernel_spmd(nc, in_maps, core_ids=list(range(8)))
    return res.results


def sample_tables(inp):
    f = np.float32
    sc = np.zeros((128, SC_N), f)
    for n in range(4):
        sc[n, SC_RROW + n * 128:SC_RROW + (n + 1) * 128] = 1.0
    sc[:, SC_CS + 3] = 1.0
    sc[:, SC_ST + 127] = 1.0
    for k in range(4):
        sc[k, SC_REP + k * 32:SC_REP + (k + 1) * 32] = 1.0
    for h in range(8):
        sc[h, SC_E8 + h * 128:SC_E8 + (h + 1) * 128] = 1.0
    slopes = (2.0 ** (-np.arange(1, NH + 1, dtype=np.float64)))
    p = np.arange(128)[:, None, None]
    pg = np.arange(64)[None, :, None]
    sc[:, SC_AL:SC_AL + 512] = (-slopes[None, None, :] * (8192.0 - 128.0 * pg - p)).astype(f).reshape(128, 512)
    sc[:, SC_PI] = np.arange(128, dtype=f)
    for n in range(4):
        sc[n * 30:(n + 1) * 30, SC_IND + n] = 1.0
    vr = np.zeros((9, 5632), f)
    vr[0, :512] = np.asarray(inp["w_dw_a"], f)[30]
    vr[1, :512] = np.asarray(inp["b_dw_a"], f)
    vr[2, :512] = np.asarray(inp["ln_a_g"], f)
    vr[3, :512] = np.asarray(inp["ln_a_b"], f)
    vr[4, :3072] = np.asarray(inp["b_gate"], f)
    vr[5:8, :] = np.asarray(inp["w_dw_f"], f)
    vr[8, :] = np.asarray(inp["b_dw_f"], f)
    w120 = np.ascontiguousarray(np.tile(np.asarray(inp["w_dw_a"], f)[:30], (4, 1)))
    return sc, vr, w120


def run_all(inp):
    key = ("all",)
    if key not in _NC_CACHE:
        _NC_CACHE[key] = build(nch=NCHUNK, do_sample=True)
    nc = _NC_CACHE[key]
    tabs = host_tables(inp)
    f = np.float32
    wnames = ["w_in", "w_proj_a", "w_proj_b", "w_proj_c", "w_mem_kv", "w_out", "w_up", "w_down"]
    shared = {n: np.ascontiguousarray(np.asarray(inp[n], f)) for n in wnames}
    shared.update(tabs)
    sc, vr, w120 = sample_tables(inp)
    shared["sconst"] = sc
    shared["vrows"] = vr
    shared["w120"] = w120
    shared["cache_k"] = np.ascontiguousarray(np.asarray(inp["cache_k"], f)).reshape(N_POOL * 128, 512)
    shared["cache_v"] = np.ascontiguousarray(np.asarray(inp["cache_v"], f)).reshape(N_POOL * 128, 512)
    pt = np.asarray(inp["page_table"]).astype(np.int32)
    in_maps = []
    for c in range(8):
        s = c % 4
        m = dict(shared)
        m["x"] = np.ascontiguousarray(np.asarray(inp["x_prompt"][s], f))
        m["mem"] = np.ascontiguousarray(np.asarray(inp["mem_prompt"][s], f))
        sl = slice(4 * c, 4 * c + 4)
        xs = np.zeros((128, D), f)
        xs[0:4] = np.asarray(inp["x_sample"], f)[sl, 0, :]
        m["xs"] = xs
        m["stc"] = np.ascontiguousarray(np.asarray(inp["state_conv"], f)[sl].reshape(120, 512))
        m["stf"] = np.ascontiguousarray(np.asarray(inp["state_ffn_conv"], f)[sl].reshape(4, 2 * NUP))
        m["cmk"] = np.ascontiguousarray(np.asarray(inp["cache_mem_k"], f)[sl].reshape(4, 256, 512))
        m["cmv"] = np.ascontiguousarray(np.asarray(inp["cache_mem_v"], f)[sl].reshape(4, 256, 512))
        m["ptrep"] = np.ascontiguousarray(np.broadcast_to(pt[sl].reshape(1, 256), (128, 256))).astype(np.int32)
        in_maps.append(m)
    res = run_bass_kernel_spmd(nc, in_maps, core_ids=list(range(8)))
    return res.results


def kernel(**inp):
    r = run_all(inp)
    f = np.float32
    y_p = np.stack([r[s]["y"] for s in range(4)]).astype(f)
    k_p = np.stack([r[s]["k"] for s in range(4)]).reshape(4, SEQ, NH, HD).astype(f)
    v_p = np.stack([r[s]["v"] for s in range(4)]).reshape(4, SEQ, NH, HD).astype(f)
    conv_p = np.stack([r[s]["conv"][2:32] for s in range(4)]).astype(f)
    ffn_p = np.stack([r[s]["ffnc"].reshape(44, 2, 128).transpose(1, 0, 2).reshape(2, NUP) for s in range(4)]).astype(f)
    mk_p = np.stack([r[s]["mk"] for s in range(4)]).reshape(4, 256, 4, 128).astype(f)
    mv_p = np.stack([r[s]["mv"] for s in range(4)]).reshape(4, 256, 4, 128).astype(f)
    y_s = np.concatenate([r[c]["ys"] for c in range(8)]).reshape(32, 1, D).astype(f)
    k_s = np.concatenate([r[c]["ks"] for c in range(8)]).reshape(32, 1, NH, HD).astype(f)
    v_s = np.concatenate([r[c]["vs"] for c in range(8)]).reshape(32, 1, NH, HD).astype(f)
    conv_s = np.concatenate([r[c]["convs"] for c in range(8)]).reshape(32, 30, 512).astype(f)
    ffn_s = np.concatenate([r[c]["ffns"] for c in range(8)]).reshape(32, 2, NUP).astype(f)
    return (y_p, y_s, k_p, v_p, k_s, v_s, conv_p, conv_s, ffn_p, ffn_s, mk_p, mv_p)
```

```python
import contextlib
import numpy as np
import concourse.bass as bass
import concourse.mybir as mybir
from concourse.bass_utils import run_bass_kernel_spmd

F32 = mybir.dt.float32
BF16 = mybir.dt.bfloat16
I32 = mybir.dt.int32
AF = mybir.ActivationFunctionType
ALU = mybir.AluOpType
AX = mybir.AxisListType

D = 1024
SEQ = 4096
NT = 512
NCHUNK = SEQ // NT
CONV_CH = 512
CW = 31
NH = 8
HD = 64
DFF = 2816
NUP = 2 * DFF
NIN = 6144
EPS = 1e-6
NEG = -30000.0
FFN_MUL_ENG = "dve"
CONV_POOL_CC = 0
CONV_PER_HEAD = (31 * (4 - CONV_POOL_CC) + 7) // 8
N_POOL = 2560


class Sched:
    ENGS = ("pe", "act", "dve", "pool", "sp")

    def __init__(self, nc):
        self.nc = nc
        self.q = {e: [] for e in self.ENGS}
        self.last_w = {}
        self.readers = {}
        self.dma_cnt = {}
        self.n_ops = 0
        self.alias = {}

    def op(self, eng, fn, reads=(), writes=(), dma=False, semkey=None):
        rec = {"fn": fn, "eng": eng, "deps": [], "need_inc": False, "dma": dma,
               "semkey": None, "dval": 0}
        self.n_ops += 1
        deps = []
        for k in reads:
            w = self.last_w.get(k)
            if w is not None:
                deps.append(w)
        for k in writes:
            w = self.last_w.get(k)
            if w is not None:
                deps.append(w)
            for r in self.readers.get(k, ()):
                deps.append(r)
            for a in self.alias.get(k, ()):
                w = self.last_w.get(a)
                if w is not None:
                    deps.append(w)
                for r in self.readers.get(a, ()):
                    deps.append(r)
        if dma:
            assert semkey is not None
            rec["semkey"] = semkey
            self.dma_cnt[semkey] = self.dma_cnt.get(semkey, 0) + 16
            rec["dval"] = self.dma_cnt[semkey]
            me = ("d", semkey, rec["dval"])
        else:
            me = ("c", rec)
        seen = set()
        for d in deps:
            if d[0] == "c":
                p = d[1]
                if p is rec:
                    continue
                if p["eng"] == eng and eng == "pe":
                    continue
                if id(p) in seen:
                    continue
                seen.add(id(p))
                p["need_inc"] = True
                rec["deps"].append(d)
            else:
                key = (d[1], d[2])
                if key in seen:
                    continue
                seen.add(key)
                rec["deps"].append(d)
        for k in writes:
            self.last_w[k] = me
            self.readers[k] = []
        for k in reads:
            if k in writes:
                continue
            self.readers.setdefault(k, []).append(me)
        self.q[eng].append(rec)
        return rec

    def emit(self, st):
        nc = self.nc
        esem = {e: st.enter_context(nc.semaphore("e_" + e)) for e in self.ENGS}
        dsem = {}
        for i, k in enumerate(self.dma_cnt):
            dsem[k] = st.enter_context(nc.semaphore("d%d" % i))
        for e in self.ENGS:
            c = 0
            for rec in self.q[e]:
                if rec["need_inc"] and not rec["dma"]:
                    c += 1
                    rec["inc_val"] = c
        block = st.enter_context(nc.Block())
        hw = {"pe": nc.tensor, "act": nc.scalar, "dve": nc.vector, "pool": nc.gpsimd, "sp": nc.sync}

        def run(e):
            eng = hw[e]
            waited = {}
            for rec in self.q[e]:
                for d in rec["deps"]:
                    if d[0] == "c":
                        p = d[1]
                        sem = esem[p["eng"]]
                        val = p["inc_val"]
                        key = ("e", p["eng"])
                    else:
                        sem = dsem[d[1]]
                        val = d[2]
                        key = ("d", d[1])
                    if waited.get(key, 0) >= val:
                        continue
                    waited[key] = val
                    eng.wait_ge(sem, val)
                ins = rec["fn"](eng)
                if rec["dma"]:
                    ins.then_inc(dsem[rec["semkey"]], 16)
                elif rec["need_inc"]:
                    ins.then_inc(esem[e], 1)
            if e == "sp":
                for k, v in self.dma_cnt.items():
                    eng.wait_ge(dsem[k], v)

        block.tensor(lambda _e: run("pe"))
        block.scalar(lambda _e: run("act"))
        block.vector(lambda _e: run("dve"))
        block.gpsimd(lambda _e: run("pool"))
        block.sync(lambda _e: run("sp"))


FM_G1, FM_G2, FM_GM, FM_BG, FM_WDA, FM_BDA, FM_LAG, FM_LAB, FM_WDF, FM_BDF = 0, 8, 16, 24, 48, 172, 176, 180, 184, 316
FM_N = 360
BC_GQ, BC_GK, BC_GMQ, BC_GMK = 0, 512, 1024, 1536
BC_N = 2048


SC_RROW, SC_CS, SC_ST, SC_REP, SC_E8, SC_AL, SC_PI, SC_IND = 0, 512, 643, 898, 1026, 2050, 2562, 2563
SC_N = 2691


def build(nch=NCHUNK, do_sample=True, dbg=False):
    nc = bass.Bass("TRN2", target_bir_lowering=False)

    def din(name, shape, dt=F32):
        return nc.dram_tensor(name, list(shape), dt, kind="ExternalInput").ap()

    def dout(name, shape, dt=F32):
        return nc.dram_tensor(name, list(shape), dt, kind="ExternalOutput").ap()

    xin = din("x", [SEQ, D])
    memin = din("mem", [256, D])
    w_in = din("w_in", [D, NIN])
    w_pa = din("w_proj_a", [512, D])
    w_pb = din("w_proj_b", [512, D])
    w_pc = din("w_proj_c", [512, D])
    w_mkv = din("w_mem_kv", [D, D])
    w_out = din("w_out", [D, D])
    w_up = din("w_up", [D, NUP])
    w_down = din("w_down", [DFF, D])
    fm_in = din("fm", [128, FM_N])
    bc_in = din("bc", [128, BC_N])
    ident_in = din("ident", [128, 128])
    tri_in = din("tri", [128, 128])
    abase_in = din("abase", [128, NH * 32])
    qal_in = din("qal", [128, 4 * NH * 2])

    if do_sample:
        xs_in = din("xs", [128, D])
        stc_in = din("stc", [120, 512])
        w120_in = din("w120", [120, 512])
        stf_in = din("stf", [4, 2 * NUP])
        cmk_in = din("cmk", [4, 256, 512])
        cmv_in = din("cmv", [4, 256, 512])
        pt_in = din("ptrep", [128, 256], I32)
        ck_in = din("cache_k", [N_POOL * 128, 512])
        cv_in = din("cache_v", [N_POOL * 128, 512])
        sc_in = din("sconst", [128, SC_N])
        vrow_in = din("vrows", [9, 5632])
        ys_o = dout("ys", [4, D])
        ks_o = dout("ks", [4, 512])
        vs_o = dout("vs", [4, 512])
        convs_o = dout("convs", [4, 30, 512])
        ffns_o = dout("ffns", [4, 2, NUP])
    y_o = dout("y", [SEQ, D])
    k_o = dout("k", [SEQ, 512])
    v_o = dout("v", [SEQ, 512])
    conv_o = dout("conv", [32, 512])
    ffnc_o = dout("ffnc", [88, 128])
    mk_o = dout("mk", [256, 512])
    mv_o = dout("mv", [256, 512])
    kt_scr = nc.dram_tensor("kt_scr", [NH, 82, SEQ], BF16, kind="Internal").ap()
    v_scr = nc.dram_tensor("v_scr", [NH, SEQ // 128, 128, 128], BF16, kind="Internal").ap()

    st = contextlib.ExitStack()
    with st:
        def sb(name, shape, dt):
            return st.enter_context(nc.sbuf_tensor("s_" + name, list(shape), dt))

        def psum(name, shape, dt):
            return st.enter_context(nc.psum_tensor("p_" + name, list(shape), dt))

        S = Sched(nc)
        op = S.op
        dbg_n = [0]

        def dbgout(src_ap, shape, keys, dt=F32):
            if not dbg:
                return
            name = "dbg%d" % dbg_n[0]
            dbg_n[0] += 1
            t = nc.dram_tensor(name, list(shape), dt, kind="ExternalOutput").ap()
            op("pool", lambda e: e.dma_start(out=t, in_=src_ap), reads=keys, writes=["hbm_" + name], dma=True, semkey="dbg")

        identb = sb("identb", [128, 128], BF16)
        identf = sb("identf", [128, 128], F32)
        trib = sb("trib", [128, 128], BF16)
        onesb = sb("onesb", [128, 128], BF16)
        onesf = sb("onesf", [128, 128], F32)
        fm = sb("fm", [128, FM_N], F32)
        bc = sb("bc", [128, BC_N], F32)
        btab = sb("btab", [128, NH * 32], F32)
        mvals = sb("mvals", [128, 8], F32)
        epsb = sb("epsb", [128, 1], F32)
        X0 = sb("X0", [128, 4, D], F32)
        Xb = [X0, X0]
        X = Xb[0]
        xn = sb("xn", [128, D], BF16)
        ss = sb("ss", [128, 16], F32)
        hT = sb("hT", [128, 8, NT], BF16)
        uT = sb("uT", [128, 4, 30 + NT], F32)
        cT = sb("cT", [128, 4, NT], F32)
        sT = sb("sT", [128, 4, NT], BF16)
        tmpA = sb("tmpA", [128, NT], F32)
        tmpB = sb("tmpB", [128, NT], F32)
        Qaug = sb("Qaug", [128, 4, NH, 82], BF16)
        Kaug = sb("Kaug", [128, 4, NH, 82], BF16)
        Vloc = sb("Vloc", [128, 4, NH, 128], BF16)
        mqT = sb("mqT", [128, 4, NT], BF16)
        qn_bf = sb("qn_bf", [128, 512], BF16)
        kn_bf = sb("kn_bf", [128, 2, 512], BF16)
        kst = sb("kst", [128, 2, 512], F32)
        vst = sb("vst", [128, 2, 512], F32)
        kmBD = sb("kmBD", [128, 4, 2, 16], BF16)
        gatef = sb("gatef", [128, NH, 16], F32)
        top8 = sb("top8", [128, NH, 8], F32)
        thr = sb("thr", [128, NH], F32)
        nsel = sb("nsel", [128, NH, 16], F32)
        PT = sb("PT", [128, 3, NT], BF16)
        OT = sb("OT", [128, 4, NT], BF16)
        OmT = sb("OmT", [128, 4, NT], BF16)
        rden = sb("rden", [128, NT], F32)
        mkT = sb("mkT", [128, 4, 256], BF16)
        mVa = sb("mVa", [128, 2, 512], BF16)
        merged = sb("merged", [128, 8, NT], BF16)
        scrA = sb("scrA", [128, 22 * NT], BF16)
        sa = scrA[:, :].rearrange("p (j t) -> p j t", j=22)
        KTloc = scrA[:, 0:4096].rearrange("p (j t) -> p j t", j=NH)
        qTa = scrA[:, 4096:8192].rearrange("p (j t) -> p j t", j=NH)
        mqn = scrA[:, 8192:10240].rearrange("p (j t) -> p j t", j=4)
        qTp = scrA[:, 10240:10752].rearrange("p (j t) -> p j t", j=4)
        cbuf = sb("cbuf", [128, 3, NT], F32)
        tmpC = cbuf[:, 0, :]
        tmpD = cbuf[:, 1, :]
        sig = cT
        junk = kn_bf[:, :, :].rearrange("p a t -> p (a t)")
        for _k in ("KTloc", "qTa", "mqn", "qTp"):
            S.alias[_k] = [("sa", j) for j in range(22)]
        for j in range(22):
            S.alias[("sa", j)] = ["KTloc", "qTa", "mqn", "qTp"]
        hist = sb("hist", [128, 44, 2], F32)
        corr = sb("corr", [128, 44, 2], F32)
        corrt = sb("corrt", [128, 44], F32)
        ring = sb("ring", [128, 4, 4096], BF16)
        gring = sb("gring", [128, 4, 1024], BF16)
        kvK = sb("kvK", [128, 4, NT], BF16)
        kvV = sb("kvV", [128, 4, 4, 128], BF16)
        outst = sb("outst", [128, 512], F32)
        idx_t = sb("idx_t", [128, 256], I32)
        orowb = sb("orowb", [128, 132], BF16)
        ptf = sb("ptf", [128, 256], F32)

        psA = [psum("psA%d" % i, [128, 512], F32) for i in range(6)]
        psT = psum("psT", [128, 1024], BF16)
        psM = psum("psM", [128, 512], F32)
        PA = ["psA%d" % i for i in range(6)]

        dma_uid = [0]

        def dma(eng, out, in_, reads, writes, semkey):
            w2 = []
            for w in writes:
                if isinstance(w, str) and w.startswith("hbm_"):
                    dma_uid[0] += 1
                    w2.append((w, dma_uid[0]))
                else:
                    w2.append(w)
            return op(eng, lambda e: e.dma_start(out=out, in_=in_), reads=reads, writes=w2, dma=True, semkey=semkey)

        def mm(out, lhsT, rhs, start, stop, reads, writes):
            return op("pe", lambda e: e.matmul(out, lhsT=lhsT, rhs=rhs, start=start, stop=stop, skip_group_check=True),
                      reads=reads, writes=writes)

        def tr(out, in_, ident, reads, writes):
            return op("pe", lambda e: e.transpose(out=out, in_=in_, identity=ident), reads=reads, writes=writes)

        rstate = {"i": 0, "g": 0}

        def load_w(view, nk, ncols):
            i = rstate["i"] % 4
            rstate["i"] += 1
            dst = ring[:, i, 0:nk * ncols].rearrange("p (k n) -> p k n", k=nk)
            dma("pool", dst, view, [], [("ring", i)], "ring%d" % i)
            return dst, ("ring", i)

        def load_g(view):
            i = rstate["g"] % 4
            rstate["g"] += 1
            dst = gring[:, i, :].rearrange("p (k n) -> p k n", k=8)
            dma("pool", dst, view, [], [("gring", i)], "gring%d" % i)
            return dst, ("gring", i)

        win_v = w_in.rearrange("(kc p) n -> p kc n", p=128)
        wup_v = w_up.rearrange("(kc p) n -> p kc n", p=128)
        wout_v = w_out.rearrange("(kc p) n -> p kc n", p=128)
        wdn_v = w_down.rearrange("(kc p) n -> p kc n", p=128)
        wmkv_v = w_mkv.rearrange("(kc p) n -> p kc n", p=128)
        wpa_v = w_pa.rearrange("(kc p) n -> p kc n", p=128)
        wpb_v = w_pb.rearrange("(kc p) n -> p kc n", p=128)
        wpc_v = w_pc.rearrange("(kc p) n -> p kc n", p=128)

        dma("sp", fm[:, :], fm_in[:, :], [], ["fm"], "c1")
        dma("sp", bc[:, :], bc_in[:, :], [], ["bc"], "c2")
        dma("sp", identf[:, :], ident_in[:, :], [], ["identf"], "c3")
        dma("sp", btab[:, :], abase_in[:, :], [], ["btab"], "c4")
        dma("sp", tmpA[:, 0:128], tri_in[:, :], [], ["tmpA"], "c5")
        dma("sp", tmpB[:, 0:64], qal_in[:, :], [], ["tmpB"], "c6")
        op("dve", lambda e: e.tensor_copy(out=identb[:, :], in_=identf[:, :]), ["identf"], ["identb"])
        op("dve", lambda e: e.tensor_copy(out=trib[:, :], in_=tmpA[:, 0:128]), ["tmpA"], ["trib"])
        op("dve", lambda e: e.memset(onesb[:, :], 1.0), [], ["onesb"])
        op("dve", lambda e: e.memset(onesf[:, :], 1.0), [], ["onesf"])
        op("dve", lambda e: e.memset(epsb[:, :], EPS), [], ["epsb"])
        op("dve", lambda e: e.memset(uT[:, :, :], 0.0), [], ["uT"])
        op("dve", lambda e: e.memset(hist[:, :, :], 0.0), [], ["hist"])
        op("dve", lambda e: e.memset(kmBD[:, :, :, :], 0.0), [], ["kmBD"])
        op("dve", lambda e: e.memset(gatef[:, :, :], -1e30), [], ["gatef"])
        op("dve", lambda e: e.memset(Vloc[:, :, :, 0:64], 1.0), [], ["Vloc"])
        op("dve", lambda e: e.memset(Kaug[:, :, :, 80:82], 1.0), [], ["Kaug"])
        op("dve", lambda e: e.tensor_copy(out=Qaug[:, :, :, 80:82],
                                          in_=tmpB[:, 0:64].rearrange("p (s h r) -> p s h r", s=4, h=NH)),
           ["tmpB"], ["Qaug"])
        for j, (off, n) in enumerate(((BC_GQ, 64), (BC_GK, 64), (BC_GMQ, 128), (BC_GMK, 128))):
            op("dve", lambda e, j=j, off=off, n=n: e.tensor_reduce(out=mvals[:, j:j + 1], in_=bc[:, off:off + n], axis=AX.X,
                                                                  op=ALU.max, apply_absolute_value=True),
               ["bc"], ["mvals"])
        op("dve", lambda e: e.tensor_tensor(out=mvals[:, 4:5], in0=mvals[:, 0:1], in1=mvals[:, 1:2], op=ALU.mult), ["mvals"], ["mvals"])
        op("dve", lambda e: e.tensor_scalar(out=mvals[:, 4:5], in0=mvals[:, 4:5], scalar1=8.0, scalar2=None, op0=ALU.mult), ["mvals"], ["mvals"])
        op("dve", lambda e: e.tensor_tensor(out=mvals[:, 5:6], in0=mvals[:, 2:3], in1=mvals[:, 3:4], op=ALU.mult), ["mvals"], ["mvals"])
        op("dve", lambda e: e.tensor_scalar(out=mvals[:, 5:6], in0=mvals[:, 5:6], scalar1=-(128.0 ** 0.5), scalar2=None, op0=ALU.mult), ["mvals"], ["mvals"])
        op("dve", lambda e: e.tensor_scalar(out=btab[:, :], in0=btab[:, :], scalar1=mvals[:, 4:5], scalar2=None, op0=ALU.subtract),
           ["btab", "mvals"], ["btab"])
        op("dve", lambda e: e.tensor_scalar(out=bc[:, BC_GQ:BC_GQ + 512], in0=bc[:, BC_GQ:BC_GQ + 512], scalar1=HD ** -0.5, scalar2=None, op0=ALU.mult), ["bc"], ["bc"])
        op("dve", lambda e: e.tensor_scalar(out=bc[:, BC_GMQ:BC_GMQ + 512], in0=bc[:, BC_GMQ:BC_GMQ + 512], scalar1=128.0 ** -0.5, scalar2=None, op0=ALU.mult), ["bc"], ["bc"])

        def XK(b):
            return [("X", 0, sub) for sub in range(4)]

        def rms_rows(src3, sub, col, srckey="X"):
            op("dve", lambda e: e.memset(ss[:, col:col + 1], 0.0), [], [("ss", col)])
            op("act", lambda e: e.activation(out=junk[:, :], in_=src3[:, sub, :], func=AF.Square, accum_out=ss[:, col:col + 1]),
               [srckey, ("ss", col)], ["kn_bf", ("ss", col)])
            op("act", lambda e: e.activation(out=ss[:, col:col + 1], in_=ss[:, col:col + 1], func=AF.Sqrt, bias=epsb[:, 0:1], scale=1.0 / D),
               [("ss", col), "epsb"], [("ss", col)])
            op("dve", lambda e: e.reciprocal(out=ss[:, col:col + 1], in_=ss[:, col:col + 1]), [("ss", col)], [("ss", col)])

        def norm_A(src3, sub, srckey):
            op("dve", lambda e: e.tensor_scalar(out=xn[:, :], in0=src3[:, sub, :], scalar1=ss[:, sub:sub + 1], scalar2=None, op0=ALU.mult),
               [srckey, ("ss", sub)], ["xn"])

        def norm_B(sub, goff, dstT, dkey):
            for kc in range(8):
                tr(psT[:, kc * 128:(kc + 1) * 128], xn[:, kc * 128:(kc + 1) * 128], identb[:, :], ["xn", "identb"], ["psT"])
            op("dve", lambda e: e.tensor_tensor(
                out=dstT[:, :, sub * 128:(sub + 1) * 128], in0=psT[:, :].rearrange("p (k t) -> p k t", k=8),
                in1=fm[:, goff:goff + 8].unsqueeze(2).broadcast_to([128, 8, 128]), op=ALU.mult),
               ["psT", "fm"], [dkey])

        def norm_to_T(src3, nsub, goff, dstT, dkey, srckey):
            keys = srckey if isinstance(srckey, list) else [srckey] * nsub
            for sub in range(nsub):
                rms_rows(src3, sub, sub, keys[sub])
            for sub in range(nsub):
                norm_A(src3, sub, keys[sub])
                norm_B(sub, goff, dstT, dkey)

        def headnorm(ps, pkey, nh, hd, goff, out_ap, okey, extra_reads=()):
            op("act", lambda e: e.activation(out=tmpA[:, :], in_=ps[:, :], func=AF.Square), [pkey], ["tmpA"])
            op("dve", lambda e: e.tensor_reduce(out=ss[:, 8:8 + nh], in_=tmpA[:, :].rearrange("p (h d) -> p h d", h=nh), axis=AX.X, op=ALU.add),
               ["tmpA"], ["ss8"])
            op("act", lambda e: e.activation(out=ss[:, 8:8 + nh], in_=ss[:, 8:8 + nh], func=AF.Sqrt, bias=epsb[:, 0:1], scale=1.0 / hd),
               ["ss8", "epsb"], ["ss8"])
            op("dve", lambda e: e.reciprocal(out=ss[:, 8:8 + nh], in_=ss[:, 8:8 + nh]), ["ss8"], ["ss8"])
            op("dve", lambda e: e.tensor_tensor(out=tmpB[:, :].rearrange("p (h d) -> p h d", h=nh), in0=ps[:, :].rearrange("p (h d) -> p h d", h=nh),
                                                in1=ss[:, 8:8 + nh].unsqueeze(2).broadcast_to([128, nh, hd]), op=ALU.mult),
               [pkey, "ss8"], ["tmpB"])
            op("dve", lambda e: e.tensor_tensor(out=out_ap, in0=tmpB[:, :].rearrange("p (h d) -> p h d", h=nh),
                                                in1=bc[:, goff:goff + 512].rearrange("p (h d) -> p h d", h=nh), op=ALU.mult),
               ["tmpB", "bc"] + list(extra_reads), [okey])

        dma("sp", Xb[1][:, 0:2, :], memin.rearrange("(s p) d -> p s d", p=128), [], XK(1)[0:2], "xload2")
        norm_to_T(Xb[1], 2, FM_GM, hT, "hT", XK(1)[0:2])
        for half in range(2):
            W, wk = load_w(wmkv_v[:, :, half * 512:(half + 1) * 512], 8, 512)
            for sub in range(2):
                ps = psA[sub]
                for kc in range(8):
                    mm(ps[:, :], hT[:, kc, sub * 128:(sub + 1) * 128], W[:, kc, :], kc == 0, kc == 7, ["hT", wk], [PA[sub]])
                if half == 0:
                    headnorm(ps, PA[sub], 4, 128, BC_GMK, kst[:, sub, :].rearrange("p (h d) -> p h d", h=4), ("kst", sub))
                    dma("sp", mk_o[sub * 128:(sub + 1) * 128, :], kst[:, sub, :], [("kst", sub)], ["hbm_mk"], "st_k%d" % sub)
                    op("dve", lambda e, sub=sub: e.tensor_copy(out=mqn[:, sub, :], in_=kst[:, sub, :]), [("kst", sub)], ["mqn"])
                    for mh in range(4):
                        tr(psT[:, mh * 128:(mh + 1) * 128], mqn[:, sub, mh * 128:(mh + 1) * 128], identb[:, :], ["mqn", "identb"], ["psT"])
                    op("dve", lambda e, sub=sub: e.tensor_copy(out=mkT[:, :, sub * 128:(sub + 1) * 128],
                                                               in_=psT[:, 0:512].rearrange("p (k t) -> p k t", k=4)), ["psT"], ["mkT"])
                else:
                    op("act", lambda e, sub=sub, ps=ps: e.activation(out=vst[:, sub, :], in_=ps[:, :], func=AF.Copy), [PA[sub]], [("vst", sub)])
                    dma("sp", mv_o[sub * 128:(sub + 1) * 128, :], vst[:, sub, :], [("vst", sub)], ["hbm_mv"], "st_v%d" % sub)
                    op("dve", lambda e, sub=sub: e.tensor_copy(out=mVa[:, sub, :], in_=vst[:, sub, :]), [("vst", sub)], ["mVa"])

        kvstate = {"i": 0}
        for c in range(nch):
            t0 = c * NT
            xb = c % 2
            X = Xb[xb]
            xks = XK(xb)
            dma("sp", X[:, :, :], xin[t0:t0 + NT, :].rearrange("(s p) d -> p s d", p=128), [], xks, "xload")
            norm_to_T(X, 4, FM_G1, hT, "hT", xks)

            Wa, wka = load_w(win_v[:, :, 0:512], 8, 512)
            Wg, wkg = load_w(win_v[:, :, 512:1024], 8, 512)
            for cc in range(4):
                pa, pg = psA[2 * (cc % 2)], psA[2 * (cc % 2) + 1]
                ka, kg = PA[2 * (cc % 2)], PA[2 * (cc % 2) + 1]
                for kc in range(8):
                    mm(pa[:, :], Wa[:, kc, cc * 128:(cc + 1) * 128], hT[:, kc, :], kc == 0, kc == 7, ["hT", wka], [ka])
                for kc in range(8):
                    mm(pg[:, :], Wg[:, kc, cc * 128:(cc + 1) * 128], hT[:, kc, :], kc == 0, kc == 7, ["hT", wkg], [kg])
                tk = ("cbuf", 0) if cc % 2 == 0 else ("cbuf", 1)
                tt = tmpC if cc % 2 == 0 else tmpD
                op("act", lambda e, pg=pg, tt=tt: e.activation(out=tt[:, :], in_=pg[:, :], func=AF.Sigmoid), [kg], [tk])
                op("dve", lambda e, pa=pa, tt=tt, cc=cc: e.tensor_tensor(out=uT[:, cc, 30:30 + NT], in0=pa[:, :], in1=tt[:, :], op=ALU.mult),
                   [ka, tk], ["uT"])
            conv_steps = []
            pool_steps = []
            for cc in range(4):
                ceng = "pool" if cc >= 4 - CONV_POOL_CC else "dve"
                clist = pool_steps if ceng == "pool" else conv_steps
                clist.append(lambda cc=cc, ceng=ceng: op(ceng, lambda e: e.tensor_scalar(
                    out=cT[:, cc, :], in0=uT[:, cc, 0:NT], scalar1=fm[:, FM_WDA + cc * 31:FM_WDA + cc * 31 + 1],
                    scalar2=fm[:, FM_BDA + cc:FM_BDA + cc + 1], op0=ALU.mult, op1=ALU.add), ["uT", "fm"], [("cT", cc)]))
                for k in range(1, CW):
                    clist.append(lambda cc=cc, k=k, ceng=ceng: op(ceng, lambda e: e.scalar_tensor_tensor(
                        out=cT[:, cc, :], in0=uT[:, cc, k:k + NT], scalar=fm[:, FM_WDA + cc * 31 + k:FM_WDA + cc * 31 + k + 1],
                        in1=cT[:, cc, :], op0=ALU.mult, op1=ALU.add), ["uT", "fm", ("cT", cc)], [("cT", cc)]))

            def conv_finish(c=c):
                if c == nch - 1:
                    for cc in range(4):
                        op("pe", lambda e, cc=cc: e.transpose(out=psM[0:32, cc * 128:(cc + 1) * 128], in_=uT[:, cc, 30 + NT - 32:30 + NT], identity=identf[:, :]),
                           ["uT", "identf"], ["psM"])
                    op("dve", lambda e: e.tensor_copy(out=outst[0:32, :], in_=psM[0:32, :]), ["psM"], ["outst"])
                    dma("sp", conv_o[:, :], outst[0:32, :], ["outst"], ["hbm_conv"], "st_misc")
                op("dve", lambda e: e.tensor_copy(out=uT[:, :, 0:30], in_=uT[:, :, NT:NT + 30]), ["uT"], ["uT"])
                for cc in range(4):
                    mm(psA[4][:, :], onesf[:, :], cT[:, cc, :], cc == 0, cc == 3, ["onesf", ("cT", cc)], [PA[4]])
                for cc in range(4):
                    tt, tk = (tmpC, ("cbuf", 0)) if cc % 2 == 0 else (tmpD, ("cbuf", 1))
                    op("act", lambda e, cc=cc, tt=tt: e.activation(out=tt[:, :], in_=cT[:, cc, :], func=AF.Square), [("cT", cc)], [tk])
                    mm(psA[5][:, :], onesf[:, :], tt[:, :], cc == 0, cc == 3, ["onesf", tk], [PA[5]])
                op("dve", lambda e: e.tensor_scalar(out=tmpA[:, :], in0=psA[4][:, :], scalar1=1.0 / 512, scalar2=None, op0=ALU.mult), [PA[4]], ["tmpA"])
                op("dve", lambda e: e.tensor_tensor(out=tmpB[:, :], in0=tmpA[:, :], in1=tmpA[:, :], op=ALU.mult), ["tmpA"], ["tmpB"])
                op("dve", lambda e: e.scalar_tensor_tensor(out=tmpB[:, :], in0=psA[5][:, :], scalar=1.0 / 512, in1=tmpB[:, :], op0=ALU.mult, op1=ALU.subtract),
                   [PA[5], "tmpB"], ["tmpB"])
                op("act", lambda e: e.activation(out=tmpB[:, :], in_=tmpB[:, :], func=AF.Sqrt, bias=epsb[:, 0:1], scale=1.0), ["tmpB", "epsb"], ["tmpB"])
                op("dve", lambda e: e.reciprocal(out=tmpB[:, :], in_=tmpB[:, :]), ["tmpB"], ["tmpB"])
                for cc in range(4):
                    op("dve", lambda e, cc=cc: e.tensor_tensor(out=cT[:, cc, :], in0=cT[:, cc, :], in1=tmpA[:, :], op=ALU.subtract), [("cT", cc), "tmpA"], [("cT", cc)])
                    op("dve", lambda e, cc=cc: e.tensor_tensor(out=cT[:, cc, :], in0=cT[:, cc, :], in1=tmpB[:, :], op=ALU.mult), [("cT", cc), "tmpB"], [("cT", cc)])
                    op("act", lambda e, cc=cc: e.activation(out=sT[:, cc, :], in_=cT[:, cc, :], func=AF.Silu, scale=fm[:, FM_LAG + cc:FM_LAG + cc + 1],
                                                            bias=fm[:, FM_LAB + cc:FM_LAB + cc + 1]), [("cT", cc), "fm"], ["sT"])

            op("dve", lambda e: e.memset(Kaug[:, :, :, 64:80], 0.0), [], ["Kaug"])
            op("dve", lambda e, c=c: e.memset(Kaug[:, 0:2, :, 64 + 2 * c:65 + 2 * c], 1.0), [], ["Kaug"])
            op("dve", lambda e, c=c: e.memset(Kaug[:, 2:4, :, 65 + 2 * c:66 + 2 * c], 1.0), [], ["Kaug"])
            kvqm = (("k", 1536), ("v", 2048), ("q", 1024), ("mq", 2560))
            wcur = {}

            def emit_mm(i):
                name, col0 = kvqm[i // 4]
                sub = i % 4
                if sub == 0:
                    wcur[name] = load_w(win_v[:, :, col0:col0 + 512], 8, 512)
                W, wk = wcur[name]
                ps, pk = psA[sub], PA[sub]
                for kc in range(8):
                    mm(ps[:, :], hT[:, kc, sub * 128:(sub + 1) * 128], W[:, kc, :], kc == 0, kc == 7, ["hT", wk], [pk])

            def post(i, c=c, t0=t0):
                name = kvqm[i // 4][0]
                sub = i % 4
                ps, pk = psA[sub], PA[sub]
                if name == "k":
                    headnorm(ps, pk, NH, HD, BC_GK, kst[:, sub % 2, :].rearrange("p (h d) -> p h d", h=NH), ("kst", sub % 2))
                    dma("sp", k_o[t0 + sub * 128:t0 + (sub + 1) * 128, :], kst[:, sub % 2, :], [("kst", sub % 2)], ["hbm_k"], "st_k%d" % (sub % 2))
                    op("dve", lambda e, sub=sub: e.tensor_copy(out=Kaug[:, sub, :, 0:64], in_=kst[:, sub % 2, :].rearrange("p (h d) -> p h d", h=NH)),
                       [("kst", sub % 2)], ["Kaug"])
                    op("dve", lambda e, sub=sub: e.tensor_copy(out=kn_bf[:, sub % 2, :], in_=kst[:, sub % 2, :]), [("kst", sub % 2)], ["kn_bf"])
                    for h in range(NH):
                        tr(psT[0:82, h * 128:(h + 1) * 128], Kaug[:, sub, h, :], identb[:, :], ["Kaug", "identb"], ["psT"])
                    op("dve", lambda e, sub=sub: e.tensor_copy(out=KTloc[0:82, :, sub * 128:(sub + 1) * 128],
                                                               in_=psT[0:82, :].rearrange("p (k t) -> p k t", k=NH)), ["psT"], ["KTloc"])
                    if sub % 2 == 1:
                        blk = 2 * c + sub // 2
                        for pair in range(4):
                            for s2 in range(2):
                                mm(psM[:, pair:pair + 1], kn_bf[:, s2, pair * 128:(pair + 1) * 128], onesb[:, 0:1], s2 == 0, s2 == 1,
                                   ["kn_bf", "onesb"], ["psM"])
                        op("dve", lambda e, blk=blk: e.tensor_scalar(out=kmBD[0:64, :, 0, blk], in0=psM[0:64, 0:4], scalar1=1.0 / 256, scalar2=None, op0=ALU.mult),
                           ["psM"], ["kmBD"])
                        op("dve", lambda e, blk=blk: e.tensor_scalar(out=kmBD[64:128, :, 1, blk], in0=psM[64:128, 0:4], scalar1=1.0 / 256, scalar2=None, op0=ALU.mult),
                           ["psM"], ["kmBD"])
                elif name == "v":
                    op("act", lambda e, sub=sub, ps=ps: e.activation(out=vst[:, sub % 2, :], in_=ps[:, :], func=AF.Copy), [pk], [("vst", sub % 2)])
                    dma("sp", v_o[t0 + sub * 128:t0 + (sub + 1) * 128, :], vst[:, sub % 2, :], [("vst", sub % 2)], ["hbm_v"], "st_v%d" % (sub % 2))
                    op("dve", lambda e, sub=sub: e.tensor_copy(out=Vloc[:, sub, :, 64:128], in_=vst[:, sub % 2, :].rearrange("p (h d) -> p h d", h=NH)),
                       [("vst", sub % 2)], ["Vloc"])
                elif name == "q":
                    cur = 2 * c + sub // 2
                    headnorm(ps, pk, NH, HD, BC_GQ, Qaug[:, sub, :, 0:64], "Qaug")
                    if cur == 0:
                        op("dve", lambda e, sub=sub: e.memset(Qaug[:, sub, :, 64:80], NEG), [], ["Qaug"])
                    else:
                        op("dve", lambda e, sub=sub: e.tensor_copy(out=qn_bf[:, :].rearrange("p (h d) -> p h d", h=NH), in_=Qaug[:, sub, :, 0:64]), ["Qaug"], ["qn_bf"])
                        for pair in range(4):
                            tr(psT[:, pair * 128:(pair + 1) * 128], qn_bf[:, pair * 128:(pair + 1) * 128], identb[:, :], ["qn_bf", "identb"], ["psT"])
                        op("dve", lambda e: e.tensor_copy(out=qTp[:, :, :], in_=psT[:, 0:512].rearrange("p (k t) -> p k t", k=4)), ["psT"], ["qTp"])
                        for pair in range(4):
                            mm(psM[:, 32 * pair:32 * pair + 32], qTp[:, pair, :], kmBD[:, pair, :, :].rearrange("p a b -> p (a b)"), True, True, ["qTp", "kmBD"], ["psM"])
                        op("dve", lambda e, cur=cur: e.tensor_copy(out=gatef[:, :, 0:cur], in_=psM[:, 0:128].rearrange("p (h b) -> p h b", h=NH)[:, :, 0:cur]),
                           ["psM"], ["gatef"])
                        for h in range(NH):
                            op("dve", lambda e, h=h: e.max(out=top8[:, h, :], in_=gatef[:, h, :]), ["gatef"], ["top8"])
                        op("dve", lambda e: e.tensor_scalar(out=thr[:, :], in0=top8[:, :, 2], scalar1=-1e29, scalar2=None, op0=ALU.max), ["top8"], ["thr"])
                        op("dve", lambda e: e.tensor_tensor(out=nsel[:, :, :], in0=gatef[:, :, :], in1=thr[:, :].unsqueeze(2).broadcast_to([128, NH, 16]), op=ALU.is_lt),
                           ["gatef", "thr"], ["nsel"])
                        op("dve", lambda e, sub=sub: e.tensor_scalar(out=Qaug[:, sub, :, 64:80], in0=nsel[:, :, :], scalar1=NEG, scalar2=None, op0=ALU.mult),
                           ["nsel"], ["Qaug"])
                    op("dve", lambda e, sub=sub, cur=cur: e.memset(Qaug[:, sub, :, 64 + cur:65 + cur], 0.0), [], ["Qaug"])
                    for h in range(NH):
                        tr(psT[0:82, h * 128:(h + 1) * 128], Qaug[:, sub, h, :], identb[:, :], ["Qaug", "identb"], ["psT"])
                    op("dve", lambda e, sub=sub: e.tensor_copy(out=qTa[0:82, :, sub * 128:(sub + 1) * 128],
                                                               in_=psT[0:82, :].rearrange("p (k t) -> p k t", k=NH)), ["psT"], ["qTa"])
                else:
                    headnorm(ps, pk, 4, 128, BC_GMQ, mqn[:, sub, :].rearrange("p (h d) -> p h d", h=4), "mqn")
                    for mh in range(4):
                        tr(psT[:, mh * 128:(mh + 1) * 128], mqn[:, sub, mh * 128:(mh + 1) * 128], identb[:, :], ["mqn", "identb"], ["psT"])
                    op("dve", lambda e, sub=sub: e.tensor_copy(out=mqT[:, :, sub * 128:(sub + 1) * 128],
                                                               in_=psT[:, 0:512].rearrange("p (k t) -> p k t", k=4)), ["psT"], ["mqT"])
                if name == "v" and sub == 3:
                    dma("sp", kt_scr[:, :, t0:t0 + NT].rearrange("h r k -> r h k"), KTloc[0:82, :, :], ["KTloc"], [("kscr", c)], "st_kscr")
                    for t4 in range(4):
                        dma("sp", v_scr[:, 4 * c + t4, :, :].rearrange("h k c -> k h c"), Vloc[:, t4, :, :], ["Vloc"], [("vscr", c)], "st_vscr")


            emit_mm(0)
            emit_mm(1)
            for i in range(16):
                if i + 2 < 16:
                    emit_mm(i + 2)
                post(i)

            nexp = 0
            steps = []
            for h in range(NH):
                for g in range(c + 1):
                    for tile in range(4):
                        steps.append((h, g, tile))
            LA = 2
            slots = {}

            def qk(i):
                h, g, tile = steps[i]
                if tile == 0:
                    sl = kvstate["i"] % 4
                    kvstate["i"] += 1
                    slots[(h, g)] = sl
                    dma("sp", kvK[0:82, sl, :], kt_scr[h, :, g * NT:(g + 1) * NT], [("kscr", g)], [("kvK", sl)], "kvK%d" % sl)
                    dma("sp", kvV[:, sl, :, :], v_scr[h, 4 * g:4 * g + 4, :, :].rearrange("t k c -> k t c"), [("vscr", g)], [("kvV", sl)], "kvV%d" % sl)
                sl = slots[(h, g)]
                diag = (g == c)
                col0 = 128 * tile if diag else 0
                sp_, sk = psA[2 + i % 4], PA[2 + i % 4]
                mm(sp_[:, col0:NT], kvK[0:82, sl, tile * 128:(tile + 1) * 128], qTa[0:82, h, col0:NT], True, True, [("kvK", sl), "qTa"], [sk])
                if diag:
                    mm(sp_[:, col0:col0 + 128], identb[:, :], trib[:, :], False, True, ["identb", "trib"], [sk])

            def expv(i):
                h, g, tile = steps[i]
                sl = slots[(h, g)]
                acc, ak = psA[h % 2], PA[h % 2]
                kt = 4 * g + tile
                diag = (g == c)
                col0 = 128 * tile if diag else 0
                sp_, sk = psA[2 + i % 4], PA[2 + i % 4]
                pslot = i % 3
                bcol = h * 32 + (kt - 4 * c + 28)
                op("act", lambda e: e.activation(out=PT[:, pslot, col0:NT], in_=sp_[:, col0:NT], func=AF.Exp, bias=btab[:, bcol:bcol + 1], scale=1.0),
                   [sk, "btab"], [("PT", pslot)])
                mm(acc[:, col0:NT], kvV[:, sl, tile, :], PT[:, pslot, col0:NT], g == 0 and tile == 0, diag and tile == 3, [("kvV", sl), ("PT", pslot)], [ak])
                if diag and tile == 3:
                    op("dve", lambda e: e.reciprocal(out=rden[0:64, :], in_=acc[0:64, :]), [ak], ["rden"])
                    op("dve", lambda e: e.tensor_tensor(out=OT[64 * (h % 2):64 * (h % 2) + 64, h // 2, :], in0=acc[64:128, :], in1=rden[0:64, :], op=ALU.mult),
                       [ak, "rden"], ["OT"])
                    for stp in conv_steps[CONV_PER_HEAD * h:CONV_PER_HEAD * (h + 1)]:
                        stp()

            for stp in pool_steps:
                stp()
            for i in range(min(LA, len(steps))):
                qk(i)
            for i in range(len(steps)):
                if i + LA < len(steps):
                    qk(i + LA)
                expv(i)
            nexp = len(steps)
            conv_finish()

            msteps = [(mh, t) for mh in range(4) for t in range(2)]

            def mqk(i):
                mh, t = msteps[i]
                sp_, sk = psA[4 + i % 2], PA[4 + i % 2]
                mm(sp_[:, :], mkT[:, mh, t * 128:(t + 1) * 128], mqT[:, mh, :], True, True, ["mkT", "mqT"], [sk])

            def mpv(i):
                mh, t = msteps[i]
                sp_, sk = psA[4 + i % 2], PA[4 + i % 2]
                pslot = i % 3
                o_ps, ok = psA[2 * (mh % 2)], PA[2 * (mh % 2)]
                d_ps, dk = psA[2 * (mh % 2) + 1], PA[2 * (mh % 2) + 1]
                op("act", lambda e: e.activation(out=PT[:, pslot, :], in_=sp_[:, :], func=AF.Exp, bias=mvals[:, 5:6], scale=1.0),
                   [sk, "mvals"], [("PT", pslot)])
                mm(o_ps[:, :], mVa[:, t, mh * 128:(mh + 1) * 128], PT[:, pslot, :], t == 0, t == 1, ["mVa", ("PT", pslot)], [ok])
                mm(d_ps[:, :], onesb[:, :], PT[:, pslot, :], t == 0, t == 1, ["onesb", ("PT", pslot)], [dk])
                if t == 1:
                    op("dve", lambda e: e.reciprocal(out=rden[:, :], in_=d_ps[:, :]), [dk], ["rden"])
                    op("dve", lambda e: e.tensor_tensor(out=OmT[:, mh, :], in0=o_ps[:, :], in1=rden[:, :], op=ALU.mult), [ok, "rden"], ["OmT"])

            mqk(0)
            for i in range(8):
                if i + 1 < 8:
                    mqk(i + 1)
                mpv(i)

            Wpa, kpa = load_w(wpa_v, 4, 1024)
            Wpb, kpb = load_w(wpb_v, 4, 1024)
            Wpc, kpc = load_w(wpc_v, 4, 1024)
            for dc in range(8):
                srcs = ((Wpa, kpa, sT, "sT"), (Wpb, kpb, OT, "OT"), (Wpc, kpc, OmT, "OmT"))
                for j in range(3):
                    Wp, kp, src, skey = srcs[j]
                    for kc in range(4):
                        mm(psA[j][:, :], Wp[:, kc, dc * 128:(dc + 1) * 128], src[:, kc, :], kc == 0, kc == 3, [kp, skey], [PA[j]])
                for j in range(3):
                    Wgt, kgt = load_g(win_v[:, :, 3072 + j * 1024 + dc * 128:3072 + j * 1024 + (dc + 1) * 128])
                    for kc in range(8):
                        mm(psA[3 + j][:, :], Wgt[:, kc, :], hT[:, kc, :], kc == 0, kc == 7, [kgt, "hT"], [PA[3 + j]])
                    op("act", lambda e, j=j, dc=dc: e.activation(out=sig[:, j, :], in_=psA[3 + j][:, :], func=AF.Sigmoid,
                                                                 bias=fm[:, FM_BG + j * 8 + dc:FM_BG + j * 8 + dc + 1], scale=1.0),
                       [PA[3 + j], "fm"], [("cT", j)])
                op("dve", lambda e: e.tensor_tensor(out=tmpA[:, :], in0=psA[0][:, :], in1=sig[:, 0, :], op=ALU.mult), [PA[0], ("cT", 0)], ["tmpA"])
                op("dve", lambda e: e.tensor_tensor(out=tmpB[:, :], in0=psA[1][:, :], in1=sig[:, 1, :], op=ALU.mult), [PA[1], ("cT", 1)], ["tmpB"])
                op("dve", lambda e: e.tensor_tensor(out=tmpA[:, :], in0=tmpA[:, :], in1=tmpB[:, :], op=ALU.add), ["tmpA", "tmpB"], ["tmpA"])
                op("dve", lambda e: e.tensor_tensor(out=tmpB[:, :], in0=psA[2][:, :], in1=sig[:, 2, :], op=ALU.mult), [PA[2], ("cT", 2)], ["tmpB"])
                op("dve", lambda e, dc=dc: e.tensor_tensor(out=merged[:, dc, :], in0=tmpA[:, :], in1=tmpB[:, :], op=ALU.add), ["tmpA", "tmpB"], ["merged"])
            Wo = [load_w(wout_v[:, :, half * 512:(half + 1) * 512], 8, 512) for half in range(2)]
            for sub in range(4):
                for half in range(2):
                    W, wk = Wo[half]
                    pb, pbk = psA[(2 * sub + half) % 4], PA[(2 * sub + half) % 4]
                    for kc in range(8):
                        mm(pb[:, :], merged[:, kc, sub * 128:(sub + 1) * 128], W[:, kc, :], kc == 0, kc == 7, ["merged", wk], [pbk])
                    op("dve", lambda e, sub=sub, half=half, pb=pb: e.tensor_tensor(out=X[:, sub, half * 512:(half + 1) * 512], in0=X[:, sub, half * 512:(half + 1) * 512],
                                                                                   in1=pb[:, :], op=ALU.add), [xks[sub], pbk], [xks[sub]])
                rms_rows(X, sub, sub, xks[sub])
                if sub >= 1:
                    norm_A(X, sub - 1, xks[sub - 1])
                    norm_B(sub - 1, FM_G2, hT, "hT")
            norm_A(X, 3, xks[3])
            norm_B(3, FM_G2, hT, "hT")

            wdf3 = fm[:, FM_WDF:FM_WDF + 132].rearrange("p (j k) -> p j k", k=3)
            allhist_k = [("hist", j) for j in range(44)]
            op("dve", lambda e: e.tensor_tensor(out=corr[:, :, 0], in0=hist[:, :, 0], in1=wdf3[:, :, 0], op=ALU.mult), allhist_k + ["fm"], ["corr"])
            op("dve", lambda e: e.tensor_tensor(out=corrt[:, :], in0=hist[:, :, 1], in1=wdf3[:, :, 1], op=ALU.mult), allhist_k + ["fm"], ["corrt"])
            op("dve", lambda e: e.tensor_tensor(out=corr[:, :, 0], in0=corr[:, :, 0], in1=corrt[:, :], op=ALU.add), ["corr", "corrt"], ["corr"])
            op("dve", lambda e: e.tensor_tensor(out=corr[:, :, 1], in0=hist[:, :, 1], in1=wdf3[:, :, 0], op=ALU.mult), allhist_k + ["fm"], ["corr"])
            op("dve", lambda e: e.tensor_tensor(out=corr[:, :, :], in0=corr[:, :, :], in1=fm[:, FM_BDF:FM_BDF + 44].unsqueeze(2).broadcast_to([128, 44, 2]), op=ALU.add),
               ["corr", "fm"], ["corr"])
            upw = {}

            def emit_up(j):
                piece, jj = j // 4, j % 4
                if jj == 0:
                    upw[piece] = load_w(wup_v[:, :, piece * 512:(piece + 1) * 512], 8, 512)
                W, wk = upw[piece]
                ps, pk = psA[j % 4], PA[j % 4]
                cb, ck = cbuf[:, j % 3, :], ("cbuf", j % 3)
                for kc in range(8):
                    mm(ps[:, :], W[:, kc, jj * 128:(jj + 1) * 128], hT[:, kc, :], kc == 0, kc == 7, [wk, "hT"], [pk])
                w2 = fm[:, FM_WDF + 3 * j + 2:FM_WDF + 3 * j + 3]
                bb = fm[:, FM_BDF + j:FM_BDF + j + 1]
                op("act", lambda e: e.activation(out=cb[:, 2:NT], in_=ps[:, 2:NT], func=AF.Identity, scale=w2, bias=bb), [pk, "fm"], [ck])
                op("act", lambda e: e.activation(out=cb[:, 0:1], in_=ps[:, 0:1], func=AF.Identity, scale=w2, bias=corr[:, j, 0:1]), [pk, "fm", "corr"], [ck])
                op("act", lambda e: e.activation(out=cb[:, 1:2], in_=ps[:, 1:2], func=AF.Identity, scale=w2, bias=corr[:, j, 1:2]), [pk, "fm", "corr"], [ck])
                op("act", lambda e: e.activation(out=hist[:, j, :], in_=ps[:, NT - 2:NT], func=AF.Copy), [pk], [("hist", j)])

            def chain_up(j):
                ps, pk = psA[j % 4], PA[j % 4]
                cb, ck = cbuf[:, j % 3, :], ("cbuf", j % 3)
                w0 = fm[:, FM_WDF + 3 * j:FM_WDF + 3 * j + 1]
                w1 = fm[:, FM_WDF + 3 * j + 1:FM_WDF + 3 * j + 2]
                op("dve", lambda e: e.scalar_tensor_tensor(out=cb[:, 1:NT], in0=ps[:, 0:NT - 1], scalar=w1, in1=cb[:, 1:NT], op0=ALU.mult, op1=ALU.add),
                   [pk, ck, "fm"], [ck])
                op("dve", lambda e: e.scalar_tensor_tensor(out=cb[:, 2:NT], in0=ps[:, 0:NT - 2], scalar=w0, in1=cb[:, 2:NT], op0=ALU.mult, op1=ALU.add),
                   [pk, ck, "fm"], [ck])
                if j < 22:
                    op("act", lambda e: e.activation(out=sa[:, j, :], in_=cb, func=AF.Silu), [ck], [("sa", j)])
                else:
                    op(FFN_MUL_ENG, lambda e: e.tensor_tensor(out=sa[:, j - 22, :], in0=sa[:, j - 22, :], in1=cb, op=ALU.mult), [ck, ("sa", j - 22)], [("sa", j - 22)])

            emit_up(0)
            for j in range(44):
                if j + 1 < 44:
                    emit_up(j + 1)
                chain_up(j)
            sakeys = [("sa", j) for j in range(22)]
            Xn, xnk = Xb[1 - xb], XK(1 - xb)
            if False:
                for sub in range(4):
                    rms_rows(Xn, sub, sub, xnk[sub])
                norm_A(Xn, 0, xnk[0])
            grp = 0
            for half in range(2):
                for kp, (k0, k1) in enumerate(((0, 8), (8, 16), (16, 22))):
                    W, wk = load_w(wdn_v[:, k0:k1, half * 512:(half + 1) * 512], k1 - k0, 512)
                    for sub in range(4):
                        for kc in range(k0, k1):
                            mm(psA[sub][:, :], sa[:, kc, sub * 128:(sub + 1) * 128], W[:, kc - k0, :], kc == 0, kc == 21, sakeys + [wk], [PA[sub]])
                    if False and grp < 4:
                        norm_B(grp, FM_G1, hT, "hT")
                        if grp + 1 < 4:
                            norm_A(Xn, grp + 1, xnk[grp + 1])
                    grp += 1
                for sub in range(4):
                    op("dve", lambda e, sub=sub, half=half: e.tensor_tensor(out=X[:, sub, half * 512:(half + 1) * 512], in0=X[:, sub, half * 512:(half + 1) * 512],
                                                                            in1=psA[sub][:, :], op=ALU.add), [xks[sub], PA[sub]], [xks[sub]])
            dma("sp", y_o[t0:t0 + NT, :].rearrange("(s p) d -> p s d", p=128), X[:, :, :], xks, ["hbm_y"], "st_y")


        if do_sample:
            X = Xb[0]
            S.alias["X"] = XK(0) + XK(1)
            sc = X[:, 1:4, :].rearrange("p a t -> p (a t)")
            dma("sp", X[:, 0, :], xs_in[:, :], [], ["X"], "xload")
            dma("sp", sc[:, 0:SC_N], sc_in[:, :], [], ["X"], "xload")
            RROW = lambda n: sc[:, SC_RROW + n * 128:SC_RROW + (n + 1) * 128]
            ONEROW = lambda n: sc[:, SC_CS + 3 - n:SC_CS + 131 - n]
            ONECOL = lambda r: sc[:, SC_ST + 127 - r:SC_ST + 255 - r]
            REP = sc[:, SC_REP:SC_REP + 128]
            E8 = lambda h: sc[0:8, SC_E8 + h * 128:SC_E8 + (h + 1) * 128]
            ALT = sc[:, SC_AL:SC_AL + 512]
            PIOTA = sc[:, SC_PI:SC_PI + 1]
            IND120 = sc[0:120, SC_IND:SC_IND + 128]
            op("dve", lambda e: e.tensor_copy(out=orowb[:, 0:131], in_=sc[:, SC_CS:SC_CS + 131]), ["X"], ["orowb"])
            ONEROWB = lambda n: orowb[:, 3 - n:131 - n]
            HCOL = lambda h: sc[:, SC_ST + 127 - h:SC_ST + 135 - h]
            dma("sp", idx_t[:, :], pt_in[:, :], [], ["idx_t"], "c7")
            op("dve", lambda e: e.tensor_copy(out=ptf[:, :], in_=idx_t[:, :]), ["idx_t"], ["ptf"])
            op("dve", lambda e: e.tensor_scalar(out=ptf[:, :], in0=ptf[:, :], scalar1=128.0, scalar2=None, op0=ALU.mult), ["ptf"], ["ptf"])
            op("dve", lambda e: e.tensor_scalar(out=ptf[:, :], in0=ptf[:, :], scalar1=PIOTA, scalar2=None, op0=ALU.add), ["ptf", "X"], ["ptf"])
            op("dve", lambda e: e.tensor_copy(out=idx_t[:, :], in_=ptf[:, :]), ["ptf"], ["idx_t"])

            for _k in ("stc", "prd", "qb", "mg"):
                S.alias[_k] = ["uT"]
            S.alias["mg"] = ["uT", "stc", "prd"]
            S.alias["cs"] = [("sa", j) for j in range(22)] + ["KTloc", "qTa", "mqn", "qTp"]
            S.alias["stf"] = ["Vloc"]
            norm_to_T(X, 1, FM_G1, hT, "hT", "X")
            us, qs, mqs, ks = cT[:, 0, :], cT[:, 1, :], cT[:, 2, :], cT[:, 3, :]
            vs, stc, prd, qb = uT[:, 0, 0:512], uT[:, 1, 0:512], uT[:, 2, 0:512], uT[:, 3, 0:512]

            def bload(dst, dkey, src_row):
                dma("sp", dst, src_row.partition_broadcast(128), [], [dkey], "bl_" + str(dkey))

            def zpiece(col0):
                W, wk = load_w(win_v[:, :, col0:col0 + 512], 8, 512)
                for kc in range(8):
                    mm(psA[0][:, :], hT[:, kc, 0:128], W[:, kc, :], kc == 0, kc == 7, ["hT", wk], [PA[0]])
                return psA[0], PA[0]

            ps, pk = zpiece(0)
            op("act", lambda e: e.activation(out=tmpA[:, :], in_=psA[0][:, :], func=AF.Copy), [pk], ["tmpA"])
            op("dve", lambda e: e.tensor_copy(out=rden[:, :], in_=tmpA[:, :]), ["tmpA"], ["rden"])
            ps, pk = zpiece(512)
            op("act", lambda e: e.activation(out=tmpB[:, :], in_=psA[0][:, :], func=AF.Sigmoid), [pk], ["tmpB"])
            op("dve", lambda e: e.tensor_tensor(out=us, in0=rden[:, :], in1=tmpB[:, :], op=ALU.mult), ["rden", "tmpB"], [("cT", 0)])
            ps, pk = zpiece(1024)
            headnorm(ps, pk, NH, HD, BC_GQ, qs.rearrange("p (h d) -> p h d", h=NH), ("cT", 1))
            ps, pk = zpiece(1536)
            headnorm(ps, pk, NH, HD, BC_GK, ks.rearrange("p (h d) -> p h d", h=NH), ("cT", 3))
            dma("sp", ks_o[:, :], cT[0:4, 3, :], [("cT", 3)], ["hbm_ks"], "st_s1")
            ps, pk = zpiece(2048)
            op("act", lambda e: e.activation(out=vs, in_=psA[0][:, :], func=AF.Copy), [pk], ["uT"])
            dma("sp", vs_o[:, :], uT[0:4, 0, 0:512], ["uT"], ["hbm_vs"], "st_s2")
            ps, pk = zpiece(2560)
            headnorm(ps, pk, 4, 128, BC_GMQ, mqs.rearrange("p (h d) -> p h d", h=4), ("cT", 2))

            dma("sp", uT[0:120, 1, 0:512], stc_in[:, :], [], ["stc"], "c8")
            dma("sp", uT[0:120, 2, 0:512], w120_in[:, :], [], ["prd"], "c9")
            for n in range(4):
                dma("sp", convs_o[n, 0:29, :], uT[n * 30 + 1:n * 30 + 30, 1, 0:512], ["stc"], ["hbm_convs"], "st_s3")
            dma("sp", convs_o[:, 29, :], cT[0:4, 0, :], [("cT", 0)], ["hbm_convs"], "st_s3")
            op("dve", lambda e: e.tensor_tensor(out=uT[0:120, 2, 0:512], in0=uT[0:120, 2, 0:512], in1=uT[0:120, 1, 0:512], op=ALU.mult), ["stc", "prd"], ["prd"])
            mm(psA[1][:, :], IND120, uT[0:120, 2, 0:512], True, True, ["prd", "X"], [PA[1]])
            bload(cbuf[:, 0, :], ("cbuf", 0), vrow_in[0, 0:512])
            bload(cbuf[:, 1, :], ("cbuf", 1), vrow_in[1, 0:512])
            op("dve", lambda e: e.tensor_tensor(out=tmpA[:, :], in0=us, in1=cbuf[:, 0, :], op=ALU.mult), [("cT", 0), ("cbuf", 0)], ["tmpA"])
            op("dve", lambda e: e.tensor_tensor(out=tmpA[:, :], in0=tmpA[:, :], in1=cbuf[:, 1, :], op=ALU.add), ["tmpA", ("cbuf", 1)], ["tmpA"])
            op("dve", lambda e: e.tensor_tensor(out=tmpA[:, :], in0=tmpA[:, :], in1=psA[1][:, :], op=ALU.add), ["tmpA", PA[1]], ["tmpA"])
            op("dve", lambda e: e.tensor_reduce(out=ss[:, 0:1], in_=tmpA[:, :], axis=AX.X, op=ALU.add), ["tmpA"], [("ss", 0)])
            op("dve", lambda e: e.tensor_scalar(out=ss[:, 0:1], in0=ss[:, 0:1], scalar1=1.0 / 512, scalar2=None, op0=ALU.mult), [("ss", 0)], [("ss", 0)])
            op("dve", lambda e: e.tensor_scalar(out=tmpA[:, :], in0=tmpA[:, :], scalar1=ss[:, 0:1], scalar2=None, op0=ALU.subtract), ["tmpA", ("ss", 0)], ["tmpA"])
            op("dve", lambda e: e.memset(ss[:, 1:2], 0.0), [], [("ss", 1)])
            op("act", lambda e: e.activation(out=tmpB[:, :], in_=tmpA[:, :], func=AF.Square, accum_out=ss[:, 1:2]), ["tmpA", ("ss", 1)], ["tmpB", ("ss", 1)])
            op("act", lambda e: e.activation(out=ss[:, 1:2], in_=ss[:, 1:2], func=AF.Sqrt, bias=epsb[:, 0:1], scale=1.0 / 512), [("ss", 1), "epsb"], [("ss", 1)])
            op("dve", lambda e: e.reciprocal(out=ss[:, 1:2], in_=ss[:, 1:2]), [("ss", 1)], [("ss", 1)])
            bload(cbuf[:, 0, :], ("cbuf", 0), vrow_in[2, 0:512])
            bload(cbuf[:, 1, :], ("cbuf", 1), vrow_in[3, 0:512])
            op("dve", lambda e: e.tensor_scalar(out=tmpA[:, :], in0=tmpA[:, :], scalar1=ss[:, 1:2], scalar2=None, op0=ALU.mult), ["tmpA", ("ss", 1)], ["tmpA"])
            op("dve", lambda e: e.tensor_tensor(out=tmpA[:, :], in0=tmpA[:, :], in1=cbuf[:, 0, :], op=ALU.mult), ["tmpA", ("cbuf", 0)], ["tmpA"])
            op("dve", lambda e: e.tensor_tensor(out=tmpA[:, :], in0=tmpA[:, :], in1=cbuf[:, 1, :], op=ALU.add), ["tmpA", ("cbuf", 1)], ["tmpA"])
            op("act", lambda e: e.activation(out=xn[:, 0:512], in_=tmpA[:, :], func=AF.Silu), ["tmpA"], ["xn"])
            dbgout(xn[0:4, 0:512], [4, 512], ["xn"])
            for kc in range(4):
                tr(psT[:, kc * 128:(kc + 1) * 128], xn[:, kc * 128:(kc + 1) * 128], identb[:, :], ["xn", "identb"], ["psT"])
            op("dve", lambda e: e.tensor_copy(out=sT[:, :, 0:128], in_=psT[:, 0:512].rearrange("p (k t) -> p k t", k=4)), ["psT"], ["sT"])

            Lt = bc
            pbufs = [(kst[:, 0, :], ("kst", 0)), (kst[:, 1, :], ("kst", 1)), (vst[:, 0, :], ("vst", 0)), (vst[:, 1, :], ("vst", 1))]
            pstate = {"i": 0}
            pvstate = {"i": 0}

            def get_page(src_kind, n, pg):
                buf, bk = pbufs[pstate["i"] % 4]
                pstate["i"] += 1
                if src_kind in ("k", "v"):
                    src = ck_in if src_kind == "k" else cv_in
                    col = n * 64 + pg
                    op("pool", lambda e: e.indirect_dma_start(out=buf, out_offset=None, in_=src[:, :],
                                                              in_offset=bass.IndirectOffsetOnAxis(ap=idx_t[:, col:col + 1], axis=0)),
                       ["idx_t"], [bk], dma=True, semkey="pg_" + str(bk))
                else:
                    src = cmk_in if src_kind == "mk" else cmv_in
                    dma("sp", buf, src[n, pg * 128:(pg + 1) * 128, :], [], [bk], "pg_" + str(bk))
                return buf, bk

            def bcast_row(n, src, skey):
                mm(psA[2][:, :], RROW(n), src, True, True, ["X", skey], [PA[2]])
                op("act", lambda e: e.activation(out=qb, in_=psA[2][:, :], func=AF.Copy), [PA[2]], ["qb"])

            def dense_attn(kkind, vkind, npg, nh, hd, qsrc, qkey, moba):
                G = npg * nh
                first_sum = [True]
                for n in range(4):
                    bcast_row(n, qsrc, qkey)
                    for pg in range(npg):
                        buf, bk = get_page(kkind, n, pg)
                        op("dve", lambda e, buf=buf: e.tensor_tensor(out=tmpA[:, :], in0=buf, in1=qb, op=ALU.mult), [bk, "qb"], ["tmpA"])
                        o0 = n * G + pg * nh
                        op("dve", lambda e, o0=o0: e.tensor_reduce(out=Lt[:, o0:o0 + nh], in_=tmpA[:, :].rearrange("p (h d) -> p h d", h=nh), axis=AX.X, op=ALU.add),
                           ["tmpA"], ["bc"])
                if moba:
                    Lv = Lt[:, :].rearrange("p (g h) -> p g h", h=NH)
                    for h in range(NH):
                        mm(psA[3][0:8, 0:256], HCOL(h), Lv[:, :, h], h == 0, h == NH - 1, ["X", "bc"], [PA[3]])
                    op("dve", lambda e: e.tensor_reduce(out=rden[0:8, 0:128], in_=psA[3][0:8, 0:256].rearrange("p (g e) -> p g e", e=2), axis=AX.X, op=ALU.add),
                       [PA[3]], ["rden"])
                    for n in range(4):
                        op("dve", lambda e, n=n: e.max(out=top8[0:8, n, :], in_=rden[0:8, n * 32:(n + 1) * 32]), ["rden"], ["top8"])
                    op("dve", lambda e: e.tensor_tensor(out=rden[0:8, 128:256].rearrange("p (n b) -> p n b", n=4), in0=rden[0:8, 0:128].rearrange("p (n b) -> p n b", n=4),
                                                        in1=top8[0:8, 0:4, 2:3].broadcast_to([8, 4, 32]), op=ALU.is_ge), ["rden", "top8"], ["rden"])
                    op("dve", lambda e: e.tensor_copy(out=tmpB[0:8, 0:256].rearrange("p (g e) -> p g e", e=2), in_=rden[0:8, 128:256].unsqueeze(2).broadcast_to([8, 128, 2])),
                       ["rden"], ["tmpB"])
                for n in range(4):
                    if moba:
                        op("dve", lambda e, n=n: e.tensor_tensor(out=Lt[:, n * G:(n + 1) * G], in0=Lt[:, n * G:(n + 1) * G], in1=ALT, op=ALU.add), ["bc", "X"], ["bc"])
                    op("dve", lambda e, n=n: e.tensor_reduce(out=thr[:, n:n + 1], in_=Lt[:, n * G:(n + 1) * G], axis=AX.X, op=ALU.max), ["bc"], ["thr"])
                op("pe", lambda e: e.transpose(out=psM[0:4, 128:256], in_=thr[:, 0:4], identity=identf[:, :]), ["thr", "identf"], ["psM"])
                op("dve", lambda e: e.tensor_reduce(out=mvals[0:4, 6:7], in_=psM[0:4, 128:256], axis=AX.X, op=ALU.max), ["psM"], ["mvals"])
                if moba:
                    op("dve", lambda e: e.tensor_tensor(out=tmpA[:, :], in0=qs, in1=ks, op=ALU.mult), [("cT", 1), ("cT", 3)], ["tmpA"])
                    op("dve", lambda e: e.tensor_reduce(out=nsel[:, 0, 0:8], in_=tmpA[:, :].rearrange("p (h d) -> p h d", h=NH), axis=AX.X, op=ALU.add), ["tmpA"], ["nsel"])
                    op("dve", lambda e: e.tensor_reduce(out=mvals[0:4, 7:8], in_=nsel[0:4, 0, 0:8], axis=AX.X, op=ALU.max), ["nsel"], ["mvals"])
                    op("dve", lambda e: e.tensor_tensor(out=mvals[0:4, 6:7], in0=mvals[0:4, 6:7], in1=mvals[0:4, 7:8], op=ALU.max), ["mvals"], ["mvals"])
                op("dve", lambda e: e.tensor_scalar(out=mvals[0:4, 6:7], in0=mvals[0:4, 6:7], scalar1=-1.0, scalar2=None, op0=ALU.mult), ["mvals"], ["mvals"])
                op("dve", lambda e: e.tensor_scalar(out=nsel[0:4, 1, 0:4], in0=identf[0:4, 0:4], scalar1=mvals[0:4, 6:7], scalar2=None, op0=ALU.mult), ["mvals", "identf"], ["nsel"])
                mm(psM[:, 256:260], onesf[0:4, :], nsel[0:4, 1, 0:4], True, True, ["onesf", "nsel"], ["psM"])
                op("dve", lambda e: e.tensor_copy(out=thr[:, 4:8], in_=psM[:, 256:260]), ["psM"], ["thr"])
                for n in range(4):
                    op("act", lambda e, n=n: e.activation(out=Lt[:, n * G:(n + 1) * G], in_=Lt[:, n * G:(n + 1) * G], func=AF.Exp, bias=thr[:, 4 + n:5 + n], scale=1.0),
                       ["bc", "thr"], ["bc"])
                if moba:
                    op("act", lambda e: e.activation(out=nsel[0:4, 0, 0:8], in_=nsel[0:4, 0, 0:8], func=AF.Exp, bias=mvals[0:4, 6:7], scale=1.0), ["nsel", "mvals"], ["nsel"])
                    for h in range(NH):
                        mm(psA[h // 2][:, (h % 2) * 256:(h % 2) * 256 + 256], E8(h), tmpB[0:8, 0:256], True, True, ["X", "tmpB"], [PA[h // 2]])
                    for hp in range(4):
                        op("dve", lambda e, hp=hp: e.tensor_tensor(
                            out=Lt[:, :].rearrange("p (g h) -> p g h", h=NH)[:, :, 2 * hp:2 * hp + 2],
                            in0=Lt[:, :].rearrange("p (g h) -> p g h", h=NH)[:, :, 2 * hp:2 * hp + 2],
                            in1=psA[hp][:, :].rearrange("p (h g) -> p g h", h=2), op=ALU.mult), ["bc", PA[hp]], ["bc"])
                for n in range(4):
                    for c0 in range(0, G, 512):
                        w = min(512, G - c0)
                        mm(psA[4][:, 0:w], ONEROW(n), Lt[:, n * G + c0:n * G + c0 + w], n == 0 and c0 == 0, n == 3 and c0 + w >= G, ["X", "bc"], [PA[4]])
                Gc = min(G, 512)
                op("dve", lambda e: e.tensor_reduce(out=ss[:, 8:8 + nh], in_=psA[4][:, 0:Gc].rearrange("p (g h) -> p h g", h=nh), axis=AX.X, op=ALU.add), [PA[4]], ["ss8"])
                firstv = [True]
                for n in range(4):
                    for pg in range(npg):
                        buf, bk = get_page(vkind, n, pg)
                        o0 = n * G + pg * nh
                        slot = pvstate["i"] % 3
                        pvstate["i"] += 1
                        op("dve", lambda e, buf=buf, o0=o0, slot=slot: e.tensor_tensor(out=PT[:, slot, :].rearrange("p (h d) -> p h d", h=nh), in0=buf.rearrange("p (h d) -> p h d", h=nh),
                                                                                     in1=Lt[:, o0:o0 + nh].unsqueeze(2).broadcast_to([128, nh, hd]), op=ALU.mult), [bk, "bc"], [("PT", slot)])
                        mm(psA[5][:, :], ONEROWB(n), PT[:, slot, :], firstv[0], n == 3 and pg == npg - 1, ["orowb", ("PT", slot)], [PA[5]])
                        firstv[0] = False

            dense_attn("k", "v", 64, NH, HD, qs, ("cT", 1), True)
            op("dve", lambda e: e.tensor_tensor(out=ss[:, 8:16], in0=ss[:, 8:16], in1=nsel[:, 0, 0:8], op=ALU.add), ["ss8", "nsel"], ["ss8"])
            op("dve", lambda e: e.tensor_scalar(out=ss[:, 8:16], in0=ss[:, 8:16], scalar1=1e-30, scalar2=None, op0=ALU.add), ["ss8"], ["ss8"])
            op("dve", lambda e: e.reciprocal(out=ss[:, 8:16], in_=ss[:, 8:16]), ["ss8"], ["ss8"])
            op("dve", lambda e: e.tensor_tensor(out=tmpB[:, :].rearrange("p (h d) -> p h d", h=NH), in0=vs.rearrange("p (h d) -> p h d", h=NH),
                                                in1=nsel[:, 0, 0:8].unsqueeze(2).broadcast_to([128, NH, HD]), op=ALU.mult), ["uT", "nsel"], ["tmpB"])
            op("dve", lambda e: e.tensor_tensor(out=tmpB[:, :], in0=tmpB[:, :], in1=psA[5][:, :], op=ALU.add), ["tmpB", PA[5]], ["tmpB"])
            op("dve", lambda e: e.tensor_tensor(out=xn[:, 0:512].rearrange("p (h d) -> p h d", h=NH), in0=tmpB[:, :].rearrange("p (h d) -> p h d", h=NH),
                                                in1=ss[:, 8:16].unsqueeze(2).broadcast_to([128, NH, HD]), op=ALU.mult), ["tmpB", "ss8"], ["xn"])
            dbgout(rden[0:8, 0:256], [8, 256], ["rden"])
            dbgout(ss[0:4, 8:16], [4, 8], ["ss8"])
            dbgout(xn[0:4, 0:512], [4, 512], ["xn"])
            dbgout(bc[:, :], [128, 2048], ["bc"])
            dbgout(nsel[0:4, 0, 0:8], [4, 8], ["nsel"])
            dbgout(tmpB[0:4, :], [4, 512], ["tmpB"])
            for kc in range(4):
                tr(psT[:, kc * 128:(kc + 1) * 128], xn[:, kc * 128:(kc + 1) * 128], identb[:, :], ["xn", "identb"], ["psT"])
            op("dve", lambda e: e.tensor_copy(out=OT[:, :, 0:128], in_=psT[:, 0:512].rearrange("p (k t) -> p k t", k=4)), ["psT"], ["OT"])

            dense_attn("mk", "mv", 2, 4, 128, mqs, ("cT", 2), False)
            op("dve", lambda e: e.tensor_scalar(out=ss[:, 8:12], in0=ss[:, 8:12], scalar1=1e-30, scalar2=None, op0=ALU.add), ["ss8"], ["ss8"])
            op("dve", lambda e: e.reciprocal(out=ss[:, 8:12], in_=ss[:, 8:12]), ["ss8"], ["ss8"])
            op("dve", lambda e: e.tensor_tensor(out=xn[:, 0:512].rearrange("p (h d) -> p h d", h=4), in0=psA[5][:, :].rearrange("p (h d) -> p h d", h=4),
                                                in1=ss[:, 8:12].unsqueeze(2).broadcast_to([128, 4, 128]), op=ALU.mult), [PA[5], "ss8"], ["xn"])
            dbgout(xn[0:4, 0:512], [4, 512], ["xn"])
            for kc in range(4):
                tr(psT[:, kc * 128:(kc + 1) * 128], xn[:, kc * 128:(kc + 1) * 128], identb[:, :], ["xn", "identb"], ["psT"])
            op("dve", lambda e: e.tensor_copy(out=OmT[:, :, 0:128], in_=psT[:, 0:512].rearrange("p (k t) -> p k t", k=4)), ["psT"], ["OmT"])

            mg = uT[:, 1:3, :].rearrange("p a t -> p (a t)")[:, 0:1024]
            for half in range(2):
                srcs = ((wpa_v, sT, "sT"), (wpb_v, OT, "OT"), (wpc_v, OmT, "OmT"))
                for j in range(3):
                    wpv, src, skey = srcs[j]
                    Wp, kp = load_w(wpv[:, :, half * 512:(half + 1) * 512], 4, 512)
                    for kc in range(4):
                        mm(psA[j][:, :], src[:, kc, 0:128], Wp[:, kc, :], kc == 0, kc == 3, [kp, skey], [PA[j]])
                    col0 = 3072 + j * 1024 + half * 512
                    W, wk = load_w(win_v[:, :, col0:col0 + 512], 8, 512)
                    for kc in range(8):
                        mm(psA[3][:, :], hT[:, kc, 0:128], W[:, kc, :], kc == 0, kc == 7, ["hT", wk], [PA[3]])
                    bload(cbuf[:, 2, :], ("cbuf", 2), vrow_in[4, j * 1024 + half * 512:j * 1024 + half * 512 + 512])
                    op("dve", lambda e: e.tensor_tensor(out=tmpA[:, :], in0=psA[3][:, :], in1=cbuf[:, 2, :], op=ALU.add), [PA[3], ("cbuf", 2)], ["tmpA"])
                    op("act", lambda e: e.activation(out=tmpA[:, :], in_=tmpA[:, :], func=AF.Sigmoid), ["tmpA"], ["tmpA"])
                    if j == 0:
                        op("dve", lambda e, half=half: e.tensor_tensor(out=mg[:, half * 512:(half + 1) * 512], in0=psA[0][:, :], in1=tmpA[:, :], op=ALU.mult), [PA[0], "tmpA"], ["mg"])
                    else:
                        op("dve", lambda e, j=j: e.tensor_tensor(out=tmpB[:, :], in0=psA[j][:, :], in1=tmpA[:, :], op=ALU.mult), [PA[j], "tmpA"], ["tmpB"])
                        op("dve", lambda e, half=half: e.tensor_tensor(out=mg[:, half * 512:(half + 1) * 512], in0=mg[:, half * 512:(half + 1) * 512], in1=tmpB[:, :], op=ALU.add),
                           ["mg", "tmpB"], ["mg"])
            op("dve", lambda e: e.tensor_copy(out=xn[:, :], in_=mg), ["mg"], ["xn"])
            dbgout(xn[0:4, :], [4, 1024], ["xn"])
            for kc in range(8):
                tr(psT[:, kc * 128:(kc + 1) * 128], xn[:, kc * 128:(kc + 1) * 128], identb[:, :], ["xn", "identb"], ["psT"])
            op("dve", lambda e: e.tensor_copy(out=merged[:, :, 0:128], in_=psT[:, :].rearrange("p (k t) -> p k t", k=8)), ["psT"], ["merged"])
            for half in range(2):
                W, wk = load_w(wout_v[:, :, half * 512:(half + 1) * 512], 8, 512)
                for kc in range(8):
                    mm(psA[0][:, :], merged[:, kc, 0:128], W[:, kc, :], kc == 0, kc == 7, ["merged", wk], [PA[0]])
                op("dve", lambda e, half=half: e.tensor_tensor(out=X[:, 0, half * 512:(half + 1) * 512], in0=X[:, 0, half * 512:(half + 1) * 512], in1=psA[0][:, :], op=ALU.add),
                   ["X", PA[0]], ["X"])

            dbgout(X[0:4, 0, :], [4, 1024], ["X"])
            norm_to_T(X, 1, FM_G2, hT, "hT", "X")
            cs = scrA[:, :].bitcast(F32)
            stf = Vloc[:, :, :, :].rearrange("p a b c -> p (a b c)").bitcast(F32)
            op("dve", lambda e: e.memset(stf[:, 0:1024], 0.0), [], ["stf"])
            for piece in range(11):
                c0 = piece * 512
                W, wk = load_w(wup_v[:, :, c0:c0 + 512], 8, 512)
                for kc in range(8):
                    mm(psA[piece % 2][:, :], hT[:, kc, 0:128], W[:, kc, :], kc == 0, kc == 7, ["hT", wk], [PA[piece % 2]])
                pp, pkk = psA[piece % 2], PA[piece % 2]
                op("act", lambda e, pp=pp: e.activation(out=tmpA[:, :], in_=pp[:, :], func=AF.Copy), [pkk], ["tmpA"])
                dma("sp", ffns_o[:, 1, c0:c0 + 512], tmpA[0:4, :], ["tmpA"], ["hbm_ffns"], "st_s4")
                dma("sp", stf[0:4, 0:512], stf_in[:, c0:c0 + 512], [], ["stf"], "ld_stf")
                dma("sp", stf[0:4, 512:1024], stf_in[:, NUP + c0:NUP + c0 + 512], [], ["stf"], "ld_stf")
                dma("sp", ffns_o[:, 0, c0:c0 + 512], stf[0:4, 512:1024], ["stf"], ["hbm_ffns"], "st_s5")
                bload(cbuf[:, 0, :], ("cbuf", 0), vrow_in[5, c0:c0 + 512])
                op("dve", lambda e, c0=c0: e.tensor_tensor(out=cs[:, c0:c0 + 512], in0=stf[:, 0:512], in1=cbuf[:, 0, :], op=ALU.mult), ["stf", ("cbuf", 0)], ["cs"])
                bload(cbuf[:, 1, :], ("cbuf", 1), vrow_in[6, c0:c0 + 512])
                op("dve", lambda e: e.tensor_tensor(out=tmpB[:, :], in0=stf[:, 512:1024], in1=cbuf[:, 1, :], op=ALU.mult), ["stf", ("cbuf", 1)], ["tmpB"])
                op("dve", lambda e, c0=c0: e.tensor_tensor(out=cs[:, c0:c0 + 512], in0=cs[:, c0:c0 + 512], in1=tmpB[:, :], op=ALU.add), [("sa", 0), "tmpB"], ["cs"])
                bload(cbuf[:, 2, :], ("cbuf", 2), vrow_in[7, c0:c0 + 512])
                op("dve", lambda e: e.tensor_tensor(out=tmpB[:, :], in0=tmpA[:, :], in1=cbuf[:, 2, :], op=ALU.mult), ["tmpA", ("cbuf", 2)], ["tmpB"])
                op("dve", lambda e, c0=c0: e.tensor_tensor(out=cs[:, c0:c0 + 512], in0=cs[:, c0:c0 + 512], in1=tmpB[:, :], op=ALU.add), [("sa", 0), "tmpB"], ["cs"])
                bload(cbuf[:, 0, :], ("cbuf", 0), vrow_in[8, c0:c0 + 512])
                op("dve", lambda e, c0=c0: e.tensor_tensor(out=cs[:, c0:c0 + 512], in0=cs[:, c0:c0 + 512], in1=cbuf[:, 0, :], op=ALU.add), [("sa", 0), ("cbuf", 0)], ["cs"])
            actb = hT[:, 0:6, :].rearrange("p a t -> p (a t)")
            for c0 in range(0, DFF, 512):
                w = min(512, DFF - c0)
                op("act", lambda e, c0=c0, w=w: e.activation(out=tmpA[:, 0:w], in_=cs[:, c0:c0 + w], func=AF.Silu), ["cs"], ["tmpA"])
                op("dve", lambda e, c0=c0, w=w: e.tensor_tensor(out=actb[:, c0:c0 + w], in0=tmpA[:, 0:w], in1=cs[:, DFF + c0:DFF + c0 + w], op=ALU.mult), ["tmpA", ("sa", 0)], ["hT"])
            actT = qTa_s = None
            aT = sT[:, :, :].rearrange("p a t -> p (a t)")
            aT2 = OT[:, :, :].rearrange("p a t -> p (a t)")
            for grp in range(3):
                k0 = grp * 8
                nk = min(8, 22 - k0)
                for kk in range(nk):
                    tr(psT[:, kk * 128:(kk + 1) * 128], actb[:, (k0 + kk) * 128:(k0 + kk + 1) * 128], identb[:, :], ["hT", "identb"], ["psT"])
                if grp < 2:
                    op("dve", lambda e, k0=k0, nk=nk: e.tensor_copy(out=aT[:, k0 * 128:(k0 + nk) * 128], in_=psT[:, 0:nk * 128]), ["psT"], ["sT"])
                else:
                    op("dve", lambda e, nk=nk: e.tensor_copy(out=aT2[:, 0:nk * 128], in_=psT[:, 0:nk * 128]), ["psT"], ["OT"])

            def aTk(kc):
                return aT[:, kc * 128:(kc + 1) * 128] if kc < 16 else aT2[:, (kc - 16) * 128:(kc - 15) * 128]

            for half in range(2):
                for kp, (k0, k1) in enumerate(((0, 8), (8, 16), (16, 22))):
                    W, wk = load_w(wdn_v[:, k0:k1, half * 512:(half + 1) * 512], k1 - k0, 512)
                    for kc in range(k0, k1):
                        mm(psA[2][:, :], aTk(kc), W[:, kc - k0, :], kc == 0, kc == 21, ["sT", "OT", wk], [PA[2]])
                op("dve", lambda e, half=half: e.tensor_tensor(out=X[:, 0, half * 512:(half + 1) * 512], in0=X[:, 0, half * 512:(half + 1) * 512], in1=psA[2][:, :], op=ALU.add),
                   ["X", PA[2]], ["X"])
            dma("sp", ys_o[:, :], X[0:4, 0, :], ["X"], ["hbm_ys"], "st_y")

        allhist = [("hist", j) for j in range(44)]
        op("pe", lambda e: e.transpose(out=psM[0:88, 0:128], in_=hist[:, :, :].rearrange("p j r -> p (j r)"), identity=identf[:, :]), allhist + ["identf"], ["psM"])
        op("dve", lambda e: e.tensor_copy(out=outst[0:88, 0:128], in_=psM[0:88, 0:128]), ["psM"], ["outst"])
        dma("sp", ffnc_o[:, :], outst[0:88, 0:128], ["outst"], ["hbm_ffnc"], "st_misc")

        S.emit(st)
    return nc


def host_tables(inp):
    f = np.float32
    fm = np.zeros((128, FM_N), f)

    def fmaj(v):
        return np.ascontiguousarray(np.asarray(v, f).reshape(-1, 128).T)

    fm[:, FM_G1:FM_G1 + 8] = fmaj(inp["norm1_g"])
    fm[:, FM_G2:FM_G2 + 8] = fmaj(inp["norm2_g"])
    fm[:, FM_GM:FM_GM + 8] = fmaj(inp["mem_norm_g"])
    fm[:, FM_BG:FM_BG + 24] = fmaj(inp["b_gate"])
    wda = np.asarray(inp["w_dw_a"], f)
    fm[:, FM_WDA:FM_WDA + 124] = wda.T.reshape(4, 128, 31).transpose(1, 0, 2).reshape(128, 124)
    fm[:, FM_BDA:FM_BDA + 4] = fmaj(inp["b_dw_a"])
    fm[:, FM_LAG:FM_LAG + 4] = fmaj(inp["ln_a_g"])
    fm[:, FM_LAB:FM_LAB + 4] = fmaj(inp["ln_a_b"])
    wdf = np.asarray(inp["w_dw_f"], f)
    fm[:, FM_WDF:FM_WDF + 132] = wdf.T.reshape(44, 128, 3).transpose(1, 0, 2).reshape(128, 132)
    fm[:, FM_BDF:FM_BDF + 44] = fmaj(inp["b_dw_f"])
    bc = np.zeros((128, BC_N), f)
    bc[:, BC_GQ:BC_GQ + 512] = np.tile(np.asarray(inp["q_norm_g"], f), 8)[None, :]
    bc[:, BC_GK:BC_GK + 512] = np.tile(np.asarray(inp["k_norm_g"], f), 8)[None, :]
    bc[:, BC_GMQ:BC_GMQ + 512] = np.tile(np.asarray(inp["mq_norm_g"], f), 4)[None, :]
    bc[:, BC_GMK:BC_GMK + 512] = np.tile(np.asarray(inp["mk_norm_g"], f), 4)[None, :]
    ident = np.eye(128, dtype=f)
    p = np.arange(128)
    tri = np.where(p[:, None] > p[None, :], f(NEG), f(0)).astype(f)
    slopes = (2.0 ** (-np.arange(1, NH + 1, dtype=np.float64))).astype(f)
    j = np.arange(32)
    abase = (slopes[None, :, None] * (p[:, None, None] + 128.0 * (j[None, None, :] - 28))).astype(f).reshape(128, NH * 32)
    dt = (np.arange(4)[None, :] * 128 + p[:, None]).astype(np.int64)
    hi = (16 * (dt // 16)).astype(f)
    lo = (dt % 16).astype(f)
    qal = np.stack([-slopes[None, None, :] * hi[:, :, None], -slopes[None, None, :] * lo[:, :, None]], axis=-1).astype(f).reshape(128, 64)
    return {"fm": fm, "bc": bc, "ident": ident, "tri": tri, "abase": abase, "qal": qal}


_NC_CACHE = {}


def run_prompt(inp, nch=NCHUNK):
    key = ("p", nch)
    if key not in _NC_CACHE:
        _NC_CACHE[key] = build(nch=nch, do_sample=False)
    nc = _NC_CACHE[key]
    tabs = host_tables(inp)
    f = np.float32
    wnames = ["w_in", "w_proj_a", "w_proj_b", "w_proj_c", "w_mem_kv", "w_out", "w_up", "w_down"]
    shared = {n: np.ascontiguousarray(np.asarray(inp[n], f)) for n in wnames}
    shared.update(tabs)
    in_maps = []
    for c in range(8):
        s = c % 4
        m = dict(shared)
        m["x"] = np.ascontiguousarray(np.asarray(inp["x_prompt"][s], f))
        m["mem"] = np.ascontiguousarray(np.asarray(inp["mem_prompt"][s], f))
        in_maps.append(m)
    res = run_bass_kernel_spmd(nc, in_maps, core_ids=list(range(8)))
    return res.results


def sample_tables(inp):
    f = np.float32
    sc = np.zeros((128, SC_N), f)
    for n in range(4):
        sc[n, SC_RROW + n * 128:SC_RROW + (n + 1) * 128] = 1.0
    sc[:, SC_CS + 3] = 1.0
    sc[:, SC_ST + 127] = 1.0
    for k in range(4):
        sc[k, SC_REP + k * 32:SC_REP + (k + 1) * 32] = 1.0
    for h in range(8):
        sc[h, SC_E8 + h * 128:SC_E8 + (h + 1) * 128] = 1.0
    slopes = (2.0 ** (-np.arange(1, NH + 1, dtype=np.float64)))
    p = np.arange(128)[:, None, None]
    pg = np.arange(64)[None, :, None]
    sc[:, SC_AL:SC_AL + 512] = (-slopes[None, None, :] * (8192.0 - 128.0 * pg - p)).astype(f).reshape(128, 512)
    sc[:, SC_PI] = np.arange(128, dtype=f)
    for n in range(4):
        sc[n * 30:(n + 1) * 30, SC_IND + n] = 1.0
    vr = np.zeros((9, 5632), f)
    vr[0, :512] = np.asarray(inp["w_dw_a"], f)[30]
    vr[1, :512] = np.asarray(inp["b_dw_a"], f)
    vr[2, :512] = np.asarray(inp["ln_a_g"], f)
    vr[3, :512] = np.asarray(inp["ln_a_b"], f)
    vr[4, :3072] = np.asarray(inp["b_gate"], f)
    vr[5:8, :] = np.asarray(inp["w_dw_f"], f)
    vr[8, :] = np.asarray(inp["b_dw_f"], f)
    w120 = np.ascontiguousarray(np.tile(np.asarray(inp["w_dw_a"], f)[:30], (4, 1)))
    return sc, vr, w120


def run_all(inp):
    key = ("all",)
    if key not in _NC_CACHE:
        _NC_CACHE[key] = build(nch=NCHUNK, do_sample=True)
    nc = _NC_CACHE[key]
    tabs = host_tables(inp)
    f = np.float32
    wnames = ["w_in", "w_proj_a", "w_proj_b", "w_proj_c", "w_mem_kv", "w_out", "w_up", "w_down"]
    shared = {n: np.ascontiguousarray(np.asarray(inp[n], f)) for n in wnames}
    shared.update(tabs)
    sc, vr, w120 = sample_tables(inp)
    shared["sconst"] = sc
    shared["vrows"] = vr
    shared["w120"] = w120
    shared["cache_k"] = np.ascontiguousarray(np.asarray(inp["cache_k"], f)).reshape(N_POOL * 128, 512)
    shared["cache_v"] = np.ascontiguousarray(np.asarray(inp["cache_v"], f)).reshape(N_POOL * 128, 512)
    pt = np.asarray(inp["page_table"]).astype(np.int32)
    in_maps = []
    for c in range(8):
        s = c % 4
        m = dict(shared)
        m["x"] = np.ascontiguousarray(np.asarray(inp["x_prompt"][s], f))
        m["mem"] = np.ascontiguousarray(np.asarray(inp["mem_prompt"][s], f))
        sl = slice(4 * c, 4 * c + 4)
        xs = np.zeros((128, D), f)
        xs[0:4] = np.asarray(inp["x_sample"], f)[sl, 0, :]
        m["xs"] = xs
        m["stc"] = np.ascontiguousarray(np.asarray(inp["state_conv"], f)[sl].reshape(120, 512))
        m["stf"] = np.ascontiguousarray(np.asarray(inp["state_ffn_conv"], f)[sl].reshape(4, 2 * NUP))
        m["cmk"] = np.ascontiguousarray(np.asarray(inp["cache_mem_k"], f)[sl].reshape(4, 256, 512))
        m["cmv"] = np.ascontiguousarray(np.asarray(inp["cache_mem_v"], f)[sl].reshape(4, 256, 512))
        m["ptrep"] = np.ascontiguousarray(np.broadcast_to(pt[sl].reshape(1, 256), (128, 256))).astype(np.int32)
        in_maps.append(m)
    res = run_bass_kernel_spmd(nc, in_maps, core_ids=list(range(8)))
    return res.results


def kernel(**inp):
    r = run_all(inp)
    f = np.float32
    y_p = np.stack([r[s]["y"] for s in range(4)]).astype(f)
    k_p = np.stack([r[s]["k"] for s in range(4)]).reshape(4, SEQ, NH, HD).astype(f)
    v_p = np.stack([r[s]["v"] for s in range(4)]).reshape(4, SEQ, NH, HD).astype(f)
    conv_p = np.stack([r[s]["conv"][2:32] for s in range(4)]).astype(f)
    ffn_p = np.stack([r[s]["ffnc"].reshape(44, 2, 128).transpose(1, 0, 2).reshape(2, NUP) for s in range(4)]).astype(f)
    mk_p = np.stack([r[s]["mk"] for s in range(4)]).reshape(4, 256, 4, 128).astype(f)
    mv_p = np.stack([r[s]["mv"] for s in range(4)]).reshape(4, 256, 4, 128).astype(f)
    y_s = np.concatenate([r[c]["ys"] for c in range(8)]).reshape(32, 1, D).astype(f)
    k_s = np.concatenate([r[c]["ks"] for c in range(8)]).reshape(32, 1, NH, HD).astype(f)
    v_s = np.concatenate([r[c]["vs"] for c in range(8)]).reshape(32, 1, NH, HD).astype(f)
    conv_s = np.concatenate([r[c]["convs"] for c in range(8)]).reshape(32, 30, 512).astype(f)
    ffn_s = np.concatenate([r[c]["ffns"] for c in range(8)]).reshape(32, 2, NUP).astype(f)
    return (y_p, y_s, k_p, v_p, k_s, v_s, conv_p, conv_s, ffn_p, ffn_s, mk_p, mv_p)
```

```python
import contextlib
import numpy as np
import concourse.bass as bass
import concourse.mybir as mybir
from concourse.bass_utils import run_bass_kernel_spmd

F32 = mybir.dt.float32
BF16 = mybir.dt.bfloat16
I32 = mybir.dt.int32
AF = mybir.ActivationFunctionType
ALU = mybir.AluOpType
AX = mybir.AxisListType

D = 1024
SEQ = 4096
NT = 512
NCHUNK = SEQ // NT
CONV_CH = 512
CW = 31
NH = 8
HD = 64
DFF = 2816
NUP = 2 * DFF
NIN = 6144
EPS = 1e-6
NEG = -30000.0
FFN_MUL_ENG = "dve"
CONV_POOL_CC = 0
CONV_PER_HEAD = (31 * (4 - CONV_POOL_CC) + 7) // 8
N_POOL = 2560


class Sched:
    ENGS = ("pe", "act", "dve", "pool", "sp")

    def __init__(self, nc):
        self.nc = nc
        self.q = {e: [] for e in self.ENGS}
        self.last_w = {}
        self.readers = {}
        self.dma_cnt = {}
        self.n_ops = 0
        self.alias = {}

    def op(self, eng, fn, reads=(), writes=(), dma=False, semkey=None):
        rec = {"fn": fn, "eng": eng, "deps": [], "need_inc": False, "dma": dma,
               "semkey": None, "dval": 0}
        self.n_ops += 1
        deps = []
        for k in reads:
            w = self.last_w.get(k)
            if w is not None:
                deps.append(w)
        for k in writes:
            w = self.last_w.get(k)
            if w is not None:
                deps.append(w)
            for r in self.readers.get(k, ()):
                deps.append(r)
            for a in self.alias.get(k, ()):
                w = self.last_w.get(a)
                if w is not None:
                    deps.append(w)
                for r in self.readers.get(a, ()):
                    deps.append(r)
        if dma:
            assert semkey is not None
            rec["semkey"] = semkey
            self.dma_cnt[semkey] = self.dma_cnt.get(semkey, 0) + 16
            rec["dval"] = self.dma_cnt[semkey]
            me = ("d", semkey, rec["dval"])
        else:
            me = ("c", rec)
        seen = set()
        for d in deps:
            if d[0] == "c":
                p = d[1]
                if p is rec:
                    continue
                if p["eng"] == eng and eng == "pe":
                    continue
                if id(p) in seen:
                    continue
                seen.add(id(p))
                p["need_inc"] = True
                rec["deps"].append(d)
            else:
                key = (d[1], d[2])
                if key in seen:
                    continue
                seen.add(key)
                rec["deps"].append(d)
        for k in writes:
            self.last_w[k] = me
            self.readers[k] = []
        for k in reads:
            if k in writes:
                continue
            self.readers.setdefault(k, []).append(me)
        self.q[eng].append(rec)
        return rec

    def emit(self, st):
        nc = self.nc
        esem = {e: st.enter_context(nc.semaphore("e_" + e)) for e in self.ENGS}
        dsem = {}
        for i, k in enumerate(self.dma_cnt):
            dsem[k] = st.enter_context(nc.semaphore("d%d" % i))
        for e in self.ENGS:
            c = 0
            for rec in self.q[e]:
                if rec["need_inc"] and not rec["dma"]:
                    c += 1
                    rec["inc_val"] = c
        block = st.enter_context(nc.Block())
        hw = {"pe": nc.tensor, "act": nc.scalar, "dve": nc.vector, "pool": nc.gpsimd, "sp": nc.sync}

        def run(e):
            eng = hw[e]
            waited = {}
            for rec in self.q[e]:
                for d in rec["deps"]:
                    if d[0] == "c":
                        p = d[1]
                        sem = esem[p["eng"]]
                        val = p["inc_val"]
                        key = ("e", p["eng"])
                    else:
                        sem = dsem[d[1]]
                        val = d[2]
                        key = ("d", d[1])
                    if waited.get(key, 0) >= val:
                        continue
                    waited[key] = val
                    eng.wait_ge(sem, val)
                ins = rec["fn"](eng)
                if rec["dma"]:
                    ins.then_inc(dsem[rec["semkey"]], 16)
                elif rec["need_inc"]:
                    ins.then_inc(esem[e], 1)
            if e == "sp":
                for k, v in self.dma_cnt.items():
                    eng.wait_ge(dsem[k], v)

        block.tensor(lambda _e: run("pe"))
        block.scalar(lambda _e: run("act"))
        block.vector(lambda _e: run("dve"))
        block.gpsimd(lambda _e: run("pool"))
        block.sync(lambda _e: run("sp"))


FM_G1, FM_G2, FM_GM, FM_BG, FM_WDA, FM_BDA, FM_LAG, FM_LAB, FM_WDF, FM_BDF = 0, 8, 16, 24, 48, 172, 176, 180, 184, 316
FM_N = 360
BC_GQ, BC_GK, BC_GMQ, BC_GMK = 0, 512, 1024, 1536
BC_N = 2048


SC_RROW, SC_CS, SC_ST, SC_REP, SC_E8, SC_AL, SC_PI, SC_IND = 0, 512, 643, 898, 1026, 2050, 2562, 2563
SC_N = 2691


def build(nch=NCHUNK, do_sample=True, dbg=False):
    nc = bass.Bass("TRN2", target_bir_lowering=False)

    def din(name, shape, dt=F32):
        return nc.dram_tensor(name, list(shape), dt, kind="ExternalInput").ap()

    def dout(name, shape, dt=F32):
        return nc.dram_tensor(name, list(shape), dt, kind="ExternalOutput").ap()

    xin = din("x", [SEQ, D])
    memin = din("mem", [256, D])
    w_in = din("w_in", [D, NIN])
    w_pa = din("w_proj_a", [512, D])
    w_pb = din("w_proj_b", [512, D])
    w_pc = din("w_proj_c", [512, D])
    w_mkv = din("w_mem_kv", [D, D])
    w_out = din("w_out", [D, D])
    w_up = din("w_up", [D, NUP])
    w_down = din("w_down", [DFF, D])
    fm_in = din("fm", [128, FM_N])
    bc_in = din("bc", [128, BC_N])
    ident_in = din("ident", [128, 128])
    tri_in = din("tri", [128, 128])
    abase_in = din("abase", [128, NH * 32])
    qal_in = din("qal", [128, 4 * NH * 2])

    if do_sample:
        xs_in = din("xs", [128, D])
        stc_in = din("stc", [120, 512])
        w120_in = din("w120", [120, 512])
        stf_in = din("stf", [4, 2 * NUP])
        cmk_in = din("cmk", [4, 256, 512])
        cmv_in = din("cmv", [4, 256, 512])
        pt_in = din("ptrep", [128, 256], I32)
        ck_in = din("cache_k", [N_POOL * 128, 512])
        cv_in = din("cache_v", [N_POOL * 128, 512])
        sc_in = din("sconst", [128, SC_N])
        vrow_in = din("vrows", [9, 5632])
        ys_o = dout("ys", [4, D])
        ks_o = dout("ks", [4, 512])
        vs_o = dout("vs", [4, 512])
        convs_o = dout("convs", [4, 30, 512])
        ffns_o = dout("ffns", [4, 2, NUP])
    y_o = dout("y", [SEQ, D])
    k_o = dout("k", [SEQ, 512])
    v_o = dout("v", [SEQ, 512])
    conv_o = dout("conv", [32, 512])
    ffnc_o = dout("ffnc", [88, 128])
    mk_o = dout("mk", [256, 512])
    mv_o = dout("mv", [256, 512])
    kt_scr = nc.dram_tensor("kt_scr", [NH, 82, SEQ], BF16, kind="Internal").ap()
    v_scr = nc.dram_tensor("v_scr", [NH, SEQ // 128, 128, 128], BF16, kind="Internal").ap()

    st = contextlib.ExitStack()
    with st:
        def sb(name, shape, dt):
            return st.enter_context(nc.sbuf_tensor("s_" + name, list(shape), dt))

        def psum(name, shape, dt):
            return st.enter_context(nc.psum_tensor("p_" + name, list(shape), dt))

        S = Sched(nc)
        op = S.op
        dbg_n = [0]

        def dbgout(src_ap, shape, keys, dt=F32):
            if not dbg:
                return
            name = "dbg%d" % dbg_n[0]
            dbg_n[0] += 1
            t = nc.dram_tensor(name, list(shape), dt, kind="ExternalOutput").ap()
            op("pool", lambda e: e.dma_start(out=t, in_=src_ap), reads=keys, writes=["hbm_" + name], dma=True, semkey="dbg")

        identb = sb("identb", [128, 128], BF16)
        identf = sb("identf", [128, 128], F32)
        trib = sb("trib", [128, 128], BF16)
        onesb = sb("onesb", [128, 128], BF16)
        onesf = sb("onesf", [128, 128], F32)
        fm = sb("fm", [128, FM_N], F32)
        bc = sb("bc", [128, BC_N], F32)
        btab = sb("btab", [128, NH * 32], F32)
        mvals = sb("mvals", [128, 8], F32)
        epsb = sb("epsb", [128, 1], F32)
        X0 = sb("X0", [128, 4, D], F32)
        Xb = [X0, X0]
        X = Xb[0]
        xn = sb("xn", [128, D], BF16)
        ss = sb("ss", [128, 16], F32)
        hT = sb("hT", [128, 8, NT], BF16)
        uT = sb("uT", [128, 4, 30 + NT], F32)
        cT = sb("cT", [128, 4, NT], F32)
        sT = sb("sT", [128, 4, NT], BF16)
        tmpA = sb("tmpA", [128, NT], F32)
        tmpB = sb("tmpB", [128, NT], F32)
        Qaug = sb("Qaug", [128, 4, NH, 82], BF16)
        Kaug = sb("Kaug", [128, 4, NH, 82], BF16)
        Vloc = sb("Vloc", [128, 4, NH, 128], BF16)
        mqT = sb("mqT", [128, 4, NT], BF16)
        qn_bf = sb("qn_bf", [128, 512], BF16)
        kn_bf = sb("kn_bf", [128, 2, 512], BF16)
        kst = sb("kst", [128, 2, 512], F32)
        vst = sb("vst", [128, 2, 512], F32)
        kmBD = sb("kmBD", [128, 4, 2, 16], BF16)
        gatef = sb("gatef", [128, NH, 16], F32)
        top8 = sb("top8", [128, NH, 8], F32)
        thr = sb("thr", [128, NH], F32)
        nsel = sb("nsel", [128, NH, 16], F32)
        PT = sb("PT", [128, 3, NT], BF16)
        OT = sb("OT", [128, 4, NT], BF16)
        OmT = sb("OmT", [128, 4, NT], BF16)
        rden = sb("rden", [128, NT], F32)
        mkT = sb("mkT", [128, 4, 256], BF16)
        mVa = sb("mVa", [128, 2, 512], BF16)
        merged = sb("merged", [128, 8, NT], BF16)
        scrA = sb("scrA", [128, 22 * NT], BF16)
        sa = scrA[:, :].rearrange("p (j t) -> p j t", j=22)
        KTloc = scrA[:, 0:4096].rearrange("p (j t) -> p j t", j=NH)
        qTa = scrA[:, 4096:8192].rearrange("p (j t) -> p j t", j=NH)
        mqn = scrA[:, 8192:10240].rearrange("p (j t) -> p j t", j=4)
        qTp = scrA[:, 10240:10752].rearrange("p (j t) -> p j t", j=4)
        cbuf = sb("cbuf", [128, 3, NT], F32)
        tmpC = cbuf[:, 0, :]
        tmpD = cbuf[:, 1, :]
        sig = cT
        junk = kn_bf[:, :, :].rearrange("p a t -> p (a t)")
        for _k in ("KTloc", "qTa", "mqn", "qTp"):
            S.alias[_k] = [("sa", j) for j in range(22)]
        for j in range(22):
            S.alias[("sa", j)] = ["KTloc", "qTa", "mqn", "qTp"]
        hist = sb("hist", [128, 44, 2], F32)
        corr = sb("corr", [128, 44, 2], F32)
        corrt = sb("corrt", [128, 44], F32)
        ring = sb("ring", [128, 4, 4096], BF16)
        gring = sb("gring", [128, 4, 1024], BF16)
        kvK = sb("kvK", [128, 4, NT], BF16)
        kvV = sb("kvV", [128, 4, 4, 128], BF16)
        outst = sb("outst", [128, 512], F32)
        idx_t = sb("idx_t", [128, 256], I32)
        orowb = sb("orowb", [128, 132], BF16)
        ptf = sb("ptf", [128, 256], F32)

        psA = [psum("psA%d" % i, [128, 512], F32) for i in range(6)]
        psT = psum("psT", [128, 1024], BF16)
        psM = psum("psM", [128, 512], F32)
        PA = ["psA%d" % i for i in range(6)]

        def dma(eng, out, in_, reads, writes, semkey):
            return op(eng, lambda e: e.dma_start(out=out, in_=in_), reads=reads, writes=writes, dma=True, semkey=semkey)

        def mm(out, lhsT, rhs, start, stop, reads, writes):
            return op("pe", lambda e: e.matmul(out, lhsT=lhsT, rhs=rhs, start=start, stop=stop, skip_group_check=True),
                      reads=reads, writes=writes)

        def tr(out, in_, ident, reads, writes):
            return op("pe", lambda e: e.transpose(out=out, in_=in_, identity=ident), reads=reads, writes=writes)

        rstate = {"i": 0, "g": 0}

        def load_w(view, nk, ncols):
            i = rstate["i"] % 4
            rstate["i"] += 1
            dst = ring[:, i, 0:nk * ncols].rearrange("p (k n) -> p k n", k=nk)
            dma("pool", dst, view, [], [("ring", i)], "ring%d" % i)
            return dst, ("ring", i)

        def load_g(view):
            i = rstate["g"] % 4
            rstate["g"] += 1
            dst = gring[:, i, :].rearrange("p (k n) -> p k n", k=8)
            dma("pool", dst, view, [], [("gring", i)], "gring%d" % i)
            return dst, ("gring", i)

        win_v = w_in.rearrange("(kc p) n -> p kc n", p=128)
        wup_v = w_up.rearrange("(kc p) n -> p kc n", p=128)
        wout_v = w_out.rearrange("(kc p) n -> p kc n", p=128)
        wdn_v = w_down.rearrange("(kc p) n -> p kc n", p=128)
        wmkv_v = w_mkv.rearrange("(kc p) n -> p kc n", p=128)
        wpa_v = w_pa.rearrange("(kc p) n -> p kc n", p=128)
        wpb_v = w_pb.rearrange("(kc p) n -> p kc n", p=128)
        wpc_v = w_pc.rearrange("(kc p) n -> p kc n", p=128)

        dma("sp", fm[:, :], fm_in[:, :], [], ["fm"], "c1")
        dma("sp", bc[:, :], bc_in[:, :], [], ["bc"], "c2")
        dma("sp", identf[:, :], ident_in[:, :], [], ["identf"], "c3")
        dma("sp", btab[:, :], abase_in[:, :], [], ["btab"], "c4")
        dma("sp", tmpA[:, 0:128], tri_in[:, :], [], ["tmpA"], "c5")
        dma("sp", tmpB[:, 0:64], qal_in[:, :], [], ["tmpB"], "c6")
        op("dve", lambda e: e.tensor_copy(out=identb[:, :], in_=identf[:, :]), ["identf"], ["identb"])
        op("dve", lambda e: e.tensor_copy(out=trib[:, :], in_=tmpA[:, 0:128]), ["tmpA"], ["trib"])
        op("dve", lambda e: e.memset(onesb[:, :], 1.0), [], ["onesb"])
        op("dve", lambda e: e.memset(onesf[:, :], 1.0), [], ["onesf"])
        op("dve", lambda e: e.memset(epsb[:, :], EPS), [], ["epsb"])
        op("dve", lambda e: e.memset(uT[:, :, :], 0.0), [], ["uT"])
        op("dve", lambda e: e.memset(hist[:, :, :], 0.0), [], ["hist"])
        op("dve", lambda e: e.memset(kmBD[:, :, :, :], 0.0), [], ["kmBD"])
        op("dve", lambda e: e.memset(gatef[:, :, :], -1e30), [], ["gatef"])
        op("dve", lambda e: e.memset(Vloc[:, :, :, 0:64], 1.0), [], ["Vloc"])
        op("dve", lambda e: e.memset(Kaug[:, :, :, 80:82], 1.0), [], ["Kaug"])
        op("dve", lambda e: e.tensor_copy(out=Qaug[:, :, :, 80:82],
                                          in_=tmpB[:, 0:64].rearrange("p (s h r) -> p s h r", s=4, h=NH)),
           ["tmpB"], ["Qaug"])
        for j, (off, n) in enumerate(((BC_GQ, 64), (BC_GK, 64), (BC_GMQ, 128), (BC_GMK, 128))):
            op("dve", lambda e, j=j, off=off, n=n: e.tensor_reduce(out=mvals[:, j:j + 1], in_=bc[:, off:off + n], axis=AX.X,
                                                                  op=ALU.max, apply_absolute_value=True),
               ["bc"], ["mvals"])
        op("dve", lambda e: e.tensor_tensor(out=mvals[:, 4:5], in0=mvals[:, 0:1], in1=mvals[:, 1:2], op=ALU.mult), ["mvals"], ["mvals"])
        op("dve", lambda e: e.tensor_scalar(out=mvals[:, 4:5], in0=mvals[:, 4:5], scalar1=8.0, scalar2=None, op0=ALU.mult), ["mvals"], ["mvals"])
        op("dve", lambda e: e.tensor_tensor(out=mvals[:, 5:6], in0=mvals[:, 2:3], in1=mvals[:, 3:4], op=ALU.mult), ["mvals"], ["mvals"])
        op("dve", lambda e: e.tensor_scalar(out=mvals[:, 5:6], in0=mvals[:, 5:6], scalar1=-(128.0 ** 0.5), scalar2=None, op0=ALU.mult), ["mvals"], ["mvals"])
        op("dve", lambda e: e.tensor_scalar(out=btab[:, :], in0=btab[:, :], scalar1=mvals[:, 4:5], scalar2=None, op0=ALU.subtract),
           ["btab", "mvals"], ["btab"])
        op("dve", lambda e: e.tensor_scalar(out=bc[:, BC_GQ:BC_GQ + 512], in0=bc[:, BC_GQ:BC_GQ + 512], scalar1=HD ** -0.5, scalar2=None, op0=ALU.mult), ["bc"], ["bc"])
        op("dve", lambda e: e.tensor_scalar(out=bc[:, BC_GMQ:BC_GMQ + 512], in0=bc[:, BC_GMQ:BC_GMQ + 512], scalar1=128.0 ** -0.5, scalar2=None, op0=ALU.mult), ["bc"], ["bc"])

        def XK(b):
            return [("X", 0, sub) for sub in range(4)]

        def rms_rows(src3, sub, col, srckey="X"):
            op("dve", lambda e: e.memset(ss[:, col:col + 1], 0.0), [], [("ss", col)])
            op("act", lambda e: e.activation(out=junk[:, :], in_=src3[:, sub, :], func=AF.Square, accum_out=ss[:, col:col + 1]),
               [srckey, ("ss", col)], ["kn_bf", ("ss", col)])
            op("act", lambda e: e.activation(out=ss[:, col:col + 1], in_=ss[:, col:col + 1], func=AF.Sqrt, bias=epsb[:, 0:1], scale=1.0 / D),
               [("ss", col), "epsb"], [("ss", col)])
            op("dve", lambda e: e.reciprocal(out=ss[:, col:col + 1], in_=ss[:, col:col + 1]), [("ss", col)], [("ss", col)])

        def norm_A(src3, sub, srckey):
            op("dve", lambda e: e.tensor_scalar(out=xn[:, :], in0=src3[:, sub, :], scalar1=ss[:, sub:sub + 1], scalar2=None, op0=ALU.mult),
               [srckey, ("ss", sub)], ["xn"])

        def norm_B(sub, goff, dstT, dkey):
            for kc in range(8):
                tr(psT[:, kc * 128:(kc + 1) * 128], xn[:, kc * 128:(kc + 1) * 128], identb[:, :], ["xn", "identb"], ["psT"])
            op("dve", lambda e: e.tensor_tensor(
                out=dstT[:, :, sub * 128:(sub + 1) * 128], in0=psT[:, :].rearrange("p (k t) -> p k t", k=8),
                in1=fm[:, goff:goff + 8].unsqueeze(2).broadcast_to([128, 8, 128]), op=ALU.mult),
               ["psT", "fm"], [dkey])

        def norm_to_T(src3, nsub, goff, dstT, dkey, srckey):
            keys = srckey if isinstance(srckey, list) else [srckey] * nsub
            for sub in range(nsub):
                rms_rows(src3, sub, sub, keys[sub])
            for sub in range(nsub):
                norm_A(src3, sub, keys[sub])
                norm_B(sub, goff, dstT, dkey)

        def headnorm(ps, pkey, nh, hd, goff, out_ap, okey, extra_reads=()):
            op("act", lambda e: e.activation(out=tmpA[:, :], in_=ps[:, :], func=AF.Square), [pkey], ["tmpA"])
            op("dve", lambda e: e.tensor_reduce(out=ss[:, 8:8 + nh], in_=tmpA[:, :].rearrange("p (h d) -> p h d", h=nh), axis=AX.X, op=ALU.add),
               ["tmpA"], ["ss8"])
            op("act", lambda e: e.activation(out=ss[:, 8:8 + nh], in_=ss[:, 8:8 + nh], func=AF.Sqrt, bias=epsb[:, 0:1], scale=1.0 / hd),
               ["ss8", "epsb"], ["ss8"])
            op("dve", lambda e: e.reciprocal(out=ss[:, 8:8 + nh], in_=ss[:, 8:8 + nh]), ["ss8"], ["ss8"])
            op("dve", lambda e: e.tensor_tensor(out=tmpB[:, :].rearrange("p (h d) -> p h d", h=nh), in0=ps[:, :].rearrange("p (h d) -> p h d", h=nh),
                                                in1=ss[:, 8:8 + nh].unsqueeze(2).broadcast_to([128, nh, hd]), op=ALU.mult),
               [pkey, "ss8"], ["tmpB"])
            op("dve", lambda e: e.tensor_tensor(out=out_ap, in0=tmpB[:, :].rearrange("p (h d) -> p h d", h=nh),
                                                in1=bc[:, goff:goff + 512].rearrange("p (h d) -> p h d", h=nh), op=ALU.mult),
               ["tmpB", "bc"] + list(extra_reads), [okey])

        dma("sp", Xb[1][:, 0:2, :], memin.rearrange("(s p) d -> p s d", p=128), [], XK(1)[0:2], "xload2")
        norm_to_T(Xb[1], 2, FM_GM, hT, "hT", XK(1)[0:2])
        for half in range(2):
            W, wk = load_w(wmkv_v[:, :, half * 512:(half + 1) * 512], 8, 512)
            for sub in range(2):
                ps = psA[sub]
                for kc in range(8):
                    mm(ps[:, :], hT[:, kc, sub * 128:(sub + 1) * 128], W[:, kc, :], kc == 0, kc == 7, ["hT", wk], [PA[sub]])
                if half == 0:
                    headnorm(ps, PA[sub], 4, 128, BC_GMK, kst[:, sub, :].rearrange("p (h d) -> p h d", h=4), ("kst", sub))
                    dma("sp", mk_o[sub * 128:(sub + 1) * 128, :], kst[:, sub, :], [("kst", sub)], ["hbm_mk"], "st_k%d" % sub)
                    op("dve", lambda e, sub=sub: e.tensor_copy(out=mqn[:, sub, :], in_=kst[:, sub, :]), [("kst", sub)], ["mqn"])
                    for mh in range(4):
                        tr(psT[:, mh * 128:(mh + 1) * 128], mqn[:, sub, mh * 128:(mh + 1) * 128], identb[:, :], ["mqn", "identb"], ["psT"])
                    op("dve", lambda e, sub=sub: e.tensor_copy(out=mkT[:, :, sub * 128:(sub + 1) * 128],
                                                               in_=psT[:, 0:512].rearrange("p (k t) -> p k t", k=4)), ["psT"], ["mkT"])
                else:
                    op("act", lambda e, sub=sub, ps=ps: e.activation(out=vst[:, sub, :], in_=ps[:, :], func=AF.Copy), [PA[sub]], [("vst", sub)])
                    dma("sp", mv_o[sub * 128:(sub + 1) * 128, :], vst[:, sub, :], [("vst", sub)], ["hbm_mv"], "st_v%d" % sub)
                    op("dve", lambda e, sub=sub: e.tensor_copy(out=mVa[:, sub, :], in_=vst[:, sub, :]), [("vst", sub)], ["mVa"])

        kvstate = {"i": 0}
        for c in range(nch):
            t0 = c * NT
            xb = c % 2
            X = Xb[xb]
            xks = XK(xb)
            dma("sp", X[:, :, :], xin[t0:t0 + NT, :].rearrange("(s p) d -> p s d", p=128), [], xks, "xload")
            norm_to_T(X, 4, FM_G1, hT, "hT", xks)

            Wa, wka = load_w(win_v[:, :, 0:512], 8, 512)
            Wg, wkg = load_w(win_v[:, :, 512:1024], 8, 512)
            for cc in range(4):
                pa, pg = psA[2 * (cc % 2)], psA[2 * (cc % 2) + 1]
                ka, kg = PA[2 * (cc % 2)], PA[2 * (cc % 2) + 1]
                for kc in range(8):
                    mm(pa[:, :], Wa[:, kc, cc * 128:(cc + 1) * 128], hT[:, kc, :], kc == 0, kc == 7, ["hT", wka], [ka])
                for kc in range(8):
                    mm(pg[:, :], Wg[:, kc, cc * 128:(cc + 1) * 128], hT[:, kc, :], kc == 0, kc == 7, ["hT", wkg], [kg])
                tk = ("cbuf", 0) if cc % 2 == 0 else ("cbuf", 1)
                tt = tmpC if cc % 2 == 0 else tmpD
                op("act", lambda e, pg=pg, tt=tt: e.activation(out=tt[:, :], in_=pg[:, :], func=AF.Sigmoid), [kg], [tk])
                op("dve", lambda e, pa=pa, tt=tt, cc=cc: e.tensor_tensor(out=uT[:, cc, 30:30 + NT], in0=pa[:, :], in1=tt[:, :], op=ALU.mult),
                   [ka, tk], ["uT"])
            conv_steps = []
            pool_steps = []
            for cc in range(4):
                ceng = "pool" if cc >= 4 - CONV_POOL_CC else "dve"
                clist = pool_steps if ceng == "pool" else conv_steps
                clist.append(lambda cc=cc, ceng=ceng: op(ceng, lambda e: e.tensor_scalar(
                    out=cT[:, cc, :], in0=uT[:, cc, 0:NT], scalar1=fm[:, FM_WDA + cc * 31:FM_WDA + cc * 31 + 1],
                    scalar2=fm[:, FM_BDA + cc:FM_BDA + cc + 1], op0=ALU.mult, op1=ALU.add), ["uT", "fm"], [("cT", cc)]))
                for k in range(1, CW):
                    clist.append(lambda cc=cc, k=k, ceng=ceng: op(ceng, lambda e: e.scalar_tensor_tensor(
                        out=cT[:, cc, :], in0=uT[:, cc, k:k + NT], scalar=fm[:, FM_WDA + cc * 31 + k:FM_WDA + cc * 31 + k + 1],
                        in1=cT[:, cc, :], op0=ALU.mult, op1=ALU.add), ["uT", "fm", ("cT", cc)], [("cT", cc)]))

            def conv_finish(c=c):
                if c == nch - 1:
                    for cc in range(4):
                        op("pe", lambda e, cc=cc: e.transpose(out=psM[0:32, cc * 128:(cc + 1) * 128], in_=uT[:, cc, 30 + NT - 32:30 + NT], identity=identf[:, :]),
                           ["uT", "identf"], ["psM"])
                    op("dve", lambda e: e.tensor_copy(out=outst[0:32, :], in_=psM[0:32, :]), ["psM"], ["outst"])
                    dma("sp", conv_o[:, :], outst[0:32, :], ["outst"], ["hbm_conv"], "st_misc")
                op("dve", lambda e: e.tensor_copy(out=uT[:, :, 0:30], in_=uT[:, :, NT:NT + 30]), ["uT"], ["uT"])
                for cc in range(4):
                    mm(psA[4][:, :], onesf[:, :], cT[:, cc, :], cc == 0, cc == 3, ["onesf", ("cT", cc)], [PA[4]])
                for cc in range(4):
                    tt, tk = (tmpC, ("cbuf", 0)) if cc % 2 == 0 else (tmpD, ("cbuf", 1))
                    op("act", lambda e, cc=cc, tt=tt: e.activation(out=tt[:, :], in_=cT[:, cc, :], func=AF.Square), [("cT", cc)], [tk])
                    mm(psA[5][:, :], onesf[:, :], tt[:, :], cc == 0, cc == 3, ["onesf", tk], [PA[5]])
                op("dve", lambda e: e.tensor_scalar(out=tmpA[:, :], in0=psA[4][:, :], scalar1=1.0 / 512, scalar2=None, op0=ALU.mult), [PA[4]], ["tmpA"])
                op("dve", lambda e: e.tensor_tensor(out=tmpB[:, :], in0=tmpA[:, :], in1=tmpA[:, :], op=ALU.mult), ["tmpA"], ["tmpB"])
                op("dve", lambda e: e.scalar_tensor_tensor(out=tmpB[:, :], in0=psA[5][:, :], scalar=1.0 / 512, in1=tmpB[:, :], op0=ALU.mult, op1=ALU.subtract),
                   [PA[5], "tmpB"], ["tmpB"])
                op("act", lambda e: e.activation(out=tmpB[:, :], in_=tmpB[:, :], func=AF.Sqrt, bias=epsb[:, 0:1], scale=1.0), ["tmpB", "epsb"], ["tmpB"])
                op("dve", lambda e: e.reciprocal(out=tmpB[:, :], in_=tmpB[:, :]), ["tmpB"], ["tmpB"])
                for cc in range(4):
                    op("dve", lambda e, cc=cc: e.tensor_tensor(out=cT[:, cc, :], in0=cT[:, cc, :], in1=tmpA[:, :], op=ALU.subtract), [("cT", cc), "tmpA"], [("cT", cc)])
                    op("dve", lambda e, cc=cc: e.tensor_tensor(out=cT[:, cc, :], in0=cT[:, cc, :], in1=tmpB[:, :], op=ALU.mult), [("cT", cc), "tmpB"], [("cT", cc)])
                    op("act", lambda e, cc=cc: e.activation(out=sT[:, cc, :], in_=cT[:, cc, :], func=AF.Silu, scale=fm[:, FM_LAG + cc:FM_LAG + cc + 1],
                                                            bias=fm[:, FM_LAB + cc:FM_LAB + cc + 1]), [("cT", cc), "fm"], ["sT"])

            op("dve", lambda e: e.memset(Kaug[:, :, :, 64:80], 0.0), [], ["Kaug"])
            op("dve", lambda e, c=c: e.memset(Kaug[:, 0:2, :, 64 + 2 * c:65 + 2 * c], 1.0), [], ["Kaug"])
            op("dve", lambda e, c=c: e.memset(Kaug[:, 2:4, :, 65 + 2 * c:66 + 2 * c], 1.0), [], ["Kaug"])
            kvqm = (("k", 1536), ("v", 2048), ("q", 1024), ("mq", 2560))
            wcur = {}

            def emit_mm(i):
                name, col0 = kvqm[i // 4]
                sub = i % 4
                if sub == 0:
                    wcur[name] = load_w(win_v[:, :, col0:col0 + 512], 8, 512)
                W, wk = wcur[name]
                ps, pk = psA[sub], PA[sub]
                for kc in range(8):
                    mm(ps[:, :], hT[:, kc, sub * 128:(sub + 1) * 128], W[:, kc, :], kc == 0, kc == 7, ["hT", wk], [pk])

            def post(i, c=c, t0=t0):
                name = kvqm[i // 4][0]
                sub = i % 4
                ps, pk = psA[sub], PA[sub]
                if name == "k":
                    headnorm(ps, pk, NH, HD, BC_GK, kst[:, sub % 2, :].rearrange("p (h d) -> p h d", h=NH), ("kst", sub % 2))
                    dma("sp", k_o[t0 + sub * 128:t0 + (sub + 1) * 128, :], kst[:, sub % 2, :], [("kst", sub % 2)], ["hbm_k"], "st_k%d" % (sub % 2))
                    op("dve", lambda e, sub=sub: e.tensor_copy(out=Kaug[:, sub, :, 0:64], in_=kst[:, sub % 2, :].rearrange("p (h d) -> p h d", h=NH)),
                       [("kst", sub % 2)], ["Kaug"])
                    op("dve", lambda e, sub=sub: e.tensor_copy(out=kn_bf[:, sub % 2, :], in_=kst[:, sub % 2, :]), [("kst", sub % 2)], ["kn_bf"])
                    for h in range(NH):
                        tr(psT[0:82, h * 128:(h + 1) * 128], Kaug[:, sub, h, :], identb[:, :], ["Kaug", "identb"], ["psT"])
                    op("dve", lambda e, sub=sub: e.tensor_copy(out=KTloc[0:82, :, sub * 128:(sub + 1) * 128],
                                                               in_=psT[0:82, :].rearrange("p (k t) -> p k t", k=NH)), ["psT"], ["KTloc"])
                    if sub % 2 == 1:
                        blk = 2 * c + sub // 2
                        for pair in range(4):
                            for s2 in range(2):
                                mm(psM[:, pair:pair + 1], kn_bf[:, s2, pair * 128:(pair + 1) * 128], onesb[:, 0:1], s2 == 0, s2 == 1,
                                   ["kn_bf", "onesb"], ["psM"])
                        op("dve", lambda e, blk=blk: e.tensor_scalar(out=kmBD[0:64, :, 0, blk], in0=psM[0:64, 0:4], scalar1=1.0 / 256, scalar2=None, op0=ALU.mult),
                           ["psM"], ["kmBD"])
                        op("dve", lambda e, blk=blk: e.tensor_scalar(out=kmBD[64:128, :, 1, blk], in0=psM[64:128, 0:4], scalar1=1.0 / 256, scalar2=None, op0=ALU.mult),
                           ["psM"], ["kmBD"])
                elif name == "v":
                    op("act", lambda e, sub=sub, ps=ps: e.activation(out=vst[:, sub % 2, :], in_=ps[:, :], func=AF.Copy), [pk], [("vst", sub % 2)])
                    dma("sp", v_o[t0 + sub * 128:t0 + (sub + 1) * 128, :], vst[:, sub % 2, :], [("vst", sub % 2)], ["hbm_v"], "st_v%d" % (sub % 2))
                    op("dve", lambda e, sub=sub: e.tensor_copy(out=Vloc[:, sub, :, 64:128], in_=vst[:, sub % 2, :].rearrange("p (h d) -> p h d", h=NH)),
                       [("vst", sub % 2)], ["Vloc"])
                elif name == "q":
                    cur = 2 * c + sub // 2
                    headnorm(ps, pk, NH, HD, BC_GQ, Qaug[:, sub, :, 0:64], "Qaug")
                    if cur == 0:
                        op("dve", lambda e, sub=sub: e.memset(Qaug[:, sub, :, 64:80], NEG), [], ["Qaug"])
                    else:
                        op("dve", lambda e, sub=sub: e.tensor_copy(out=qn_bf[:, :].rearrange("p (h d) -> p h d", h=NH), in_=Qaug[:, sub, :, 0:64]), ["Qaug"], ["qn_bf"])
                        for pair in range(4):
                            tr(psT[:, pair * 128:(pair + 1) * 128], qn_bf[:, pair * 128:(pair + 1) * 128], identb[:, :], ["qn_bf", "identb"], ["psT"])
                        op("dve", lambda e: e.tensor_copy(out=qTp[:, :, :], in_=psT[:, 0:512].rearrange("p (k t) -> p k t", k=4)), ["psT"], ["qTp"])
                        for pair in range(4):
                            mm(psM[:, 32 * pair:32 * pair + 32], qTp[:, pair, :], kmBD[:, pair, :, :].rearrange("p a b -> p (a b)"), True, True, ["qTp", "kmBD"], ["psM"])
                        op("dve", lambda e, cur=cur: e.tensor_copy(out=gatef[:, :, 0:cur], in_=psM[:, 0:128].rearrange("p (h b) -> p h b", h=NH)[:, :, 0:cur]),
                           ["psM"], ["gatef"])
                        for h in range(NH):
                            op("dve", lambda e, h=h: e.max(out=top8[:, h, :], in_=gatef[:, h, :]), ["gatef"], ["top8"])
                        op("dve", lambda e: e.tensor_scalar(out=thr[:, :], in0=top8[:, :, 2], scalar1=-1e29, scalar2=None, op0=ALU.max), ["top8"], ["thr"])
                        op("dve", lambda e: e.tensor_tensor(out=nsel[:, :, :], in0=gatef[:, :, :], in1=thr[:, :].unsqueeze(2).broadcast_to([128, NH, 16]), op=ALU.is_lt),
                           ["gatef", "thr"], ["nsel"])
                        op("dve", lambda e, sub=sub: e.tensor_scalar(out=Qaug[:, sub, :, 64:80], in0=nsel[:, :, :], scalar1=NEG, scalar2=None, op0=ALU.mult),
                           ["nsel"], ["Qaug"])
                    op("dve", lambda e, sub=sub, cur=cur: e.memset(Qaug[:, sub, :, 64 + cur:65 + cur], 0.0), [], ["Qaug"])
                    for h in range(NH):
                        tr(psT[0:82, h * 128:(h + 1) * 128], Qaug[:, sub, h, :], identb[:, :], ["Qaug", "identb"], ["psT"])
                    op("dve", lambda e, sub=sub: e.tensor_copy(out=qTa[0:82, :, sub * 128:(sub + 1) * 128],
                                                               in_=psT[0:82, :].rearrange("p (k t) -> p k t", k=NH)), ["psT"], ["qTa"])
                else:
                    headnorm(ps, pk, 4, 128, BC_GMQ, mqn[:, sub, :].rearrange("p (h d) -> p h d", h=4), "mqn")
                    for mh in range(4):
                        tr(psT[:, mh * 128:(mh + 1) * 128], mqn[:, sub, mh * 128:(mh + 1) * 128], identb[:, :], ["mqn", "identb"], ["psT"])
                    op("dve", lambda e, sub=sub: e.tensor_copy(out=mqT[:, :, sub * 128:(sub + 1) * 128],
                                                               in_=psT[:, 0:512].rearrange("p (k t) -> p k t", k=4)), ["psT"], ["mqT"])
                if name == "v" and sub == 3:
                    dma("sp", kt_scr[:, :, t0:t0 + NT].rearrange("h r k -> r h k"), KTloc[0:82, :, :], ["KTloc"], [("kscr", c)], "st_kscr")
                    for t4 in range(4):
                        dma("sp", v_scr[:, 4 * c + t4, :, :].rearrange("h k c -> k h c"), Vloc[:, t4, :, :], ["Vloc"], [("vscr", c)], "st_vscr")


            emit_mm(0)
            emit_mm(1)
            for i in range(16):
                if i + 2 < 16:
                    emit_mm(i + 2)
                post(i)

            nexp = 0
            steps = []
            for h in range(NH):
                for g in range(c + 1):
                    for tile in range(4):
                        steps.append((h, g, tile))
            LA = 2
            slots = {}

            def qk(i):
                h, g, tile = steps[i]
                if tile == 0:
                    sl = kvstate["i"] % 4
                    kvstate["i"] += 1
                    slots[(h, g)] = sl
                    dma("sp", kvK[0:82, sl, :], kt_scr[h, :, g * NT:(g + 1) * NT], [("kscr", g)], [("kvK", sl)], "kvK%d" % sl)
                    dma("sp", kvV[:, sl, :, :], v_scr[h, 4 * g:4 * g + 4, :, :].rearrange("t k c -> k t c"), [("vscr", g)], [("kvV", sl)], "kvV%d" % sl)
                sl = slots[(h, g)]
                diag = (g == c)
                col0 = 128 * tile if diag else 0
                sp_, sk = psA[2 + i % 4], PA[2 + i % 4]
                mm(sp_[:, col0:NT], kvK[0:82, sl, tile * 128:(tile + 1) * 128], qTa[0:82, h, col0:NT], True, True, [("kvK", sl), "qTa"], [sk])
                if diag:
                    mm(sp_[:, col0:col0 + 128], identb[:, :], trib[:, :], False, True, ["identb", "trib"], [sk])

            def expv(i):
                h, g, tile = steps[i]
                sl = slots[(h, g)]
                acc, ak = psA[h % 2], PA[h % 2]
                kt = 4 * g + tile
                diag = (g == c)
                col0 = 128 * tile if diag else 0
                sp_, sk = psA[2 + i % 4], PA[2 + i % 4]
                pslot = i % 3
                bcol = h * 32 + (kt - 4 * c + 28)
                op("act", lambda e: e.activation(out=PT[:, pslot, col0:NT], in_=sp_[:, col0:NT], func=AF.Exp, bias=btab[:, bcol:bcol + 1], scale=1.0),
                   [sk, "btab"], [("PT", pslot)])
                mm(acc[:, col0:NT], kvV[:, sl, tile, :], PT[:, pslot, col0:NT], g == 0 and tile == 0, diag and tile == 3, [("kvV", sl), ("PT", pslot)], [ak])
                if diag and tile == 3:
                    op("dve", lambda e: e.reciprocal(out=rden[0:64, :], in_=acc[0:64, :]), [ak], ["rden"])
                    op("dve", lambda e: e.tensor_tensor(out=OT[64 * (h % 2):64 * (h % 2) + 64, h // 2, :], in0=acc[64:128, :], in1=rden[0:64, :], op=ALU.mult),
                       [ak, "rden"], ["OT"])
                    for stp in conv_steps[CONV_PER_HEAD * h:CONV_PER_HEAD * (h + 1)]:
                        stp()

            for stp in pool_steps:
                stp()
            for i in range(min(LA, len(steps))):
                qk(i)
            for i in range(len(steps)):
                if i + LA < len(steps):
                    qk(i + LA)
                expv(i)
            nexp = len(steps)

            msteps = [(mh, t) for mh in range(4) for t in range(2)]

            def mqk(i):
                mh, t = msteps[i]
                sp_, sk = psA[4 + i % 2], PA[4 + i % 2]
                mm(sp_[:, :], mkT[:, mh, t * 128:(t + 1) * 128], mqT[:, mh, :], True, True, ["mkT", "mqT"], [sk])

            def mpv(i):
                mh, t = msteps[i]
                sp_, sk = psA[4 + i % 2], PA[4 + i % 2]
                pslot = i % 3
                o_ps, ok = psA[2 * (mh % 2)], PA[2 * (mh % 2)]
                d_ps, dk = psA[2 * (mh % 2) + 1], PA[2 * (mh % 2) + 1]
                op("act", lambda e: e.activation(out=PT[:, pslot, :], in_=sp_[:, :], func=AF.Exp, bias=mvals[:, 5:6], scale=1.0),
                   [sk, "mvals"], [("PT", pslot)])
                mm(o_ps[:, :], mVa[:, t, mh * 128:(mh + 1) * 128], PT[:, pslot, :], t == 0, t == 1, ["mVa", ("PT", pslot)], [ok])
                mm(d_ps[:, :], onesb[:, :], PT[:, pslot, :], t == 0, t == 1, ["onesb", ("PT", pslot)], [dk])
                if t == 1:
                    op("dve", lambda e: e.reciprocal(out=rden[:, :], in_=d_ps[:, :]), [dk], ["rden"])
                    op("dve", lambda e: e.tensor_tensor(out=OmT[:, mh, :], in0=o_ps[:, :], in1=rden[:, :], op=ALU.mult), [ok, "rden"], ["OmT"])

            mqk(0)
            for i in range(8):
                if i + 1 < 8:
                    mqk(i + 1)
                mpv(i)

            conv_finish()

            Wpa, kpa = load_w(wpa_v, 4, 1024)
            Wpb, kpb = load_w(wpb_v, 4, 1024)
            Wpc, kpc = load_w(wpc_v, 4, 1024)
            for dc in range(8):
                srcs = ((Wpa, kpa, sT, "sT"), (Wpb, kpb, OT, "OT"), (Wpc, kpc, OmT, "OmT"))
                for j in range(3):
                    Wp, kp, src, skey = srcs[j]
                    for kc in range(4):
                        mm(psA[j][:, :], Wp[:, kc, dc * 128:(dc + 1) * 128], src[:, kc, :], kc == 0, kc == 3, [kp, skey], [PA[j]])
                for j in range(3):
                    Wgt, kgt = load_g(win_v[:, :, 3072 + j * 1024 + dc * 128:3072 + j * 1024 + (dc + 1) * 128])
                    for kc in range(8):
                        mm(psA[3 + j][:, :], Wgt[:, kc, :], hT[:, kc, :], kc == 0, kc == 7, [kgt, "hT"], [PA[3 + j]])
                    op("act", lambda e, j=j, dc=dc: e.activation(out=sig[:, j, :], in_=psA[3 + j][:, :], func=AF.Sigmoid,
                                                                 bias=fm[:, FM_BG + j * 8 + dc:FM_BG + j * 8 + dc + 1], scale=1.0),
                       [PA[3 + j], "fm"], [("cT", j)])
                op("dve", lambda e: e.tensor_tensor(out=tmpA[:, :], in0=psA[0][:, :], in1=sig[:, 0, :], op=ALU.mult), [PA[0], ("cT", 0)], ["tmpA"])
                op("dve", lambda e: e.tensor_tensor(out=tmpB[:, :], in0=psA[1][:, :], in1=sig[:, 1, :], op=ALU.mult), [PA[1], ("cT", 1)], ["tmpB"])
                op("dve", lambda e: e.tensor_tensor(out=tmpA[:, :], in0=tmpA[:, :], in1=tmpB[:, :], op=ALU.add), ["tmpA", "tmpB"], ["tmpA"])
                op("dve", lambda e: e.tensor_tensor(out=tmpB[:, :], in0=psA[2][:, :], in1=sig[:, 2, :], op=ALU.mult), [PA[2], ("cT", 2)], ["tmpB"])
                op("dve", lambda e, dc=dc: e.tensor_tensor(out=merged[:, dc, :], in0=tmpA[:, :], in1=tmpB[:, :], op=ALU.add), ["tmpA", "tmpB"], ["merged"])
            Wo = [load_w(wout_v[:, :, half * 512:(half + 1) * 512], 8, 512) for half in range(2)]
            for sub in range(4):
                for half in range(2):
                    W, wk = Wo[half]
                    pb, pbk = psA[(2 * sub + half) % 4], PA[(2 * sub + half) % 4]
                    for kc in range(8):
                        mm(pb[:, :], merged[:, kc, sub * 128:(sub + 1) * 128], W[:, kc, :], kc == 0, kc == 7, ["merged", wk], [pbk])
                    op("dve", lambda e, sub=sub, half=half, pb=pb: e.tensor_tensor(out=X[:, sub, half * 512:(half + 1) * 512], in0=X[:, sub, half * 512:(half + 1) * 512],
                                                                                   in1=pb[:, :], op=ALU.add), [xks[sub], pbk], [xks[sub]])
                rms_rows(X, sub, sub, xks[sub])
                if sub >= 1:
                    norm_A(X, sub - 1, xks[sub - 1])
                    norm_B(sub - 1, FM_G2, hT, "hT")
            norm_A(X, 3, xks[3])
            norm_B(3, FM_G2, hT, "hT")

            wdf3 = fm[:, FM_WDF:FM_WDF + 132].rearrange("p (j k) -> p j k", k=3)
            allhist_k = [("hist", j) for j in range(44)]
            op("dve", lambda e: e.tensor_tensor(out=corr[:, :, 0], in0=hist[:, :, 0], in1=wdf3[:, :, 0], op=ALU.mult), allhist_k + ["fm"], ["corr"])
            op("dve", lambda e: e.tensor_tensor(out=corrt[:, :], in0=hist[:, :, 1], in1=wdf3[:, :, 1], op=ALU.mult), allhist_k + ["fm"], ["corrt"])
            op("dve", lambda e: e.tensor_tensor(out=corr[:, :, 0], in0=corr[:, :, 0], in1=corrt[:, :], op=ALU.add), ["corr", "corrt"], ["corr"])
            op("dve", lambda e: e.tensor_tensor(out=corr[:, :, 1], in0=hist[:, :, 1], in1=wdf3[:, :, 0], op=ALU.mult), allhist_k + ["fm"], ["corr"])
            upw = {}

            def emit_up(j):
                piece, jj = j // 4, j % 4
                if jj == 0:
                    upw[piece] = load_w(wup_v[:, :, piece * 512:(piece + 1) * 512], 8, 512)
                W, wk = upw[piece]
                ps, pk = psA[j % 4], PA[j % 4]
                cb, ck = cbuf[:, j % 3, :], ("cbuf", j % 3)
                for kc in range(8):
                    mm(ps[:, :], W[:, kc, jj * 128:(jj + 1) * 128], hT[:, kc, :], kc == 0, kc == 7, [wk, "hT"], [pk])
                w2 = fm[:, FM_WDF + 3 * j + 2:FM_WDF + 3 * j + 3]
                bb = fm[:, FM_BDF + j:FM_BDF + j + 1]
                op("act", lambda e: e.activation(out=cb, in_=ps[:, :], func=AF.Identity, scale=w2, bias=bb), [pk, "fm"], [ck])
                op("act", lambda e: e.activation(out=hist[:, j, :], in_=ps[:, NT - 2:NT], func=AF.Copy), [pk], [("hist", j)])

            def chain_up(j):
                ps, pk = psA[j % 4], PA[j % 4]
                cb, ck = cbuf[:, j % 3, :], ("cbuf", j % 3)
                w0 = fm[:, FM_WDF + 3 * j:FM_WDF + 3 * j + 1]
                w1 = fm[:, FM_WDF + 3 * j + 1:FM_WDF + 3 * j + 2]
                op("dve", lambda e: e.scalar_tensor_tensor(out=cb[:, 1:NT], in0=ps[:, 0:NT - 1], scalar=w1, in1=cb[:, 1:NT], op0=ALU.mult, op1=ALU.add),
                   [pk, ck, "fm"], [ck])
                op("dve", lambda e: e.scalar_tensor_tensor(out=cb[:, 2:NT], in0=ps[:, 0:NT - 2], scalar=w0, in1=cb[:, 2:NT], op0=ALU.mult, op1=ALU.add),
                   [pk, ck, "fm"], [ck])
                op("dve", lambda e: e.tensor_tensor(out=cb[:, 0:2], in0=cb[:, 0:2], in1=corr[:, j, :], op=ALU.add), [ck, "corr"], [ck])
                if j < 22:
                    op("act", lambda e: e.activation(out=sa[:, j, :], in_=cb, func=AF.Silu), [ck], [("sa", j)])
                else:
                    op(FFN_MUL_ENG, lambda e: e.tensor_tensor(out=sa[:, j - 22, :], in0=sa[:, j - 22, :], in1=cb, op=ALU.mult), [ck, ("sa", j - 22)], [("sa", j - 22)])

            emit_up(0)
            for j in range(44):
                if j + 1 < 44:
                    emit_up(j + 1)
                chain_up(j)
            sakeys = [("sa", j) for j in range(22)]
            Xn, xnk = Xb[1 - xb], XK(1 - xb)
            if False:
                for sub in range(4):
                    rms_rows(Xn, sub, sub, xnk[sub])
                norm_A(Xn, 0, xnk[0])
            grp = 0
            for half in range(2):
                for kp, (k0, k1) in enumerate(((0, 8), (8, 16), (16, 22))):
                    W, wk = load_w(wdn_v[:, k0:k1, half * 512:(half + 1) * 512], k1 - k0, 512)
                    for sub in range(4):
                        for kc in range(k0, k1):
                            mm(psA[sub][:, :], sa[:, kc, sub * 128:(sub + 1) * 128], W[:, kc - k0, :], kc == 0, kc == 21, sakeys + [wk], [PA[sub]])
                    if False and grp < 4:
                        norm_B(grp, FM_G1, hT, "hT")
                        if grp + 1 < 4:
                            norm_A(Xn, grp + 1, xnk[grp + 1])
                    grp += 1
                for sub in range(4):
                    op("dve", lambda e, sub=sub, half=half: e.tensor_tensor(out=X[:, sub, half * 512:(half + 1) * 512], in0=X[:, sub, half * 512:(half + 1) * 512],
                                                                            in1=psA[sub][:, :], op=ALU.add), [xks[sub], PA[sub]], [xks[sub]])
            dma("sp", y_o[t0:t0 + NT, :].rearrange("(s p) d -> p s d", p=128), X[:, :, :], xks, ["hbm_y"], "st_y")


        if do_sample:
            X = Xb[0]
            S.alias["X"] = XK(0) + XK(1)
            sc = X[:, 1:4, :].rearrange("p a t -> p (a t)")
            dma("sp", X[:, 0, :], xs_in[:, :], [], ["X"], "xload")
            dma("sp", sc[:, 0:SC_N], sc_in[:, :], [], ["X"], "xload")
            RROW = lambda n: sc[:, SC_RROW + n * 128:SC_RROW + (n + 1) * 128]
            ONEROW = lambda n: sc[:, SC_CS + 3 - n:SC_CS + 131 - n]
            ONECOL = lambda r: sc[:, SC_ST + 127 - r:SC_ST + 255 - r]
            REP = sc[:, SC_REP:SC_REP + 128]
            E8 = lambda h: sc[0:8, SC_E8 + h * 128:SC_E8 + (h + 1) * 128]
            ALT = sc[:, SC_AL:SC_AL + 512]
            PIOTA = sc[:, SC_PI:SC_PI + 1]
            IND120 = sc[0:120, SC_IND:SC_IND + 128]
            op("dve", lambda e: e.tensor_copy(out=orowb[:, 0:131], in_=sc[:, SC_CS:SC_CS + 131]), ["X"], ["orowb"])
            ONEROWB = lambda n: orowb[:, 3 - n:131 - n]
            HCOL = lambda h: sc[:, SC_ST + 127 - h:SC_ST + 135 - h]
            dma("sp", idx_t[:, :], pt_in[:, :], [], ["idx_t"], "c7")
            op("dve", lambda e: e.tensor_copy(out=ptf[:, :], in_=idx_t[:, :]), ["idx_t"], ["ptf"])
            op("dve", lambda e: e.tensor_scalar(out=ptf[:, :], in0=ptf[:, :], scalar1=128.0, scalar2=None, op0=ALU.mult), ["ptf"], ["ptf"])
            op("dve", lambda e: e.tensor_scalar(out=ptf[:, :], in0=ptf[:, :], scalar1=PIOTA, scalar2=None, op0=ALU.add), ["ptf", "X"], ["ptf"])
            op("dve", lambda e: e.tensor_copy(out=idx_t[:, :], in_=ptf[:, :]), ["ptf"], ["idx_t"])

            for _k in ("stc", "prd", "qb", "mg"):
                S.alias[_k] = ["uT"]
            S.alias["mg"] = ["uT", "stc", "prd"]
            S.alias["cs"] = [("sa", j) for j in range(22)] + ["KTloc", "qTa", "mqn", "qTp"]
            S.alias["stf"] = ["Vloc"]
            norm_to_T(X, 1, FM_G1, hT, "hT", "X")
            us, qs, mqs, ks = cT[:, 0, :], cT[:, 1, :], cT[:, 2, :], cT[:, 3, :]
            vs, stc, prd, qb = uT[:, 0, 0:512], uT[:, 1, 0:512], uT[:, 2, 0:512], uT[:, 3, 0:512]

            def bload(dst, dkey, src_row):
                dma("sp", dst, src_row.partition_broadcast(128), [], [dkey], "bl_" + str(dkey))

            def zpiece(col0):
                W, wk = load_w(win_v[:, :, col0:col0 + 512], 8, 512)
                for kc in range(8):
                    mm(psA[0][:, :], hT[:, kc, 0:128], W[:, kc, :], kc == 0, kc == 7, ["hT", wk], [PA[0]])
                return psA[0], PA[0]

            ps, pk = zpiece(0)
            op("act", lambda e: e.activation(out=tmpA[:, :], in_=psA[0][:, :], func=AF.Copy), [pk], ["tmpA"])
            op("dve", lambda e: e.tensor_copy(out=rden[:, :], in_=tmpA[:, :]), ["tmpA"], ["rden"])
            ps, pk = zpiece(512)
            op("act", lambda e: e.activation(out=tmpB[:, :], in_=psA[0][:, :], func=AF.Sigmoid), [pk], ["tmpB"])
            op("dve", lambda e: e.tensor_tensor(out=us, in0=rden[:, :], in1=tmpB[:, :], op=ALU.mult), ["rden", "tmpB"], [("cT", 0)])
            ps, pk = zpiece(1024)
            headnorm(ps, pk, NH, HD, BC_GQ, qs.rearrange("p (h d) -> p h d", h=NH), ("cT", 1))
            ps, pk = zpiece(1536)
            headnorm(ps, pk, NH, HD, BC_GK, ks.rearrange("p (h d) -> p h d", h=NH), ("cT", 3))
            dma("sp", ks_o[:, :], cT[0:4, 3, :], [("cT", 3)], ["hbm_ks"], "st_s1")
            ps, pk = zpiece(2048)
            op("act", lambda e: e.activation(out=vs, in_=psA[0][:, :], func=AF.Copy), [pk], ["uT"])
            dma("sp", vs_o[:, :], uT[0:4, 0, 0:512], ["uT"], ["hbm_vs"], "st_s2")
            ps, pk = zpiece(2560)
            headnorm(ps, pk, 4, 128, BC_GMQ, mqs.rearrange("p (h d) -> p h d", h=4), ("cT", 2))

            dma("sp", uT[0:120, 1, 0:512], stc_in[:, :], [], ["stc"], "c8")
            dma("sp", uT[0:120, 2, 0:512], w120_in[:, :], [], ["prd"], "c9")
            for n in range(4):
                dma("sp", convs_o[n, 0:29, :], uT[n * 30 + 1:n * 30 + 30, 1, 0:512], ["stc"], ["hbm_convs"], "st_s3")
            dma("sp", convs_o[:, 29, :], cT[0:4, 0, :], [("cT", 0)], ["hbm_convs"], "st_s3")
            op("dve", lambda e: e.tensor_tensor(out=uT[0:120, 2, 0:512], in0=uT[0:120, 2, 0:512], in1=uT[0:120, 1, 0:512], op=ALU.mult), ["stc", "prd"], ["prd"])
            mm(psA[1][:, :], IND120, uT[0:120, 2, 0:512], True, True, ["prd", "X"], [PA[1]])
            bload(cbuf[:, 0, :], ("cbuf", 0), vrow_in[0, 0:512])
            bload(cbuf[:, 1, :], ("cbuf", 1), vrow_in[1, 0:512])
            op("dve", lambda e: e.tensor_tensor(out=tmpA[:, :], in0=us, in1=cbuf[:, 0, :], op=ALU.mult), [("cT", 0), ("cbuf", 0)], ["tmpA"])
            op("dve", lambda e: e.tensor_tensor(out=tmpA[:, :], in0=tmpA[:, :], in1=cbuf[:, 1, :], op=ALU.add), ["tmpA", ("cbuf", 1)], ["tmpA"])
            op("dve", lambda e: e.tensor_tensor(out=tmpA[:, :], in0=tmpA[:, :], in1=psA[1][:, :], op=ALU.add), ["tmpA", PA[1]], ["tmpA"])
            op("dve", lambda e: e.tensor_reduce(out=ss[:, 0:1], in_=tmpA[:, :], axis=AX.X, op=ALU.add), ["tmpA"], [("ss", 0)])
            op("dve", lambda e: e.tensor_scalar(out=ss[:, 0:1], in0=ss[:, 0:1], scalar1=1.0 / 512, scalar2=None, op0=ALU.mult), [("ss", 0)], [("ss", 0)])
            op("dve", lambda e: e.tensor_scalar(out=tmpA[:, :], in0=tmpA[:, :], scalar1=ss[:, 0:1], scalar2=None, op0=ALU.subtract), ["tmpA", ("ss", 0)], ["tmpA"])
            op("dve", lambda e: e.memset(ss[:, 1:2], 0.0), [], [("ss", 1)])
            op("act", lambda e: e.activation(out=tmpB[:, :], in_=tmpA[:, :], func=AF.Square, accum_out=ss[:, 1:2]), ["tmpA", ("ss", 1)], ["tmpB", ("ss", 1)])
            op("act", lambda e: e.activation(out=ss[:, 1:2], in_=ss[:, 1:2], func=AF.Sqrt, bias=epsb[:, 0:1], scale=1.0 / 512), [("ss", 1), "epsb"], [("ss", 1)])
            op("dve", lambda e: e.reciprocal(out=ss[:, 1:2], in_=ss[:, 1:2]), [("ss", 1)], [("ss", 1)])
            bload(cbuf[:, 0, :], ("cbuf", 0), vrow_in[2, 0:512])
            bload(cbuf[:, 1, :], ("cbuf", 1), vrow_in[3, 0:512])
            op("dve", lambda e: e.tensor_scalar(out=tmpA[:, :], in0=tmpA[:, :], scalar1=ss[:, 1:2], scalar2=None, op0=ALU.mult), ["tmpA", ("ss", 1)], ["tmpA"])
            op("dve", lambda e: e.tensor_tensor(out=tmpA[:, :], in0=tmpA[:, :], in1=cbuf[:, 0, :], op=ALU.mult), ["tmpA", ("cbuf", 0)], ["tmpA"])
            op("dve", lambda e: e.tensor_tensor(out=tmpA[:, :], in0=tmpA[:, :], in1=cbuf[:, 1, :], op=ALU.add), ["tmpA", ("cbuf", 1)], ["tmpA"])
            op("act", lambda e: e.activation(out=xn[:, 0:512], in_=tmpA[:, :], func=AF.Silu), ["tmpA"], ["xn"])
            dbgout(xn[0:4, 0:512], [4, 512], ["xn"])
            for kc in range(4):
                tr(psT[:, kc * 128:(kc + 1) * 128], xn[:, kc * 128:(kc + 1) * 128], identb[:, :], ["xn", "identb"], ["psT"])
            op("dve", lambda e: e.tensor_copy(out=sT[:, :, 0:128], in_=psT[:, 0:512].rearrange("p (k t) -> p k t", k=4)), ["psT"], ["sT"])

            Lt = bc
            pbufs = [(kst[:, 0, :], ("kst", 0)), (kst[:, 1, :], ("kst", 1)), (vst[:, 0, :], ("vst", 0)), (vst[:, 1, :], ("vst", 1))]
            pstate = {"i": 0}
            pvstate = {"i": 0}

            def get_page(src_kind, n, pg):
                buf, bk = pbufs[pstate["i"] % 4]
                pstate["i"] += 1
                if src_kind in ("k", "v"):
                    src = ck_in if src_kind == "k" else cv_in
                    col = n * 64 + pg
                    op("pool", lambda e: e.indirect_dma_start(out=buf, out_offset=None, in_=src[:, :],
                                                              in_offset=bass.IndirectOffsetOnAxis(ap=idx_t[:, col:col + 1], axis=0)),
                       ["idx_t"], [bk], dma=True, semkey="pg_" + str(bk))
                else:
                    src = cmk_in if src_kind == "mk" else cmv_in
                    dma("sp", buf, src[n, pg * 128:(pg + 1) * 128, :], [], [bk], "pg_" + str(bk))
                return buf, bk

            def bcast_row(n, src, skey):
                mm(psA[2][:, :], RROW(n), src, True, True, ["X", skey], [PA[2]])
                op("act", lambda e: e.activation(out=qb, in_=psA[2][:, :], func=AF.Copy), [PA[2]], ["qb"])

            def dense_attn(kkind, vkind, npg, nh, hd, qsrc, qkey, moba):
                G = npg * nh
                first_sum = [True]
                for n in range(4):
                    bcast_row(n, qsrc, qkey)
                    for pg in range(npg):
                        buf, bk = get_page(kkind, n, pg)
                        op("dve", lambda e, buf=buf: e.tensor_tensor(out=tmpA[:, :], in0=buf, in1=qb, op=ALU.mult), [bk, "qb"], ["tmpA"])
                        o0 = n * G + pg * nh
                        op("dve", lambda e, o0=o0: e.tensor_reduce(out=Lt[:, o0:o0 + nh], in_=tmpA[:, :].rearrange("p (h d) -> p h d", h=nh), axis=AX.X, op=ALU.add),
                           ["tmpA"], ["bc"])
                if moba:
                    Lv = Lt[:, :].rearrange("p (g h) -> p g h", h=NH)
                    for h in range(NH):
                        mm(psA[3][0:8, 0:256], HCOL(h), Lv[:, :, h], h == 0, h == NH - 1, ["X", "bc"], [PA[3]])
                    op("dve", lambda e: e.tensor_reduce(out=rden[0:8, 0:128], in_=psA[3][0:8, 0:256].rearrange("p (g e) -> p g e", e=2), axis=AX.X, op=ALU.add),
                       [PA[3]], ["rden"])
                    for n in range(4):
                        op("dve", lambda e, n=n: e.max(out=top8[0:8, n, :], in_=rden[0:8, n * 32:(n + 1) * 32]), ["rden"], ["top8"])
                    op("dve", lambda e: e.tensor_tensor(out=rden[0:8, 128:256].rearrange("p (n b) -> p n b", n=4), in0=rden[0:8, 0:128].rearrange("p (n b) -> p n b", n=4),
                                                        in1=top8[0:8, 0:4, 2:3].broadcast_to([8, 4, 32]), op=ALU.is_ge), ["rden", "top8"], ["rden"])
                    op("dve", lambda e: e.tensor_copy(out=tmpB[0:8, 0:256].rearrange("p (g e) -> p g e", e=2), in_=rden[0:8, 128:256].unsqueeze(2).broadcast_to([8, 128, 2])),
                       ["rden"], ["tmpB"])
                for n in range(4):
                    if moba:
                        op("dve", lambda e, n=n: e.tensor_tensor(out=Lt[:, n * G:(n + 1) * G], in0=Lt[:, n * G:(n + 1) * G], in1=ALT, op=ALU.add), ["bc", "X"], ["bc"])
                    op("dve", lambda e, n=n: e.tensor_reduce(out=thr[:, n:n + 1], in_=Lt[:, n * G:(n + 1) * G], axis=AX.X, op=ALU.max), ["bc"], ["thr"])
                op("pe", lambda e: e.transpose(out=psM[0:4, 128:256], in_=thr[:, 0:4], identity=identf[:, :]), ["thr", "identf"], ["psM"])
                op("dve", lambda e: e.tensor_reduce(out=mvals[0:4, 6:7], in_=psM[0:4, 128:256], axis=AX.X, op=ALU.max), ["psM"], ["mvals"])
                if moba:
                    op("dve", lambda e: e.tensor_tensor(out=tmpA[:, :], in0=qs, in1=ks, op=ALU.mult), [("cT", 1), ("cT", 3)], ["tmpA"])
                    op("dve", lambda e: e.tensor_reduce(out=nsel[:, 0, 0:8], in_=tmpA[:, :].rearrange("p (h d) -> p h d", h=NH), axis=AX.X, op=ALU.add), ["tmpA"], ["nsel"])
                    op("dve", lambda e: e.tensor_reduce(out=mvals[0:4, 7:8], in_=nsel[0:4, 0, 0:8], axis=AX.X, op=ALU.max), ["nsel"], ["mvals"])
                    op("dve", lambda e: e.tensor_tensor(out=mvals[0:4, 6:7], in0=mvals[0:4, 6:7], in1=mvals[0:4, 7:8], op=ALU.max), ["mvals"], ["mvals"])
                op("dve", lambda e: e.tensor_scalar(out=mvals[0:4, 6:7], in0=mvals[0:4, 6:7], scalar1=-1.0, scalar2=None, op0=ALU.mult), ["mvals"], ["mvals"])
                op("dve", lambda e: e.tensor_scalar(out=nsel[0:4, 1, 0:4], in0=identf[0:4, 0:4], scalar1=mvals[0:4, 6:7], scalar2=None, op0=ALU.mult), ["mvals", "identf"], ["nsel"])
                mm(psM[:, 256:260], onesf[0:4, :], nsel[0:4, 1, 0:4], True, True, ["onesf", "nsel"], ["psM"])
                op("dve", lambda e: e.tensor_copy(out=thr[:, 4:8], in_=psM[:, 256:260]), ["psM"], ["thr"])
                for n in range(4):
                    op("act", lambda e, n=n: e.activation(out=Lt[:, n * G:(n + 1) * G], in_=Lt[:, n * G:(n + 1) * G], func=AF.Exp, bias=thr[:, 4 + n:5 + n], scale=1.0),
                       ["bc", "thr"], ["bc"])
                if moba:
                    op("act", lambda e: e.activation(out=nsel[0:4, 0, 0:8], in_=nsel[0:4, 0, 0:8], func=AF.Exp, bias=mvals[0:4, 6:7], scale=1.0), ["nsel", "mvals"], ["nsel"])
                    for h in range(NH):
                        mm(psA[h // 2][:, (h % 2) * 256:(h % 2) * 256 + 256], E8(h), tmpB[0:8, 0:256], True, True, ["X", "tmpB"], [PA[h // 2]])
                    for hp in range(4):
                        op("dve", lambda e, hp=hp: e.tensor_tensor(
                            out=Lt[:, :].rearrange("p (g h) -> p g h", h=NH)[:, :, 2 * hp:2 * hp + 2],
                            in0=Lt[:, :].rearrange("p (g h) -> p g h", h=NH)[:, :, 2 * hp:2 * hp + 2],
                            in1=psA[hp][:, :].rearrange("p (h g) -> p g h", h=2), op=ALU.mult), ["bc", PA[hp]], ["bc"])
                for n in range(4):
                    for c0 in range(0, G, 512):
                        w = min(512, G - c0)
                        mm(psA[4][:, 0:w], ONEROW(n), Lt[:, n * G + c0:n * G + c0 + w], n == 0 and c0 == 0, n == 3 and c0 + w >= G, ["X", "bc"], [PA[4]])
                Gc = min(G, 512)
                op("dve", lambda e: e.tensor_reduce(out=ss[:, 8:8 + nh], in_=psA[4][:, 0:Gc].rearrange("p (g h) -> p h g", h=nh), axis=AX.X, op=ALU.add), [PA[4]], ["ss8"])
                firstv = [True]
                for n in range(4):
                    for pg in range(npg):
                        buf, bk = get_page(vkind, n, pg)
                        o0 = n * G + pg * nh
                        slot = pvstate["i"] % 3
                        pvstate["i"] += 1
                        op("dve", lambda e, buf=buf, o0=o0, slot=slot: e.tensor_tensor(out=PT[:, slot, :].rearrange("p (h d) -> p h d", h=nh), in0=buf.rearrange("p (h d) -> p h d", h=nh),
                                                                                     in1=Lt[:, o0:o0 + nh].unsqueeze(2).broadcast_to([128, nh, hd]), op=ALU.mult), [bk, "bc"], [("PT", slot)])
                        mm(psA[5][:, :], ONEROWB(n), PT[:, slot, :], firstv[0], n == 3 and pg == npg - 1, ["orowb", ("PT", slot)], [PA[5]])
                        firstv[0] = False

            dense_attn("k", "v", 64, NH, HD, qs, ("cT", 1), True)
            op("dve", lambda e: e.tensor_tensor(out=ss[:, 8:16], in0=ss[:, 8:16], in1=nsel[:, 0, 0:8], op=ALU.add), ["ss8", "nsel"], ["ss8"])
            op("dve", lambda e: e.tensor_scalar(out=ss[:, 8:16], in0=ss[:, 8:16], scalar1=1e-30, scalar2=None, op0=ALU.add), ["ss8"], ["ss8"])
            op("dve", lambda e: e.reciprocal(out=ss[:, 8:16], in_=ss[:, 8:16]), ["ss8"], ["ss8"])
            op("dve", lambda e: e.tensor_tensor(out=tmpB[:, :].rearrange("p (h d) -> p h d", h=NH), in0=vs.rearrange("p (h d) -> p h d", h=NH),
                                                in1=nsel[:, 0, 0:8].unsqueeze(2).broadcast_to([128, NH, HD]), op=ALU.mult), ["uT", "nsel"], ["tmpB"])
            op("dve", lambda e: e.tensor_tensor(out=tmpB[:, :], in0=tmpB[:, :], in1=psA[5][:, :], op=ALU.add), ["tmpB", PA[5]], ["tmpB"])
            op("dve", lambda e: e.tensor_tensor(out=xn[:, 0:512].rearrange("p (h d) -> p h d", h=NH), in0=tmpB[:, :].rearrange("p (h d) -> p h d", h=NH),
                                                in1=ss[:, 8:16].unsqueeze(2).broadcast_to([128, NH, HD]), op=ALU.mult), ["tmpB", "ss8"], ["xn"])
            dbgout(rden[0:8, 0:256], [8, 256], ["rden"])
            dbgout(ss[0:4, 8:16], [4, 8], ["ss8"])
            dbgout(xn[0:4, 0:512], [4, 512], ["xn"])
            dbgout(bc[:, :], [128, 2048], ["bc"])
            dbgout(nsel[0:4, 0, 0:8], [4, 8], ["nsel"])
            dbgout(tmpB[0:4, :], [4, 512], ["tmpB"])
            for kc in range(4):
                tr(psT[:, kc * 128:(kc + 1) * 128], xn[:, kc * 128:(kc + 1) * 128], identb[:, :], ["xn", "identb"], ["psT"])
            op("dve", lambda e: e.tensor_copy(out=OT[:, :, 0:128], in_=psT[:, 0:512].rearrange("p (k t) -> p k t", k=4)), ["psT"], ["OT"])

            dense_attn("mk", "mv", 2, 4, 128, mqs, ("cT", 2), False)
            op("dve", lambda e: e.tensor_scalar(out=ss[:, 8:12], in0=ss[:, 8:12], scalar1=1e-30, scalar2=None, op0=ALU.add), ["ss8"], ["ss8"])
            op("dve", lambda e: e.reciprocal(out=ss[:, 8:12], in_=ss[:, 8:12]), ["ss8"], ["ss8"])
            op("dve", lambda e: e.tensor_tensor(out=xn[:, 0:512].rearrange("p (h d) -> p h d", h=4), in0=psA[5][:, :].rearrange("p (h d) -> p h d", h=4),
                                                in1=ss[:, 8:12].unsqueeze(2).broadcast_to([128, 4, 128]), op=ALU.mult), [PA[5], "ss8"], ["xn"])
            dbgout(xn[0:4, 0:512], [4, 512], ["xn"])
            for kc in range(4):
                tr(psT[:, kc * 128:(kc + 1) * 128], xn[:, kc * 128:(kc + 1) * 128], identb[:, :], ["xn", "identb"], ["psT"])
            op("dve", lambda e: e.tensor_copy(out=OmT[:, :, 0:128], in_=psT[:, 0:512].rearrange("p (k t) -> p k t", k=4)), ["psT"], ["OmT"])

            mg = uT[:, 1:3, :].rearrange("p a t -> p (a t)")[:, 0:1024]
            for half in range(2):
                srcs = ((wpa_v, sT, "sT"), (wpb_v, OT, "OT"), (wpc_v, OmT, "OmT"))
                for j in range(3):
                    wpv, src, skey = srcs[j]
                    Wp, kp = load_w(wpv[:, :, half * 512:(half + 1) * 512], 4, 512)
                    for kc in range(4):
                        mm(psA[j][:, :], src[:, kc, 0:128], Wp[:, kc, :], kc == 0, kc == 3, [kp, skey], [PA[j]])
                    col0 = 3072 + j * 1024 + half * 512
                    W, wk = load_w(win_v[:, :, col0:col0 + 512], 8, 512)
                    for kc in range(8):
                        mm(psA[3][:, :], hT[:, kc, 0:128], W[:, kc, :], kc == 0, kc == 7, ["hT", wk], [PA[3]])
                    bload(cbuf[:, 2, :], ("cbuf", 2), vrow_in[4, j * 1024 + half * 512:j * 1024 + half * 512 + 512])
                    op("dve", lambda e: e.tensor_tensor(out=tmpA[:, :], in0=psA[3][:, :], in1=cbuf[:, 2, :], op=ALU.add), [PA[3], ("cbuf", 2)], ["tmpA"])
                    op("act", lambda e: e.activation(out=tmpA[:, :], in_=tmpA[:, :], func=AF.Sigmoid), ["tmpA"], ["tmpA"])
                    if j == 0:
                        op("dve", lambda e, half=half: e.tensor_tensor(out=mg[:, half * 512:(half + 1) * 512], in0=psA[0][:, :], in1=tmpA[:, :], op=ALU.mult), [PA[0], "tmpA"], ["mg"])
                    else:
                        op("dve", lambda e, j=j: e.tensor_tensor(out=tmpB[:, :], in0=psA[j][:, :], in1=tmpA[:, :], op=ALU.mult), [PA[j], "tmpA"], ["tmpB"])
                        op("dve", lambda e, half=half: e.tensor_tensor(out=mg[:, half * 512:(half + 1) * 512], in0=mg[:, half * 512:(half + 1) * 512], in1=tmpB[:, :], op=ALU.add),
                           ["mg", "tmpB"], ["mg"])
            op("dve", lambda e: e.tensor_copy(out=xn[:, :], in_=mg), ["mg"], ["xn"])
            dbgout(xn[0:4, :], [4, 1024], ["xn"])
            for kc in range(8):
                tr(psT[:, kc * 128:(kc + 1) * 128], xn[:, kc * 128:(kc + 1) * 128], identb[:, :], ["xn", "identb"], ["psT"])
            op("dve", lambda e: e.tensor_copy(out=merged[:, :, 0:128], in_=psT[:, :].rearrange("p (k t) -> p k t", k=8)), ["psT"], ["merged"])
            for half in range(2):
                W, wk = load_w(wout_v[:, :, half * 512:(half + 1) * 512], 8, 512)
                for kc in range(8):
                    mm(psA[0][:, :], merged[:, kc, 0:128], W[:, kc, :], kc == 0, kc == 7, ["merged", wk], [PA[0]])
                op("dve", lambda e, half=half: e.tensor_tensor(out=X[:, 0, half * 512:(half + 1) * 512], in0=X[:, 0, half * 512:(half + 1) * 512], in1=psA[0][:, :], op=ALU.add),
                   ["X", PA[0]], ["X"])

            dbgout(X[0:4, 0, :], [4, 1024], ["X"])
            norm_to_T(X, 1, FM_G2, hT, "hT", "X")
            cs = scrA[:, :].bitcast(F32)
            stf = Vloc[:, :, :, :].rearrange("p a b c -> p (a b c)").bitcast(F32)
            op("dve", lambda e: e.memset(stf[:, 0:1024], 0.0), [], ["stf"])
            for piece in range(11):
                c0 = piece * 512
                W, wk = load_w(wup_v[:, :, c0:c0 + 512], 8, 512)
                for kc in range(8):
                    mm(psA[piece % 2][:, :], hT[:, kc, 0:128], W[:, kc, :], kc == 0, kc == 7, ["hT", wk], [PA[piece % 2]])
                pp, pkk = psA[piece % 2], PA[piece % 2]
                op("act", lambda e, pp=pp: e.activation(out=tmpA[:, :], in_=pp[:, :], func=AF.Copy), [pkk], ["tmpA"])
                dma("sp", ffns_o[:, 1, c0:c0 + 512], tmpA[0:4, :], ["tmpA"], ["hbm_ffns"], "st_s4")
                dma("sp", stf[0:4, 0:512], stf_in[:, c0:c0 + 512], [], ["stf"], "ld_stf")
                dma("sp", stf[0:4, 512:1024], stf_in[:, NUP + c0:NUP + c0 + 512], [], ["stf"], "ld_stf")
                dma("sp", ffns_o[:, 0, c0:c0 + 512], stf[0:4, 512:1024], ["stf"], ["hbm_ffns"], "st_s5")
                bload(cbuf[:, 0, :], ("cbuf", 0), vrow_in[5, c0:c0 + 512])
                op("dve", lambda e, c0=c0: e.tensor_tensor(out=cs[:, c0:c0 + 512], in0=stf[:, 0:512], in1=cbuf[:, 0, :], op=ALU.mult), ["stf", ("cbuf", 0)], ["cs"])
                bload(cbuf[:, 1, :], ("cbuf", 1), vrow_in[6, c0:c0 + 512])
                op("dve", lambda e: e.tensor_tensor(out=tmpB[:, :], in0=stf[:, 512:1024], in1=cbuf[:, 1, :], op=ALU.mult), ["stf", ("cbuf", 1)], ["tmpB"])
                op("dve", lambda e, c0=c0: e.tensor_tensor(out=cs[:, c0:c0 + 512], in0=cs[:, c0:c0 + 512], in1=tmpB[:, :], op=ALU.add), [("sa", 0), "tmpB"], ["cs"])
                bload(cbuf[:, 2, :], ("cbuf", 2), vrow_in[7, c0:c0 + 512])
                op("dve", lambda e: e.tensor_tensor(out=tmpB[:, :], in0=tmpA[:, :], in1=cbuf[:, 2, :], op=ALU.mult), ["tmpA", ("cbuf", 2)], ["tmpB"])
                op("dve", lambda e, c0=c0: e.tensor_tensor(out=cs[:, c0:c0 + 512], in0=cs[:, c0:c0 + 512], in1=tmpB[:, :], op=ALU.add), [("sa", 0), "tmpB"], ["cs"])
                bload(cbuf[:, 0, :], ("cbuf", 0), vrow_in[8, c0:c0 + 512])
                op("dve", lambda e, c0=c0: e.tensor_tensor(out=cs[:, c0:c0 + 512], in0=cs[:, c0:c0 + 512], in1=cbuf[:, 0, :], op=ALU.add), [("sa", 0), ("cbuf", 0)], ["cs"])
            actb = hT[:, 0:6, :].rearrange("p a t -> p (a t)")
            for c0 in range(0, DFF, 512):
                w = min(512, DFF - c0)
                op("act", lambda e, c0=c0, w=w: e.activation(out=tmpA[:, 0:w], in_=cs[:, c0:c0 + w], func=AF.Silu), ["cs"], ["tmpA"])
                op("dve", lambda e, c0=c0, w=w: e.tensor_tensor(out=actb[:, c0:c0 + w], in0=tmpA[:, 0:w], in1=cs[:, DFF + c0:DFF + c0 + w], op=ALU.mult), ["tmpA", ("sa", 0)], ["hT"])
            actT = qTa_s = None
            aT = sT[:, :, :].rearrange("p a t -> p (a t)")
            aT2 = OT[:, :, :].rearrange("p a t -> p (a t)")
            for grp in range(3):
                k0 = grp * 8
                nk = min(8, 22 - k0)
                for kk in range(nk):
                    tr(psT[:, kk * 128:(kk + 1) * 128], actb[:, (k0 + kk) * 128:(k0 + kk + 1) * 128], identb[:, :], ["hT", "identb"], ["psT"])
                if grp < 2:
                    op("dve", lambda e, k0=k0, nk=nk: e.tensor_copy(out=aT[:, k0 * 128:(k0 + nk) * 128], in_=psT[:, 0:nk * 128]), ["psT"], ["sT"])
                else:
                    op("dve", lambda e, nk=nk: e.tensor_copy(out=aT2[:, 0:nk * 128], in_=psT[:, 0:nk * 128]), ["psT"], ["OT"])

            def aTk(kc):
                return aT[:, kc * 128:(kc + 1) * 128] if kc < 16 else aT2[:, (kc - 16) * 128:(kc - 15) * 128]

            for half in range(2):
                for kp, (k0, k1) in enumerate(((0, 8), (8, 16), (16, 22))):
                    W, wk = load_w(wdn_v[:, k0:k1, half * 512:(half + 1) * 512], k1 - k0, 512)
                    for kc in range(k0, k1):
                        mm(psA[2][:, :], aTk(kc), W[:, kc - k0, :], kc == 0, kc == 21, ["sT", "OT", wk], [PA[2]])
                op("dve", lambda e, half=half: e.tensor_tensor(out=X[:, 0, half * 512:(half + 1) * 512], in0=X[:, 0, half * 512:(half + 1) * 512], in1=psA[2][:, :], op=ALU.add),
                   ["X", PA[2]], ["X"])
            dma("sp", ys_o[:, :], X[0:4, 0, :], ["X"], ["hbm_ys"], "st_y")

        allhist = [("hist", j) for j in range(44)]
        op("pe", lambda e: e.transpose(out=psM[0:88, 0:128], in_=hist[:, :, :].rearrange("p j r -> p (j r)"), identity=identf[:, :]), allhist + ["identf"], ["psM"])
        op("dve", lambda e: e.tensor_copy(out=outst[0:88, 0:128], in_=psM[0:88, 0:128]), ["psM"], ["outst"])
        dma("sp", ffnc_o[:, :], outst[0:88, 0:128], ["outst"], ["hbm_ffnc"], "st_misc")

        S.emit(st)
    return nc


def host_tables(inp):
    f = np.float32
    fm = np.zeros((128, FM_N), f)

    def fmaj(v):
        return np.ascontiguousarray(np.asarray(v, f).reshape(-1, 128).T)

    fm[:, FM_G1:FM_G1 + 8] = fmaj(inp["norm1_g"])
    fm[:, FM_G2:FM_G2 + 8] = fmaj(inp["norm2_g"])
    fm[:, FM_GM:FM_GM + 8] = fmaj(inp["mem_norm_g"])
    fm[:, FM_BG:FM_BG + 24] = fmaj(inp["b_gate"])
    wda = np.asarray(inp["w_dw_a"], f)
    fm[:, FM_WDA:FM_WDA + 124] = wda.T.reshape(4, 128, 31).transpose(1, 0, 2).reshape(128, 124)
    fm[:, FM_BDA:FM_BDA + 4] = fmaj(inp["b_dw_a"])
    fm[:, FM_LAG:FM_LAG + 4] = fmaj(inp["ln_a_g"])
    fm[:, FM_LAB:FM_LAB + 4] = fmaj(inp["ln_a_b"])
    wdf = np.asarray(inp["w_dw_f"], f)
    fm[:, FM_WDF:FM_WDF + 132] = wdf.T.reshape(44, 128, 3).transpose(1, 0, 2).reshape(128, 132)
    fm[:, FM_BDF:FM_BDF + 44] = fmaj(inp["b_dw_f"])
    bc = np.zeros((128, BC_N), f)
    bc[:, BC_GQ:BC_GQ + 512] = np.tile(np.asarray(inp["q_norm_g"], f), 8)[None, :]
    bc[:, BC_GK:BC_GK + 512] = np.tile(np.asarray(inp["k_norm_g"], f), 8)[None, :]
    bc[:, BC_GMQ:BC_GMQ + 512] = np.tile(np.asarray(inp["mq_norm_g"], f), 4)[None, :]
    bc[:, BC_GMK:BC_GMK + 512] = np.tile(np.asarray(inp["mk_norm_g"], f), 4)[None, :]
    ident = np.eye(128, dtype=f)
    p = np.arange(128)
    tri = np.where(p[:, None] > p[None, :], f(NEG), f(0)).astype(f)
    slopes = (2.0 ** (-np.arange(1, NH + 1, dtype=np.float64))).astype(f)
    j = np.arange(32)
    abase = (slopes[None, :, None] * (p[:, None, None] + 128.0 * (j[None, None, :] - 28))).astype(f).reshape(128, NH * 32)
    dt = (np.arange(4)[None, :] * 128 + p[:, None]).astype(np.int64)
    hi = (16 * (dt // 16)).astype(f)
    lo = (dt % 16).astype(f)
    qal = np.stack([-slopes[None, None, :] * hi[:, :, None], -slopes[None, None, :] * lo[:, :, None]], axis=-1).astype(f).reshape(128, 64)
    return {"fm": fm, "bc": bc, "ident": ident, "tri": tri, "abase": abase, "qal": qal}


_NC_CACHE = {}


def run_prompt(inp, nch=NCHUNK):
    key = ("p", nch)
    if key not in _NC_CACHE:
        _NC_CACHE[key] = build(nch=nch, do_sample=False)
    nc = _NC_CACHE[key]
    tabs = host_tables(inp)
    f = np.float32
    wnames = ["w_in", "w_proj_a", "w_proj_b", "w_proj_c", "w_mem_kv", "w_out", "w_up", "w_down"]
    shared = {n: np.ascontiguousarray(np.asarray(inp[n], f)) for n in wnames}
    shared.update(tabs)
    in_maps = []
    for c in range(8):
        s = c % 4
        m = dict(shared)
        m["x"] = np.ascontiguousarray(np.asarray(inp["x_prompt"][s], f))
        m["mem"] = np.ascontiguousarray(np.asarray(inp["mem_prompt"][s], f))
        in_maps.append(m)
    res = run_bass_kernel_spmd(nc, in_maps, core_ids=list(range(8)))
    return res.results


def sample_tables(inp):
    f = np.float32
    sc = np.zeros((128, SC_N), f)
    for n in range(4):
        sc[n, SC_RROW + n * 128:SC_RROW + (n + 1) * 128] = 1.0
    sc[:, SC_CS + 3] = 1.0
    sc[:, SC_ST + 127] = 1.0
    for k in range(4):
        sc[k, SC_REP + k * 32:SC_REP + (k + 1) * 32] = 1.0
    for h in range(8):
        sc[h, SC_E8 + h * 128:SC_E8 + (h + 1) * 128] = 1.0
    slopes = (2.0 ** (-np.arange(1, NH + 1, dtype=np.float64)))
    p = np.arange(128)[:, None, None]
    pg = np.arange(64)[None, :, None]
    sc[:, SC_AL:SC_AL + 512] = (-slopes[None, None, :] * (8192.0 - 128.0 * pg - p)).astype(f).reshape(128, 512)
    sc[:, SC_PI] = np.arange(128, dtype=f)
    for n in range(4):
        sc[n * 30:(n + 1) * 30, SC_IND + n] = 1.0
    vr = np.zeros((9, 5632), f)
    vr[0, :512] = np.asarray(inp["w_dw_a"], f)[30]
    vr[1, :512] = np.asarray(inp["b_dw_a"], f)
    vr[2, :512] = np.asarray(inp["ln_a_g"], f)
    vr[3, :512] = np.asarray(inp["ln_a_b"], f)
    vr[4, :3072] = np.asarray(inp["b_gate"], f)
    vr[5:8, :] = np.asarray(inp["w_dw_f"], f)
    vr[8, :] = np.asarray(inp["b_dw_f"], f)
    w120 = np.ascontiguousarray(np.tile(np.asarray(inp["w_dw_a"], f)[:30], (4, 1)))
    return sc, vr, w120


def run_all(inp):
    key = ("all",)
    if key not in _NC_CACHE:
        _NC_CACHE[key] = build(nch=NCHUNK, do_sample=True)
    nc = _NC_CACHE[key]
    tabs = host_tables(inp)
    f = np.float32
    wnames = ["w_in", "w_proj_a", "w_proj_b", "w_proj_c", "w_mem_kv", "w_out", "w_up", "w_down"]
    shared = {n: np.ascontiguousarray(np.asarray(inp[n], f)) for n in wnames}
    shared.update(tabs)
    sc, vr, w120 = sample_tables(inp)
    shared["sconst"] = sc
    shared["vrows"] = vr
    shared["w120"] = w120
    shared["cache_k"] = np.ascontiguousarray(np.asarray(inp["cache_k"], f)).reshape(N_POOL * 128, 512)
    shared["cache_v"] = np.ascontiguousarray(np.asarray(inp["cache_v"], f)).reshape(N_POOL * 128, 512)
    pt = np.asarray(inp["page_table"]).astype(np.int32)
    in_maps = []
    for c in range(8):
        s = c % 4
        m = dict(shared)
        m["x"] = np.ascontiguousarray(np.asarray(inp["x_prompt"][s], f))
        m["mem"] = np.ascontiguousarray(np.asarray(inp["mem_prompt"][s], f))
        sl = slice(4 * c, 4 * c + 4)
        xs = np.zeros((128, D), f)
        xs[0:4] = np.asarray(inp["x_sample"], f)[sl, 0, :]
        m["xs"] = xs
        m["stc"] = np.ascontiguousarray(np.asarray(inp["state_conv"], f)[sl].reshape(120, 512))
        m["stf"] = np.ascontiguousarray(np.asarray(inp["state_ffn_conv"], f)[sl].reshape(4, 2 * NUP))
        m["cmk"] = np.ascontiguousarray(np.asarray(inp["cache_mem_k"], f)[sl].reshape(4, 256, 512))
        m["cmv"] = np.ascontiguousarray(np.asarray(inp["cache_mem_v"], f)[sl].reshape(4, 256, 512))
        m["ptrep"] = np.ascontiguousarray(np.broadcast_to(pt[sl].reshape(1, 256), (128, 256))).astype(np.int32)
        in_maps.append(m)
    res = run_bass_kernel_spmd(nc, in_maps, core_ids=list(range(8)))
    return res.results


def kernel(**inp):
    r = run_all(inp)
    f = np.float32
    y_p = np.stack([r[s]["y"] for s in range(4)]).astype(f)
    k_p = np.stack([r[s]["k"] for s in range(4)]).reshape(4, SEQ, NH, HD).astype(f)
    v_p = np.stack([r[s]["v"] for s in range(4)]).reshape(4, SEQ, NH, HD).astype(f)
    conv_p = np.stack([r[s]["conv"][2:32] for s in range(4)]).astype(f)
    ffn_p = np.stack([r[s]["ffnc"].reshape(44, 2, 128).transpose(1, 0, 2).reshape(2, NUP) for s in range(4)]).astype(f)
    mk_p = np.stack([r[s]["mk"] for s in range(4)]).reshape(4, 256, 4, 128).astype(f)
    mv_p = np.stack([r[s]["mv"] for s in range(4)]).reshape(4, 256, 4, 128).astype(f)
    y_s = np.concatenate([r[c]["ys"] for c in range(8)]).reshape(32, 1, D).astype(f)
    k_s = np.concatenate([r[c]["ks"] for c in range(8)]).reshape(32, 1, NH, HD).astype(f)
    v_s = np.concatenate([r[c]["vs"] for c in range(8)]).reshape(32, 1, NH, HD).astype(f)
    conv_s = np.concatenate([r[c]["convs"] for c in range(8)]).reshape(32, 30, 512).astype(f)
    ffn_s = np.concatenate([r[c]["ffns"] for c in range(8)]).reshape(32, 2, NUP).astype(f)
    return (y_p, y_s, k_p, v_p, k_s, v_s, conv_p, conv_s, ffn_p, ffn_s, mk_p, mv_p)
```
